# Optimizing a Trainium2 kernel written in Bass

```python
import jax
import jax.numpy as jnp
from jax import lax
import numpy as np


D_MODEL = 1024
BATCH = 32
SEQ = 2048
DEPTH = 2

MIX_WIDTH = D_MODEL
N_MIXERS = 4
GROUP_WIDTH = MIX_WIDTH // N_MIXERS
NORM_EPS = 1e-6
NEG_INF = -1e30

MOBA_HEADS = 4
MOBA_HEAD_DIM = GROUP_WIDTH // MOBA_HEADS
MOBA_BLOCK = 256
MOBA_TOPK = 3
MOBA_Q_CHUNK = 64
ROPE_THETA = 10000.0

SSM_HEADS = 4
SSM_D_INNER = GROUP_WIDTH
SSM_HEAD_DIM = SSM_D_INNER // SSM_HEADS
SSM_GROUPS = 2
SSM_STATE = 64
SSM_CONV = 4
SSM_CHUNK = 128
SSM_CONV_CH = SSM_D_INNER + 2 * SSM_GROUPS * SSM_STATE

GLA_HEADS = 4
GLA_DV = GROUP_WIDTH // GLA_HEADS
GLA_DK = GLA_DV // 2
GLA_GATE_RANK = 16
GLA_GATE_TAU = 16.0
GLA_CHUNK = 64

RET_HEADS = 4
RET_DV = GROUP_WIDTH // RET_HEADS
RET_DK = RET_DV // 2
RET_CHUNK = 128

FFN_DIM = 2816
FFN_CONV = 3

IN_SIZES = (GROUP_WIDTH, GROUP_WIDTH, GROUP_WIDTH,
            SSM_D_INNER, SSM_CONV_CH, SSM_HEADS,
            GLA_HEADS * GLA_DK, GLA_HEADS * GLA_DK, GLA_HEADS * GLA_DV, GLA_HEADS * GLA_DV, GLA_GATE_RANK,
            RET_HEADS * RET_DK, RET_HEADS * RET_DK, RET_HEADS * RET_DV, RET_HEADS * RET_DV)
IN_WIDTH = sum(IN_SIZES)

kernel_name = 'hybrid_moba_ssd_gla_retention_convglu'


def rms_norm(x, g):
    xf = x.astype(jnp.float32)
    y = xf * lax.rsqrt(jnp.mean(xf * xf, axis=-1, keepdims=True) + NORM_EPS)
    return (y * g.astype(jnp.float32)).astype(x.dtype)


def rope_inv_freq(dim):
    return ROPE_THETA ** (-jnp.arange(0, dim, 2, dtype=jnp.float32) / dim)


def retnet_inv_freq(dim):
    return 1.0 / (ROPE_THETA ** jnp.linspace(0.0, 1.0, dim // 2, dtype=jnp.float32))


def apply_rotary(x, inv_freq):
    s = x.shape[1]
    ang = jnp.arange(s, dtype=jnp.float32)[:, None] * inv_freq[None, :]
    cos = jnp.cos(ang)[None, :, None, :]
    sin = jnp.sin(ang)[None, :, None, :]
    x1, x2 = jnp.split(x.astype(jnp.float32), 2, axis=-1)
    return jnp.concatenate([x1 * cos - x2 * sin, x2 * cos + x1 * sin], axis=-1).astype(x.dtype)


def causal_dwconv(x, w, b):
    k, c = w.shape
    y = lax.conv_general_dilated(x, w[:, None, :].astype(x.dtype), window_strides=(1,),
                                 padding=[(k - 1, 0)], dimension_numbers=('NWC', 'WIO', 'NWC'),
                                 feature_group_count=c)
    return y + b.astype(x.dtype)


def moba_attention(q, k, v, q_norm_g, k_norm_g):
    bsz, s, h, dh = q.shape
    inv = rope_inv_freq(dh)
    q = apply_rotary(rms_norm(q, q_norm_g), inv).transpose(0, 2, 1, 3)
    k = apply_rotary(rms_norm(k, k_norm_g), inv).transpose(0, 2, 1, 3)
    v = v.transpose(0, 2, 1, 3)
    n_blk = -(-s // MOBA_BLOCK)
    pad = n_blk * MOBA_BLOCK - s
    kb = jnp.pad(k, ((0, 0), (0, 0), (0, pad), (0, 0))).reshape(bsz, h, n_blk, MOBA_BLOCK, dh)
    vb = jnp.pad(v, ((0, 0), (0, 0), (0, pad), (0, 0))).reshape(bsz, h, n_blk, MOBA_BLOCK, dh)
    pos = jnp.arange(s)
    q_blk = pos // MOBA_BLOCK
    k_mean = jnp.mean(kb.astype(jnp.float32), axis=3)
    gate = jnp.einsum('bhsd,bhnd->bhsn', q.astype(jnp.float32), k_mean)
    fully_past = jnp.arange(n_blk)[None, :] < q_blk[:, None]
    gate = jnp.where(fully_past, gate, -jnp.inf)
    n_sel = min(MOBA_TOPK, n_blk)
    _, sel = lax.top_k(gate, n_sel)
    sel_valid = sel < q_blk[:, None]

    n_qc = s // MOBA_Q_CHUNK

    def to_chunks(t):
        t = t.reshape((bsz, h, n_qc, MOBA_Q_CHUNK) + t.shape[3:])
        return jnp.moveaxis(t, 2, 0)

    b_idx = jnp.arange(bsz)[:, None, None, None]
    h_idx = jnp.arange(h)[None, :, None, None]
    scale = dh ** -0.5

    def attend(args):
        q_c, sel_c, valid_c, c = args
        start = c * MOBA_Q_CHUNK
        own = start // MOBA_BLOCK
        q_pos = start + jnp.arange(MOBA_Q_CHUNK)
        k_pos = own * MOBA_BLOCK + jnp.arange(MOBA_BLOCK)
        k_sel = kb[b_idx, h_idx, sel_c].astype(jnp.float32)
        v_sel = vb[b_idx, h_idx, sel_c].astype(jnp.float32)
        k_own = lax.dynamic_index_in_dim(kb, own, axis=2, keepdims=False).astype(jnp.float32)
        v_own = lax.dynamic_index_in_dim(vb, own, axis=2, keepdims=False).astype(jnp.float32)
        qf = q_c.astype(jnp.float32)
        s_sel = jnp.einsum('bhqd,bhqnjd->bhqnj', qf, k_sel) * scale
        s_sel = jnp.where(valid_c[..., None], s_sel, NEG_INF)
        s_own = jnp.einsum('bhqd,bhjd->bhqj', qf, k_own) * scale
        s_own = jnp.where(k_pos[None, :] <= q_pos[:, None], s_own, NEG_INF)
        n_far = n_sel * MOBA_BLOCK
        probs = jax.nn.softmax(jnp.concatenate([s_sel.reshape(bsz, h, MOBA_Q_CHUNK, n_far), s_own], axis=-1), axis=-1)
        p_sel = probs[..., :n_far].reshape(bsz, h, MOBA_Q_CHUNK, n_sel, MOBA_BLOCK)
        p_own = probs[..., n_far:]
        o = jnp.einsum('bhqnj,bhqnjd->bhqd', p_sel, v_sel) + jnp.einsum('bhqj,bhjd->bhqd', p_own, v_own)
        return o.astype(v.dtype)

    out = lax.map(attend, (to_chunks(q), to_chunks(sel), to_chunks(sel_valid), jnp.arange(n_qc)))
    return out.transpose(1, 0, 3, 2, 4).reshape(bsz, s, h * dh)


def ssd_chunked(x, a, b, c):
    bsz, s, h, p = x.shape
    n = b.shape[-1]
    L = SSM_CHUNK
    nc = s // L
    x = x.reshape(bsz, nc, L, h, p)
    b = b.reshape(bsz, nc, L, h, n)
    c = c.reshape(bsz, nc, L, h, n)
    a = a.reshape(bsz, nc, L, h).transpose(0, 3, 1, 2)
    a_cs = jnp.cumsum(a, axis=-1)
    causal = jnp.tril(jnp.ones((L, L), dtype=bool))
    seg = a_cs[..., :, None] - a_cs[..., None, :]
    decay = jnp.exp(jnp.where(causal, seg, -jnp.inf))
    scores = jnp.einsum('bclhn,bcshn->bhcls', c, b) * decay
    y_diag = jnp.einsum('bhcls,bcshp->bclhp', scores, x)
    decay_to_end = jnp.exp(a_cs[..., -1:] - a_cs)
    states = jnp.einsum('bclhn,bhcl,bclhp->bchpn', b, decay_to_end, x)
    chunk_decay = jnp.exp(a_cs[..., -1])

    def step(hs, inp):
        st, dec = inp
        return hs * dec[..., None, None] + st, hs

    h0 = jnp.zeros((bsz, h, p, n), jnp.float32)
    _, prev = lax.scan(step, h0, (jnp.moveaxis(states, 1, 0), jnp.moveaxis(chunk_decay, 2, 0)))
    prev = jnp.moveaxis(prev, 0, 1)
    y_off = jnp.einsum('bclhn,bchpn,bhcl->bclhp', c, prev, jnp.exp(a_cs))
    return (y_diag + y_off).reshape(bsz, s, h, p)


def mamba2_mixer(z, xbc, dt_raw, conv_w, conv_b, dt_bias, a_log, d_skip, norm_g):
    bsz, s, _ = z.shape
    f32 = jnp.float32
    xbc = jax.nn.silu(causal_dwconv(xbc, conv_w, conv_b)).astype(f32)
    xs, bm, cm = jnp.split(xbc, [SSM_D_INNER, SSM_D_INNER + SSM_GROUPS * SSM_STATE], axis=-1)
    xs = xs.reshape(bsz, s, SSM_HEADS, SSM_HEAD_DIM)
    rep = SSM_HEADS // SSM_GROUPS
    bm = jnp.repeat(bm.reshape(bsz, s, SSM_GROUPS, SSM_STATE), rep, axis=2)
    cm = jnp.repeat(cm.reshape(bsz, s, SSM_GROUPS, SSM_STATE), rep, axis=2)
    dt = jax.nn.softplus(dt_raw.astype(f32) + dt_bias.astype(f32))
    a = -jnp.exp(a_log.astype(f32))
    y = ssd_chunked(xs * dt[..., None], dt * a, bm, cm)
    y = y + xs * d_skip.astype(f32)[:, None]
    y = y.reshape(bsz, s, SSM_D_INNER) * jax.nn.silu(z.astype(f32))
    y = rms_norm(y.reshape(bsz, s, SSM_GROUPS, SSM_D_INNER // SSM_GROUPS),
                 norm_g.reshape(SSM_GROUPS, SSM_D_INNER // SSM_GROUPS))
    return y.reshape(bsz, s, SSM_D_INNER).astype(z.dtype)


def gla_chunked(q, k, v, g):
    bsz, s, h, dk = q.shape
    dv = v.shape[-1]
    L = GLA_CHUNK
    nc = s // L
    q = q.reshape(bsz, nc, L, h, dk)
    k = k.reshape(bsz, nc, L, h, dk)
    v = v.reshape(bsz, nc, L, h, dv)
    bcs = jnp.cumsum(g.reshape(bsz, nc, L, h, dk), axis=2)
    q_dec = q * jnp.exp(bcs)
    k_inv = k * jnp.exp(-bcs)
    causal = jnp.tril(jnp.ones((L, L), dtype=bool))
    att = jnp.where(causal, jnp.einsum('bclhd,bcshd->bchls', q_dec, k_inv), 0.0)
    o_intra = jnp.einsum('bchls,bcshe->bclhe', att, v)
    b_last = bcs[:, :, -1]
    k_end = k * jnp.exp(b_last[:, :, None] - bcs)
    states = jnp.einsum('bclhd,bclhe->bchde', k_end, v)

    def step(st, inp):
        new, dec = inp
        return st * dec[..., None] + new, st

    s0 = jnp.zeros((bsz, h, dk, dv), jnp.float32)
    _, prev = lax.scan(step, s0, (jnp.moveaxis(states, 1, 0), jnp.moveaxis(jnp.exp(b_last), 1, 0)))
    prev = jnp.moveaxis(prev, 0, 1)
    o_inter = jnp.einsum('bclhd,bchde->bclhe', q_dec, prev)
    return (o_intra + o_inter).reshape(bsz, s, h, dv)


def gla_mixer(q, k, v, r, g_lr, w2, b2, norm_g):
    bsz, s, _ = q.shape
    f32 = jnp.float32
    q = q.astype(f32).reshape(bsz, s, GLA_HEADS, GLA_DK) * (GLA_DK ** -0.5)
    k = k.astype(f32).reshape(bsz, s, GLA_HEADS, GLA_DK)
    v = v.astype(f32).reshape(bsz, s, GLA_HEADS, GLA_DV)
    g_pre = jnp.einsum('bsr,rk->bsk', g_lr.astype(f32), w2.astype(f32)) + b2.astype(f32)
    g = (jax.nn.log_sigmoid(g_pre) / GLA_GATE_TAU).reshape(bsz, s, GLA_HEADS, GLA_DK)
    o = rms_norm(gla_chunked(q, k, v, g), norm_g).reshape(bsz, s, GLA_HEADS * GLA_DV)
    return (o * jax.nn.silu(r.astype(f32))).astype(r.dtype)


def retention_chunked(q, k, v, lg):
    bsz, s, h, dk = q.shape
    dv = v.shape[-1]
    L = RET_CHUNK
    nc = s // L
    q = q.reshape(bsz, nc, L, h, dk)
    k = k.reshape(bsz, nc, L, h, dk)
    v = v.reshape(bsz, nc, L, h, dv)
    idx = jnp.arange(L, dtype=jnp.float32)
    diff = idx[:, None] - idx[None, :]
    decay_mat = jnp.where(diff >= 0, jnp.exp(jnp.maximum(diff, 0.0)[None] * lg[:, None, None]), 0.0)
    att = jnp.einsum('bclhd,bcshd->bchls', q, k) * decay_mat[None, None]
    o_intra = jnp.einsum('bchls,bcshe->bclhe', att, v)
    k_end = k * jnp.exp((L - 1 - idx)[:, None] * lg[None, :])[None, None, :, :, None]
    states = jnp.einsum('bclhd,bclhe->bchde', k_end, v)
    chunk_dec = jnp.exp(L * lg)

    def step(st, new):
        return st * chunk_dec[None, :, None, None] + new, st

    s0 = jnp.zeros((bsz, h, dk, dv), jnp.float32)
    _, prev = lax.scan(step, s0, jnp.moveaxis(states, 1, 0))
    prev = jnp.moveaxis(prev, 0, 1)
    q_dec = q * jnp.exp((idx + 1.0)[:, None] * lg[None, :])[None, None, :, :, None]
    o_inter = jnp.einsum('bclhd,bchde->bclhe', q_dec, prev)
    return (o_intra + o_inter).reshape(bsz, s, h, dv)


def retention_mixer(q, k, v, g, norm_g):
    bsz, s, _ = q.shape
    f32 = jnp.float32
    inv = retnet_inv_freq(RET_DK)
    q = apply_rotary(q.astype(f32).reshape(bsz, s, RET_HEADS, RET_DK), inv)
    k = apply_rotary(k.astype(f32).reshape(bsz, s, RET_HEADS, RET_DK), inv) * (RET_DK ** -0.5)
    v = v.astype(f32).reshape(bsz, s, RET_HEADS, RET_DV)
    lg = jnp.log(1.0 - 2.0 ** (-5.0 - jnp.arange(RET_HEADS, dtype=f32)))
    o = rms_norm(retention_chunked(q, k, v, lg), norm_g).reshape(bsz, s, RET_HEADS * RET_DV)
    return (o * jax.nn.silu(g.astype(f32))).astype(g.dtype)


def conv_glu_ffn(x, w_up, conv_w, conv_b, w_down):
    u = jnp.einsum('bsd,df->bsf', x, w_up)
    u = causal_dwconv(u, conv_w, conv_b)
    gate, val = jnp.split(u, 2, axis=-1)
    return jnp.einsum('bsf,fd->bsd', jax.nn.silu(gate) * val, w_down)


def setup_inputs(seed: int = 0) -> dict:
    key = jax.random.key(seed)
    ks = jax.random.split(key, 24)
    f32 = jnp.float32

    def nrm(k, shape, scale):
        return scale * jax.random.normal(k, shape, f32)

    def gain(k, n):
        return 1.0 + 0.01 * jax.random.normal(k, (DEPTH, n), f32)

    dt = jnp.exp(jax.random.uniform(ks[7], (DEPTH, SSM_HEADS), f32, float(np.log(1e-3)), float(np.log(1e-1))))
    return {
        'x': jax.random.normal(ks[0], (BATCH, SEQ, D_MODEL), f32),
        'attn_norm_g': gain(ks[1], D_MODEL),
        'w_in': nrm(ks[2], (DEPTH, D_MODEL, IN_WIDTH), D_MODEL ** -0.5),
        'moba_q_norm_g': gain(ks[3], MOBA_HEAD_DIM),
        'moba_k_norm_g': gain(ks[4], MOBA_HEAD_DIM),
        'ssm_conv_w': nrm(ks[5], (DEPTH, SSM_CONV, SSM_CONV_CH), SSM_CONV ** -0.5),
        'ssm_conv_b': nrm(ks[6], (DEPTH, SSM_CONV_CH), 0.01),
        'ssm_dt_bias': dt + jnp.log(-jnp.expm1(-dt)),
        'ssm_a_log': jnp.log(jax.random.uniform(ks[8], (DEPTH, SSM_HEADS), f32, 1.0, 16.0)),
        'ssm_d': gain(ks[9], SSM_HEADS),
        'ssm_norm_g': gain(ks[10], SSM_D_INNER),
        'gla_gate_w2': nrm(ks[11], (DEPTH, GLA_GATE_RANK, GLA_HEADS * GLA_DK), GLA_GATE_RANK ** -0.5),
        'gla_gate_b': nrm(ks[12], (DEPTH, GLA_HEADS * GLA_DK), 0.01),
        'gla_norm_g': gain(ks[13], GLA_DV),
        'ret_norm_g': gain(ks[14], RET_DV),
        'w_out': nrm(ks[15], (DEPTH, MIX_WIDTH, D_MODEL), MIX_WIDTH ** -0.5),
        'ffn_norm_g': gain(ks[16], D_MODEL),
        'ffn_w_up': nrm(ks[17], (DEPTH, D_MODEL, 2 * FFN_DIM), D_MODEL ** -0.5),
        'ffn_conv_w': nrm(ks[18], (DEPTH, FFN_CONV, 2 * FFN_DIM), FFN_CONV ** -0.5),
        'ffn_conv_b': nrm(ks[19], (DEPTH, 2 * FFN_DIM), 0.01),
        'ffn_w_down': nrm(ks[20], (DEPTH, FFN_DIM, D_MODEL), FFN_DIM ** -0.5),
    }


def reference(x, attn_norm_g, w_in, moba_q_norm_g, moba_k_norm_g, ssm_conv_w, ssm_conv_b,
              ssm_dt_bias, ssm_a_log, ssm_d, ssm_norm_g, gla_gate_w2, gla_gate_b, gla_norm_g,
              ret_norm_g, w_out, ffn_norm_g, ffn_w_up, ffn_conv_w, ffn_conv_b, ffn_w_down):
    bsz, s, _ = x.shape
    splits = [int(v) for v in np.cumsum(IN_SIZES)[:-1]]
    for l in range(DEPTH):
        h = rms_norm(x, attn_norm_g[l])
        proj = jnp.einsum('bsd,de->bse', h, w_in[l])
        (mq, mk, mv, sz, sxbc, sdt, gq, gk, gv, gr, gg, rq, rk, rv, rg) = jnp.split(proj, splits, axis=-1)
        y_moba = moba_attention(mq.reshape(bsz, s, MOBA_HEADS, MOBA_HEAD_DIM),
                                mk.reshape(bsz, s, MOBA_HEADS, MOBA_HEAD_DIM),
                                mv.reshape(bsz, s, MOBA_HEADS, MOBA_HEAD_DIM),
                                moba_q_norm_g[l], moba_k_norm_g[l])
        y_ssm = mamba2_mixer(sz, sxbc, sdt, ssm_conv_w[l], ssm_conv_b[l], ssm_dt_bias[l],
                             ssm_a_log[l], ssm_d[l], ssm_norm_g[l])
        y_gla = gla_mixer(gq, gk, gv, gr, gg, gla_gate_w2[l], gla_gate_b[l], gla_norm_g[l])
        y_ret = retention_mixer(rq, rk, rv, rg, ret_norm_g[l])
        mixed = jnp.concatenate([y_moba, y_ssm, y_gla, y_ret], axis=-1)
        x = x + jnp.einsum('bse,ed->bsd', mixed, w_out[l])
        x = x + conv_glu_ffn(rms_norm(x, ffn_norm_g[l]), ffn_w_up[l], ffn_conv_w[l],
                             ffn_conv_b[l], ffn_w_down[l])
    return x
```

```python
import contextlib
import numpy as np
import concourse.bass as bass
import concourse.mybir as mybir
from concourse.bass_utils import run_bass_kernel_spmd
from concourse.alu_op_type import AluOpType as ALU

F32 = mybir.dt.float32
BF16 = mybir.dt.bfloat16
AF = mybir.ActivationFunctionType
AX = mybir.AxisListType

D = 1024
S = 2048
KC = 8
NCORES = 8
FF = 2816
NJ = 22
EPS = 1e-6
IN_W = 3092
GOFF = (0, 768, 1540, 2324)
GW = 784


class Dep:
    __slots__ = ("w", "r")

    def __init__(self):
        self.w = None
        self.r = {}


class Chan:
    def __init__(self, sem):
        self.sem = sem
        self.n = 0


class KB:
    def __init__(self, nc, es):
        self.nc = nc
        self.es = es
        self.eng = {"pe": nc.tensor, "act": nc.scalar, "dve": nc.vector, "pool": nc.gpsimd, "sp": nc.sync}
        self.sem = {e: es.enter_context(nc.semaphore("s_" + e)) for e in self.eng}
        self.cnt = {e: 0 for e in self.eng}
        self.seen = {e: {} for e in self.eng}
        self.chans = []
        self.nchan = 0

    def un(self, name):
        self.nuniq = getattr(self, "nuniq", 0) + 1
        return "%s_u%d" % (name, self.nuniq)

    def chan(self):
        if getattr(self, "free", None):
            c = self.free.pop()
        else:
            self.nchan += 1
            c = Chan(self.es.enter_context(self.nc.semaphore("c%d" % self.nchan)))
            self.chans.append(c)
        if not hasattr(self, "live"):
            self.live = []
        self.live.append(c)
        return c

    def mark(self):
        if not hasattr(self, "live"):
            self.live = []
        return len(self.live)

    def release(self, mark):
        if not hasattr(self, "free"):
            self.free = []
        self.free.extend(self.live[mark:])
        del self.live[mark:]

    @staticmethod
    def _need(reads, writes):
        need = {}

        def add(k, v):
            if need.get(k, 0) < v:
                need[k] = v

        for d in reads:
            if d.w is not None:
                add(*d.w)
        for d in writes:
            if d.w is not None:
                add(*d.w)
            for k, v in d.r.items():
                add(k, v)
        return need

    def _waits(self, eng, need):
        e = self.eng[eng]
        seen = self.seen[eng]
        for k, v in need.items():
            if k == "pe" and eng == "pe":
                continue
            if seen.get(k, 0) >= v:
                continue
            seen[k] = v
            if isinstance(k, Chan):
                e.wait_ge(k.sem, v)
            else:
                e.wait_ge(self.sem[k], v)

    def op(self, eng, fn, reads=(), writes=(), inc=True):
        self._waits(eng, self._need(reads, writes))
        ins = fn()
        if inc:
            self.cnt[eng] += 1
            ins.then_inc(self.sem[eng], 1)
            c = self.cnt[eng]
        else:
            c = self.cnt[eng] + 1
        for d in reads:
            if d.r.get(eng, 0) < c:
                d.r[eng] = c
        for d in writes:
            d.w = (eng, c)
            d.r = {}
        return ins

    def dma(self, chan, out, in_, reads=(), writes=(), q="sp"):
        self._waits(q, self._need(reads, writes))
        ins = self.eng[q].dma_start(out=out, in_=in_)
        chan.n += 16
        ins.then_inc(chan.sem, 16)
        for d in reads:
            d.r[chan] = chan.n
        for d in writes:
            d.w = (chan, chan.n)
            d.r = {}
        return ins

    def barrier(self):
        for e in self.eng:
            need = {k: v for k, v in self.cnt.items() if v > 0 and k != e}
            for c in self.chans:
                if c.n > 0:
                    need[c] = c.n
            self._waits(e, need)
        for e in ("act", "dve", "pool"):
            if self.cnt[e] > 0 and self.seen[e].get(e, 0) < self.cnt[e]:
                self.seen[e][e] = self.cnt[e]
                self.eng[e].wait_ge(self.sem[e], self.cnt[e])


class Ring:
    def __init__(self, items):
        self.items = items
        self.i = 0

    def next(self):
        it = self.items[self.i % len(self.items)]
        self.i += 1
        return it


def pp_layout(L):
    off = {}
    n = 0

    def add(name, cnt):
        nonlocal n
        off[name] = n
        n += cnt

    add("attn_g", L * KC)
    add("ffn_g", L * KC)
    add("fcw", L * 3 * 2 * NJ)
    add("fcb", L * 2 * NJ)
    add("gla_b2n", L)
    add("gla_ng", L)
    add("ret_ng", L)
    add("ssm_ng", L * 2)
    add("ssm_cw", L * 4 * 4)
    add("ssm_cb", L * 4)
    add("moba_qg", L)
    add("moba_kg", L)
    return off, n


def host_prep(inputs, L):
    f = np.float32
    w_in = np.asarray(inputs["w_in"], f)[:L]
    win = np.zeros((L, 4, 128, KC, GW), f)
    for g in range(4):
        w = (GOFF + (IN_W,))[g + 1] - GOFF[g]
        blk = w_in[:, :, GOFF[g]:GOFF[g] + w].reshape(L, KC, 128, w)
        win[:, g, :, :, :w] = blk.transpose(0, 2, 1, 3)
    win = win.reshape(L * 4 * 128, KC * GW)
    w_out = np.asarray(inputs["w_out"], f)[:L]
    wout = w_out.reshape(L, 4, 2, 128, D).transpose(0, 1, 3, 2, 4).reshape(L * 4 * 128, 2 * D)
    w_up = np.asarray(inputs["ffn_w_up"], f)[:L]
    wu = w_up.reshape(L, KC, 128, 2, NJ, 128)
    wup = wu.transpose(0, 4, 2, 1, 3, 5).reshape(L * NJ * 128, KC * 256)
    wdn = np.asarray(inputs["ffn_w_down"], f)[:L].reshape(L * FF, D)
    off, npp = pp_layout(L)
    pp = np.zeros((128, npp), f)

    def fm(v):
        return np.asarray(v, f).reshape(-1, 128).T

    for l in range(L):
        pp[:, off["attn_g"] + l * KC: off["attn_g"] + (l + 1) * KC] = fm(inputs["attn_norm_g"][l])
        pp[:, off["ffn_g"] + l * KC: off["ffn_g"] + (l + 1) * KC] = fm(inputs["ffn_norm_g"][l])
        for i in range(3):
            o = off["fcw"] + (l * 3 + i) * 2 * NJ
            pp[:, o:o + 2 * NJ] = fm(inputs["ffn_conv_w"][l][i])
        o = off["fcb"] + l * 2 * NJ
        pp[:, o:o + 2 * NJ] = fm(inputs["ffn_conv_b"][l])
    for l in range(L):
        pp[:, off["gla_b2n"] + l] = np.asarray(inputs["gla_gate_b"][l], f)
        pp[:, off["gla_ng"] + l] = np.tile(np.asarray(inputs["gla_norm_g"][l], f), 2)
        pp[:, off["ret_ng"] + l] = np.tile(np.asarray(inputs["ret_norm_g"][l], f), 2)
        pp[:, off["ssm_ng"] + 2 * l: off["ssm_ng"] + 2 * l + 2] = fm(inputs["ssm_norm_g"][l])
        for i in range(4):
            o = off["ssm_cw"] + (l * 4 + i) * 4
            pp[:, o:o + 4] = fm(inputs["ssm_conv_w"][l][i])
        pp[:, off["ssm_cb"] + 4 * l: off["ssm_cb"] + 4 * l + 4] = fm(inputs["ssm_conv_b"][l])
        pp[:, off["moba_qg"] + l] = np.tile(np.asarray(inputs["moba_q_norm_g"][l], f), 2)
        pp[:, off["moba_kg"] + l] = np.tile(np.asarray(inputs["moba_k_norm_g"][l], f), 2)
    consts = host_consts()
    w2 = np.asarray(inputs["gla_gate_w2"], f)[:L].reshape(L * 16, 128)
    consts["gw2"] = np.ascontiguousarray(w2)
    rows = np.zeros((128, L * 384), f)
    for l in range(L):
        rows[:, l * 384: l * 384 + 64] = np.tile(np.asarray(inputs["ssm_dt_bias"][l], f), 16)[None, :]
        rows[:, l * 384 + 64: l * 384 + 128] = np.tile(np.asarray(inputs["ssm_a_log"][l], f), 16)[None, :]
        rows[:, l * 384 + 128: l * 384 + 384] = np.repeat(np.asarray(inputs["ssm_d"][l], f), 64)[None, :]
    consts["rows"] = rows
    return dict(win=np.ascontiguousarray(win), wout=np.ascontiguousarray(wout),
                wup=np.ascontiguousarray(wup), wdn=np.ascontiguousarray(wdn), pp=pp, **consts)


CM_OFF = {}
STAGE = 0
SKIP_FFN = False


def host_consts():
    f = np.float32
    p = np.arange(128)
    cols = []

    def add(name, a):
        CM_OFF[name] = sum(c.shape[1] for c in cols)
        cols.append(np.asarray(a, f))

    add("ident", np.eye(128))
    add("ones", np.ones((128, 128)))
    caus = (p[:, None] <= p[None, :]).astype(f)
    add("causal", np.tile(caus, (1, 4)))
    add("bdm4", (p[:, None] // 32 == np.arange(256)[None, :] // 64))
    add("bdm2", (p[:, None] // 64 == np.arange(256)[None, :] // 128))
    add("hm4", (p[:, None] // 32 == np.arange(4)[None, :]))
    add("hm2", (p[:, None] // 64 == np.arange(2)[None, :]))
    def perm(hd):
        half = hd // 2
        m = np.arange(128)
        partner = (m // hd) * hd + ((m % hd) + half) % hd
        P = np.zeros((128, 128), f)
        P[partner, m] = 1.0
        return P
    add("perm32", perm(32))
    add("perm64", perm(64))
    add("bo64", (p[:, None] // 64 == p[None, :] // 64))
    add("sgt", (p[:, None] > p[None, :]))
    gmk = np.zeros((128, 8, 4, 8))
    for b_ in range(8):
        gmk[:, b_, :, b_:] = -1e30
    add("gmask", gmk.reshape(128, 256))
    cm = np.concatenate(cols, axis=1)
    lg = np.log(1.0 - 2.0 ** (-5.0 - np.arange(4, dtype=np.float64)))
    h = p // 32
    d = p % 32
    inv = 1.0 / (10000.0 ** np.linspace(0.0, 1.0, 16))
    t = np.arange(S, dtype=np.float64)
    ang = t[None, :] * inv[d % 16][:, None]
    rcos = np.cos(ang)
    rsin = np.sin(ang) * np.where(d < 16, -1.0, 1.0)[:, None]
    idx = np.arange(512) % 128
    sc = 32.0 ** -0.5
    rdq = np.exp((idx[None, :] + 1.0) * lg[h][:, None])
    rdki = np.exp(-(idx[None, :] + 1.0) * lg[h][:, None]) * sc
    rdke = np.exp((127.0 - idx[None, :]) * lg[h][:, None]) * sc
    rcd = np.repeat(np.exp(128.0 * lg[h])[:, None], 16, axis=1)
    rett = np.concatenate([rdq, rdki, rdke, rcd], axis=1).astype(f)
    dm = p % 64
    invm = 10000.0 ** (-np.arange(0, 64, 2, dtype=np.float64) / 64)
    angm = t[None, :] * invm[dm % 32][:, None]
    mcos = np.cos(angm)
    msin = np.sin(angm) * np.where(dm < 32, -1.0, 1.0)[:, None]
    blk = (np.arange(S)[None, :] // 256 == np.arange(8)[:, None]).astype(f)
    return {"blk1h": blk, "cm": cm, "ret_cs": np.concatenate([rcos, rsin], axis=1).astype(f), "ret_t": rett,
            "moba_cs": np.concatenate([mcos, msin], axis=1).astype(f)}


def build(n_seq, L, mixers=(0, 1, 2, 3), debug=False):
    nc = bass.Bass("TRN2", target_bir_lowering=False)
    off, npp = pp_layout(L)
    x_d = nc.dram_tensor("x", [n_seq * S, D], F32, kind="ExternalInput").ap()
    y_d = nc.dram_tensor("y", [n_seq * S, D], F32, kind="ExternalOutput").ap()
    win_d = nc.dram_tensor("win", [L * 4 * 128, KC * GW], F32, kind="ExternalInput").ap()
    wout_d = nc.dram_tensor("wout", [L * 4 * 128, 2 * D], F32, kind="ExternalInput").ap()
    wup_d = nc.dram_tensor("wup", [L * NJ * 128, KC * 256], F32, kind="ExternalInput").ap()
    wdn_d = nc.dram_tensor("wdn", [L * FF, D], F32, kind="ExternalInput").ap()
    pp_d = nc.dram_tensor("pp", [128, npp], F32, kind="ExternalInput").ap()
    if not CM_OFF:
        host_consts()
    NCM = CM_OFF["gmask"] + 256
    cm_d = nc.dram_tensor("cm", [128, NCM], F32, kind="ExternalInput").ap()
    retcs_d = nc.dram_tensor("ret_cs", [128, 2 * S], F32, kind="ExternalInput").ap()
    rett_d = nc.dram_tensor("ret_t", [128, 1552], F32, kind="ExternalInput").ap()
    mobacs_d = nc.dram_tensor("moba_cs", [128, 2 * S], F32, kind="ExternalInput").ap()
    gw2_d = nc.dram_tensor("gw2", [L * 16, 128], F32, kind="ExternalInput").ap()
    rows_d = nc.dram_tensor("rows", [128, L * 384], F32, kind="ExternalInput").ap()
    blk_d = nc.dram_tensor("blk1h", [8, S], F32, kind="ExternalInput").ap()
    dbg_d = nc.dram_tensor("dbg", [4, 128, 2 * S], BF16, kind="ExternalOutput").ap() if debug else None
    win_b = nc.dram_tensor("win_b", [L * 4 * 128, KC * GW], BF16, kind="Internal").ap()
    wout_b = nc.dram_tensor("wout_b", [L * 4 * 128, 2 * D], BF16, kind="Internal").ap()
    wup_b = nc.dram_tensor("wup_b", [L * NJ * 128, KC * 256], BF16, kind="Internal").ap()
    wdn_b = nc.dram_tensor("wdn_b", [L * FF, D], BF16, kind="Internal").ap()

    with contextlib.ExitStack() as es:
        kb = KB(nc, es)
        sb = lambda name, shape, dt: es.enter_context(nc.sbuf_tensor(name, shape, dt))

        cc = kb.chan()
        for src, dst in ((win_d, win_b), (wout_d, wout_b), (wup_d, wup_b), (wdn_d, wdn_b)):
            rows = src.shape[0]
            for r in range(0, rows, 128):
                kb.dma(cc, dst[r:r + 128, :], src[r:r + 128, :], q="pool")

        xT = sb("xT", [128, KC, S], F32)
        hT = sb("hT", [128, KC, S], BF16)
        xT_d = [[Dep() for _ in range(4)] for _ in range(KC)]
        hT_d = [[Dep() for _ in range(4)] for _ in range(KC)]
        ppt = sb("ppt", [128, npp], F32)
        cmt = sb("cmt", [128, NCM], F32)
        cmb = sb("cmb", [128, 5, 128], BF16)
        identf = cmt[:, 0:128]
        onesb = cmb[:, 1, :]
        identb = cmb[:, 0, :]

        def cmc(name, n):
            return cmt[:, CM_OFF[name]:CM_OFF[name] + n]
        const_d = Dep()
        c0 = kb.chan()
        kb.dma(c0, ppt[:], pp_d[:, :], writes=[const_d])
        kb.dma(c0, cmt[:], cm_d[:, :], writes=[const_d])
        for ii, nm in enumerate(("ident", "ones", "perm32", "perm64", "bo64")):
            kb.op("dve", lambda: nc.vector.tensor_copy(out=cmb[:, ii, :], in_=cmc(nm, 128)), reads=[const_d], writes=[const_d])
        cv = sb("cv", [128, 8], F32)
        kb.op("pool", lambda: nc.gpsimd.memset(cv[:, 0:1], EPS), writes=[const_d])
        ps = [es.enter_context(nc.psum_tensor("ps%d" % i, [128, 512], F32)) for i in range(7)]
        ps_d = [Dep() for _ in range(7)]
        psb = es.enter_context(nc.psum_tensor("psb", [128, 1024], BF16))
        psb_d = [Dep()] * 8
        kb.barrier()

        def ppc(name, idx):
            o = off[name] + idx
            return ppt[:, o:o + 1]

        def load_x(s):
            mk_ = kb.mark()
            with contextlib.ExitStack() as sc:
                xin = [sc.enter_context(nc.sbuf_tensor(kb.un("xin"), [128, D], F32)) for i in range(2)]
                xin_d = [Dep(), Dep()]
                xc = [kb.chan(), kb.chan()]
                for i in range(16):
                    b = i % 2
                    kb.dma(xc[b], xin[b][:], x_d[s * S + i * 128: s * S + (i + 1) * 128, :], writes=[xin_d[b]])
                    for hh in range(2):
                        pi = (i * 2 + hh) % 4
                        for k in range(4):
                            kc = hh * 4 + k
                            kb.op("pe", lambda: nc.tensor.transpose(out=ps[pi][:, k * 128:(k + 1) * 128],
                                                                    in_=xin[b][:, kc * 128:(kc + 1) * 128],
                                                                    identity=identf),
                                  reads=[xin_d[b], const_d], writes=[ps_d[pi]], inc=(k == 3))
                        eng = "act" if hh == 0 else "dve"
                        dst = xT[:, hh * 4:(hh + 1) * 4, i * 128:(i + 1) * 128]
                        src = ps[pi][:].rearrange("p (k c) -> p k c", k=4)
                        wr = [xT_d[hh * 4 + k][i // 4] for k in range(4)]
                        if eng == "act":
                            kb.op("act", lambda: nc.scalar.copy(out=dst, in_=src), reads=[ps_d[pi]], writes=wr)
                        else:
                            kb.op("dve", lambda: nc.vector.tensor_copy(out=dst, in_=src), reads=[ps_d[pi]], writes=wr)
                kb.barrier()
            kb.release(mk_)

        def store_x(s):
            mk_ = kb.mark()
            with contextlib.ExitStack() as sc:
                xo = [sc.enter_context(nc.sbuf_tensor(kb.un("xo"), [128, D], F32)) for i in range(2)]
                xo_d = [Dep(), Dep()]
                xc = [kb.chan(), kb.chan()]
                for i in range(16):
                    b = i % 2
                    for hh in range(2):
                        pi = (i * 2 + hh) % 4
                        for k in range(4):
                            kc = hh * 4 + k
                            kb.op("pe", lambda: nc.tensor.transpose(out=ps[pi][:, k * 128:(k + 1) * 128],
                                                                    in_=xT[:, kc, i * 128:(i + 1) * 128],
                                                                    identity=identf),
                                  reads=[xT_d[kc][i // 4], const_d], writes=[ps_d[pi]], inc=(k == 3))
                        dst = xo[b][:, hh * 512:(hh + 1) * 512]
                        if hh == 0:
                            kb.op("act", lambda: nc.scalar.copy(out=dst, in_=ps[pi][:]), reads=[ps_d[pi]], writes=[xo_d[b]])
                        else:
                            kb.op("dve", lambda: nc.vector.tensor_copy(out=dst, in_=ps[pi][:]), reads=[ps_d[pi]], writes=[xo_d[b]])
                    kb.dma(xc[b], y_d[s * S + i * 128: s * S + (i + 1) * 128, :], xo[b][:], reads=[xo_d[b]])
                kb.barrier()

            kb.release(mk_)

        def norm(gname, l):
            with contextlib.ExitStack() as sc:
                sq = [sc.enter_context(nc.sbuf_tensor(kb.un("sq"), [128, KC, 512], BF16)) for i in range(2)]
                rs = [sc.enter_context(nc.sbuf_tensor(kb.un("rs"), [128, 512], F32)) for i in range(2)]
                sq_d = [Dep(), Dep()]
                rs_d = [Dep(), Dep()]
                for tt in range(4):
                    b = tt % 2
                    sl = slice(tt * 512, (tt + 1) * 512)
                    kb.op("act", lambda: nc.scalar.activation(out=sq[b][:], in_=xT[:, :, sl], func=AF.Square),
                          reads=[xT_d[k][tt] for k in range(KC)], writes=[sq_d[b]])
                    pi = 4 + b
                    for kc in range(KC):
                        kb.op("pe", lambda: nc.tensor.matmul(ps[pi][:], lhsT=onesb, rhs=sq[b][:, kc, :],
                                                             start=(kc == 0), stop=(kc == KC - 1)),
                              reads=[sq_d[b], const_d], writes=[ps_d[pi]], inc=(kc == KC - 1))
                    kb.op("act", lambda: nc.scalar.activation(out=rs[b][:], in_=ps[pi][:], func=AF.Ln, bias=cv[:, 0:1], scale=1.0 / D),
                          reads=[ps_d[pi], const_d], writes=[rs_d[b]])
                    kb.op("act", lambda: nc.scalar.activation(out=rs[b][:], in_=rs[b][:], func=AF.Exp, scale=-0.5),
                          reads=[rs_d[b]], writes=[rs_d[b]])
                    for kc in range(KC):
                        kb.op("dve", lambda: nc.vector.scalar_tensor_tensor(out=hT[:, kc, sl], in0=xT[:, kc, sl],
                                                                            scalar=ppc(gname, l * KC + kc), in1=rs[b][:],
                                                                            op0=ALU.mult, op1=ALU.mult),
                              reads=[xT_d[kc][tt], rs_d[b], const_d], writes=[hT_d[kc][tt]])
                kb.barrier()

        def ffn(l):
            mk_ = kb.mark()
            G = 6
            groups = [list(range(a, min(a + G, NJ))) for a in range(0, NJ, G)]
            with contextlib.ExitStack() as sc:
                st = lambda name, shape, dt: sc.enter_context(nc.sbuf_tensor(kb.un(name), shape, dt))
                upre = [[st("upre%d_%d" % (b, h), [128, S + 2], BF16) for h in range(2)] for b in range(2)]
                upre_d = [[[Dep() for _ in range(5)] for h in range(2)] for b in range(2)]
                acc = [[st("acc%d_%d" % (r, h), [128, 512], F32) for h in range(2)] for r in range(3)]
                acc_d = [[Dep() for h in range(2)] for r in range(3)]
                actT = st("actT", [128, G, S], BF16)
                act_d = [[Dep() for _ in range(4)] for _ in range(G)]
                wup = [st("wup%d" % i, [128, KC, 256], BF16) for i in range(3)]
                wup_dd = [Dep() for _ in range(3)]
                wup_c = [kb.chan() for _ in range(3)]
                wdn = [st("wdn%d" % i, [128, D], BF16) for i in range(G)]
                wdn_dd = [Dep() for _ in range(G)]
                wdn_c = [kb.chan() for _ in range(G)]
                for b in range(2):
                    for h in range(2):
                        kb.op("pool", lambda: nc.gpsimd.memset(upre[b][h][:, 0:2], 0.0), writes=[upre_d[b][h][0]])
                accr = 0
                jcount = 0
                for grp in groups:
                    for jj, j in enumerate(grp):
                        kb.dma(wdn_c[jj], wdn[jj][:], wdn_b[l * FF + j * 128: l * FF + (j + 1) * 128, :], writes=[wdn_dd[jj]])
                    for jj, j in enumerate(grp):
                        ws = jcount % 3
                        ub = jcount % 2
                        jcount += 1
                        r0 = (l * NJ + j) * 128
                        kb.dma(wup_c[ws], wup[ws][:].rearrange("p k c -> p (k c)"), wup_b[r0:r0 + 128, :], writes=[wup_dd[ws]])
                        for tt in range(4):
                            sl = slice(tt * 512, (tt + 1) * 512)
                            ar = accr % 3
                            accr += 1
                            for h in range(2):
                                pi = (tt % 2) * 2 + h
                                for kc in range(KC):
                                    kb.op("pe", lambda: nc.tensor.matmul(ps[pi][:], lhsT=wup[ws][:, kc, h * 128:(h + 1) * 128],
                                                                         rhs=hT[:, kc, sl], start=(kc == 0), stop=(kc == KC - 1)),
                                          reads=[wup_dd[ws], hT_d[kc][tt]], writes=[ps_d[pi]], inc=(kc == KC - 1))
                                kb.op("act", lambda: nc.scalar.copy(out=upre[ub][h][:, 2 + tt * 512: 2 + (tt + 1) * 512], in_=ps[pi][:]),
                                      reads=[ps_d[pi]], writes=[upre_d[ub][h][tt + 1]])
                                w2 = ppc("fcw", (l * 3 + 2) * 2 * NJ + h * NJ + j)
                                w1 = ppc("fcw", (l * 3 + 1) * 2 * NJ + h * NJ + j)
                                w0 = ppc("fcw", (l * 3 + 0) * 2 * NJ + h * NJ + j)
                                bb = ppc("fcb", l * 2 * NJ + h * NJ + j)
                                kb.op("act", lambda: nc.scalar.activation(out=acc[ar][h][:], in_=ps[pi][:], func=AF.Identity,
                                                                          bias=bb, scale=w2),
                                      reads=[ps_d[pi], const_d], writes=[acc_d[ar][h]])
                                kb.op("dve", lambda: nc.vector.scalar_tensor_tensor(
                                    out=acc[ar][h][:], in0=upre[ub][h][:, 1 + tt * 512: 1 + (tt + 1) * 512], scalar=w1,
                                    in1=acc[ar][h][:], op0=ALU.mult, op1=ALU.add),
                                    reads=[upre_d[ub][h][tt + 1], upre_d[ub][h][tt], acc_d[ar][h], const_d], writes=[acc_d[ar][h]])
                                kb.op("dve", lambda: nc.vector.scalar_tensor_tensor(
                                    out=acc[ar][h][:], in0=upre[ub][h][:, tt * 512: (tt + 1) * 512], scalar=w0,
                                    in1=acc[ar][h][:], op0=ALU.mult, op1=ALU.add),
                                    reads=[upre_d[ub][h][tt + 1], upre_d[ub][h][tt], acc_d[ar][h], const_d], writes=[acc_d[ar][h]])
                            kb.op("act", lambda: nc.scalar.activation(out=acc[ar][0][:], in_=acc[ar][0][:], func=AF.Silu),
                                  reads=[acc_d[ar][0]], writes=[acc_d[ar][0]])
                            kb.op("pool", lambda: nc.gpsimd.tensor_tensor(out=actT[:, jj, sl], in0=acc[ar][0][:], in1=acc[ar][1][:],
                                                                          op=ALU.mult),
                                  reads=[acc_d[ar][0], acc_d[ar][1]], writes=[act_d[jj][tt]])
                    for dc in range(KC):
                        for tt in range(4):
                            sl = slice(tt * 512, (tt + 1) * 512)
                            pi = 4 + (dc * 4 + tt) % 3
                            for jj, j in enumerate(grp):
                                kb.op("pe", lambda: nc.tensor.matmul(ps[pi][:], lhsT=wdn[jj][:, dc * 128:(dc + 1) * 128],
                                                                     rhs=actT[:, jj, sl], start=(jj == 0), stop=(jj == len(grp) - 1)),
                                      reads=[wdn_dd[jj], act_d[jj][tt]], writes=[ps_d[pi]], inc=(jj == len(grp) - 1))
                            kb.op("dve", lambda: nc.vector.tensor_tensor(out=xT[:, dc, sl], in0=ps[pi][:], in1=xT[:, dc, sl], op=ALU.add),
                                  reads=[ps_d[pi], xT_d[dc][tt]], writes=[xT_d[dc][tt]])
                kb.barrier()
            kb.release(mk_)

        kb.op("pool", lambda: nc.gpsimd.memset(cv[:, 1:2], 1.0), writes=[const_d])
        kb.op("pool", lambda: nc.gpsimd.memset(cv[:, 2:3], float(np.log(32.0 ** -0.5))), writes=[const_d])
        kb.barrier()
        prr = Ring(list(range(7)))

        def proj_fm(pi, wg, wg_d, c0, M, tt):
            sl = slice(tt * 512, (tt + 1) * 512)
            for kc in range(KC):
                kb.op("pe", lambda: nc.tensor.matmul(ps[pi][0:M, :], lhsT=wg[:, kc, c0:c0 + M], rhs=hT[:, kc, sl],
                                                     start=(kc == 0), stop=(kc == KC - 1)),
                      reads=[wg_d, hT_d[kc][tt]], writes=[ps_d[pi]], inc=(kc == KC - 1))

        def proj_tm(pi, col0, wg, wg_d, c0, N, i):
            for kc in range(KC):
                kb.op("pe", lambda: nc.tensor.matmul(ps[pi][:, col0:col0 + N], lhsT=hT[:, kc, i * 128:(i + 1) * 128],
                                                     rhs=wg[:, kc, c0:c0 + N], start=(kc == 0), stop=(kc == KC - 1)),
                      reads=[wg_d, hT_d[kc][i // 4]], writes=[ps_d[pi]], inc=(kc == KC - 1))

        def evac(i, out, in_, reads, writes):
            if i % 2 == 0:
                kb.op("act", lambda: nc.scalar.copy(out=out, in_=in_), reads=reads, writes=writes)
            else:
                kb.op("dve", lambda: nc.vector.tensor_copy(out=out, in_=in_), reads=reads, writes=writes)

        def v_tokmajor(vt, v_d, wg, wg_d, c0):
            for i in range(16):
                pi = prr.next()
                proj_tm(pi, 0, wg, wg_d, c0, 256, i)
                evac(i, vt[:, i, :], ps[pi][:, 0:256], [ps_d[pi]], [v_d[i]])

        def gate_fm(sg, sg_d, wg, wg_d, c0):
            for cc in range(2):
                for tt in range(4):
                    pi = prr.next()
                    proj_fm(pi, wg, wg_d, c0 + cc * 128, 128, tt)
                    kb.op("act", lambda: nc.scalar.activation(out=sg[:, cc, tt * 512:(tt + 1) * 512], in_=ps[pi][:], func=AF.Silu),
                          reads=[ps_d[pi]], writes=[sg_d[cc][tt]])

        def post_norm(c, src, src_d, ng, gname_idx, sg, sg_d, mixT, mix_d, tl):
            w = 256 // ng
            b = c % 2
            sq, sq_d, ss, ss_d, on, on_d = tl["sq"][b], tl["sq_d"][b], tl["ss"][b], tl["ss_d"][b], tl["on"][b], tl["on_d"][b]
            kb.op("act", lambda: nc.scalar.activation(out=sq[:], in_=src, func=AF.Square), reads=[src_d], writes=[sq_d])
            kb.op("dve", lambda: nc.vector.tensor_reduce(out=ss[:, 0:ng], in_=sq[:].rearrange("p (g e) -> p g e", g=ng),
                                                         axis=AX.X, op=ALU.add), reads=[sq_d], writes=[ss_d])
            kb.op("act", lambda: nc.scalar.activation(out=ss[:, 0:ng], in_=ss[:, 0:ng], func=AF.Ln, bias=cv[:, 0:1], scale=1.0 / w),
                  reads=[ss_d, const_d], writes=[ss_d])
            kb.op("act", lambda: nc.scalar.activation(out=ss[:, 0:ng], in_=ss[:, 0:ng], func=AF.Exp, scale=-0.5),
                  reads=[ss_d], writes=[ss_d])
            for g in range(ng):
                kb.op("act", lambda: nc.scalar.activation(out=on[:, g * w:(g + 1) * w], in_=src[:, g * w:(g + 1) * w], func=AF.Copy,
                                                          scale=ss[:, g:g + 1]), reads=[src_d, ss_d], writes=[on_d])
            if STAGE == 3:
                if c == 15:
                    mix_zero(0, None, None, mixT, mix_d, None)
                return
            pt = 5 + b
            for cc in range(2):
                tin = sq if STAGE == 5 else on
                tin_d = sq_d if STAGE == 5 else on_d
                if STAGE == 7:
                    continue
                kb.op("pe", lambda: nc.tensor.transpose(out=ps[pt][:, cc * 128:(cc + 1) * 128], in_=tin[:, cc * 128:(cc + 1) * 128],
                                                        identity=identf), reads=[tin_d, const_d], writes=[ps_d[pt]], inc=(cc == 1))
            if STAGE == 6:
                if c == 15:
                    mix_zero(0, None, None, mixT, mix_d, None)
                return
            for cc in range(2):
                dst = mixT[:, cc, c * 128:(c + 1) * 128]
                srcT = ps[pt][:, cc * 128:(cc + 1) * 128]
                if sg is not None:
                    kb.op("dve", lambda: nc.vector.scalar_tensor_tensor(out=dst, in0=srcT,
                                                                        scalar=ppc(*gname_idx(cc)), in1=sg[:, cc, c * 128:(c + 1) * 128],
                                                                        op0=ALU.mult, op1=ALU.mult),
                          reads=[ps_d[pt], sg_d[cc][c // 4], const_d], writes=[mix_d[cc][c // 4]])
                else:
                    kb.op("dve", lambda: nc.vector.tensor_scalar(out=dst, in0=srcT,
                                                                 scalar1=ppc(*gname_idx(cc)), scalar2=None, op0=ALU.mult),
                          reads=[ps_d[pt], const_d], writes=[mix_d[cc][c // 4]])

        def post_tiles(st):
            return dict(sq=[st("sq", [128, 256], F32) for _ in range(2)], sq_d=[Dep(), Dep()],
                        ss=[st("ss", [128, 4], F32) for _ in range(2)], ss_d=[Dep(), Dep()],
                        on=[st("on", [128, 256], F32) for _ in range(2)], on_d=[Dep(), Dep()])

        def linattn(st, Kmask, Km_d, QdT, Qd_d, kendT, ke_d, vt, v_d, cdec, cdec_d, gname_idx, sg, sg_d, mixT, mix_d):
            tl = post_tiles(st)
            attm = [st("attm", [128, 512], BF16) for _ in range(2)]
            attm_d = [Dep(), Dep()]
            ketm = [st("ketm", [128, 128], BF16) for _ in range(2)]
            ketm_d = [Dep(), Dep()]
            S_run = st("S_run", [128, 256], F32)
            S_tmp = st("S_tmp", [128, 256], F32)
            Sbf = st("Sbf", [128, 256], BF16)
            S_d, St_d, Sb_d = Dep(), Dep(), Dep()
            for c in range(16):
                ch = slice(c * 128, (c + 1) * 128)
                b = c % 2
                tq = c // 4
                if c < 15:
                    kb.op("pe", lambda: nc.tensor.transpose(out=psb[:, b * 128:(b + 1) * 128], in_=kendT[:, ch], identity=identb),
                          reads=[ke_d[tq], const_d], writes=[psb_d[b]])
                    kb.op("act", lambda: nc.scalar.copy(out=ketm[b][:], in_=psb[:, b * 128:(b + 1) * 128]),
                          reads=[psb_d[b]], writes=[ketm_d[b]])
                pa = b
                for g in range(4):
                    kb.op("pe", lambda: nc.tensor.matmul(ps[pa][:, g * 128:(g + 1) * 128], lhsT=Kmask[:, g, ch], rhs=QdT[:, ch],
                                                         start=True, stop=True),
                          reads=[Km_d[tq], Qd_d[tq]], writes=[ps_d[pa]], inc=(g == 3))
                kb.op("dve", lambda: nc.vector.tensor_tensor(out=attm[b][:], in0=ps[pa][:], in1=cmc("causal", 512), op=ALU.mult),
                      reads=[ps_d[pa], const_d], writes=[attm_d[b]])
                po = 2 + b
                if c > 0:
                    kb.op("pe", lambda: nc.tensor.matmul(ps[po][:, 0:256], lhsT=QdT[:, ch], rhs=Sbf[:], start=True, stop=False),
                          reads=[Qd_d[tq], Sb_d], writes=[ps_d[po]], inc=False)
                for h in range(4):
                    kb.op("pe", lambda: nc.tensor.matmul(ps[po][:, h * 64:(h + 1) * 64], lhsT=attm[b][:, h * 128:(h + 1) * 128],
                                                         rhs=vt[:, c, h * 64:(h + 1) * 64], start=(c == 0 and h == 0), stop=(h == 3)),
                          reads=[attm_d[b], v_d[c]], writes=[ps_d[po]], inc=(h == 3))
                if c < 15:
                    kb.op("pe", lambda: nc.tensor.matmul(ps[4][:, 0:256], lhsT=ketm[b][:], rhs=vt[:, c, :], start=True, stop=True),
                          reads=[ketm_d[b], v_d[c]], writes=[ps_d[4]])
                    if c == 0:
                        kb.op("dve", lambda: nc.vector.tensor_tensor(out=S_run[:], in0=ps[4][:, 0:256], in1=cmc("bdm4", 256), op=ALU.mult),
                              reads=[ps_d[4], const_d, Sb_d], writes=[S_d])
                    else:
                        kb.op("dve", lambda: nc.vector.tensor_tensor(out=S_tmp[:], in0=ps[4][:, 0:256], in1=cmc("bdm4", 256), op=ALU.mult),
                              reads=[ps_d[4], const_d], writes=[St_d])
                        kb.op("dve", lambda: nc.vector.scalar_tensor_tensor(out=S_run[:], in0=S_run[:], scalar=cdec[:, c:c + 1], in1=S_tmp[:],
                                                                            op0=ALU.mult, op1=ALU.add),
                              reads=[S_d, St_d, cdec_d], writes=[S_d])
                    kb.op("act", lambda: nc.scalar.copy(out=Sbf[:], in_=S_run[:]), reads=[S_d], writes=[Sb_d])
                if STAGE == 2:
                    if c == 15:
                        mix_zero(0, None, None, mixT, mix_d, st)
                    continue
                post_norm(c, ps[po][:, 0:256], ps_d[po], 4, gname_idx, sg, sg_d, mixT, mix_d, tl)

        def kq_finish(st, qsrc, q_d, ksrc, k_d, dq, dki, dke, dec_d, QdT, Qd_d, Kmask, Km_d, kendT, ke_d, tt, kinv, kinv_d):
            sl = slice(tt * 512, (tt + 1) * 512)
            kb.op("dve", lambda: nc.vector.tensor_tensor(out=QdT[:, sl], in0=qsrc, in1=dq, op=ALU.mult),
                  reads=[q_d, dec_d], writes=[Qd_d[tt]])
            kb.op("dve", lambda: nc.vector.tensor_tensor(out=kinv[:], in0=ksrc, in1=dki, op=ALU.mult),
                  reads=[k_d, dec_d], writes=[kinv_d])
            kb.op("dve", lambda: nc.vector.tensor_tensor(out=kendT[:, sl], in0=ksrc, in1=dke, op=ALU.mult),
                  reads=[k_d, dec_d], writes=[ke_d[tt]])
            for h in range(4):
                eng = "pool" if h % 2 == 0 else "dve"
                e = nc.gpsimd if eng == "pool" else nc.vector
                kb.op(eng, lambda: e.tensor_scalar(out=Kmask[:, h, sl], in0=kinv[:], scalar1=cmc("hm4", 4)[:, h:h + 1], scalar2=None,
                                                   op0=ALU.mult), reads=[kinv_d, const_d], writes=[Km_d[tt]])

        def la_tiles(st):
            return dict(QdT=st("QdT", [128, S], BF16), Qd_d=[Dep() for _ in range(4)],
                        Kmask=st("Kmask", [128, 4, S], BF16), Km_d=[Dep() for _ in range(4)],
                        kendT=st("kendT", [128, S], BF16), ke_d=[Dep() for _ in range(4)],
                        vt=st("vt", [128, 16, 256], BF16), v_d=[Dep() for _ in range(16)],
                        sg=st("sg", [128, 2, S], BF16), sg_d=[[Dep() for _ in range(4)] for _ in range(2)],
                        kinv=st("kinv", [128, 512], F32), kinv_d=Dep())

        def mix_gla(l, wg, wg_d, mixT, mix_d, st):
            T = la_tiles(st)
            w2f = st("w2f", [16, 128], F32)
            w2b = st("w2b", [16, 128], BF16)
            w2_d = Dep()
            c1 = kb.chan()
            kb.dma(c1, w2f[:], gw2_d[l * 16:(l + 1) * 16, :], writes=[w2_d])
            kb.op("dve", lambda: nc.vector.tensor_copy(out=w2b[:], in_=w2f[:]), reads=[w2_d], writes=[w2_d])
            nb2 = st("nb2", [128, 1], F32)
            kb.op("pool", lambda: nc.gpsimd.tensor_scalar(out=nb2[:], in0=ppc("gla_b2n", l), scalar1=-1.0, scalar2=None, op0=ALU.mult),
                  reads=[const_d], writes=[w2_d])
            ggT = st("ggT", [16, S], BF16)
            gg_d = [Dep() for _ in range(4)]
            bcs = st("bcs", [128, S], F32)
            bcs_d = [Dep() for _ in range(4)]
            spt = [st("spt", [128, 512], F32) for _ in range(2)]
            spt_d = [Dep(), Dep()]
            for tt in range(4):
                sl = slice(tt * 512, (tt + 1) * 512)
                pi = prr.next()
                proj_fm(pi, wg, wg_d, 768, 16, tt)
                kb.op("act", lambda: nc.scalar.copy(out=ggT[:, sl], in_=ps[pi][0:16, :]), reads=[ps_d[pi]], writes=[gg_d[tt]])
                pj = prr.next()
                kb.op("pe", lambda: nc.tensor.matmul(ps[pj][:], lhsT=w2b[:], rhs=ggT[:, sl], start=True, stop=True),
                      reads=[w2_d, gg_d[tt]], writes=[ps_d[pj]])
                b = tt % 2
                kb.op("act", lambda: nc.scalar.activation(out=spt[b][:], in_=ps[pj][:], func=AF.Exp, bias=nb2[:], scale=-1.0),
                      reads=[ps_d[pj], w2_d], writes=[spt_d[b]])
                kb.op("act", lambda: nc.scalar.activation(out=spt[b][:], in_=spt[b][:], func=AF.Ln, bias=cv[:, 1:2], scale=1.0),
                      reads=[spt_d[b], const_d], writes=[spt_d[b]])
                for ci in range(4):
                    cs_ = slice(ci * 128, (ci + 1) * 128)
                    gs_ = slice(tt * 512 + ci * 128, tt * 512 + (ci + 1) * 128)
                    kb.op("dve", lambda: nc.vector.tensor_tensor_scan(out=bcs[:, gs_], data0=cmc("ones", 128), data1=spt[b][:, cs_],
                                                                      initial=0.0, op0=ALU.mult, op1=ALU.add),
                          reads=[spt_d[b], const_d], writes=[bcs_d[tt]])
            nbl = st("nbl", [128, 16], F32)
            cdec = st("cdec", [128, 16], F32)
            nbl_d, cdec_d = Dep(), Dep()
            blast = bcs[:].rearrange("p (c i) -> p c i", i=128)[:, :, 127]
            kb.op("dve", lambda: nc.vector.tensor_scalar(out=nbl[:], in0=blast, scalar1=-1.0 / 16.0, scalar2=None, op0=ALU.mult),
                  reads=bcs_d, writes=[nbl_d])
            kb.op("act", lambda: nc.scalar.activation(out=cdec[:], in_=nbl[:], func=AF.Exp), reads=[nbl_d], writes=[cdec_d])
            dq = [st("dq", [128, 512], F32)] * 2
            dki = [st("dki", [128, 512], F32)] * 2
            dke = [st("dke", [128, 512], F32)] * 2
            dec_d = [Dep()] * 2
            for tt in range(4):
                sl = slice(tt * 512, (tt + 1) * 512)
                b = tt % 2
                kb.op("act", lambda: nc.scalar.activation(out=dq[b][:], in_=bcs[:, sl], func=AF.Exp, bias=cv[:, 2:3], scale=-1.0 / 16.0),
                      reads=[bcs_d[tt], const_d], writes=[dec_d[b]])
                kb.op("act", lambda: nc.scalar.activation(out=dki[b][:], in_=bcs[:, sl], func=AF.Exp, scale=1.0 / 16.0),
                      reads=[bcs_d[tt]], writes=[dec_d[b]])
                for ci in range(4):
                    c = tt * 4 + ci
                    kb.op("act", lambda: nc.scalar.activation(out=dke[b][:, ci * 128:(ci + 1) * 128], in_=bcs[:, c * 128:(c + 1) * 128],
                                                              func=AF.Exp, bias=nbl[:, c:c + 1], scale=1.0 / 16.0),
                          reads=[bcs_d[tt], nbl_d], writes=[dec_d[b]])
                pq = prr.next()
                proj_fm(pq, wg, wg_d, 0, 128, tt)
                pk = prr.next()
                proj_fm(pk, wg, wg_d, 128, 128, tt)
                kq_finish(st, ps[pq][:], ps_d[pq], ps[pk][:], ps_d[pk], dq[b][:], dki[b][:], dke[b][:], dec_d[b],
                          T["QdT"], T["Qd_d"], T["Kmask"], T["Km_d"], T["kendT"], T["ke_d"], tt, T["kinv"], T["kinv_d"])
            v_tokmajor(T["vt"], T["v_d"], wg, wg_d, 256)
            gate_fm(T["sg"], T["sg_d"], wg, wg_d, 512)
            linattn(st, T["Kmask"], T["Km_d"], T["QdT"], T["Qd_d"], T["kendT"], T["ke_d"], T["vt"], T["v_d"], cdec, cdec_d,
                    lambda cc: ("gla_ng", l), T["sg"], T["sg_d"], mixT, mix_d)

        def mix_ret(l, wg, wg_d, mixT, mix_d, st):
            T = la_tiles(st)
            rcs2 = [st("rcs", [128, 1024], F32) for _ in range(2)]
            rcs_d = [Dep(), Dep()]
            rcs_c = [kb.chan(), kb.chan()]
            rt = st("rt", [128, 1552], F32)
            rt_d = Dep()
            c1 = kb.chan()
            kb.dma(c1, rt[:], rett_d[:, :], writes=[rt_d])
            qb = [st("qb", [128, 512], BF16) for _ in range(2)]
            t1 = [st("t1", [128, 512], F32)] * 2
            t2 = [st("t2", [128, 512], F32)] * 2
            qr = [st("qr", [128, 512], F32) for _ in range(2)]
            qb_d, t1_d, t2_d, qr_d = [Dep(), Dep()], [Dep()] * 2, [Dep()] * 2, [Dep(), Dep()]
            for tt in range(4):
                sl = slice(tt * 512, (tt + 1) * 512)
                rb = tt % 2
                rcs = rcs2[rb]
                kb.dma(rcs_c[rb], rcs[:, 0:512], retcs_d[:, sl], writes=[rcs_d[rb]])
                kb.dma(rcs_c[rb], rcs[:, 512:1024], retcs_d[:, S + tt * 512: S + (tt + 1) * 512], writes=[rcs_d[rb]])
                for w in range(2):
                    pi = prr.next()
                    proj_fm(pi, wg, wg_d, w * 128, 128, tt)
                    kb.op("act", lambda: nc.scalar.copy(out=qb[w][:], in_=ps[pi][:]), reads=[ps_d[pi]], writes=[qb_d[w]])
                    pj = prr.next()
                    kb.op("pe", lambda: nc.tensor.matmul(ps[pj][:], lhsT=cmb[:, 2, :], rhs=qb[w][:], start=True, stop=True),
                          reads=[qb_d[w], const_d], writes=[ps_d[pj]])
                    kb.op("dve", lambda: nc.vector.tensor_tensor(out=t1[w][:], in0=ps[pi][:], in1=rcs[:, 0:512], op=ALU.mult),
                          reads=[ps_d[pi], rcs_d[rb]], writes=[t1_d[w]])
                    kb.op("dve", lambda: nc.vector.tensor_tensor(out=t2[w][:], in0=ps[pj][:], in1=rcs[:, 512:1024],
                                                                 op=ALU.mult), reads=[ps_d[pj], rcs_d[rb]], writes=[t2_d[w]])
                    kb.op("pool", lambda: nc.gpsimd.tensor_tensor(out=qr[w][:], in0=t1[w][:], in1=t2[w][:], op=ALU.add),
                          reads=[t1_d[w], t2_d[w]], writes=[qr_d[w]])
                kq_finish(st, qr[0][:], qr_d[0], qr[1][:], qr_d[1], rt[:, 0:512], rt[:, 512:1024], rt[:, 1024:1536], rt_d,
                          T["QdT"], T["Qd_d"], T["Kmask"], T["Km_d"], T["kendT"], T["ke_d"], tt, T["kinv"], T["kinv_d"])
            v_tokmajor(T["vt"], T["v_d"], wg, wg_d, 256)
            gate_fm(T["sg"], T["sg_d"], wg, wg_d, 512)
            if STAGE == 1:
                return mix_zero(l, wg, wg_d, mixT, mix_d, st)
            linattn(st, T["Kmask"], T["Km_d"], T["QdT"], T["Qd_d"], T["kendT"], T["ke_d"], T["vt"], T["v_d"], rt[:, 1536:1552], rt_d,
                    lambda cc: ("ret_ng", l), T["sg"], T["sg_d"], mixT, mix_d)

        def mix_ssm(l, wg, wg_d, mixT, mix_d, st):
            rw = st("rw", [128, 384], F32)
            rw_d = Dep()
            c1 = kb.chan()
            kb.dma(c1, rw[:], rows_d[:, l * 384:(l + 1) * 384], writes=[rw_d])
            kb.op("act", lambda: nc.scalar.activation(out=rw[:, 64:128], in_=rw[:, 64:128], func=AF.Exp), reads=[rw_d], writes=[rw_d])
            kb.op("dve", lambda: nc.vector.tensor_scalar(out=rw[:, 64:128], in0=rw[:, 64:128], scalar1=-1.0, scalar2=None, op0=ALU.mult),
                  reads=[rw_d], writes=[rw_d])
            dtt = st("dtt", [128, 64], F32)
            at = st("at", [128, 64], F32)
            acs = st("acs", [128, 64], F32)
            alast = st("alast", [128, 64], F32)
            eacs = st("eacs", [128, 64], F32)
            cdec = st("cdec", [128, 64], F32)
            dte = st("dte", [128, 64], F32)
            sm_d = Dep()
            for i in range(16):
                pi = prr.next()
                proj_tm(pi, 0, wg, wg_d, 768, 4, i)
                kb.op("dve", lambda: nc.vector.tensor_tensor(out=dtt[:, i * 4:(i + 1) * 4], in0=ps[pi][:, 0:4], in1=rw[:, i * 4:(i + 1) * 4], op=ALU.add),
                      reads=[ps_d[pi], rw_d], writes=[sm_d])
            kb.op("act", lambda: nc.scalar.activation(out=dtt[:], in_=dtt[:], func=AF.Exp), reads=[sm_d], writes=[sm_d])
            kb.op("act", lambda: nc.scalar.activation(out=dtt[:], in_=dtt[:], func=AF.Ln, bias=cv[:, 1:2], scale=1.0), reads=[sm_d, const_d], writes=[sm_d])
            kb.op("dve", lambda: nc.vector.tensor_tensor(out=at[:], in0=dtt[:], in1=rw[:, 64:128], op=ALU.mult), reads=[sm_d, rw_d], writes=[sm_d])
            pa = prr.next()
            kb.op("pe", lambda: nc.tensor.matmul(ps[pa][:, 0:64], lhsT=cmc("causal", 128), rhs=at[:], start=True, stop=True),
                  reads=[sm_d, const_d], writes=[ps_d[pa]])
            kb.op("act", lambda: nc.scalar.copy(out=acs[:], in_=ps[pa][:, 0:64]), reads=[ps_d[pa]], writes=[sm_d])
            pb = prr.next()
            kb.op("pe", lambda: nc.tensor.matmul(ps[pb][:, 0:64], lhsT=cmc("ones", 128), rhs=at[:], start=True, stop=True),
                  reads=[sm_d, const_d], writes=[ps_d[pb]])
            kb.op("act", lambda: nc.scalar.copy(out=alast[:], in_=ps[pb][:, 0:64]), reads=[ps_d[pb]], writes=[sm_d])
            kb.op("act", lambda: nc.scalar.activation(out=eacs[:], in_=acs[:], func=AF.Exp), reads=[sm_d], writes=[sm_d])
            kb.op("act", lambda: nc.scalar.activation(out=cdec[:], in_=alast[:], func=AF.Exp), reads=[sm_d], writes=[sm_d])
            kb.op("dve", lambda: nc.vector.tensor_tensor(out=dte[:], in0=alast[:], in1=acs[:], op=ALU.subtract), reads=[sm_d], writes=[sm_d])
            kb.op("act", lambda: nc.scalar.activation(out=dte[:], in_=dte[:], func=AF.Exp), reads=[sm_d], writes=[sm_d])
            if STAGE == 11:
                return mix_zero(l, wg, wg_d, mixT, mix_d, st)
            upre = st("upre", [128, S + 3], BF16)
            up_d = [Dep() for _ in range(5)]
            kb.op("pool", lambda: nc.gpsimd.memset(upre[:, 0:3], 0.0), writes=[up_d[0]])
            acc = st("acc", [128, 512], F32)
            acc_d = Dep()
            xsT = st("xsT", [128, 2, S], BF16)
            xs_d = [[Dep() for _ in range(4)] for _ in range(2)]
            Bm = st("Bm", [128, 2, S], BF16)
            Bm_d = [Dep() for _ in range(4)]
            CT = st("CT", [128, S], BF16)
            CT_d = [Dep() for _ in range(4)]
            for cc in range(4):
                for tt in range(4):
                    sl = slice(tt * 512, (tt + 1) * 512)
                    pi = prr.next()
                    proj_fm(pi, wg, wg_d, 256 + cc * 128, 128, tt)
                    kb.op("act", lambda: nc.scalar.copy(out=upre[:, 3 + tt * 512: 3 + (tt + 1) * 512], in_=ps[pi][:]),
                          reads=[ps_d[pi]], writes=[up_d[tt + 1]])
                    kb.op("act", lambda: nc.scalar.activation(out=acc[:], in_=ps[pi][:], func=AF.Identity,
                                                              bias=ppc("ssm_cb", l * 4 + cc), scale=ppc("ssm_cw", (l * 4 + 3) * 4 + cc)),
                          reads=[ps_d[pi], const_d], writes=[acc_d])
                    for k in range(1, 4):
                        kb.op("dve", lambda: nc.vector.scalar_tensor_tensor(
                            out=acc[:], in0=upre[:, 3 - k + tt * 512: 3 - k + (tt + 1) * 512], scalar=ppc("ssm_cw", (l * 4 + 3 - k) * 4 + cc),
                            in1=acc[:], op0=ALU.mult, op1=ALU.add),
                            reads=[up_d[tt + 1], up_d[tt], acc_d, const_d], writes=[acc_d])
                    if cc < 2:
                        kb.op("act", lambda: nc.scalar.activation(out=xsT[:, cc, sl], in_=acc[:], func=AF.Silu), reads=[acc_d], writes=[xs_d[cc][tt]])
                    elif cc == 3:
                        kb.op("act", lambda: nc.scalar.activation(out=CT[:, sl], in_=acc[:], func=AF.Silu), reads=[acc_d], writes=[CT_d[tt]])
                    else:
                        kb.op("act", lambda: nc.scalar.activation(out=acc[:], in_=acc[:], func=AF.Silu), reads=[acc_d], writes=[acc_d])
                        for g in range(2):
                            kb.op("pool", lambda: nc.gpsimd.tensor_scalar(out=Bm[:, g, sl], in0=acc[:], scalar1=cmc("hm2", 2)[:, g:g + 1],
                                                                          scalar2=None, op0=ALU.mult), reads=[acc_d, const_d], writes=[Bm_d[tt]])
            if STAGE == 12:
                return mix_zero(l, wg, wg_d, mixT, mix_d, st)
            vt = st("vt", [128, 16, 256], BF16)
            xsD = st("xsD", [128, 16, 256], BF16)
            Btm = st("Btm", [128, 16, 128], BF16)
            szt = st("szt", [128, 16, 256], BF16)
            xs_tm = [st("xs_tm", [128, 256], BF16) for _ in range(2)]
            xtm_d = [Dep(), Dep()]
            v_d = [Dep() for _ in range(16)]
            xd_d = [Dep() for _ in range(16)]
            bt_d = [Dep() for _ in range(16)]
            sz_d = [Dep() for _ in range(16)]
            for c in range(16):
                ch = slice(c * 128, (c + 1) * 128)
                s0 = 0
                srcs = [(xsT[:, 0, ch], xs_d[0][c // 4]), (xsT[:, 1, ch], xs_d[1][c // 4]), (Bm[:, 0, ch], Bm_d[c // 4]), (Bm[:, 1, ch], Bm_d[c // 4])]
                for k, (ap_, d_) in enumerate(srcs):
                    kb.op("pe", lambda: nc.tensor.transpose(out=psb[:, (s0 + k) * 128:(s0 + k + 1) * 128], in_=ap_, identity=identb),
                          reads=[d_, const_d], writes=[psb_d[s0 + k]], inc=(k == 3))
                xb = xs_tm[c % 2]
                kb.op("act", lambda: nc.scalar.copy(out=xb[:], in_=psb[:, 0:256]), reads=[psb_d[0]], writes=[xtm_d[c % 2]])
                kb.op("pool", lambda: nc.gpsimd.tensor_tensor(out=xsD[:, c, :], in0=xb[:], in1=rw[:, 128:384], op=ALU.mult),
                      reads=[xtm_d[c % 2], rw_d], writes=[xd_d[c]])
                for cc in range(2):
                    pt_ = psb[:, (s0 + cc) * 128:(s0 + cc + 1) * 128]
                    for hh in range(2):
                        h = cc * 2 + hh
                        kb.op("act", lambda: nc.scalar.activation(out=vt[:, c, h * 64:(h + 1) * 64], in_=pt_[:, hh * 64:(hh + 1) * 64], func=AF.Copy,
                                                                  scale=dtt[:, c * 4 + h: c * 4 + h + 1]),
                              reads=[psb_d[s0 + cc], sm_d], writes=[v_d[c]])
                for g in range(2):
                    kb.op("act", lambda: nc.scalar.copy(out=Btm[:, c, g * 64:(g + 1) * 64],
                                                        in_=psb[:, (s0 + 2 + g) * 128 + g * 64:(s0 + 2 + g) * 128 + (g + 1) * 64]),
                          reads=[psb_d[s0 + 2 + g]], writes=[bt_d[c]])
                pi = prr.next()
                proj_tm(pi, 0, wg, wg_d, 0, 256, c)
                kb.op("act", lambda: nc.scalar.activation(out=szt[:, c, :], in_=ps[pi][:, 0:256], func=AF.Silu), reads=[ps_d[pi]], writes=[sz_d[c]])
            if STAGE == 13:
                return mix_zero(l, wg, wg_d, mixT, mix_d, st)
            tl = post_tiles(st)
            scm = st("scm", [128, 256], F32)
            Mh = [st("Mh", [128, 128], F32) for _ in range(4)]
            dec = st("dec", [128, 512], F32)
            attm = [st("attm", [128, 512], BF16) for _ in range(2)]
            yt = [st("yt", [128, 256], F32) for _ in range(2)]
            vend = st("vend", [128, 256], BF16)
            S_run = st("S_run", [128, 256], F32)
            S_tmp = st("S_tmp", [128, 256], F32)
            Sbf = st("Sbf", [128, 256], BF16)
            scm_d, Mh_d, dec_d, attm_d, yt_d, vend_d = Dep(), [Dep() for _ in range(4)], Dep(), [Dep(), Dep()], [Dep(), Dep()], Dep()
            S_d, St_d, Sb_d = Dep(), Dep(), Dep()
            for c in range(16):
                ch = slice(c * 128, (c + 1) * 128)
                b = c % 2
                tq = c // 4
                pa = b
                for g in range(2):
                    kb.op("pe", lambda: nc.tensor.matmul(ps[pa][:, g * 128:(g + 1) * 128], lhsT=Bm[:, g, ch], rhs=CT[:, ch], start=True, stop=True),
                          reads=[Bm_d[tq], CT_d[tq]], writes=[ps_d[pa]], inc=(g == 1))
                kb.op("dve", lambda: nc.vector.tensor_tensor(out=scm[:], in0=ps[pa][:, 0:256], in1=cmc("causal", 256), op=ALU.mult),
                      reads=[ps_d[pa], const_d], writes=[scm_d])
                pg = 5 + b
                for h in range(4):
                    mb = h
                    kb.op("pool", lambda: nc.gpsimd.tensor_scalar(out=Mh[mb][:], in0=cmc("sgt", 128), scalar1=at[:, c * 4 + h: c * 4 + h + 1],
                                                                  scalar2=None, op0=ALU.mult), reads=[const_d, sm_d], writes=[Mh_d[mb]])
                    kb.op("pe", lambda: nc.tensor.matmul(ps[pg][:, h * 128:(h + 1) * 128], lhsT=Mh[mb][:], rhs=cmc("causal", 128), start=True, stop=True),
                          reads=[Mh_d[mb], const_d], writes=[ps_d[pg]], inc=(h == 3))
                kb.op("act", lambda: nc.scalar.activation(out=dec[:], in_=ps[pg][:], func=AF.Exp), reads=[ps_d[pg]], writes=[dec_d])
                for h in range(4):
                    g = h // 2
                    kb.op("dve", lambda: nc.vector.tensor_tensor(out=attm[b][:, h * 128:(h + 1) * 128], in0=scm[:, g * 128:(g + 1) * 128],
                                                                 in1=dec[:, h * 128:(h + 1) * 128], op=ALU.mult),
                          reads=[scm_d, dec_d], writes=[attm_d[b]])
                po = 2 + b
                if c > 0:
                    kb.op("pe", lambda: nc.tensor.matmul(ps[po][:, 256:512], lhsT=CT[:, ch], rhs=Sbf[:], start=True, stop=True),
                          reads=[CT_d[tq], Sb_d], writes=[ps_d[po]], inc=False)
                for h in range(4):
                    kb.op("pe", lambda: nc.tensor.matmul(ps[po][:, h * 64:(h + 1) * 64], lhsT=attm[b][:, h * 128:(h + 1) * 128],
                                                         rhs=vt[:, c, h * 64:(h + 1) * 64], start=(h == 0), stop=(h == 3)),
                          reads=[attm_d[b], v_d[c]], writes=[ps_d[po]], inc=(h == 3))
                kb.op("dve", lambda: nc.vector.tensor_tensor(out=yt[b][:], in0=ps[po][:, 0:256], in1=xsD[:, c, :], op=ALU.add),
                      reads=[ps_d[po], xd_d[c]], writes=[yt_d[b]])
                if c > 0:
                    for h in range(4):
                        kb.op("dve", lambda: nc.vector.scalar_tensor_tensor(out=yt[b][:, h * 64:(h + 1) * 64], in0=ps[po][:, 256 + h * 64: 256 + (h + 1) * 64],
                                                                            scalar=eacs[:, c * 4 + h: c * 4 + h + 1], in1=yt[b][:, h * 64:(h + 1) * 64],
                                                                            op0=ALU.mult, op1=ALU.add),
                              reads=[ps_d[po], sm_d, yt_d[b]], writes=[yt_d[b]])
                kb.op("pool", lambda: nc.gpsimd.tensor_tensor(out=yt[b][:], in0=yt[b][:], in1=szt[:, c, :], op=ALU.mult),
                      reads=[yt_d[b], sz_d[c]], writes=[yt_d[b]])
                if c < 15:
                    for h in range(4):
                        kb.op("pool", lambda: nc.gpsimd.tensor_scalar(out=vend[:, h * 64:(h + 1) * 64], in0=vt[:, c, h * 64:(h + 1) * 64],
                                                                      scalar1=dte[:, c * 4 + h: c * 4 + h + 1], scalar2=None, op0=ALU.mult),
                              reads=[v_d[c], sm_d], writes=[vend_d])
                    kb.op("pe", lambda: nc.tensor.matmul(ps[4][:, 0:256], lhsT=Btm[:, c, :], rhs=vend[:], start=True, stop=True),
                          reads=[bt_d[c], vend_d], writes=[ps_d[4]])
                    if c == 0:
                        kb.op("dve", lambda: nc.vector.tensor_tensor(out=S_run[:], in0=ps[4][:, 0:256], in1=cmc("bdm2", 256), op=ALU.mult),
                              reads=[ps_d[4], const_d, Sb_d], writes=[S_d])
                    else:
                        kb.op("dve", lambda: nc.vector.tensor_tensor(out=S_tmp[:], in0=ps[4][:, 0:256], in1=cmc("bdm2", 256), op=ALU.mult),
                              reads=[ps_d[4], const_d], writes=[St_d])
                        for h in range(4):
                            kb.op("dve", lambda: nc.vector.scalar_tensor_tensor(out=S_run[:, h * 64:(h + 1) * 64], in0=S_run[:, h * 64:(h + 1) * 64],
                                                                                scalar=cdec[:, c * 4 + h: c * 4 + h + 1], in1=S_tmp[:, h * 64:(h + 1) * 64],
                                                                                op0=ALU.mult, op1=ALU.add),
                                  reads=[S_d, St_d, sm_d], writes=[S_d])
                    kb.op("act", lambda: nc.scalar.copy(out=Sbf[:], in_=S_run[:]), reads=[S_d], writes=[Sb_d])
                post_norm(c, yt[b][:], yt_d[b], 2, lambda cc: ("ssm_ng", 2 * l + cc), None, None, mixT, mix_d, tl)

        def mix_moba(l, wg, wg_d, mixT, mix_d, st):
            qa = [st("qa", [72, S], BF16) for _ in range(4)]
            ka = [st("ka", [72, S], BF16) for _ in range(4)]
            qa_d = [[Dep() for _ in range(4)] for _ in range(4)]
            ka_d = [[Dep() for _ in range(4)] for _ in range(4)]
            qb_d = [Dep() for _ in range(4)]
            kb_d = [Dep()] * 4
            va = st("va", [128, 16, 4, 128], BF16)
            va_d = [Dep() for _ in range(16)]
            c1 = kb.chan()
            for h in range(4):
                kb.dma(c1, ka[h][64:72, :], blk_d[:, :], writes=[kb_d[h]], q="pool")
                kb.op("pool", lambda: nc.gpsimd.memset(qa[h][64:72, :], 0.0), writes=[qb_d[h]])
            for i in range(16):
                kb.op("pool", lambda: nc.gpsimd.memset(va[:, i, :, 64:128], 1.0), writes=[va_d[i]])
            mcs = [st("mcs", [128, 1024], F32) for _ in range(2)]
            mcs_d = [Dep(), Dep()]
            mcs_c = [kb.chan(), kb.chan()]
            sqb = st("sqb", [128, 512], BF16)
            rs = st("rs", [128, 512], F32)
            qn = st("qn", [128, 512], F32)
            qnb = st("qnb", [128, 512], BF16)
            t1 = st("t1", [128, 512], F32)
            t2 = st("t2", [128, 512], F32)
            qrot = st("qrot", [128, 512], F32)
            sqb_d, rs_d, qn_d, qnb_d, t1_d, t2_d, qrot_d = Dep(), Dep(), Dep(), Dep(), Dep(), Dep(), Dep()
            kms = st("kms", [128, 2, 8], F32)
            kms_d = Dep()
            km = [st("km", [64, 8], BF16) for _ in range(4)]
            km_d = [Dep() for _ in range(4)]
            for tt in range(4):
                sl = slice(tt * 512, (tt + 1) * 512)
                rb = tt % 2
                kb.dma(mcs_c[rb], mcs[rb][:, 0:512], mobacs_d[:, sl], writes=[mcs_d[rb]])
                kb.dma(mcs_c[rb], mcs[rb][:, 512:1024], mobacs_d[:, S + tt * 512: S + (tt + 1) * 512], writes=[mcs_d[rb]])
                for w in range(2):
                    for pr in range(2):
                        pi = prr.next()
                        proj_fm(pi, wg, wg_d, w * 256 + pr * 128, 128, tt)
                        kb.op("act", lambda: nc.scalar.activation(out=sqb[:], in_=ps[pi][:], func=AF.Square), reads=[ps_d[pi]], writes=[sqb_d])
                        pj = prr.next()
                        kb.op("pe", lambda: nc.tensor.matmul(ps[pj][:], lhsT=cmb[:, 4, :], rhs=sqb[:], start=True, stop=True),
                              reads=[sqb_d, const_d], writes=[ps_d[pj]])
                        kb.op("act", lambda: nc.scalar.activation(out=rs[:], in_=ps[pj][:], func=AF.Ln, bias=cv[:, 0:1], scale=1.0 / 64.0),
                              reads=[ps_d[pj], const_d], writes=[rs_d])
                        kb.op("act", lambda: nc.scalar.activation(out=rs[:], in_=rs[:], func=AF.Exp, scale=-0.5), reads=[rs_d], writes=[rs_d])
                        gn = "moba_qg" if w == 0 else "moba_kg"
                        kb.op("dve", lambda: nc.vector.scalar_tensor_tensor(out=qn[:], in0=ps[pi][:], scalar=ppc(gn, l), in1=rs[:],
                                                                            op0=ALU.mult, op1=ALU.mult),
                              reads=[ps_d[pi], rs_d, const_d], writes=[qn_d])
                        kb.op("act", lambda: nc.scalar.copy(out=qnb[:], in_=qn[:]), reads=[qn_d], writes=[qnb_d])
                        pk = prr.next()
                        kb.op("pe", lambda: nc.tensor.matmul(ps[pk][:], lhsT=cmb[:, 3, :], rhs=qnb[:], start=True, stop=True),
                              reads=[qnb_d, const_d], writes=[ps_d[pk]])
                        kb.op("dve", lambda: nc.vector.tensor_tensor(out=t1[:], in0=qn[:], in1=mcs[rb][:, 0:512], op=ALU.mult),
                              reads=[qn_d, mcs_d[rb]], writes=[t1_d])
                        kb.op("dve", lambda: nc.vector.tensor_tensor(out=t2[:], in0=ps[pk][:], in1=mcs[rb][:, 512:1024], op=ALU.mult),
                              reads=[ps_d[pk], mcs_d[rb]], writes=[t2_d])
                        kb.op("pool", lambda: nc.gpsimd.tensor_tensor(out=qrot[:], in0=t1[:], in1=t2[:], op=ALU.add),
                              reads=[t1_d, t2_d], writes=[qrot_d])
                        for hh in range(2):
                            h = pr * 2 + hh
                            dst = (qa if w == 0 else ka)[h]
                            dd = (qa_d if w == 0 else ka_d)[h][tt]
                            kb.op("act", lambda: nc.scalar.copy(out=dst[0:64, sl], in_=qrot[hh * 64:(hh + 1) * 64, :]), reads=[qrot_d], writes=[dd])
                        if w == 1:
                            kb.op("dve", lambda: nc.vector.tensor_reduce(out=kms[:, pr, 2 * tt:2 * tt + 2], in_=qrot[:].rearrange("p (n j) -> p n j", j=256),
                                                                         axis=AX.X, op=ALU.add), reads=[qrot_d], writes=[kms_d])
            for h in range(4):
                pr, hh = h // 2, h % 2
                kb.op("act", lambda: nc.scalar.activation(out=km[h][:], in_=kms[hh * 64:(hh + 1) * 64, pr, :], func=AF.Copy, scale=1.0 / 256.0),
                      reads=[kms_d], writes=[km_d[h]])
            for i in range(16):
                pi = prr.next()
                proj_tm(pi, 0, wg, wg_d, 512, 256, i)
                evac(i, va[:, i, :, 0:64], ps[pi][:, 0:256].rearrange("p (h e) -> p h e", h=4), [ps_d[pi]], [va_d[i]])
            gm = st("gm", [128, 32], F32)
            mx = st("mx", [128, 32], F32)
            selp = [st("selp", [128, 72], BF16) for _ in range(4)]
            gm_d, mx_d = Dep(), Dep()
            selp_d = [Dep() for _ in range(4)]
            for h in range(4):
                kb.op("pool", lambda: nc.gpsimd.memset(selp[h][:], 0.0), writes=[selp_d[h]])
            for i in range(8, 16):
                b = i // 2
                tq = i // 4
                pg = 6
                for h in range(4):
                    kb.op("pe", lambda: nc.tensor.matmul(ps[pg][:, h * 8:(h + 1) * 8], lhsT=qa[h][0:64, i * 128:(i + 1) * 128], rhs=km[h][:],
                                                         start=True, stop=True), reads=[qa_d[h][tq], km_d[h]], writes=[ps_d[pg]], inc=(h == 3))
                kb.op("dve", lambda: nc.vector.tensor_tensor(out=gm[:], in0=ps[pg][:, 0:32], in1=cmc("gmask", 256)[:, b * 32:(b + 1) * 32], op=ALU.add),
                      reads=[ps_d[pg], const_d], writes=[gm_d])
                for h in range(4):
                    kb.op("dve", lambda: nc.vector.max(out=mx[:, h * 8:(h + 1) * 8], in_=gm[:, h * 8:(h + 1) * 8]), reads=[gm_d], writes=[mx_d])
                for h in range(4):
                    kb.op("dve", lambda: nc.vector.tensor_scalar(out=selp[h][:, 64:72], in0=gm[:, h * 8:(h + 1) * 8], scalar1=mx[:, h * 8 + 2:h * 8 + 3],
                                                                 scalar2=-30000.0, op0=ALU.is_lt, op1=ALU.mult),
                          reads=[gm_d, mx_d], writes=[selp_d[h]])
                for h in range(4):
                    kb.op("pe", lambda: nc.tensor.transpose(out=psb[0:72, h * 128:(h + 1) * 128], in_=selp[h][:], identity=identb),
                          reads=[selp_d[h], const_d], writes=[psb_d[h]], inc=(h == 3))
                for h in range(4):
                    kb.op("act", lambda: nc.scalar.copy(out=qa[h][64:72, i * 128:(i + 1) * 128], in_=psb[64:72, h * 128:(h + 1) * 128]),
                          reads=[psb_d[h]], writes=[qb_d[h]])
            pt = [st("pt", [128, 256], BF16) for _ in range(3)]
            pt_d = [Dep() for _ in range(3)]
            rec = [st("rec", [64, 256], F32) for _ in range(2)]
            rec_d = [Dep(), Dep()]
            nst = 0
            nacc = 0
            for h in range(4):
                for b in range(8):
                    po = 4 + nacc % 2
                    rbi = nacc % 2
                    nacc += 1
                    qs = slice(b * 256, (b + 1) * 256)
                    tq = b // 2
                    njt = 2 * b + 2
                    for jt in range(njt):
                        own = jt >= 2 * b
                        qlo = jt - 2 * b
                        c0 = 128 if (own and qlo == 1) else 0
                        n = 256 - c0
                        qcols = slice(b * 256 + c0, (b + 1) * 256)
                        pi = nst % 4
                        pb_ = nst % 3
                        nst += 1
                        kk = 64 if own else 72
                        rds = [ka_d[h][jt // 4], qa_d[h][tq]] + ([] if own else [kb_d[h], qb_d[h]])
                        kb.op("pe", lambda: nc.tensor.matmul(ps[pi][:, 0:n], lhsT=ka[h][0:kk, jt * 128:(jt + 1) * 128], rhs=qa[h][0:kk, qcols],
                                                             start=True, stop=True), reads=rds, writes=[ps_d[pi]])
                        kb.op("act", lambda: nc.scalar.activation(out=pt[pb_][:, 0:n], in_=ps[pi][:, 0:n], func=AF.Exp, scale=0.125),
                              reads=[ps_d[pi]], writes=[pt_d[pb_]])
                        if own:
                            kb.op("dve", lambda: nc.vector.tensor_tensor(out=pt[pb_][:, 0:128], in0=pt[pb_][:, 0:128], in1=cmc("causal", 128), op=ALU.mult),
                                  reads=[pt_d[pb_], const_d], writes=[pt_d[pb_]])
                        kb.op("pe", lambda: nc.tensor.matmul(ps[po][:, c0:256], lhsT=va[:, jt, h, :], rhs=pt[pb_][:, 0:n],
                                                             start=(jt == 0), stop=(jt == njt - 1)),
                              reads=[va_d[jt], pt_d[pb_]], writes=[ps_d[po]])
                    kb.op("dve", lambda: nc.vector.reciprocal(out=rec[rbi][:], in_=ps[po][64:128, 0:256]), reads=[ps_d[po]], writes=[rec_d[rbi]])
                    hh = h % 2
                    kb.op("dve", lambda: nc.vector.tensor_tensor(out=mixT[hh * 64:(hh + 1) * 64, h // 2, qs], in0=ps[po][0:64, 0:256], in1=rec[rbi][:],
                                                                 op=ALU.mult), reads=[ps_d[po], rec_d[rbi]], writes=[mix_d[h // 2][tq]])

        def mix_zero(l, wg, wg_d, mixT, mix_d, st):
            for cc in range(2):
                kb.op("pool", lambda: nc.gpsimd.memset(mixT[:, cc, :], 0.0), writes=mix_d[cc])

        mix_fns = [mix_moba, mix_ssm, mix_gla, mix_ret]

        def mixer_phase(l, s):
            mk_ = kb.mark()
            with contextlib.ExitStack() as sc:
                st = lambda name, shape, dt: sc.enter_context(nc.sbuf_tensor(kb.un(name), shape, dt))
                wg = st("wg", [128, KC, GW], BF16)
                wo = st("wo", [128, 2, D], BF16)
                wg_d, wo_d = Dep(), Dep()
                wg_c, wo_c = kb.chan(), kb.chan()
                mixT = st("mixT", [128, 2, S], BF16)
                mix_d = [[Dep() for _ in range(4)] for _ in range(2)]
                for m in mixers:
                    r0 = (l * 4 + m) * 128
                    kb.dma(wg_c, wg[:].rearrange("p k c -> p (k c)"), win_b[r0:r0 + 128, :], writes=[wg_d])
                    kb.dma(wo_c, wo[:].rearrange("p k c -> p (k c)"), wout_b[r0:r0 + 128, :], writes=[wo_d])
                    with contextlib.ExitStack() as sc2:
                        st2 = lambda name, shape, dt: sc2.enter_context(nc.sbuf_tensor(kb.un(name), shape, dt))
                        mix_fns[m](l, wg, wg_d, mixT, mix_d, st2)
                        kb.barrier()
                    if debug and s == 0 and l == 0:
                        dc_ = kb.chan()
                        kb.dma(dc_, dbg_d[m], mixT[:].rearrange("p k c -> p (k c)"), reads=[d for dd in mix_d for d in dd])
                    for dc in range(KC):
                        for tt in range(4):
                            sl = slice(tt * 512, (tt + 1) * 512)
                            pi = (dc * 4 + tt) % 4
                            for k2 in range(2):
                                kb.op("pe", lambda: nc.tensor.matmul(ps[pi][:], lhsT=wo[:, k2, dc * 128:(dc + 1) * 128], rhs=mixT[:, k2, sl],
                                                                     start=(k2 == 0), stop=(k2 == 1)),
                                      reads=[wo_d, mix_d[k2][tt]], writes=[ps_d[pi]], inc=(k2 == 1))
                            kb.op("dve", lambda: nc.vector.tensor_tensor(out=xT[:, dc, sl], in0=ps[pi][:], in1=xT[:, dc, sl], op=ALU.add),
                                  reads=[ps_d[pi], xT_d[dc][tt]], writes=[xT_d[dc][tt]])
                    kb.barrier()
            kb.release(mk_)

        for s in range(n_seq):
            load_x(s)
            for l in range(L):
                norm("attn_g", l)
                if mixers:
                    mixer_phase(l, s)
                if not SKIP_FFN:
                    norm("ffn_g", l)
                    ffn(l)
            store_x(s)
        kb.barrier()
    return nc


def kernel(**inputs):
    L = 2
    n_seq = 4
    x = np.asarray(inputs["x"], np.float32)
    hp = host_prep(inputs, L)
    nc = build(n_seq, L)
    in_maps = []
    for c in range(NCORES):
        m = dict(hp)
        m["x"] = np.ascontiguousarray(x[c * n_seq:(c + 1) * n_seq].reshape(n_seq * S, D))
        in_maps.append(m)
    res = run_bass_kernel_spmd(nc, in_maps, core_ids=list(range(NCORES)))
    out = np.stack([r["y"].reshape(n_seq, S, D) for r in res.results], axis=0)
    return out.reshape(NCORES * n_seq, S, D).astype(np.float32)
```

```python
import contextlib
import numpy as np
import concourse.bass as bass
import concourse.mybir as mybir
from concourse.bass_utils import run_bass_kernel_spmd
from concourse.alu_op_type import AluOpType as ALU

F32 = mybir.dt.float32
BF16 = mybir.dt.bfloat16
AF = mybir.ActivationFunctionType
AX = mybir.AxisListType

D = 1024
S = 2048
KC = 8
NCORES = 8
FF = 2816
NJ = 22
EPS = 1e-6
IN_W = 3092
GOFF = (0, 768, 1540, 2324)
GW = 784


class Dep:
    __slots__ = ("w", "r")

    def __init__(self):
        self.w = None
        self.r = {}


class Chan:
    def __init__(self, sem):
        self.sem = sem
        self.n = 0


class KB:
    def __init__(self, nc, es):
        self.nc = nc
        self.es = es
        self.eng = {"pe": nc.tensor, "act": nc.scalar, "dve": nc.vector, "pool": nc.gpsimd, "sp": nc.sync}
        self.sem = {e: es.enter_context(nc.semaphore("s_" + e)) for e in self.eng}
        self.cnt = {e: 0 for e in self.eng}
        self.seen = {e: {} for e in self.eng}
        self.chans = []
        self.nchan = 0

    def un(self, name):
        self.nuniq = getattr(self, "nuniq", 0) + 1
        return "%s_u%d" % (name, self.nuniq)

    def chan(self):
        if getattr(self, "free", None):
            c = self.free.pop()
        else:
            self.nchan += 1
            c = Chan(self.es.enter_context(self.nc.semaphore("c%d" % self.nchan)))
            self.chans.append(c)
        if not hasattr(self, "live"):
            self.live = []
        self.live.append(c)
        return c

    def mark(self):
        if not hasattr(self, "live"):
            self.live = []
        return len(self.live)

    def release(self, mark):
        if not hasattr(self, "free"):
            self.free = []
        self.free.extend(self.live[mark:])
        del self.live[mark:]

    @staticmethod
    def _need(reads, writes):
        need = {}

        def add(k, v):
            if need.get(k, 0) < v:
                need[k] = v

        for d in reads:
            if d.w is not None:
                add(*d.w)
        for d in writes:
            if d.w is not None:
                add(*d.w)
            for k, v in d.r.items():
                add(k, v)
        return need

    def _waits(self, eng, need):
        e = self.eng[eng]
        seen = self.seen[eng]
        for k, v in need.items():
            if k == "pe" and eng == "pe":
                continue
            if seen.get(k, 0) >= v:
                continue
            seen[k] = v
            if isinstance(k, Chan):
                e.wait_ge(k.sem, v)
            else:
                e.wait_ge(self.sem[k], v)

    def op(self, eng, fn, reads=(), writes=(), inc=True):
        self._waits(eng, self._need(reads, writes))
        ins = fn()
        if inc:
            self.cnt[eng] += 1
            ins.then_inc(self.sem[eng], 1)
            c = self.cnt[eng]
        else:
            c = self.cnt[eng] + 1
        for d in reads:
            if d.r.get(eng, 0) < c:
                d.r[eng] = c
        for d in writes:
            d.w = (eng, c)
            d.r = {}
        return ins

    def dma(self, chan, out, in_, reads=(), writes=(), q="sp"):
        self._waits(q, self._need(reads, writes))
        ins = self.eng[q].dma_start(out=out, in_=in_)
        chan.n += 16
        ins.then_inc(chan.sem, 16)
        for d in reads:
            d.r[chan] = chan.n
        for d in writes:
            d.w = (chan, chan.n)
            d.r = {}
        return ins

    def barrier(self):
        for e in self.eng:
            need = {k: v for k, v in self.cnt.items() if v > 0 and k != e}
            for c in self.chans:
                if c.n > 0:
                    need[c] = c.n
            self._waits(e, need)
        for e in ("act", "dve", "pool"):
            if self.cnt[e] > 0 and self.seen[e].get(e, 0) < self.cnt[e]:
                self.seen[e][e] = self.cnt[e]
                self.eng[e].wait_ge(self.sem[e], self.cnt[e])


class Ring:
    def __init__(self, items):
        self.items = items
        self.i = 0

    def next(self):
        it = self.items[self.i % len(self.items)]
        self.i += 1
        return it


def pp_layout(L):
    off = {}
    n = 0

    def add(name, cnt):
        nonlocal n
        off[name] = n
        n += cnt

    add("attn_g", L * KC)
    add("ffn_g", L * KC)
    add("fcw", L * 3 * 2 * NJ)
    add("fcb", L * 2 * NJ)
    add("gla_b2n", L)
    add("gla_ng", L)
    add("ret_ng", L)
    add("ssm_ng", L * 2)
    add("ssm_cw", L * 4 * 4)
    add("ssm_cb", L * 4)
    add("moba_qg", L)
    add("moba_kg", L)
    return off, n


def host_prep(inputs, L):
    f = np.float32
    w_in = np.asarray(inputs["w_in"], f)[:L]
    win = np.zeros((L, 4, 128, KC, GW), f)
    for g in range(4):
        w = (GOFF + (IN_W,))[g + 1] - GOFF[g]
        blk = w_in[:, :, GOFF[g]:GOFF[g] + w].reshape(L, KC, 128, w)
        win[:, g, :, :, :w] = blk.transpose(0, 2, 1, 3)
    win = win.reshape(L * 4 * 128, KC * GW)
    w_out = np.asarray(inputs["w_out"], f)[:L]
    wout = w_out.reshape(L, 4, 2, 128, D).transpose(0, 1, 3, 2, 4).reshape(L * 4 * 128, 2 * D)
    w_up = np.asarray(inputs["ffn_w_up"], f)[:L]
    wu = w_up.reshape(L, KC, 128, 2, NJ, 128)
    wup = wu.transpose(0, 4, 2, 1, 3, 5).reshape(L * NJ * 128, KC * 256)
    wdn = np.asarray(inputs["ffn_w_down"], f)[:L].reshape(L * FF, D)
    off, npp = pp_layout(L)
    pp = np.zeros((128, npp), f)

    def fm(v):
        return np.asarray(v, f).reshape(-1, 128).T

    for l in range(L):
        pp[:, off["attn_g"] + l * KC: off["attn_g"] + (l + 1) * KC] = fm(inputs["attn_norm_g"][l])
        pp[:, off["ffn_g"] + l * KC: off["ffn_g"] + (l + 1) * KC] = fm(inputs["ffn_norm_g"][l])
        for i in range(3):
            o = off["fcw"] + (l * 3 + i) * 2 * NJ
            pp[:, o:o + 2 * NJ] = fm(inputs["ffn_conv_w"][l][i])
        o = off["fcb"] + l * 2 * NJ
        pp[:, o:o + 2 * NJ] = fm(inputs["ffn_conv_b"][l])
    for l in range(L):
        pp[:, off["gla_b2n"] + l] = np.asarray(inputs["gla_gate_b"][l], f)
        pp[:, off["gla_ng"] + l] = np.tile(np.asarray(inputs["gla_norm_g"][l], f), 2)
        pp[:, off["ret_ng"] + l] = np.tile(np.asarray(inputs["ret_norm_g"][l], f), 2)
        pp[:, off["ssm_ng"] + 2 * l: off["ssm_ng"] + 2 * l + 2] = fm(inputs["ssm_norm_g"][l])
        for i in range(4):
            o = off["ssm_cw"] + (l * 4 + i) * 4
            pp[:, o:o + 4] = fm(inputs["ssm_conv_w"][l][i])
        pp[:, off["ssm_cb"] + 4 * l: off["ssm_cb"] + 4 * l + 4] = fm(inputs["ssm_conv_b"][l])
        pp[:, off["moba_qg"] + l] = np.tile(np.asarray(inputs["moba_q_norm_g"][l], f), 2)
        pp[:, off["moba_kg"] + l] = np.tile(np.asarray(inputs["moba_k_norm_g"][l], f), 2)
    consts = host_consts()
    w2 = np.asarray(inputs["gla_gate_w2"], f)[:L].reshape(L * 16, 128)
    consts["gw2"] = np.ascontiguousarray(w2)
    rows = np.zeros((128, L * 384), f)
    for l in range(L):
        rows[:, l * 384: l * 384 + 64] = np.tile(np.asarray(inputs["ssm_dt_bias"][l], f), 16)[None, :]
        rows[:, l * 384 + 64: l * 384 + 128] = np.tile(np.asarray(inputs["ssm_a_log"][l], f), 16)[None, :]
        rows[:, l * 384 + 128: l * 384 + 384] = np.repeat(np.asarray(inputs["ssm_d"][l], f), 64)[None, :]
    consts["rows"] = rows
    return dict(win=np.ascontiguousarray(win), wout=np.ascontiguousarray(wout),
                wup=np.ascontiguousarray(wup), wdn=np.ascontiguousarray(wdn), pp=pp, **consts)


CM_OFF = {}
STAGE = 0
SKIP_FFN = False


def host_consts():
    f = np.float32
    p = np.arange(128)
    cols = []

    def add(name, a):
        CM_OFF[name] = sum(c.shape[1] for c in cols)
        cols.append(np.asarray(a, f))

    add("ident", np.eye(128))
    add("ones", np.ones((128, 128)))
    caus = (p[:, None] <= p[None, :]).astype(f)
    add("causal", np.tile(caus, (1, 4)))
    add("bdm4", (p[:, None] // 32 == np.arange(256)[None, :] // 64))
    add("bdm2", (p[:, None] // 64 == np.arange(256)[None, :] // 128))
    add("hm4", (p[:, None] // 32 == np.arange(4)[None, :]))
    add("hm2", (p[:, None] // 64 == np.arange(2)[None, :]))
    def perm(hd):
        half = hd // 2
        m = np.arange(128)
        partner = (m // hd) * hd + ((m % hd) + half) % hd
        P = np.zeros((128, 128), f)
        P[partner, m] = 1.0
        return P
    add("perm32", perm(32))
    add("perm64", perm(64))
    add("bo64", (p[:, None] // 64 == p[None, :] // 64))
    add("sgt", (p[:, None] > p[None, :]))
    gmk = np.zeros((128, 8, 4, 8))
    for b_ in range(8):
        gmk[:, b_, :, b_:] = -1e30
    add("gmask", gmk.reshape(128, 256))
    cm = np.concatenate(cols, axis=1)
    lg = np.log(1.0 - 2.0 ** (-5.0 - np.arange(4, dtype=np.float64)))
    h = p // 32
    d = p % 32
    inv = 1.0 / (10000.0 ** np.linspace(0.0, 1.0, 16))
    t = np.arange(S, dtype=np.float64)
    ang = t[None, :] * inv[d % 16][:, None]
    rcos = np.cos(ang)
    rsin = np.sin(ang) * np.where(d < 16, -1.0, 1.0)[:, None]
    idx = np.arange(512) % 128
    sc = 32.0 ** -0.5
    rdq = np.exp((idx[None, :] + 1.0) * lg[h][:, None])
    rdki = np.exp(-(idx[None, :] + 1.0) * lg[h][:, None]) * sc
    rdke = np.exp((127.0 - idx[None, :]) * lg[h][:, None]) * sc
    rcd = np.repeat(np.exp(128.0 * lg[h])[:, None], 16, axis=1)
    rett = np.concatenate([rdq, rdki, rdke, rcd], axis=1).astype(f)
    dm = p % 64
    invm = 10000.0 ** (-np.arange(0, 64, 2, dtype=np.float64) / 64)
    angm = t[None, :] * invm[dm % 32][:, None]
    mcos = np.cos(angm)
    msin = np.sin(angm) * np.where(dm < 32, -1.0, 1.0)[:, None]
    blk = (np.arange(S)[None, :] // 256 == np.arange(8)[:, None]).astype(f)
    return {"blk1h": blk, "cm": cm, "ret_cs": np.concatenate([rcos, rsin], axis=1).astype(f), "ret_t": rett,
            "moba_cs": np.concatenate([mcos, msin], axis=1).astype(f)}


def build(n_seq, L, mixers=(0, 1, 2, 3), debug=False):
    nc = bass.Bass("TRN2", target_bir_lowering=False)
    off, npp = pp_layout(L)
    x_d = nc.dram_tensor("x", [n_seq * S, D], F32, kind="ExternalInput").ap()
    y_d = nc.dram_tensor("y", [n_seq * S, D], F32, kind="ExternalOutput").ap()
    win_d = nc.dram_tensor("win", [L * 4 * 128, KC * GW], F32, kind="ExternalInput").ap()
    wout_d = nc.dram_tensor("wout", [L * 4 * 128, 2 * D], F32, kind="ExternalInput").ap()
    wup_d = nc.dram_tensor("wup", [L * NJ * 128, KC * 256], F32, kind="ExternalInput").ap()
    wdn_d = nc.dram_tensor("wdn", [L * FF, D], F32, kind="ExternalInput").ap()
    pp_d = nc.dram_tensor("pp", [128, npp], F32, kind="ExternalInput").ap()
    if not CM_OFF:
        host_consts()
    NCM = CM_OFF["gmask"] + 256
    cm_d = nc.dram_tensor("cm", [128, NCM], F32, kind="ExternalInput").ap()
    retcs_d = nc.dram_tensor("ret_cs", [128, 2 * S], F32, kind="ExternalInput").ap()
    rett_d = nc.dram_tensor("ret_t", [128, 1552], F32, kind="ExternalInput").ap()
    mobacs_d = nc.dram_tensor("moba_cs", [128, 2 * S], F32, kind="ExternalInput").ap()
    gw2_d = nc.dram_tensor("gw2", [L * 16, 128], F32, kind="ExternalInput").ap()
    rows_d = nc.dram_tensor("rows", [128, L * 384], F32, kind="ExternalInput").ap()
    blk_d = nc.dram_tensor("blk1h", [8, S], F32, kind="ExternalInput").ap()
    dbg_d = nc.dram_tensor("dbg", [4, 128, 2 * S], BF16, kind="ExternalOutput").ap() if debug else None
    win_b = nc.dram_tensor("win_b", [L * 4 * 128, KC * GW], BF16, kind="Internal").ap()
    wout_b = nc.dram_tensor("wout_b", [L * 4 * 128, 2 * D], BF16, kind="Internal").ap()
    wup_b = nc.dram_tensor("wup_b", [L * NJ * 128, KC * 256], BF16, kind="Internal").ap()
    wdn_b = nc.dram_tensor("wdn_b", [L * FF, D], BF16, kind="Internal").ap()

    with contextlib.ExitStack() as es:
        kb = KB(nc, es)
        sb = lambda name, shape, dt: es.enter_context(nc.sbuf_tensor(name, shape, dt))

        cc = kb.chan()
        for src, dst in ((win_d, win_b), (wout_d, wout_b), (wup_d, wup_b), (wdn_d, wdn_b)):
            rows = src.shape[0]
            for r in range(0, rows, 128):
                kb.dma(cc, dst[r:r + 128, :], src[r:r + 128, :], q="pool")

        xT = sb("xT", [128, KC, S], F32)
        hT = sb("hT", [128, KC, S], BF16)
        xT_d = [[Dep() for _ in range(4)] for _ in range(KC)]
        hT_d = [[Dep() for _ in range(4)] for _ in range(KC)]
        ppt = sb("ppt", [128, npp], F32)
        cmt = sb("cmt", [128, NCM], F32)
        cmb = sb("cmb", [128, 5, 128], BF16)
        identf = cmt[:, 0:128]
        onesb = cmb[:, 1, :]
        identb = cmb[:, 0, :]

        def cmc(name, n):
            return cmt[:, CM_OFF[name]:CM_OFF[name] + n]
        const_d = Dep()
        c0 = kb.chan()
        kb.dma(c0, ppt[:], pp_d[:, :], writes=[const_d])
        kb.dma(c0, cmt[:], cm_d[:, :], writes=[const_d])
        for ii, nm in enumerate(("ident", "ones", "perm32", "perm64", "bo64")):
            kb.op("dve", lambda: nc.vector.tensor_copy(out=cmb[:, ii, :], in_=cmc(nm, 128)), reads=[const_d], writes=[const_d])
        cv = sb("cv", [128, 8], F32)
        kb.op("pool", lambda: nc.gpsimd.memset(cv[:, 0:1], EPS), writes=[const_d])
        ps = [es.enter_context(nc.psum_tensor("ps%d" % i, [128, 512], F32)) for i in range(7)]
        ps_d = [Dep() for _ in range(7)]
        psb = es.enter_context(nc.psum_tensor("psb", [128, 1024], BF16))
        psb_d = [Dep()] * 8
        kb.barrier()

        def ppc(name, idx):
            o = off[name] + idx
            return ppt[:, o:o + 1]

        def load_x(s):
            mk_ = kb.mark()
            with contextlib.ExitStack() as sc:
                xin = [sc.enter_context(nc.sbuf_tensor(kb.un("xin"), [128, D], F32)) for i in range(2)]
                xin_d = [Dep(), Dep()]
                xc = [kb.chan(), kb.chan()]
                for i in range(16):
                    b = i % 2
                    kb.dma(xc[b], xin[b][:], x_d[s * S + i * 128: s * S + (i + 1) * 128, :], writes=[xin_d[b]])
                    for hh in range(2):
                        pi = (i * 2 + hh) % 4
                        for k in range(4):
                            kc = hh * 4 + k
                            kb.op("pe", lambda: nc.tensor.transpose(out=ps[pi][:, k * 128:(k + 1) * 128],
                                                                    in_=xin[b][:, kc * 128:(kc + 1) * 128],
                                                                    identity=identf),
                                  reads=[xin_d[b], const_d], writes=[ps_d[pi]], inc=(k == 3))
                        eng = "act" if hh == 0 else "dve"
                        dst = xT[:, hh * 4:(hh + 1) * 4, i * 128:(i + 1) * 128]
                        src = ps[pi][:].rearrange("p (k c) -> p k c", k=4)
                        wr = [xT_d[hh * 4 + k][i // 4] for k in range(4)]
                        if eng == "act":
                            kb.op("act", lambda: nc.scalar.copy(out=dst, in_=src), reads=[ps_d[pi]], writes=wr)
                        else:
                            kb.op("dve", lambda: nc.vector.tensor_copy(out=dst, in_=src), reads=[ps_d[pi]], writes=wr)
                kb.barrier()
            kb.release(mk_)

        def store_x(s):
            mk_ = kb.mark()
            with contextlib.ExitStack() as sc:
                xo = [sc.enter_context(nc.sbuf_tensor(kb.un("xo"), [128, D], F32)) for i in range(2)]
                xo_d = [Dep(), Dep()]
                xc = [kb.chan(), kb.chan()]
                for i in range(16):
                    b = i % 2
                    for hh in range(2):
                        pi = (i * 2 + hh) % 4
                        for k in range(4):
                            kc = hh * 4 + k
                            kb.op("pe", lambda: nc.tensor.transpose(out=ps[pi][:, k * 128:(k + 1) * 128],
                                                                    in_=xT[:, kc, i * 128:(i + 1) * 128],
                                                                    identity=identf),
                                  reads=[xT_d[kc][i // 4], const_d], writes=[ps_d[pi]], inc=(k == 3))
                        dst = xo[b][:, hh * 512:(hh + 1) * 512]
                        if hh == 0:
                            kb.op("act", lambda: nc.scalar.copy(out=dst, in_=ps[pi][:]), reads=[ps_d[pi]], writes=[xo_d[b]])
                        else:
                            kb.op("dve", lambda: nc.vector.tensor_copy(out=dst, in_=ps[pi][:]), reads=[ps_d[pi]], writes=[xo_d[b]])
                    kb.dma(xc[b], y_d[s * S + i * 128: s * S + (i + 1) * 128, :], xo[b][:], reads=[xo_d[b]])
                kb.barrier()

            kb.release(mk_)

        def norm(gname, l):
            with contextlib.ExitStack() as sc:
                sq = [sc.enter_context(nc.sbuf_tensor(kb.un("sq"), [128, KC, 512], BF16)) for i in range(2)]
                rs = [sc.enter_context(nc.sbuf_tensor(kb.un("rs"), [128, 512], F32)) for i in range(2)]
                sq_d = [Dep(), Dep()]
                rs_d = [Dep(), Dep()]
                for tt in range(4):
                    b = tt % 2
                    sl = slice(tt * 512, (tt + 1) * 512)
                    kb.op("act", lambda: nc.scalar.activation(out=sq[b][:], in_=xT[:, :, sl], func=AF.Square),
                          reads=[xT_d[k][tt] for k in range(KC)], writes=[sq_d[b]])
                    pi = 4 + b
                    for kc in range(KC):
                        kb.op("pe", lambda: nc.tensor.matmul(ps[pi][:], lhsT=onesb, rhs=sq[b][:, kc, :],
                                                             start=(kc == 0), stop=(kc == KC - 1)),
                              reads=[sq_d[b], const_d], writes=[ps_d[pi]], inc=(kc == KC - 1))
                    kb.op("act", lambda: nc.scalar.activation(out=rs[b][:], in_=ps[pi][:], func=AF.Ln, bias=cv[:, 0:1], scale=1.0 / D),
                          reads=[ps_d[pi], const_d], writes=[rs_d[b]])
                    kb.op("act", lambda: nc.scalar.activation(out=rs[b][:], in_=rs[b][:], func=AF.Exp, scale=-0.5),
                          reads=[rs_d[b]], writes=[rs_d[b]])
                    for kc in range(KC):
                        kb.op("dve", lambda: nc.vector.scalar_tensor_tensor(out=hT[:, kc, sl], in0=xT[:, kc, sl],
                                                                            scalar=ppc(gname, l * KC + kc), in1=rs[b][:],
                                                                            op0=ALU.mult, op1=ALU.mult),
                              reads=[xT_d[kc][tt], rs_d[b], const_d], writes=[hT_d[kc][tt]])
                kb.barrier()

        def ffn(l):
            mk_ = kb.mark()
            G = 6
            groups = [list(range(a, min(a + G, NJ))) for a in range(0, NJ, G)]
            with contextlib.ExitStack() as sc:
                st = lambda name, shape, dt: sc.enter_context(nc.sbuf_tensor(kb.un(name), shape, dt))
                upre = [[st("upre%d_%d" % (b, h), [128, S + 2], BF16) for h in range(2)] for b in range(2)]
                upre_d = [[[Dep() for _ in range(5)] for h in range(2)] for b in range(2)]
                acc = [[st("acc%d_%d" % (r, h), [128, 512], F32) for h in range(2)] for r in range(3)]
                acc_d = [[Dep() for h in range(2)] for r in range(3)]
                actT = st("actT", [128, G, S], BF16)
                act_d = [[Dep() for _ in range(4)] for _ in range(G)]
                wup = [st("wup%d" % i, [128, KC, 256], BF16) for i in range(3)]
                wup_dd = [Dep() for _ in range(3)]
                wup_c = [kb.chan() for _ in range(3)]
                wdn = [st("wdn%d" % i, [128, D], BF16) for i in range(G)]
                wdn_dd = [Dep() for _ in range(G)]
                wdn_c = [kb.chan() for _ in range(G)]
                for b in range(2):
                    for h in range(2):
                        kb.op("pool", lambda: nc.gpsimd.memset(upre[b][h][:, 0:2], 0.0), writes=[upre_d[b][h][0]])
                accr = 0
                jcount = 0
                for grp in groups:
                    for jj, j in enumerate(grp):
                        kb.dma(wdn_c[jj], wdn[jj][:], wdn_b[l * FF + j * 128: l * FF + (j + 1) * 128, :], writes=[wdn_dd[jj]])
                    for jj, j in enumerate(grp):
                        ws = jcount % 3
                        ub = jcount % 2
                        jcount += 1
                        r0 = (l * NJ + j) * 128
                        kb.dma(wup_c[ws], wup[ws][:].rearrange("p k c -> p (k c)"), wup_b[r0:r0 + 128, :], writes=[wup_dd[ws]])
                        for tt in range(4):
                            sl = slice(tt * 512, (tt + 1) * 512)
                            ar = accr % 3
                            accr += 1
                            for h in range(2):
                                pi = (tt % 2) * 2 + h
                                for kc in range(KC):
                                    kb.op("pe", lambda: nc.tensor.matmul(ps[pi][:], lhsT=wup[ws][:, kc, h * 128:(h + 1) * 128],
                                                                         rhs=hT[:, kc, sl], start=(kc == 0), stop=(kc == KC - 1)),
                                          reads=[wup_dd[ws], hT_d[kc][tt]], writes=[ps_d[pi]], inc=(kc == KC - 1))
                                kb.op("act", lambda: nc.scalar.copy(out=upre[ub][h][:, 2 + tt * 512: 2 + (tt + 1) * 512], in_=ps[pi][:]),
                                      reads=[ps_d[pi]], writes=[upre_d[ub][h][tt + 1]])
                                w2 = ppc("fcw", (l * 3 + 2) * 2 * NJ + h * NJ + j)
                                w1 = ppc("fcw", (l * 3 + 1) * 2 * NJ + h * NJ + j)
                                w0 = ppc("fcw", (l * 3 + 0) * 2 * NJ + h * NJ + j)
                                bb = ppc("fcb", l * 2 * NJ + h * NJ + j)
                                kb.op("act", lambda: nc.scalar.activation(out=acc[ar][h][:], in_=ps[pi][:], func=AF.Identity,
                                                                          bias=bb, scale=w2),
                                      reads=[ps_d[pi], const_d], writes=[acc_d[ar][h]])
                                kb.op("dve", lambda: nc.vector.scalar_tensor_tensor(
                                    out=acc[ar][h][:], in0=upre[ub][h][:, 1 + tt * 512: 1 + (tt + 1) * 512], scalar=w1,
                                    in1=acc[ar][h][:], op0=ALU.mult, op1=ALU.add),
                                    reads=[upre_d[ub][h][tt + 1], upre_d[ub][h][tt], acc_d[ar][h], const_d], writes=[acc_d[ar][h]])
                                kb.op("dve", lambda: nc.vector.scalar_tensor_tensor(
                                    out=acc[ar][h][:], in0=upre[ub][h][:, tt * 512: (tt + 1) * 512], scalar=w0,
                                    in1=acc[ar][h][:], op0=ALU.mult, op1=ALU.add),
                                    reads=[upre_d[ub][h][tt + 1], upre_d[ub][h][tt], acc_d[ar][h], const_d], writes=[acc_d[ar][h]])
                            kb.op("act", lambda: nc.scalar.activation(out=acc[ar][0][:], in_=acc[ar][0][:], func=AF.Silu),
                                  reads=[acc_d[ar][0]], writes=[acc_d[ar][0]])
                            kb.op("pool", lambda: nc.gpsimd.tensor_tensor(out=actT[:, jj, sl], in0=acc[ar][0][:], in1=acc[ar][1][:],
                                                                          op=ALU.mult),
                                  reads=[acc_d[ar][0], acc_d[ar][1]], writes=[act_d[jj][tt]])
                    for dc in range(KC):
                        for tt in range(4):
                            sl = slice(tt * 512, (tt + 1) * 512)
                            pi = 4 + (dc * 4 + tt) % 3
                            for jj, j in enumerate(grp):
                                kb.op("pe", lambda: nc.tensor.matmul(ps[pi][:], lhsT=wdn[jj][:, dc * 128:(dc + 1) * 128],
                                                                     rhs=actT[:, jj, sl], start=(jj == 0), stop=(jj == len(grp) - 1)),
                                      reads=[wdn_dd[jj], act_d[jj][tt]], writes=[ps_d[pi]], inc=(jj == len(grp) - 1))
                            kb.op("dve", lambda: nc.vector.tensor_tensor(out=xT[:, dc, sl], in0=ps[pi][:], in1=xT[:, dc, sl], op=ALU.add),
                                  reads=[ps_d[pi], xT_d[dc][tt]], writes=[xT_d[dc][tt]])
                kb.barrier()
            kb.release(mk_)

        kb.op("pool", lambda: nc.gpsimd.memset(cv[:, 1:2], 1.0), writes=[const_d])
        kb.op("pool", lambda: nc.gpsimd.memset(cv[:, 2:3], float(np.log(32.0 ** -0.5))), writes=[const_d])
        kb.barrier()
        prr = Ring(list(range(7)))

        def proj_fm(pi, wg, wg_d, c0, M, tt):
            sl = slice(tt * 512, (tt + 1) * 512)
            for kc in range(KC):
                kb.op("pe", lambda: nc.tensor.matmul(ps[pi][0:M, :], lhsT=wg[:, kc, c0:c0 + M], rhs=hT[:, kc, sl],
                                                     start=(kc == 0), stop=(kc == KC - 1)),
                      reads=[wg_d, hT_d[kc][tt]], writes=[ps_d[pi]], inc=(kc == KC - 1))

        def proj_tm(pi, col0, wg, wg_d, c0, N, i):
            for kc in range(KC):
                kb.op("pe", lambda: nc.tensor.matmul(ps[pi][:, col0:col0 + N], lhsT=hT[:, kc, i * 128:(i + 1) * 128],
                                                     rhs=wg[:, kc, c0:c0 + N], start=(kc == 0), stop=(kc == KC - 1)),
                      reads=[wg_d, hT_d[kc][i // 4]], writes=[ps_d[pi]], inc=(kc == KC - 1))

        def evac(i, out, in_, reads, writes):
            if i % 2 == 0:
                kb.op("act", lambda: nc.scalar.copy(out=out, in_=in_), reads=reads, writes=writes)
            else:
                kb.op("dve", lambda: nc.vector.tensor_copy(out=out, in_=in_), reads=reads, writes=writes)

        def v_tokmajor(vt, v_d, wg, wg_d, c0):
            for i in range(16):
                pi = prr.next()
                proj_tm(pi, 0, wg, wg_d, c0, 256, i)
                evac(i, vt[:, i, :], ps[pi][:, 0:256], [ps_d[pi]], [v_d[i]])

        def gate_fm(sg, sg_d, wg, wg_d, c0):
            for cc in range(2):
                for tt in range(4):
                    pi = prr.next()
                    proj_fm(pi, wg, wg_d, c0 + cc * 128, 128, tt)
                    kb.op("act", lambda: nc.scalar.activation(out=sg[:, cc, tt * 512:(tt + 1) * 512], in_=ps[pi][:], func=AF.Silu),
                          reads=[ps_d[pi]], writes=[sg_d[cc][tt]])

        def post_norm(c, src, src_d, ng, gname_idx, sg, sg_d, mixT, mix_d, tl):
            w = 256 // ng
            b = c % 2
            sq, sq_d, ss, ss_d, on, on_d = tl["sq"][b], tl["sq_d"][b], tl["ss"][b], tl["ss_d"][b], tl["on"][b], tl["on_d"][b]
            kb.op("act", lambda: nc.scalar.activation(out=sq[:], in_=src, func=AF.Square), reads=[src_d], writes=[sq_d])
            kb.op("dve", lambda: nc.vector.tensor_reduce(out=ss[:, 0:ng], in_=sq[:].rearrange("p (g e) -> p g e", g=ng),
                                                         axis=AX.X, op=ALU.add), reads=[sq_d], writes=[ss_d])
            kb.op("act", lambda: nc.scalar.activation(out=ss[:, 0:ng], in_=ss[:, 0:ng], func=AF.Ln, bias=cv[:, 0:1], scale=1.0 / w),
                  reads=[ss_d, const_d], writes=[ss_d])
            kb.op("act", lambda: nc.scalar.activation(out=ss[:, 0:ng], in_=ss[:, 0:ng], func=AF.Exp, scale=-0.5),
                  reads=[ss_d], writes=[ss_d])
            for g in range(ng):
                kb.op("act", lambda: nc.scalar.activation(out=on[:, g * w:(g + 1) * w], in_=src[:, g * w:(g + 1) * w], func=AF.Copy,
                                                          scale=ss[:, g:g + 1]), reads=[src_d, ss_d], writes=[on_d])
            if STAGE == 3:
                if c == 15:
                    mix_zero(0, None, None, mixT, mix_d, None)
                return
            pt = 5 + b
            for cc in range(2):
                tin = sq if STAGE == 5 else on
                tin_d = sq_d if STAGE == 5 else on_d
                if STAGE == 7:
                    continue
                kb.op("pe", lambda: nc.tensor.transpose(out=ps[pt][:, cc * 128:(cc + 1) * 128], in_=tin[:, cc * 128:(cc + 1) * 128],
                                                        identity=identf), reads=[tin_d, const_d], writes=[ps_d[pt]], inc=(cc == 1))
            if STAGE == 6:
                if c == 15:
                    mix_zero(0, None, None, mixT, mix_d, None)
                return
            for cc in range(2):
                dst = mixT[:, cc, c * 128:(c + 1) * 128]
                srcT = ps[pt][:, cc * 128:(cc + 1) * 128]
                if sg is not None:
                    kb.op("dve", lambda: nc.vector.scalar_tensor_tensor(out=dst, in0=srcT,
                                                                        scalar=ppc(*gname_idx(cc)), in1=sg[:, cc, c * 128:(c + 1) * 128],
                                                                        op0=ALU.mult, op1=ALU.mult),
                          reads=[ps_d[pt], sg_d[cc][c // 4], const_d], writes=[mix_d[cc][c // 4]])
                else:
                    kb.op("dve", lambda: nc.vector.tensor_scalar(out=dst, in0=srcT,
                                                                 scalar1=ppc(*gname_idx(cc)), scalar2=None, op0=ALU.mult),
                          reads=[ps_d[pt], const_d], writes=[mix_d[cc][c // 4]])

        def post_tiles(st):
            return dict(sq=[st("sq", [128, 256], F32) for _ in range(2)], sq_d=[Dep(), Dep()],
                        ss=[st("ss", [128, 4], F32) for _ in range(2)], ss_d=[Dep(), Dep()],
                        on=[st("on", [128, 256], F32) for _ in range(2)], on_d=[Dep(), Dep()])

        def linattn(st, Kmask, Km_d, QdT, Qd_d, kendT, ke_d, vt, v_d, cdec, cdec_d, gname_idx, sg, sg_d, mixT, mix_d):
            tl = post_tiles(st)
            attm = [st("attm", [128, 512], BF16) for _ in range(2)]
            attm_d = [Dep(), Dep()]
            ketm = [st("ketm", [128, 128], BF16) for _ in range(2)]
            ketm_d = [Dep(), Dep()]
            S_run = st("S_run", [128, 256], F32)
            S_tmp = st("S_tmp", [128, 256], F32)
            Sbf = st("Sbf", [128, 256], BF16)
            S_d, St_d, Sb_d = Dep(), Dep(), Dep()
            attm.append(st("attm", [128, 512], BF16))
            attm_d.append(Dep())
            Sbfs = [Sbf] + [st("Sbf", [128, 256], BF16) for _ in range(3)]
            Sb_ds = [Dep() for _ in range(4)]

            def stage_a(c):
                ch = slice(c * 128, (c + 1) * 128)
                b = c % 2
                b3 = c % 3
                tq = c // 4
                if c < 15:
                    kb.op("pe", lambda: nc.tensor.transpose(out=psb[:, b * 128:(b + 1) * 128], in_=kendT[:, ch], identity=identb),
                          reads=[ke_d[tq], const_d], writes=[psb_d[b]])
                    kb.op("act", lambda: nc.scalar.copy(out=ketm[b][:], in_=psb[:, b * 128:(b + 1) * 128]),
                          reads=[psb_d[b]], writes=[ketm_d[b]])
                pa = b
                for g in range(4):
                    kb.op("pe", lambda: nc.tensor.matmul(ps[pa][:, g * 128:(g + 1) * 128], lhsT=Kmask[:, g, ch], rhs=QdT[:, ch],
                                                         start=True, stop=True),
                          reads=[Km_d[tq], Qd_d[tq]], writes=[ps_d[pa]], inc=(g == 3))
                kb.op("dve", lambda: nc.vector.tensor_tensor(out=attm[b3][:], in0=ps[pa][:], in1=cmc("causal", 512), op=ALU.mult),
                      reads=[ps_d[pa], const_d], writes=[attm_d[b3]])
                if c < 15:
                    kb.op("pe", lambda: nc.tensor.matmul(ps[4][:, 0:256], lhsT=ketm[b][:], rhs=vt[:, c, :], start=True, stop=True),
                          reads=[ketm_d[b], v_d[c]], writes=[ps_d[4]])
                    if c == 0:
                        kb.op("dve", lambda: nc.vector.tensor_tensor(out=S_run[:], in0=ps[4][:, 0:256], in1=cmc("bdm4", 256), op=ALU.mult),
                              reads=[ps_d[4], const_d], writes=[S_d])
                    else:
                        kb.op("dve", lambda: nc.vector.tensor_tensor(out=S_tmp[:], in0=ps[4][:, 0:256], in1=cmc("bdm4", 256), op=ALU.mult),
                              reads=[ps_d[4], const_d], writes=[St_d])
                        kb.op("dve", lambda: nc.vector.scalar_tensor_tensor(out=S_run[:], in0=S_run[:], scalar=cdec[:, c:c + 1], in1=S_tmp[:],
                                                                            op0=ALU.mult, op1=ALU.add),
                              reads=[S_d, St_d, cdec_d], writes=[S_d])
                    kb.op("act", lambda: nc.scalar.copy(out=Sbfs[c % 4][:], in_=S_run[:]), reads=[S_d], writes=[Sb_ds[c % 4]])

            def stage_b(c):
                ch = slice(c * 128, (c + 1) * 128)
                b = c % 2
                b3 = c % 3
                tq = c // 4
                po = 2 + b
                if c > 0:
                    kb.op("pe", lambda: nc.tensor.matmul(ps[po][:, 0:256], lhsT=QdT[:, ch], rhs=Sbfs[(c - 1) % 4][:], start=True, stop=False),
                          reads=[Qd_d[tq], Sb_ds[(c - 1) % 4]], writes=[ps_d[po]], inc=False)
                for h in range(4):
                    kb.op("pe", lambda: nc.tensor.matmul(ps[po][:, h * 64:(h + 1) * 64], lhsT=attm[b3][:, h * 128:(h + 1) * 128],
                                                         rhs=vt[:, c, h * 64:(h + 1) * 64], start=(c == 0 and h == 0), stop=(h == 3)),
                          reads=[attm_d[b3], v_d[c]], writes=[ps_d[po]], inc=(h == 3))

            def stage_c(c):
                po = 2 + c % 2
                post_norm(c, ps[po][:, 0:256], ps_d[po], 4, gname_idx, sg, sg_d, mixT, mix_d, tl)

            stage_a(0)
            stage_a(1)
            for c in range(16):
                stage_b(c)
                if c + 2 < 16:
                    stage_a(c + 2)
                if c >= 1:
                    stage_c(c - 1)
            stage_c(15)

        def kq_finish(st, qsrc, q_d, ksrc, k_d, dq, dki, dke, dec_d, QdT, Qd_d, Kmask, Km_d, kendT, ke_d, tt, kinv, kinv_d):
            sl = slice(tt * 512, (tt + 1) * 512)
            kb.op("dve", lambda: nc.vector.tensor_tensor(out=QdT[:, sl], in0=qsrc, in1=dq, op=ALU.mult),
                  reads=[q_d, dec_d], writes=[Qd_d[tt]])
            kb.op("dve", lambda: nc.vector.tensor_tensor(out=kinv[:], in0=ksrc, in1=dki, op=ALU.mult),
                  reads=[k_d, dec_d], writes=[kinv_d])
            kb.op("dve", lambda: nc.vector.tensor_tensor(out=kendT[:, sl], in0=ksrc, in1=dke, op=ALU.mult),
                  reads=[k_d, dec_d], writes=[ke_d[tt]])
            for h in range(4):
                if h < 2:
                    kb.op("act", lambda: nc.scalar.activation(out=Kmask[:, h, sl], in_=kinv[:], func=AF.Copy, scale=cmc("hm4", 4)[:, h:h + 1]),
                          reads=[kinv_d, const_d], writes=[Km_d[tt]])
                else:
                    kb.op("dve", lambda: nc.vector.tensor_scalar(out=Kmask[:, h, sl], in0=kinv[:], scalar1=cmc("hm4", 4)[:, h:h + 1], scalar2=None,
                                                                 op0=ALU.mult), reads=[kinv_d, const_d], writes=[Km_d[tt]])

        def la_tiles(st):
            return dict(QdT=st("QdT", [128, S], BF16), Qd_d=[Dep() for _ in range(4)],
                        Kmask=st("Kmask", [128, 4, S], BF16), Km_d=[Dep() for _ in range(4)],
                        kendT=st("kendT", [128, S], BF16), ke_d=[Dep() for _ in range(4)],
                        vt=st("vt", [128, 16, 256], BF16), v_d=[Dep() for _ in range(16)],
                        sg=st("sg", [128, 2, S], BF16), sg_d=[[Dep() for _ in range(4)] for _ in range(2)],
                        kinv=st("kinv", [128, 512], F32), kinv_d=Dep())

        def mix_gla(l, wg, wg_d, mixT, mix_d, st):
            T = la_tiles(st)
            w2f = st("w2f", [16, 128], F32)
            w2b = st("w2b", [16, 128], BF16)
            w2_d = Dep()
            c1 = kb.chan()
            kb.dma(c1, w2f[:], gw2_d[l * 16:(l + 1) * 16, :], writes=[w2_d])
            kb.op("dve", lambda: nc.vector.tensor_copy(out=w2b[:], in_=w2f[:]), reads=[w2_d], writes=[w2_d])
            nb2 = st("nb2", [128, 1], F32)
            kb.op("pool", lambda: nc.gpsimd.tensor_scalar(out=nb2[:], in0=ppc("gla_b2n", l), scalar1=-1.0, scalar2=None, op0=ALU.mult),
                  reads=[const_d], writes=[w2_d])
            ggT = st("ggT", [16, S], BF16)
            gg_d = [Dep() for _ in range(4)]
            bcs = st("bcs", [128, S], F32)
            bcs_d = [Dep() for _ in range(4)]
            spt = [st("spt", [128, 512], F32) for _ in range(2)]
            spt_d = [Dep(), Dep()]
            for tt in range(4):
                sl = slice(tt * 512, (tt + 1) * 512)
                pi = prr.next()
                proj_fm(pi, wg, wg_d, 768, 16, tt)
                kb.op("act", lambda: nc.scalar.copy(out=ggT[:, sl], in_=ps[pi][0:16, :]), reads=[ps_d[pi]], writes=[gg_d[tt]])
                pj = prr.next()
                kb.op("pe", lambda: nc.tensor.matmul(ps[pj][:], lhsT=w2b[:], rhs=ggT[:, sl], start=True, stop=True),
                      reads=[w2_d, gg_d[tt]], writes=[ps_d[pj]])
                b = tt % 2
                kb.op("act", lambda: nc.scalar.activation(out=spt[b][:], in_=ps[pj][:], func=AF.Exp, bias=nb2[:], scale=-1.0),
                      reads=[ps_d[pj], w2_d], writes=[spt_d[b]])
                kb.op("act", lambda: nc.scalar.activation(out=spt[b][:], in_=spt[b][:], func=AF.Ln, bias=cv[:, 1:2], scale=1.0),
                      reads=[spt_d[b], const_d], writes=[spt_d[b]])
                for ci in range(4):
                    cs_ = slice(ci * 128, (ci + 1) * 128)
                    gs_ = slice(tt * 512 + ci * 128, tt * 512 + (ci + 1) * 128)
                    kb.op("dve", lambda: nc.vector.tensor_tensor_scan(out=bcs[:, gs_], data0=cmc("ones", 128), data1=spt[b][:, cs_],
                                                                      initial=0.0, op0=ALU.mult, op1=ALU.add),
                          reads=[spt_d[b], const_d], writes=[bcs_d[tt]])
            nbl = st("nbl", [128, 16], F32)
            cdec = st("cdec", [128, 16], F32)
            nbl_d, cdec_d = Dep(), Dep()
            blast = bcs[:].rearrange("p (c i) -> p c i", i=128)[:, :, 127]
            kb.op("dve", lambda: nc.vector.tensor_scalar(out=nbl[:], in0=blast, scalar1=-1.0 / 16.0, scalar2=None, op0=ALU.mult),
                  reads=bcs_d, writes=[nbl_d])
            kb.op("act", lambda: nc.scalar.activation(out=cdec[:], in_=nbl[:], func=AF.Exp), reads=[nbl_d], writes=[cdec_d])
            dq = [st("dq", [128, 512], F32)] * 2
            dki = [st("dki", [128, 512], F32)] * 2
            dke = [st("dke", [128, 512], F32)] * 2
            dec_d = [Dep()] * 2
            for tt in range(4):
                sl = slice(tt * 512, (tt + 1) * 512)
                b = tt % 2
                kb.op("act", lambda: nc.scalar.activation(out=dq[b][:], in_=bcs[:, sl], func=AF.Exp, bias=cv[:, 2:3], scale=-1.0 / 16.0),
                      reads=[bcs_d[tt], const_d], writes=[dec_d[b]])
                kb.op("act", lambda: nc.scalar.activation(out=dki[b][:], in_=bcs[:, sl], func=AF.Exp, scale=1.0 / 16.0),
                      reads=[bcs_d[tt]], writes=[dec_d[b]])
                for ci in range(4):
                    c = tt * 4 + ci
                    kb.op("act", lambda: nc.scalar.activation(out=dke[b][:, ci * 128:(ci + 1) * 128], in_=bcs[:, c * 128:(c + 1) * 128],
                                                              func=AF.Exp, bias=nbl[:, c:c + 1], scale=1.0 / 16.0),
                          reads=[bcs_d[tt], nbl_d], writes=[dec_d[b]])
                pq = prr.next()
                proj_fm(pq, wg, wg_d, 0, 128, tt)
                pk = prr.next()
                proj_fm(pk, wg, wg_d, 128, 128, tt)
                kq_finish(st, ps[pq][:], ps_d[pq], ps[pk][:], ps_d[pk], dq[b][:], dki[b][:], dke[b][:], dec_d[b],
                          T["QdT"], T["Qd_d"], T["Kmask"], T["Km_d"], T["kendT"], T["ke_d"], tt, T["kinv"], T["kinv_d"])
            v_tokmajor(T["vt"], T["v_d"], wg, wg_d, 256)
            gate_fm(T["sg"], T["sg_d"], wg, wg_d, 512)
            linattn(st, T["Kmask"], T["Km_d"], T["QdT"], T["Qd_d"], T["kendT"], T["ke_d"], T["vt"], T["v_d"], cdec, cdec_d,
                    lambda cc: ("gla_ng", l), T["sg"], T["sg_d"], mixT, mix_d)

        def linattn_v1(st, Kmask, Km_d, QdT, Qd_d, kendT, ke_d, vt, v_d, cdec, cdec_d, gname_idx, sg, sg_d, mixT, mix_d):
            tl = post_tiles(st)
            attm = [st("attm", [128, 512], BF16) for _ in range(2)]
            attm_d = [Dep(), Dep()]
            ketm = [st("ketm", [128, 128], BF16) for _ in range(2)]
            ketm_d = [Dep(), Dep()]
            S_run = st("S_run", [128, 256], F32)
            S_tmp = st("S_tmp", [128, 256], F32)
            Sbf = st("Sbf", [128, 256], BF16)
            S_d, St_d, Sb_d = Dep(), Dep(), Dep()
            for c in range(16):
                ch = slice(c * 128, (c + 1) * 128)
                b = c % 2
                tq = c // 4
                if c < 15:
                    kb.op("pe", lambda: nc.tensor.transpose(out=psb[:, b * 128:(b + 1) * 128], in_=kendT[:, ch], identity=identb),
                          reads=[ke_d[tq], const_d], writes=[psb_d[b]])
                    kb.op("act", lambda: nc.scalar.copy(out=ketm[b][:], in_=psb[:, b * 128:(b + 1) * 128]),
                          reads=[psb_d[b]], writes=[ketm_d[b]])
                pa = b
                for g in range(4):
                    kb.op("pe", lambda: nc.tensor.matmul(ps[pa][:, g * 128:(g + 1) * 128], lhsT=Kmask[:, g, ch], rhs=QdT[:, ch],
                                                         start=True, stop=True),
                          reads=[Km_d[tq], Qd_d[tq]], writes=[ps_d[pa]], inc=(g == 3))
                kb.op("dve", lambda: nc.vector.tensor_tensor(out=attm[b][:], in0=ps[pa][:], in1=cmc("causal", 512), op=ALU.mult),
                      reads=[ps_d[pa], const_d], writes=[attm_d[b]])
                po = 2 + b
                if c > 0:
                    kb.op("pe", lambda: nc.tensor.matmul(ps[po][:, 0:256], lhsT=QdT[:, ch], rhs=Sbf[:], start=True, stop=False),
                          reads=[Qd_d[tq], Sb_d], writes=[ps_d[po]], inc=False)
                for h in range(4):
                    kb.op("pe", lambda: nc.tensor.matmul(ps[po][:, h * 64:(h + 1) * 64], lhsT=attm[b][:, h * 128:(h + 1) * 128],
                                                         rhs=vt[:, c, h * 64:(h + 1) * 64], start=(c == 0 and h == 0), stop=(h == 3)),
                          reads=[attm_d[b], v_d[c]], writes=[ps_d[po]], inc=(h == 3))
                if c < 15:
                    kb.op("pe", lambda: nc.tensor.matmul(ps[4][:, 0:256], lhsT=ketm[b][:], rhs=vt[:, c, :], start=True, stop=True),
                          reads=[ketm_d[b], v_d[c]], writes=[ps_d[4]])
                    if c == 0:
                        kb.op("dve", lambda: nc.vector.tensor_tensor(out=S_run[:], in0=ps[4][:, 0:256], in1=cmc("bdm4", 256), op=ALU.mult),
                              reads=[ps_d[4], const_d, Sb_d], writes=[S_d])
                    else:
                        kb.op("dve", lambda: nc.vector.tensor_tensor(out=S_tmp[:], in0=ps[4][:, 0:256], in1=cmc("bdm4", 256), op=ALU.mult),
                              reads=[ps_d[4], const_d], writes=[St_d])
                        kb.op("dve", lambda: nc.vector.scalar_tensor_tensor(out=S_run[:], in0=S_run[:], scalar=cdec[:, c:c + 1], in1=S_tmp[:],
                                                                            op0=ALU.mult, op1=ALU.add),
                              reads=[S_d, St_d, cdec_d], writes=[S_d])
                    kb.op("act", lambda: nc.scalar.copy(out=Sbf[:], in_=S_run[:]), reads=[S_d], writes=[Sb_d])
                if STAGE == 2:
                    if c == 15:
                        mix_zero(0, None, None, mixT, mix_d, st)
                    continue
                post_norm(c, ps[po][:, 0:256], ps_d[po], 4, gname_idx, sg, sg_d, mixT, mix_d, tl)

        def kq_finish_v1(st, qsrc, q_d, ksrc, k_d, dq, dki, dke, dec_d, QdT, Qd_d, Kmask, Km_d, kendT, ke_d, tt, kinv, kinv_d):
            sl = slice(tt * 512, (tt + 1) * 512)
            kb.op("dve", lambda: nc.vector.tensor_tensor(out=QdT[:, sl], in0=qsrc, in1=dq, op=ALU.mult),
                  reads=[q_d, dec_d], writes=[Qd_d[tt]])
            kb.op("dve", lambda: nc.vector.tensor_tensor(out=kinv[:], in0=ksrc, in1=dki, op=ALU.mult),
                  reads=[k_d, dec_d], writes=[kinv_d])
            kb.op("dve", lambda: nc.vector.tensor_tensor(out=kendT[:, sl], in0=ksrc, in1=dke, op=ALU.mult),
                  reads=[k_d, dec_d], writes=[ke_d[tt]])
            for h in range(4):
                eng = "pool" if h % 2 == 0 else "dve"
                e = nc.gpsimd if eng == "pool" else nc.vector
                kb.op(eng, lambda: e.tensor_scalar(out=Kmask[:, h, sl], in0=kinv[:], scalar1=cmc("hm4", 4)[:, h:h + 1], scalar2=None,
                                                   op0=ALU.mult), reads=[kinv_d, const_d], writes=[Km_d[tt]])

        def mix_ret(l, wg, wg_d, mixT, mix_d, st):
            T = la_tiles(st)
            rcs2 = [st("rcs", [128, 1024], F32) for _ in range(2)]
            rcs_d = [Dep(), Dep()]
            rcs_c = [kb.chan(), kb.chan()]
            rt = st("rt", [128, 1552], F32)
            rt_d = Dep()
            c1 = kb.chan()
            kb.dma(c1, rt[:], rett_d[:, :], writes=[rt_d])
            qb = [st("qb", [128, 512], BF16) for _ in range(2)]
            t1 = [st("t1", [128, 512], F32)] * 2
            t2 = [st("t2", [128, 512], F32)] * 2
            qr = [st("qr", [128, 512], F32) for _ in range(2)]
            qb_d, t1_d, t2_d, qr_d = [Dep(), Dep()], [Dep()] * 2, [Dep()] * 2, [Dep(), Dep()]
            for tt in range(4):
                sl = slice(tt * 512, (tt + 1) * 512)
                rb = tt % 2
                rcs = rcs2[rb]
                kb.dma(rcs_c[rb], rcs[:, 0:512], retcs_d[:, sl], writes=[rcs_d[rb]])
                kb.dma(rcs_c[rb], rcs[:, 512:1024], retcs_d[:, S + tt * 512: S + (tt + 1) * 512], writes=[rcs_d[rb]])
                for w in range(2):
                    pi = prr.next()
                    proj_fm(pi, wg, wg_d, w * 128, 128, tt)
                    kb.op("act", lambda: nc.scalar.copy(out=qb[w][:], in_=ps[pi][:]), reads=[ps_d[pi]], writes=[qb_d[w]])
                    pj = prr.next()
                    kb.op("pe", lambda: nc.tensor.matmul(ps[pj][:], lhsT=cmb[:, 2, :], rhs=qb[w][:], start=True, stop=True),
                          reads=[qb_d[w], const_d], writes=[ps_d[pj]])
                    kb.op("dve", lambda: nc.vector.tensor_tensor(out=t1[w][:], in0=ps[pi][:], in1=rcs[:, 0:512], op=ALU.mult),
                          reads=[ps_d[pi], rcs_d[rb]], writes=[t1_d[w]])
                    kb.op("dve", lambda: nc.vector.tensor_tensor(out=t2[w][:], in0=ps[pj][:], in1=rcs[:, 512:1024],
                                                                 op=ALU.mult), reads=[ps_d[pj], rcs_d[rb]], writes=[t2_d[w]])
                    kb.op("pool", lambda: nc.gpsimd.tensor_tensor(out=qr[w][:], in0=t1[w][:], in1=t2[w][:], op=ALU.add),
                          reads=[t1_d[w], t2_d[w]], writes=[qr_d[w]])
                kq_finish_v1(st, qr[0][:], qr_d[0], qr[1][:], qr_d[1], rt[:, 0:512], rt[:, 512:1024], rt[:, 1024:1536], rt_d,
                          T["QdT"], T["Qd_d"], T["Kmask"], T["Km_d"], T["kendT"], T["ke_d"], tt, T["kinv"], T["kinv_d"])
            v_tokmajor(T["vt"], T["v_d"], wg, wg_d, 256)
            gate_fm(T["sg"], T["sg_d"], wg, wg_d, 512)
            if STAGE == 1:
                return mix_zero(l, wg, wg_d, mixT, mix_d, st)
            linattn_v1(st, T["Kmask"], T["Km_d"], T["QdT"], T["Qd_d"], T["kendT"], T["ke_d"], T["vt"], T["v_d"], rt[:, 1536:1552], rt_d,
                    lambda cc: ("ret_ng", l), T["sg"], T["sg_d"], mixT, mix_d)

        def mix_ssm(l, wg, wg_d, mixT, mix_d, st):
            rw = st("rw", [128, 384], F32)
            rw_d = Dep()
            c1 = kb.chan()
            kb.dma(c1, rw[:], rows_d[:, l * 384:(l + 1) * 384], writes=[rw_d])
            kb.op("act", lambda: nc.scalar.activation(out=rw[:, 64:128], in_=rw[:, 64:128], func=AF.Exp), reads=[rw_d], writes=[rw_d])
            kb.op("dve", lambda: nc.vector.tensor_scalar(out=rw[:, 64:128], in0=rw[:, 64:128], scalar1=-1.0, scalar2=None, op0=ALU.mult),
                  reads=[rw_d], writes=[rw_d])
            dtt = st("dtt", [128, 64], F32)
            at = st("at", [128, 64], F32)
            acs = st("acs", [128, 64], F32)
            alast = st("alast", [128, 64], F32)
            eacs = st("eacs", [128, 64], F32)
            cdec = st("cdec", [128, 64], F32)
            dte = st("dte", [128, 64], F32)
            sm_d = Dep()
            for i in range(16):
                pi = prr.next()
                proj_tm(pi, 0, wg, wg_d, 768, 4, i)
                kb.op("dve", lambda: nc.vector.tensor_tensor(out=dtt[:, i * 4:(i + 1) * 4], in0=ps[pi][:, 0:4], in1=rw[:, i * 4:(i + 1) * 4], op=ALU.add),
                      reads=[ps_d[pi], rw_d], writes=[sm_d])
            kb.op("act", lambda: nc.scalar.activation(out=dtt[:], in_=dtt[:], func=AF.Exp), reads=[sm_d], writes=[sm_d])
            kb.op("act", lambda: nc.scalar.activation(out=dtt[:], in_=dtt[:], func=AF.Ln, bias=cv[:, 1:2], scale=1.0), reads=[sm_d, const_d], writes=[sm_d])
            kb.op("dve", lambda: nc.vector.tensor_tensor(out=at[:], in0=dtt[:], in1=rw[:, 64:128], op=ALU.mult), reads=[sm_d, rw_d], writes=[sm_d])
            pa = prr.next()
            kb.op("pe", lambda: nc.tensor.matmul(ps[pa][:, 0:64], lhsT=cmc("causal", 128), rhs=at[:], start=True, stop=True),
                  reads=[sm_d, const_d], writes=[ps_d[pa]])
            kb.op("act", lambda: nc.scalar.copy(out=acs[:], in_=ps[pa][:, 0:64]), reads=[ps_d[pa]], writes=[sm_d])
            pb = prr.next()
            kb.op("pe", lambda: nc.tensor.matmul(ps[pb][:, 0:64], lhsT=cmc("ones", 128), rhs=at[:], start=True, stop=True),
                  reads=[sm_d, const_d], writes=[ps_d[pb]])
            kb.op("act", lambda: nc.scalar.copy(out=alast[:], in_=ps[pb][:, 0:64]), reads=[ps_d[pb]], writes=[sm_d])
            kb.op("act", lambda: nc.scalar.activation(out=eacs[:], in_=acs[:], func=AF.Exp), reads=[sm_d], writes=[sm_d])
            kb.op("act", lambda: nc.scalar.activation(out=cdec[:], in_=alast[:], func=AF.Exp), reads=[sm_d], writes=[sm_d])
            kb.op("dve", lambda: nc.vector.tensor_tensor(out=dte[:], in0=alast[:], in1=acs[:], op=ALU.subtract), reads=[sm_d], writes=[sm_d])
            kb.op("act", lambda: nc.scalar.activation(out=dte[:], in_=dte[:], func=AF.Exp), reads=[sm_d], writes=[sm_d])
            if STAGE == 11:
                return mix_zero(l, wg, wg_d, mixT, mix_d, st)
            upre = st("upre", [128, S + 3], BF16)
            up_d = [Dep() for _ in range(5)]
            kb.op("pool", lambda: nc.gpsimd.memset(upre[:, 0:3], 0.0), writes=[up_d[0]])
            acc = st("acc", [128, 512], F32)
            acc_d = Dep()
            xsT = st("xsT", [128, 2, S], BF16)
            xs_d = [[Dep() for _ in range(4)] for _ in range(2)]
            Bm = st("Bm", [128, 2, S], BF16)
            Bm_d = [Dep() for _ in range(4)]
            CT = st("CT", [128, S], BF16)
            CT_d = [Dep() for _ in range(4)]
            for cc in range(4):
                for tt in range(4):
                    sl = slice(tt * 512, (tt + 1) * 512)
                    pi = prr.next()
                    proj_fm(pi, wg, wg_d, 256 + cc * 128, 128, tt)
                    kb.op("act", lambda: nc.scalar.copy(out=upre[:, 3 + tt * 512: 3 + (tt + 1) * 512], in_=ps[pi][:]),
                          reads=[ps_d[pi]], writes=[up_d[tt + 1]])
                    kb.op("act", lambda: nc.scalar.activation(out=acc[:], in_=ps[pi][:], func=AF.Identity,
                                                              bias=ppc("ssm_cb", l * 4 + cc), scale=ppc("ssm_cw", (l * 4 + 3) * 4 + cc)),
                          reads=[ps_d[pi], const_d], writes=[acc_d])
                    for k in range(1, 4):
                        kb.op("dve", lambda: nc.vector.scalar_tensor_tensor(
                            out=acc[:], in0=upre[:, 3 - k + tt * 512: 3 - k + (tt + 1) * 512], scalar=ppc("ssm_cw", (l * 4 + 3 - k) * 4 + cc),
                            in1=acc[:], op0=ALU.mult, op1=ALU.add),
                            reads=[up_d[tt + 1], up_d[tt], acc_d, const_d], writes=[acc_d])
                    if cc < 2:
                        kb.op("act", lambda: nc.scalar.activation(out=xsT[:, cc, sl], in_=acc[:], func=AF.Silu), reads=[acc_d], writes=[xs_d[cc][tt]])
                    elif cc == 3:
                        kb.op("act", lambda: nc.scalar.activation(out=CT[:, sl], in_=acc[:], func=AF.Silu), reads=[acc_d], writes=[CT_d[tt]])
                    else:
                        kb.op("act", lambda: nc.scalar.activation(out=acc[:], in_=acc[:], func=AF.Silu), reads=[acc_d], writes=[acc_d])
                        for g in range(2):
                            kb.op("pool", lambda: nc.gpsimd.tensor_scalar(out=Bm[:, g, sl], in0=acc[:], scalar1=cmc("hm2", 2)[:, g:g + 1],
                                                                          scalar2=None, op0=ALU.mult), reads=[acc_d, const_d], writes=[Bm_d[tt]])
            if STAGE == 12:
                return mix_zero(l, wg, wg_d, mixT, mix_d, st)
            vt = st("vt", [128, 16, 256], BF16)
            xsD = st("xsD", [128, 16, 256], BF16)
            Btm = st("Btm", [128, 16, 128], BF16)
            szt = st("szt", [128, 16, 256], BF16)
            xs_tm = [st("xs_tm", [128, 256], BF16) for _ in range(2)]
            xtm_d = [Dep(), Dep()]
            v_d = [Dep() for _ in range(16)]
            xd_d = [Dep() for _ in range(16)]
            bt_d = [Dep() for _ in range(16)]
            sz_d = [Dep() for _ in range(16)]
            for c in range(16):
                ch = slice(c * 128, (c + 1) * 128)
                s0 = 0
                srcs = [(xsT[:, 0, ch], xs_d[0][c // 4]), (xsT[:, 1, ch], xs_d[1][c // 4]), (Bm[:, 0, ch], Bm_d[c // 4]), (Bm[:, 1, ch], Bm_d[c // 4])]
                for k, (ap_, d_) in enumerate(srcs):
                    kb.op("pe", lambda: nc.tensor.transpose(out=psb[:, (s0 + k) * 128:(s0 + k + 1) * 128], in_=ap_, identity=identb),
                          reads=[d_, const_d], writes=[psb_d[s0 + k]], inc=(k == 3))
                xb = xs_tm[c % 2]
                kb.op("act", lambda: nc.scalar.copy(out=xb[:], in_=psb[:, 0:256]), reads=[psb_d[0]], writes=[xtm_d[c % 2]])
                kb.op("pool", lambda: nc.gpsimd.tensor_tensor(out=xsD[:, c, :], in0=xb[:], in1=rw[:, 128:384], op=ALU.mult),
                      reads=[xtm_d[c % 2], rw_d], writes=[xd_d[c]])
                for cc in range(2):
                    pt_ = psb[:, (s0 + cc) * 128:(s0 + cc + 1) * 128]
                    for hh in range(2):
                        h = cc * 2 + hh
                        kb.op("act", lambda: nc.scalar.activation(out=vt[:, c, h * 64:(h + 1) * 64], in_=pt_[:, hh * 64:(hh + 1) * 64], func=AF.Copy,
                                                                  scale=dtt[:, c * 4 + h: c * 4 + h + 1]),
                              reads=[psb_d[s0 + cc], sm_d], writes=[v_d[c]])
                for g in range(2):
                    kb.op("act", lambda: nc.scalar.copy(out=Btm[:, c, g * 64:(g + 1) * 64],
                                                        in_=psb[:, (s0 + 2 + g) * 128 + g * 64:(s0 + 2 + g) * 128 + (g + 1) * 64]),
                          reads=[psb_d[s0 + 2 + g]], writes=[bt_d[c]])
                pi = prr.next()
                proj_tm(pi, 0, wg, wg_d, 0, 256, c)
                kb.op("act", lambda: nc.scalar.activation(out=szt[:, c, :], in_=ps[pi][:, 0:256], func=AF.Silu), reads=[ps_d[pi]], writes=[sz_d[c]])
            if STAGE == 13:
                return mix_zero(l, wg, wg_d, mixT, mix_d, st)
            tl = post_tiles(st)
            scm = st("scm", [128, 256], F32)
            Mh = [st("Mh", [128, 128], F32) for _ in range(4)]
            dec = st("dec", [128, 512], F32)
            attm = [st("attm", [128, 512], BF16) for _ in range(2)]
            yt = [st("yt", [128, 256], F32) for _ in range(2)]
            vend = st("vend", [128, 256], BF16)
            S_run = st("S_run", [128, 256], F32)
            S_tmp = st("S_tmp", [128, 256], F32)
            Sbf = st("Sbf", [128, 256], BF16)
            scm_d, Mh_d, dec_d, attm_d, yt_d, vend_d = Dep(), [Dep() for _ in range(4)], Dep(), [Dep(), Dep()], [Dep(), Dep()], Dep()
            S_d, St_d, Sb_d = Dep(), Dep(), Dep()
            for c in range(16):
                ch = slice(c * 128, (c + 1) * 128)
                b = c % 2
                tq = c // 4
                pa = b
                for g in range(2):
                    kb.op("pe", lambda: nc.tensor.matmul(ps[pa][:, g * 128:(g + 1) * 128], lhsT=Bm[:, g, ch], rhs=CT[:, ch], start=True, stop=True),
                          reads=[Bm_d[tq], CT_d[tq]], writes=[ps_d[pa]], inc=(g == 1))
                kb.op("dve", lambda: nc.vector.tensor_tensor(out=scm[:], in0=ps[pa][:, 0:256], in1=cmc("causal", 256), op=ALU.mult),
                      reads=[ps_d[pa], const_d], writes=[scm_d])
                pg = 5 + b
                for h in range(4):
                    mb = h
                    kb.op("pool", lambda: nc.gpsimd.tensor_scalar(out=Mh[mb][:], in0=cmc("sgt", 128), scalar1=at[:, c * 4 + h: c * 4 + h + 1],
                                                                  scalar2=None, op0=ALU.mult), reads=[const_d, sm_d], writes=[Mh_d[mb]])
                    kb.op("pe", lambda: nc.tensor.matmul(ps[pg][:, h * 128:(h + 1) * 128], lhsT=Mh[mb][:], rhs=cmc("causal", 128), start=True, stop=True),
                          reads=[Mh_d[mb], const_d], writes=[ps_d[pg]], inc=(h == 3))
                kb.op("act", lambda: nc.scalar.activation(out=dec[:], in_=ps[pg][:], func=AF.Exp), reads=[ps_d[pg]], writes=[dec_d])
                for h in range(4):
                    g = h // 2
                    kb.op("dve", lambda: nc.vector.tensor_tensor(out=attm[b][:, h * 128:(h + 1) * 128], in0=scm[:, g * 128:(g + 1) * 128],
                                                                 in1=dec[:, h * 128:(h + 1) * 128], op=ALU.mult),
                          reads=[scm_d, dec_d], writes=[attm_d[b]])
                po = 2 + b
                if c > 0:
                    kb.op("pe", lambda: nc.tensor.matmul(ps[po][:, 256:512], lhsT=CT[:, ch], rhs=Sbf[:], start=True, stop=True),
                          reads=[CT_d[tq], Sb_d], writes=[ps_d[po]], inc=False)
                for h in range(4):
                    kb.op("pe", lambda: nc.tensor.matmul(ps[po][:, h * 64:(h + 1) * 64], lhsT=attm[b][:, h * 128:(h + 1) * 128],
                                                         rhs=vt[:, c, h * 64:(h + 1) * 64], start=(h == 0), stop=(h == 3)),
                          reads=[attm_d[b], v_d[c]], writes=[ps_d[po]], inc=(h == 3))
                kb.op("dve", lambda: nc.vector.tensor_tensor(out=yt[b][:], in0=ps[po][:, 0:256], in1=xsD[:, c, :], op=ALU.add),
                      reads=[ps_d[po], xd_d[c]], writes=[yt_d[b]])
                if c > 0:
                    for h in range(4):
                        kb.op("dve", lambda: nc.vector.scalar_tensor_tensor(out=yt[b][:, h * 64:(h + 1) * 64], in0=ps[po][:, 256 + h * 64: 256 + (h + 1) * 64],
                                                                            scalar=eacs[:, c * 4 + h: c * 4 + h + 1], in1=yt[b][:, h * 64:(h + 1) * 64],
                                                                            op0=ALU.mult, op1=ALU.add),
                              reads=[ps_d[po], sm_d, yt_d[b]], writes=[yt_d[b]])
                kb.op("pool", lambda: nc.gpsimd.tensor_tensor(out=yt[b][:], in0=yt[b][:], in1=szt[:, c, :], op=ALU.mult),
                      reads=[yt_d[b], sz_d[c]], writes=[yt_d[b]])
                if c < 15:
                    for h in range(4):
                        kb.op("pool", lambda: nc.gpsimd.tensor_scalar(out=vend[:, h * 64:(h + 1) * 64], in0=vt[:, c, h * 64:(h + 1) * 64],
                                                                      scalar1=dte[:, c * 4 + h: c * 4 + h + 1], scalar2=None, op0=ALU.mult),
                              reads=[v_d[c], sm_d], writes=[vend_d])
                    kb.op("pe", lambda: nc.tensor.matmul(ps[4][:, 0:256], lhsT=Btm[:, c, :], rhs=vend[:], start=True, stop=True),
                          reads=[bt_d[c], vend_d], writes=[ps_d[4]])
                    if c == 0:
                        kb.op("dve", lambda: nc.vector.tensor_tensor(out=S_run[:], in0=ps[4][:, 0:256], in1=cmc("bdm2", 256), op=ALU.mult),
                              reads=[ps_d[4], const_d, Sb_d], writes=[S_d])
                    else:
                        kb.op("dve", lambda: nc.vector.tensor_tensor(out=S_tmp[:], in0=ps[4][:, 0:256], in1=cmc("bdm2", 256), op=ALU.mult),
                              reads=[ps_d[4], const_d], writes=[St_d])
                        for h in range(4):
                            kb.op("dve", lambda: nc.vector.scalar_tensor_tensor(out=S_run[:, h * 64:(h + 1) * 64], in0=S_run[:, h * 64:(h + 1) * 64],
                                                                                scalar=cdec[:, c * 4 + h: c * 4 + h + 1], in1=S_tmp[:, h * 64:(h + 1) * 64],
                                                                                op0=ALU.mult, op1=ALU.add),
                                  reads=[S_d, St_d, sm_d], writes=[S_d])
                    kb.op("act", lambda: nc.scalar.copy(out=Sbf[:], in_=S_run[:]), reads=[S_d], writes=[Sb_d])
                post_norm(c, yt[b][:], yt_d[b], 2, lambda cc: ("ssm_ng", 2 * l + cc), None, None, mixT, mix_d, tl)

        def mix_moba(l, wg, wg_d, mixT, mix_d, st):
            qa = [st("qa", [72, S], BF16) for _ in range(4)]
            ka = [st("ka", [72, S], BF16) for _ in range(4)]
            qa_d = [[Dep() for _ in range(4)] for _ in range(4)]
            ka_d = [[Dep() for _ in range(4)] for _ in range(4)]
            qb_d = [Dep() for _ in range(4)]
            kb_d = [Dep()] * 4
            va = st("va", [128, 16, 4, 128], BF16)
            va_d = [Dep() for _ in range(16)]
            c1 = kb.chan()
            for h in range(4):
                kb.dma(c1, ka[h][64:72, :], blk_d[:, :], writes=[kb_d[h]], q="pool")
                kb.op("pool", lambda: nc.gpsimd.memset(qa[h][64:72, :], 0.0), writes=[qb_d[h]])
            for i in range(16):
                kb.op("pool", lambda: nc.gpsimd.memset(va[:, i, :, 64:128], 1.0), writes=[va_d[i]])
            mcs = [st("mcs", [128, 1024], F32) for _ in range(2)]
            mcs_d = [Dep(), Dep()]
            mcs_c = [kb.chan(), kb.chan()]
            sqb = st("sqb", [128, 512], BF16)
            rs = st("rs", [128, 512], F32)
            qn = st("qn", [128, 512], F32)
            qnb = st("qnb", [128, 512], BF16)
            t1 = st("t1", [128, 512], F32)
            t2 = st("t2", [128, 512], F32)
            qrot = st("qrot", [128, 512], F32)
            sqb_d, rs_d, qn_d, qnb_d, t1_d, t2_d, qrot_d = Dep(), Dep(), Dep(), Dep(), Dep(), Dep(), Dep()
            kms = st("kms", [128, 2, 8], F32)
            kms_d = Dep()
            km = [st("km", [64, 8], BF16) for _ in range(4)]
            km_d = [Dep() for _ in range(4)]
            for tt in range(4):
                sl = slice(tt * 512, (tt + 1) * 512)
                rb = tt % 2
                kb.dma(mcs_c[rb], mcs[rb][:, 0:512], mobacs_d[:, sl], writes=[mcs_d[rb]])
                kb.dma(mcs_c[rb], mcs[rb][:, 512:1024], mobacs_d[:, S + tt * 512: S + (tt + 1) * 512], writes=[mcs_d[rb]])
                for w in range(2):
                    for pr in range(2):
                        pi = prr.next()
                        proj_fm(pi, wg, wg_d, w * 256 + pr * 128, 128, tt)
                        kb.op("act", lambda: nc.scalar.activation(out=sqb[:], in_=ps[pi][:], func=AF.Square), reads=[ps_d[pi]], writes=[sqb_d])
                        pj = prr.next()
                        kb.op("pe", lambda: nc.tensor.matmul(ps[pj][:], lhsT=cmb[:, 4, :], rhs=sqb[:], start=True, stop=True),
                              reads=[sqb_d, const_d], writes=[ps_d[pj]])
                        kb.op("act", lambda: nc.scalar.activation(out=rs[:], in_=ps[pj][:], func=AF.Ln, bias=cv[:, 0:1], scale=1.0 / 64.0),
                              reads=[ps_d[pj], const_d], writes=[rs_d])
                        kb.op("act", lambda: nc.scalar.activation(out=rs[:], in_=rs[:], func=AF.Exp, scale=-0.5), reads=[rs_d], writes=[rs_d])
                        gn = "moba_qg" if w == 0 else "moba_kg"
                        kb.op("dve", lambda: nc.vector.scalar_tensor_tensor(out=qn[:], in0=ps[pi][:], scalar=ppc(gn, l), in1=rs[:],
                                                                            op0=ALU.mult, op1=ALU.mult),
                              reads=[ps_d[pi], rs_d, const_d], writes=[qn_d])
                        kb.op("act", lambda: nc.scalar.copy(out=qnb[:], in_=qn[:]), reads=[qn_d], writes=[qnb_d])
                        pk = prr.next()
                        kb.op("pe", lambda: nc.tensor.matmul(ps[pk][:], lhsT=cmb[:, 3, :], rhs=qnb[:], start=True, stop=True),
                              reads=[qnb_d, const_d], writes=[ps_d[pk]])
                        kb.op("dve", lambda: nc.vector.tensor_tensor(out=t1[:], in0=qn[:], in1=mcs[rb][:, 0:512], op=ALU.mult),
                              reads=[qn_d, mcs_d[rb]], writes=[t1_d])
                        kb.op("dve", lambda: nc.vector.tensor_tensor(out=t2[:], in0=ps[pk][:], in1=mcs[rb][:, 512:1024], op=ALU.mult),
                              reads=[ps_d[pk], mcs_d[rb]], writes=[t2_d])
                        kb.op("pool", lambda: nc.gpsimd.tensor_tensor(out=qrot[:], in0=t1[:], in1=t2[:], op=ALU.add),
                              reads=[t1_d, t2_d], writes=[qrot_d])
                        for hh in range(2):
                            h = pr * 2 + hh
                            dst = (qa if w == 0 else ka)[h]
                            dd = (qa_d if w == 0 else ka_d)[h][tt]
                            kb.op("act", lambda: nc.scalar.copy(out=dst[0:64, sl], in_=qrot[hh * 64:(hh + 1) * 64, :]), reads=[qrot_d], writes=[dd])
                        if w == 1:
                            kb.op("dve", lambda: nc.vector.tensor_reduce(out=kms[:, pr, 2 * tt:2 * tt + 2], in_=qrot[:].rearrange("p (n j) -> p n j", j=256),
                                                                         axis=AX.X, op=ALU.add), reads=[qrot_d], writes=[kms_d])
            for h in range(4):
                pr, hh = h // 2, h % 2
                kb.op("act", lambda: nc.scalar.activation(out=km[h][:], in_=kms[hh * 64:(hh + 1) * 64, pr, :], func=AF.Copy, scale=1.0 / 256.0),
                      reads=[kms_d], writes=[km_d[h]])
            for i in range(16):
                pi = prr.next()
                proj_tm(pi, 0, wg, wg_d, 512, 256, i)
                evac(i, va[:, i, :, 0:64], ps[pi][:, 0:256].rearrange("p (h e) -> p h e", h=4), [ps_d[pi]], [va_d[i]])
            gm = st("gm", [128, 32], F32)
            mx = st("mx", [128, 32], F32)
            selp = [st("selp", [128, 72], BF16) for _ in range(4)]
            gm_d, mx_d = Dep(), Dep()
            selp_d = [Dep() for _ in range(4)]
            for h in range(4):
                kb.op("pool", lambda: nc.gpsimd.memset(selp[h][:], 0.0), writes=[selp_d[h]])
            for i in range(8, 16):
                b = i // 2
                tq = i // 4
                pg = 6
                for h in range(4):
                    kb.op("pe", lambda: nc.tensor.matmul(ps[pg][:, h * 8:(h + 1) * 8], lhsT=qa[h][0:64, i * 128:(i + 1) * 128], rhs=km[h][:],
                                                         start=True, stop=True), reads=[qa_d[h][tq], km_d[h]], writes=[ps_d[pg]], inc=(h == 3))
                kb.op("dve", lambda: nc.vector.tensor_tensor(out=gm[:], in0=ps[pg][:, 0:32], in1=cmc("gmask", 256)[:, b * 32:(b + 1) * 32], op=ALU.add),
                      reads=[ps_d[pg], const_d], writes=[gm_d])
                for h in range(4):
                    kb.op("dve", lambda: nc.vector.max(out=mx[:, h * 8:(h + 1) * 8], in_=gm[:, h * 8:(h + 1) * 8]), reads=[gm_d], writes=[mx_d])
                for h in range(4):
                    kb.op("dve", lambda: nc.vector.tensor_scalar(out=selp[h][:, 64:72], in0=gm[:, h * 8:(h + 1) * 8], scalar1=mx[:, h * 8 + 2:h * 8 + 3],
                                                                 scalar2=-30000.0, op0=ALU.is_lt, op1=ALU.mult),
                          reads=[gm_d, mx_d], writes=[selp_d[h]])
                for h in range(4):
                    kb.op("pe", lambda: nc.tensor.transpose(out=psb[0:72, h * 128:(h + 1) * 128], in_=selp[h][:], identity=identb),
                          reads=[selp_d[h], const_d], writes=[psb_d[h]], inc=(h == 3))
                for h in range(4):
                    kb.op("act", lambda: nc.scalar.copy(out=qa[h][64:72, i * 128:(i + 1) * 128], in_=psb[64:72, h * 128:(h + 1) * 128]),
                          reads=[psb_d[h]], writes=[qb_d[h]])
            pt = [st("pt", [128, 256], BF16) for _ in range(3)]
            pt_d = [Dep() for _ in range(3)]
            rec = [st("rec", [64, 256], F32) for _ in range(2)]
            rec_d = [Dep(), Dep()]
            items = []
            for h in range(4):
                for b in range(8):
                    for jt in range(2 * b + 2):
                        items.append((h, b, jt))
            LA = 2
            pt = pt + [st("pt", [128, 256], BF16)]
            pt_d = pt_d + [Dep()]

            def emit_score(k):
                h, b, jt = items[k]
                own = jt >= 2 * b
                qlo = jt - 2 * b
                c0 = 128 if (own and qlo == 1) else 0
                n = 256 - c0
                qcols = slice(b * 256 + c0, (b + 1) * 256)
                tq = b // 2
                pi = k % 4
                pb_ = k % 4
                kk = 64 if own else 72
                rds = [ka_d[h][jt // 4], qa_d[h][tq]] + ([] if own else [kb_d[h], qb_d[h]])
                kb.op("pe", lambda: nc.tensor.matmul(ps[pi][:, 0:n], lhsT=ka[h][0:kk, jt * 128:(jt + 1) * 128], rhs=qa[h][0:kk, qcols],
                                                     start=True, stop=True), reads=rds, writes=[ps_d[pi]])
                kb.op("act", lambda: nc.scalar.activation(out=pt[pb_][:, 0:n], in_=ps[pi][:, 0:n], func=AF.Exp, scale=0.125),
                      reads=[ps_d[pi]], writes=[pt_d[pb_]])
                if own:
                    kb.op("dve", lambda: nc.vector.tensor_tensor(out=pt[pb_][:, 0:128], in0=pt[pb_][:, 0:128], in1=cmc("causal", 128), op=ALU.mult),
                          reads=[pt_d[pb_], const_d], writes=[pt_d[pb_]])

            def emit_pv(k):
                h, b, jt = items[k]
                own = jt >= 2 * b
                qlo = jt - 2 * b
                c0 = 128 if (own and qlo == 1) else 0
                n = 256 - c0
                njt = 2 * b + 2
                nacc = h * 8 + b
                po = 4 + nacc % 2
                rbi = nacc % 2
                pb_ = k % 4
                tq = b // 2
                qs = slice(b * 256, (b + 1) * 256)
                kb.op("pe", lambda: nc.tensor.matmul(ps[po][:, c0:256], lhsT=va[:, jt, h, :], rhs=pt[pb_][:, 0:n],
                                                     start=(jt == 0), stop=(jt == njt - 1)),
                      reads=[va_d[jt], pt_d[pb_]], writes=[ps_d[po]])
                if jt == njt - 1:
                    kb.op("dve", lambda: nc.vector.reciprocal(out=rec[rbi][:], in_=ps[po][64:128, 0:256]), reads=[ps_d[po]], writes=[rec_d[rbi]])
                    hh = h % 2
                    kb.op("dve", lambda: nc.vector.tensor_tensor(out=mixT[hh * 64:(hh + 1) * 64, h // 2, qs], in0=ps[po][0:64, 0:256], in1=rec[rbi][:],
                                                                 op=ALU.mult), reads=[ps_d[po], rec_d[rbi]], writes=[mix_d[h // 2][tq]])

            for k in range(len(items) + LA):
                if k < len(items):
                    emit_score(k)
                if k >= LA:
                    emit_pv(k - LA)

        def mix_zero(l, wg, wg_d, mixT, mix_d, st):
            for cc in range(2):
                kb.op("pool", lambda: nc.gpsimd.memset(mixT[:, cc, :], 0.0), writes=mix_d[cc])

        mix_fns = [mix_moba, mix_ssm, mix_gla, mix_ret]

        def mixer_phase(l, s):
            mk_ = kb.mark()
            with contextlib.ExitStack() as sc:
                st = lambda name, shape, dt: sc.enter_context(nc.sbuf_tensor(kb.un(name), shape, dt))
                wg = st("wg", [128, KC, GW], BF16)
                wo = st("wo", [128, 2, D], BF16)
                wg_d, wo_d = Dep(), Dep()
                wg_c, wo_c = kb.chan(), kb.chan()
                mixT = st("mixT", [128, 2, S], BF16)
                mix_d = [[Dep() for _ in range(4)] for _ in range(2)]
                for m in mixers:
                    r0 = (l * 4 + m) * 128
                    kb.dma(wg_c, wg[:].rearrange("p k c -> p (k c)"), win_b[r0:r0 + 128, :], writes=[wg_d])
                    kb.dma(wo_c, wo[:].rearrange("p k c -> p (k c)"), wout_b[r0:r0 + 128, :], writes=[wo_d])
                    with contextlib.ExitStack() as sc2:
                        st2 = lambda name, shape, dt: sc2.enter_context(nc.sbuf_tensor(kb.un(name), shape, dt))
                        mix_fns[m](l, wg, wg_d, mixT, mix_d, st2)
                        kb.barrier()
                    if debug and s == 0 and l == 0:
                        dc_ = kb.chan()
                        kb.dma(dc_, dbg_d[m], mixT[:].rearrange("p k c -> p (k c)"), reads=[d for dd in mix_d for d in dd])
                    for dc in range(KC):
                        for tt in range(4):
                            sl = slice(tt * 512, (tt + 1) * 512)
                            pi = (dc * 4 + tt) % 4
                            for k2 in range(2):
                                kb.op("pe", lambda: nc.tensor.matmul(ps[pi][:], lhsT=wo[:, k2, dc * 128:(dc + 1) * 128], rhs=mixT[:, k2, sl],
                                                                     start=(k2 == 0), stop=(k2 == 1)),
                                      reads=[wo_d, mix_d[k2][tt]], writes=[ps_d[pi]], inc=(k2 == 1))
                            kb.op("dve", lambda: nc.vector.tensor_tensor(out=xT[:, dc, sl], in0=ps[pi][:], in1=xT[:, dc, sl], op=ALU.add),
                                  reads=[ps_d[pi], xT_d[dc][tt]], writes=[xT_d[dc][tt]])
                    kb.barrier()
            kb.release(mk_)

        for s in range(n_seq):
            load_x(s)
            for l in range(L):
                norm("attn_g", l)
                if mixers:
                    mixer_phase(l, s)
                if not SKIP_FFN:
                    norm("ffn_g", l)
                    ffn(l)
            store_x(s)
        kb.barrier()
    return nc


def kernel(**inputs):
    L = 2
    n_seq = 4
    x = np.asarray(inputs["x"], np.float32)
    hp = host_prep(inputs, L)
    nc = build(n_seq, L)
    in_maps = []
    for c in range(NCORES):
        m = dict(hp)
        m["x"] = np.ascontiguousarray(x[c * n_seq:(c + 1) * n_seq].reshape(n_seq * S, D))
        in_maps.append(m)
    res = run_bass_kernel_spmd(nc, in_maps, core_ids=list(range(NCORES)))
    out = np.stack([r["y"].reshape(n_seq, S, D) for r in res.results], axis=0)
    return out.reshape(NCORES * n_seq, S, D).astype(np.float32)
```

```python
import contextlib
import numpy as np
import concourse.bass as bass
import concourse.mybir as mybir
from concourse.bass_utils import run_bass_kernel_spmd
from concourse.alu_op_type import AluOpType as ALU

F32 = mybir.dt.float32
BF16 = mybir.dt.bfloat16
AF = mybir.ActivationFunctionType
AX = mybir.AxisListType

D = 1024
S = 2048
KC = 8
NCORES = 8
FF = 2816
NJ = 22
EPS = 1e-6
IN_W = 3092
GOFF = (0, 768, 1540, 2324)
GW = 784


class Dep:
    __slots__ = ("w", "r")

    def __init__(self):
        self.w = None
        self.r = {}


class Chan:
    def __init__(self, sem):
        self.sem = sem
        self.n = 0


class KB:
    def __init__(self, nc, es):
        self.nc = nc
        self.es = es
        self.eng = {"pe": nc.tensor, "act": nc.scalar, "dve": nc.vector, "pool": nc.gpsimd, "sp": nc.sync}
        self.sem = {e: es.enter_context(nc.semaphore("s_" + e)) for e in self.eng}
        self.cnt = {e: 0 for e in self.eng}
        self.seen = {e: {} for e in self.eng}
        self.chans = []
        self.nchan = 0

    def un(self, name):
        self.nuniq = getattr(self, "nuniq", 0) + 1
        return "%s_u%d" % (name, self.nuniq)

    def chan(self):
        if getattr(self, "free", None):
            c = self.free.pop()
        else:
            self.nchan += 1
            c = Chan(self.es.enter_context(self.nc.semaphore("c%d" % self.nchan)))
            self.chans.append(c)
        if not hasattr(self, "live"):
            self.live = []
        self.live.append(c)
        return c

    def mark(self):
        if not hasattr(self, "live"):
            self.live = []
        return len(self.live)

    def release(self, mark):
        if not hasattr(self, "free"):
            self.free = []
        self.free.extend(self.live[mark:])
        del self.live[mark:]

    @staticmethod
    def _need(reads, writes):
        need = {}

        def add(k, v):
            if need.get(k, 0) < v:
                need[k] = v

        for d in reads:
            if d.w is not None:
                add(*d.w)
        for d in writes:
            if d.w is not None:
                add(*d.w)
            for k, v in d.r.items():
                add(k, v)
        return need

    def _waits(self, eng, need):
        e = self.eng[eng]
        seen = self.seen[eng]
        for k, v in need.items():
            if k == "pe" and eng == "pe":
                continue
            if seen.get(k, 0) >= v:
                continue
            seen[k] = v
            if isinstance(k, Chan):
                e.wait_ge(k.sem, v)
            else:
                e.wait_ge(self.sem[k], v)

    def op(self, eng, fn, reads=(), writes=(), inc=True):
        self._waits(eng, self._need(reads, writes))
        ins = fn()
        if inc:
            self.cnt[eng] += 1
            ins.then_inc(self.sem[eng], 1)
            c = self.cnt[eng]
        else:
            c = self.cnt[eng] + 1
        for d in reads:
            if d.r.get(eng, 0) < c:
                d.r[eng] = c
        for d in writes:
            d.w = (eng, c)
            d.r = {}
        return ins

    def dma(self, chan, out, in_, reads=(), writes=(), q="sp"):
        self._waits(q, self._need(reads, writes))
        ins = self.eng[q].dma_start(out=out, in_=in_)
        chan.n += 16
        ins.then_inc(chan.sem, 16)
        for d in reads:
            d.r[chan] = chan.n
        for d in writes:
            d.w = (chan, chan.n)
            d.r = {}
        return ins

    def barrier(self):
        for e in self.eng:
            need = {k: v for k, v in self.cnt.items() if v > 0 and k != e}
            for c in self.chans:
                if c.n > 0:
                    need[c] = c.n
            self._waits(e, need)
        for e in ("act", "dve", "pool"):
            if self.cnt[e] > 0 and self.seen[e].get(e, 0) < self.cnt[e]:
                self.seen[e][e] = self.cnt[e]
                self.eng[e].wait_ge(self.sem[e], self.cnt[e])


class Ring:
    def __init__(self, items):
        self.items = items
        self.i = 0

    def next(self):
        it = self.items[self.i % len(self.items)]
        self.i += 1
        return it


def pp_layout(L):
    off = {}
    n = 0

    def add(name, cnt):
        nonlocal n
        off[name] = n
        n += cnt

    add("attn_g", L * KC)
    add("ffn_g", L * KC)
    add("fcw", L * 3 * 2 * NJ)
    add("fcb", L * 2 * NJ)
    add("gla_b2n", L)
    add("gla_ng", L)
    add("ret_ng", L)
    add("ssm_ng", L * 2)
    add("ssm_cw", L * 4 * 4)
    add("ssm_cb", L * 4)
    add("moba_qg", L)
    add("moba_kg", L)
    return off, n


def host_prep(inputs, L):
    f = np.float32
    w_in = np.asarray(inputs["w_in"], f)[:L]
    win = np.zeros((L, 4, 128, KC, GW), f)
    for g in range(4):
        w = (GOFF + (IN_W,))[g + 1] - GOFF[g]
        blk = w_in[:, :, GOFF[g]:GOFF[g] + w].reshape(L, KC, 128, w)
        win[:, g, :, :, :w] = blk.transpose(0, 2, 1, 3)
    win = win.reshape(L * 4 * 128, KC * GW)
    w_out = np.asarray(inputs["w_out"], f)[:L]
    wout = w_out.reshape(L, 4, 2, 128, D).transpose(0, 1, 3, 2, 4).reshape(L * 4 * 128, 2 * D)
    w_up = np.asarray(inputs["ffn_w_up"], f)[:L]
    wu = w_up.reshape(L, KC, 128, 2, NJ, 128)
    wup = wu.transpose(0, 4, 2, 1, 3, 5).reshape(L * NJ * 128, KC * 256)
    wdn = np.asarray(inputs["ffn_w_down"], f)[:L].reshape(L * FF, D)
    off, npp = pp_layout(L)
    pp = np.zeros((128, npp), f)

    def fm(v):
        return np.asarray(v, f).reshape(-1, 128).T

    for l in range(L):
        pp[:, off["attn_g"] + l * KC: off["attn_g"] + (l + 1) * KC] = fm(inputs["attn_norm_g"][l])
        pp[:, off["ffn_g"] + l * KC: off["ffn_g"] + (l + 1) * KC] = fm(inputs["ffn_norm_g"][l])
        for i in range(3):
            o = off["fcw"] + (l * 3 + i) * 2 * NJ
            pp[:, o:o + 2 * NJ] = fm(inputs["ffn_conv_w"][l][i])
        o = off["fcb"] + l * 2 * NJ
        pp[:, o:o + 2 * NJ] = fm(inputs["ffn_conv_b"][l])
    for l in range(L):
        pp[:, off["gla_b2n"] + l] = np.asarray(inputs["gla_gate_b"][l], f)
        pp[:, off["gla_ng"] + l] = np.tile(np.asarray(inputs["gla_norm_g"][l], f), 2)
        pp[:, off["ret_ng"] + l] = np.tile(np.asarray(inputs["ret_norm_g"][l], f), 2)
        pp[:, off["ssm_ng"] + 2 * l: off["ssm_ng"] + 2 * l + 2] = fm(inputs["ssm_norm_g"][l])
        for i in range(4):
            o = off["ssm_cw"] + (l * 4 + i) * 4
            pp[:, o:o + 4] = fm(inputs["ssm_conv_w"][l][i])
        pp[:, off["ssm_cb"] + 4 * l: off["ssm_cb"] + 4 * l + 4] = fm(inputs["ssm_conv_b"][l])
        pp[:, off["moba_qg"] + l] = np.tile(np.asarray(inputs["moba_q_norm_g"][l], f), 2)
        pp[:, off["moba_kg"] + l] = np.tile(np.asarray(inputs["moba_k_norm_g"][l], f), 2)
    consts = host_consts()
    w2 = np.asarray(inputs["gla_gate_w2"], f)[:L].reshape(L * 16, 128)
    consts["gw2"] = np.ascontiguousarray(w2)
    rows = np.zeros((128, L * 384), f)
    for l in range(L):
        rows[:, l * 384: l * 384 + 64] = np.tile(np.asarray(inputs["ssm_dt_bias"][l], f), 16)[None, :]
        rows[:, l * 384 + 64: l * 384 + 128] = np.tile(np.asarray(inputs["ssm_a_log"][l], f), 16)[None, :]
        rows[:, l * 384 + 128: l * 384 + 384] = np.repeat(np.asarray(inputs["ssm_d"][l], f), 64)[None, :]
    consts["rows"] = rows
    return dict(win=np.ascontiguousarray(win), wout=np.ascontiguousarray(wout),
                wup=np.ascontiguousarray(wup), wdn=np.ascontiguousarray(wdn), pp=pp, **consts)


CM_OFF = {}
STAGE = 0
SKIP_FFN = False


def host_consts():
    f = np.float32
    p = np.arange(128)
    cols = []

    def add(name, a):
        CM_OFF[name] = sum(c.shape[1] for c in cols)
        cols.append(np.asarray(a, f))

    add("ident", np.eye(128))
    add("ones", np.ones((128, 128)))
    caus = (p[:, None] <= p[None, :]).astype(f)
    add("causal", np.tile(caus, (1, 4)))
    add("bdm4", (p[:, None] // 32 == np.arange(256)[None, :] // 64))
    add("bdm2", (p[:, None] // 64 == np.arange(256)[None, :] // 128))
    add("hm4", (p[:, None] // 32 == np.arange(4)[None, :]))
    add("hm2", (p[:, None] // 64 == np.arange(2)[None, :]))
    def perm(hd):
        half = hd // 2
        m = np.arange(128)
        partner = (m // hd) * hd + ((m % hd) + half) % hd
        P = np.zeros((128, 128), f)
        P[partner, m] = 1.0
        return P
    add("perm32", perm(32))
    add("perm64", perm(64))
    add("bo64", (p[:, None] // 64 == p[None, :] // 64))
    add("sgt", (p[:, None] > p[None, :]))
    gmk = np.zeros((128, 8, 4, 8))
    for b_ in range(8):
        gmk[:, b_, :, b_:] = -1e30
    add("gmask", gmk.reshape(128, 256))
    cm = np.concatenate(cols, axis=1)
    lg = np.log(1.0 - 2.0 ** (-5.0 - np.arange(4, dtype=np.float64)))
    h = p // 32
    d = p % 32
    inv = 1.0 / (10000.0 ** np.linspace(0.0, 1.0, 16))
    t = np.arange(S, dtype=np.float64)
    ang = t[None, :] * inv[d % 16][:, None]
    rcos = np.cos(ang)
    rsin = np.sin(ang) * np.where(d < 16, -1.0, 1.0)[:, None]
    idx = np.arange(512) % 128
    sc = 32.0 ** -0.5
    rdq = np.exp((idx[None, :] + 1.0) * lg[h][:, None])
    rdki = np.exp(-(idx[None, :] + 1.0) * lg[h][:, None]) * sc
    rdke = np.exp((127.0 - idx[None, :]) * lg[h][:, None]) * sc
    rcd = np.repeat(np.exp(128.0 * lg[h])[:, None], 16, axis=1)
    rett = np.concatenate([rdq, rdki, rdke, rcd], axis=1).astype(f)
    dm = p % 64
    invm = 10000.0 ** (-np.arange(0, 64, 2, dtype=np.float64) / 64)
    angm = t[None, :] * invm[dm % 32][:, None]
    mcos = np.cos(angm)
    msin = np.sin(angm) * np.where(dm < 32, -1.0, 1.0)[:, None]
    blk = (np.arange(S)[None, :] // 256 == np.arange(8)[:, None]).astype(f)
    return {"blk1h": blk, "cm": cm, "ret_cs": np.concatenate([rcos, rsin], axis=1).astype(f), "ret_t": rett,
            "moba_cs": np.concatenate([mcos, msin], axis=1).astype(f)}


def build(n_seq, L, mixers=(0, 1, 2, 3), debug=False):
    nc = bass.Bass("TRN2", target_bir_lowering=False)
    off, npp = pp_layout(L)
    x_d = nc.dram_tensor("x", [n_seq * S, D], F32, kind="ExternalInput").ap()
    y_d = nc.dram_tensor("y", [n_seq * S, D], F32, kind="ExternalOutput").ap()
    win_d = nc.dram_tensor("win", [L * 4 * 128, KC * GW], F32, kind="ExternalInput").ap()
    wout_d = nc.dram_tensor("wout", [L * 4 * 128, 2 * D], F32, kind="ExternalInput").ap()
    wup_d = nc.dram_tensor("wup", [L * NJ * 128, KC * 256], F32, kind="ExternalInput").ap()
    wdn_d = nc.dram_tensor("wdn", [L * FF, D], F32, kind="ExternalInput").ap()
    pp_d = nc.dram_tensor("pp", [128, npp], F32, kind="ExternalInput").ap()
    if not CM_OFF:
        host_consts()
    NCM = CM_OFF["gmask"] + 256
    cm_d = nc.dram_tensor("cm", [128, NCM], F32, kind="ExternalInput").ap()
    retcs_d = nc.dram_tensor("ret_cs", [128, 2 * S], F32, kind="ExternalInput").ap()
    rett_d = nc.dram_tensor("ret_t", [128, 1552], F32, kind="ExternalInput").ap()
    mobacs_d = nc.dram_tensor("moba_cs", [128, 2 * S], F32, kind="ExternalInput").ap()
    gw2_d = nc.dram_tensor("gw2", [L * 16, 128], F32, kind="ExternalInput").ap()
    rows_d = nc.dram_tensor("rows", [128, L * 384], F32, kind="ExternalInput").ap()
    blk_d = nc.dram_tensor("blk1h", [8, S], F32, kind="ExternalInput").ap()
    dbg_d = nc.dram_tensor("dbg", [4, 128, 2 * S], BF16, kind="ExternalOutput").ap() if debug else None
    win_b = nc.dram_tensor("win_b", [L * 4 * 128, KC * GW], BF16, kind="Internal").ap()
    wout_b = nc.dram_tensor("wout_b", [L * 4 * 128, 2 * D], BF16, kind="Internal").ap()
    wup_b = nc.dram_tensor("wup_b", [L * NJ * 128, KC * 256], BF16, kind="Internal").ap()
    wdn_b = nc.dram_tensor("wdn_b", [L * FF, D], BF16, kind="Internal").ap()

    with contextlib.ExitStack() as es:
        kb = KB(nc, es)
        sb = lambda name, shape, dt: es.enter_context(nc.sbuf_tensor(name, shape, dt))

        cc = kb.chan()
        for src, dst in ((win_d, win_b), (wout_d, wout_b), (wup_d, wup_b), (wdn_d, wdn_b)):
            rows = src.shape[0]
            for r in range(0, rows, 128):
                kb.dma(cc, dst[r:r + 128, :], src[r:r + 128, :], q="pool")

        xT = sb("xT", [128, KC, S], F32)
        hT = sb("hT", [128, KC, S], BF16)
        xT_d = [[Dep() for _ in range(4)] for _ in range(KC)]
        hT_d = [[Dep() for _ in range(4)] for _ in range(KC)]
        ppt = sb("ppt", [128, npp], F32)
        cmt = sb("cmt", [128, NCM], F32)
        cmb = sb("cmb", [128, 5, 128], BF16)
        identf = cmt[:, 0:128]
        onesb = cmb[:, 1, :]
        identb = cmb[:, 0, :]

        def cmc(name, n):
            return cmt[:, CM_OFF[name]:CM_OFF[name] + n]
        const_d = Dep()
        c0 = kb.chan()
        kb.dma(c0, ppt[:], pp_d[:, :], writes=[const_d])
        kb.dma(c0, cmt[:], cm_d[:, :], writes=[const_d])
        for ii, nm in enumerate(("ident", "ones", "perm32", "perm64", "bo64")):
            kb.op("dve", lambda: nc.vector.tensor_copy(out=cmb[:, ii, :], in_=cmc(nm, 128)), reads=[const_d], writes=[const_d])
        cv = sb("cv", [128, 8], F32)
        kb.op("pool", lambda: nc.gpsimd.memset(cv[:, 0:1], EPS), writes=[const_d])
        ps = [es.enter_context(nc.psum_tensor("ps%d" % i, [128, 512], F32)) for i in range(7)]
        ps_d = [Dep() for _ in range(7)]
        psb = es.enter_context(nc.psum_tensor("psb", [128, 1024], BF16))
        psb_d = [Dep()] * 8
        kb.barrier()

        def ppc(name, idx):
            o = off[name] + idx
            return ppt[:, o:o + 1]

        def load_x(s):
            mk_ = kb.mark()
            with contextlib.ExitStack() as sc:
                xin = [sc.enter_context(nc.sbuf_tensor(kb.un("xin"), [128, D], F32)) for i in range(2)]
                xin_d = [Dep(), Dep()]
                xc = [kb.chan(), kb.chan()]
                for i in range(16):
                    b = i % 2
                    kb.dma(xc[b], xin[b][:], x_d[s * S + i * 128: s * S + (i + 1) * 128, :], writes=[xin_d[b]])
                    for hh in range(2):
                        pi = (i * 2 + hh) % 4
                        for k in range(4):
                            kc = hh * 4 + k
                            kb.op("pe", lambda: nc.tensor.transpose(out=ps[pi][:, k * 128:(k + 1) * 128],
                                                                    in_=xin[b][:, kc * 128:(kc + 1) * 128],
                                                                    identity=identf),
                                  reads=[xin_d[b], const_d], writes=[ps_d[pi]], inc=(k == 3))
                        eng = "act" if hh == 0 else "dve"
                        dst = xT[:, hh * 4:(hh + 1) * 4, i * 128:(i + 1) * 128]
                        src = ps[pi][:].rearrange("p (k c) -> p k c", k=4)
                        wr = [xT_d[hh * 4 + k][i // 4] for k in range(4)]
                        if eng == "act":
                            kb.op("act", lambda: nc.scalar.copy(out=dst, in_=src), reads=[ps_d[pi]], writes=wr)
                        else:
                            kb.op("dve", lambda: nc.vector.tensor_copy(out=dst, in_=src), reads=[ps_d[pi]], writes=wr)
                kb.barrier()
            kb.release(mk_)

        def store_x(s):
            mk_ = kb.mark()
            with contextlib.ExitStack() as sc:
                xo = [sc.enter_context(nc.sbuf_tensor(kb.un("xo"), [128, D], F32)) for i in range(2)]
                xo_d = [Dep(), Dep()]
                xc = [kb.chan(), kb.chan()]
                for i in range(16):
                    b = i % 2
                    for hh in range(2):
                        pi = (i * 2 + hh) % 4
                        for k in range(4):
                            kc = hh * 4 + k
                            kb.op("pe", lambda: nc.tensor.transpose(out=ps[pi][:, k * 128:(k + 1) * 128],
                                                                    in_=xT[:, kc, i * 128:(i + 1) * 128],
                                                                    identity=identf),
                                  reads=[xT_d[kc][i // 4], const_d], writes=[ps_d[pi]], inc=(k == 3))
                        dst = xo[b][:, hh * 512:(hh + 1) * 512]
                        if hh == 0:
                            kb.op("act", lambda: nc.scalar.copy(out=dst, in_=ps[pi][:]), reads=[ps_d[pi]], writes=[xo_d[b]])
                        else:
                            kb.op("dve", lambda: nc.vector.tensor_copy(out=dst, in_=ps[pi][:]), reads=[ps_d[pi]], writes=[xo_d[b]])
                    kb.dma(xc[b], y_d[s * S + i * 128: s * S + (i + 1) * 128, :], xo[b][:], reads=[xo_d[b]])
                kb.barrier()

            kb.release(mk_)

        def norm(gname, l):
            with contextlib.ExitStack() as sc:
                sq = [sc.enter_context(nc.sbuf_tensor(kb.un("sq"), [128, KC, 512], BF16)) for i in range(2)]
                rs = [sc.enter_context(nc.sbuf_tensor(kb.un("rs"), [128, 512], F32)) for i in range(2)]
                sq_d = [Dep(), Dep()]
                rs_d = [Dep(), Dep()]
                for tt in range(4):
                    b = tt % 2
                    sl = slice(tt * 512, (tt + 1) * 512)
                    kb.op("act", lambda: nc.scalar.activation(out=sq[b][:], in_=xT[:, :, sl], func=AF.Square),
                          reads=[xT_d[k][tt] for k in range(KC)], writes=[sq_d[b]])
                    pi = 4 + b
                    for kc in range(KC):
                        kb.op("pe", lambda: nc.tensor.matmul(ps[pi][:], lhsT=onesb, rhs=sq[b][:, kc, :],
                                                             start=(kc == 0), stop=(kc == KC - 1)),
                              reads=[sq_d[b], const_d], writes=[ps_d[pi]], inc=(kc == KC - 1))
                    kb.op("act", lambda: nc.scalar.activation(out=rs[b][:], in_=ps[pi][:], func=AF.Ln, bias=cv[:, 0:1], scale=1.0 / D),
                          reads=[ps_d[pi], const_d], writes=[rs_d[b]])
                    kb.op("act", lambda: nc.scalar.activation(out=rs[b][:], in_=rs[b][:], func=AF.Exp, scale=-0.5),
                          reads=[rs_d[b]], writes=[rs_d[b]])
                    for kc in range(KC):
                        kb.op("dve", lambda: nc.vector.scalar_tensor_tensor(out=hT[:, kc, sl], in0=xT[:, kc, sl],
                                                                            scalar=ppc(gname, l * KC + kc), in1=rs[b][:],
                                                                            op0=ALU.mult, op1=ALU.mult),
                              reads=[xT_d[kc][tt], rs_d[b], const_d], writes=[hT_d[kc][tt]])
                kb.barrier()

        def ffn(l):
            mk_ = kb.mark()
            G = 8
            groups = [list(range(a, min(a + G, NJ))) for a in range(0, NJ, G)]
            with contextlib.ExitStack() as sc:
                st = lambda name, shape, dt: sc.enter_context(nc.sbuf_tensor(kb.un(name), shape, dt))
                upre = [[st("upre%d_%d" % (b, h), [128, S + 2], BF16) for h in range(2)] for b in range(2)]
                upre_d = [[[Dep() for _ in range(5)] for h in range(2)] for b in range(2)]
                acc = [[st("acc%d_%d" % (r, h), [128, 512], F32) for h in range(2)] for r in range(3)]
                acc_d = [[Dep() for h in range(2)] for r in range(3)]
                actT = st("actT", [128, G, S], BF16)
                act_d = [[Dep() for _ in range(4)] for _ in range(G)]
                wup = [st("wup%d" % i, [128, KC, 256], BF16) for i in range(3)]
                wup_dd = [Dep() for _ in range(3)]
                wup_c = [kb.chan() for _ in range(3)]
                wdn = [st("wdn%d" % i, [128, D], BF16) for i in range(G)]
                wdn_dd = [Dep() for _ in range(G)]
                wdn_c = [kb.chan() for _ in range(G)]
                for b in range(2):
                    for h in range(2):
                        kb.op("pool", lambda: nc.gpsimd.memset(upre[b][h][:, 0:2], 0.0), writes=[upre_d[b][h][0]])
                accr = 0
                jcount = 0
                for grp in groups:
                    for jj, j in enumerate(grp):
                        kb.dma(wdn_c[jj], wdn[jj][:], wdn_b[l * FF + j * 128: l * FF + (j + 1) * 128, :], writes=[wdn_dd[jj]])
                    for jj, j in enumerate(grp):
                        ws = jcount % 3
                        ub = jcount % 2
                        jcount += 1
                        r0 = (l * NJ + j) * 128
                        kb.dma(wup_c[ws], wup[ws][:].rearrange("p k c -> p (k c)"), wup_b[r0:r0 + 128, :], writes=[wup_dd[ws]])
                        for tt in range(4):
                            sl = slice(tt * 512, (tt + 1) * 512)
                            ar = accr % 3
                            accr += 1
                            for h in range(2):
                                pi = (tt % 2) * 2 + h
                                for kc in range(KC):
                                    kb.op("pe", lambda: nc.tensor.matmul(ps[pi][:], lhsT=wup[ws][:, kc, h * 128:(h + 1) * 128],
                                                                         rhs=hT[:, kc, sl], start=(kc == 0), stop=(kc == KC - 1)),
                                          reads=[wup_dd[ws], hT_d[kc][tt]], writes=[ps_d[pi]], inc=(kc == KC - 1))
                                kb.op("act", lambda: nc.scalar.copy(out=upre[ub][h][:, 2 + tt * 512: 2 + (tt + 1) * 512], in_=ps[pi][:]),
                                      reads=[ps_d[pi]], writes=[upre_d[ub][h][tt + 1]])
                                w2 = ppc("fcw", (l * 3 + 2) * 2 * NJ + h * NJ + j)
                                w1 = ppc("fcw", (l * 3 + 1) * 2 * NJ + h * NJ + j)
                                w0 = ppc("fcw", (l * 3 + 0) * 2 * NJ + h * NJ + j)
                                bb = ppc("fcb", l * 2 * NJ + h * NJ + j)
                                kb.op("act", lambda: nc.scalar.activation(out=acc[ar][h][:], in_=ps[pi][:], func=AF.Identity,
                                                                          bias=bb, scale=w2),
                                      reads=[ps_d[pi], const_d], writes=[acc_d[ar][h]])
                                kb.op("dve", lambda: nc.vector.scalar_tensor_tensor(
                                    out=acc[ar][h][:], in0=upre[ub][h][:, 1 + tt * 512: 1 + (tt + 1) * 512], scalar=w1,
                                    in1=acc[ar][h][:], op0=ALU.mult, op1=ALU.add),
                                    reads=[upre_d[ub][h][tt + 1], upre_d[ub][h][tt], acc_d[ar][h], const_d], writes=[acc_d[ar][h]])
                                kb.op("dve", lambda: nc.vector.scalar_tensor_tensor(
                                    out=acc[ar][h][:], in0=upre[ub][h][:, tt * 512: (tt + 1) * 512], scalar=w0,
                                    in1=acc[ar][h][:], op0=ALU.mult, op1=ALU.add),
                                    reads=[upre_d[ub][h][tt + 1], upre_d[ub][h][tt], acc_d[ar][h], const_d], writes=[acc_d[ar][h]])
                            kb.op("act", lambda: nc.scalar.activation(out=acc[ar][0][:], in_=acc[ar][0][:], func=AF.Silu),
                                  reads=[acc_d[ar][0]], writes=[acc_d[ar][0]])
                            kb.op("pool", lambda: nc.gpsimd.tensor_tensor(out=actT[:, jj, sl], in0=acc[ar][0][:], in1=acc[ar][1][:],
                                                                          op=ALU.mult),
                                  reads=[acc_d[ar][0], acc_d[ar][1]], writes=[act_d[jj][tt]])
                    for dc in range(KC):
                        for tt in range(4):
                            sl = slice(tt * 512, (tt + 1) * 512)
                            pi = 4 + (dc * 4 + tt) % 3
                            for jj, j in enumerate(grp):
                                kb.op("pe", lambda: nc.tensor.matmul(ps[pi][:], lhsT=wdn[jj][:, dc * 128:(dc + 1) * 128],
                                                                     rhs=actT[:, jj, sl], start=(jj == 0), stop=(jj == len(grp) - 1)),
                                      reads=[wdn_dd[jj], act_d[jj][tt]], writes=[ps_d[pi]], inc=(jj == len(grp) - 1))
                            kb.op("dve", lambda: nc.vector.tensor_tensor(out=xT[:, dc, sl], in0=ps[pi][:], in1=xT[:, dc, sl], op=ALU.add),
                                  reads=[ps_d[pi], xT_d[dc][tt]], writes=[xT_d[dc][tt]])
                kb.barrier()
            kb.release(mk_)

        kb.op("pool", lambda: nc.gpsimd.memset(cv[:, 1:2], 1.0), writes=[const_d])
        kb.op("pool", lambda: nc.gpsimd.memset(cv[:, 2:3], float(np.log(32.0 ** -0.5))), writes=[const_d])
        kb.barrier()
        prr = Ring(list(range(7)))

        def proj_fm(pi, wg, wg_d, c0, M, tt):
            sl = slice(tt * 512, (tt + 1) * 512)
            for kc in range(KC):
                kb.op("pe", lambda: nc.tensor.matmul(ps[pi][0:M, :], lhsT=wg[:, kc, c0:c0 + M], rhs=hT[:, kc, sl],
                                                     start=(kc == 0), stop=(kc == KC - 1)),
                      reads=[wg_d, hT_d[kc][tt]], writes=[ps_d[pi]], inc=(kc == KC - 1))

        def proj_tm(pi, col0, wg, wg_d, c0, N, i):
            for kc in range(KC):
                kb.op("pe", lambda: nc.tensor.matmul(ps[pi][:, col0:col0 + N], lhsT=hT[:, kc, i * 128:(i + 1) * 128],
                                                     rhs=wg[:, kc, c0:c0 + N], start=(kc == 0), stop=(kc == KC - 1)),
                      reads=[wg_d, hT_d[kc][i // 4]], writes=[ps_d[pi]], inc=(kc == KC - 1))

        def evac(i, out, in_, reads, writes):
            if i % 2 == 0:
                kb.op("act", lambda: nc.scalar.copy(out=out, in_=in_), reads=reads, writes=writes)
            else:
                kb.op("dve", lambda: nc.vector.tensor_copy(out=out, in_=in_), reads=reads, writes=writes)

        def v_tokmajor(vt, v_d, wg, wg_d, c0):
            for i in range(16):
                pi = prr.next()
                proj_tm(pi, 0, wg, wg_d, c0, 256, i)
                evac(i, vt[:, i, :], ps[pi][:, 0:256], [ps_d[pi]], [v_d[i]])

        def gate_fm(sg, sg_d, wg, wg_d, c0):
            for cc in range(2):
                for tt in range(4):
                    pi = prr.next()
                    proj_fm(pi, wg, wg_d, c0 + cc * 128, 128, tt)
                    kb.op("act", lambda: nc.scalar.activation(out=sg[:, cc, tt * 512:(tt + 1) * 512], in_=ps[pi][:], func=AF.Silu),
                          reads=[ps_d[pi]], writes=[sg_d[cc][tt]])

        def post_norm(c, src, src_d, ng, gname_idx, sg, sg_d, mixT, mix_d, tl):
            w = 256 // ng
            b = c % 2
            sq, sq_d, ss, ss_d, on, on_d = tl["sq"][b], tl["sq_d"][b], tl["ss"][b], tl["ss_d"][b], tl["on"][b], tl["on_d"][b]
            kb.op("act", lambda: nc.scalar.activation(out=sq[:], in_=src, func=AF.Square), reads=[src_d], writes=[sq_d])
            kb.op("dve", lambda: nc.vector.tensor_reduce(out=ss[:, 0:ng], in_=sq[:].rearrange("p (g e) -> p g e", g=ng),
                                                         axis=AX.X, op=ALU.add), reads=[sq_d], writes=[ss_d])
            kb.op("act", lambda: nc.scalar.activation(out=ss[:, 0:ng], in_=ss[:, 0:ng], func=AF.Ln, bias=cv[:, 0:1], scale=1.0 / w),
                  reads=[ss_d, const_d], writes=[ss_d])
            kb.op("act", lambda: nc.scalar.activation(out=ss[:, 0:ng], in_=ss[:, 0:ng], func=AF.Exp, scale=-0.5),
                  reads=[ss_d], writes=[ss_d])
            for g in range(ng):
                kb.op("act", lambda: nc.scalar.activation(out=on[:, g * w:(g + 1) * w], in_=src[:, g * w:(g + 1) * w], func=AF.Copy,
                                                          scale=ss[:, g:g + 1]), reads=[src_d, ss_d], writes=[on_d])
            if STAGE == 3:
                if c == 15:
                    mix_zero(0, None, None, mixT, mix_d, None)
                return
            pt = 5 + b
            for cc in range(2):
                tin = sq if STAGE == 5 else on
                tin_d = sq_d if STAGE == 5 else on_d
                if STAGE == 7:
                    continue
                kb.op("pe", lambda: nc.tensor.transpose(out=ps[pt][:, cc * 128:(cc + 1) * 128], in_=tin[:, cc * 128:(cc + 1) * 128],
                                                        identity=identf), reads=[tin_d, const_d], writes=[ps_d[pt]], inc=(cc == 1))
            if STAGE == 6:
                if c == 15:
                    mix_zero(0, None, None, mixT, mix_d, None)
                return
            for cc in range(2):
                dst = mixT[:, cc, c * 128:(c + 1) * 128]
                srcT = ps[pt][:, cc * 128:(cc + 1) * 128]
                if sg is not None:
                    kb.op("dve", lambda: nc.vector.scalar_tensor_tensor(out=dst, in0=srcT,
                                                                        scalar=ppc(*gname_idx(cc)), in1=sg[:, cc, c * 128:(c + 1) * 128],
                                                                        op0=ALU.mult, op1=ALU.mult),
                          reads=[ps_d[pt], sg_d[cc][c // 4], const_d], writes=[mix_d[cc][c // 4]])
                else:
                    kb.op("dve", lambda: nc.vector.tensor_scalar(out=dst, in0=srcT,
                                                                 scalar1=ppc(*gname_idx(cc)), scalar2=None, op0=ALU.mult),
                          reads=[ps_d[pt], const_d], writes=[mix_d[cc][c // 4]])

        def post_tiles(st):
            return dict(sq=[st("sq", [128, 256], F32) for _ in range(2)], sq_d=[Dep(), Dep()],
                        ss=[st("ss", [128, 4], F32) for _ in range(2)], ss_d=[Dep(), Dep()],
                        on=[st("on", [128, 256], F32) for _ in range(2)], on_d=[Dep(), Dep()])

        def linattn(st, Kmask, Km_d, QdT, Qd_d, kendT, ke_d, vt, v_d, cdec, cdec_d, gname_idx, sg, sg_d, mixT, mix_d):
            tl = post_tiles(st)
            attm = [st("attm", [128, 512], BF16) for _ in range(2)]
            attm_d = [Dep(), Dep()]
            ketm = [st("ketm", [128, 128], BF16) for _ in range(2)]
            ketm_d = [Dep(), Dep()]
            S_run = st("S_run", [128, 256], F32)
            S_tmp = st("S_tmp", [128, 256], F32)
            Sbf = st("Sbf", [128, 256], BF16)
            S_d, St_d, Sb_d = Dep(), Dep(), Dep()
            attm.append(st("attm", [128, 512], BF16))
            attm_d.append(Dep())
            Sbfs = [Sbf] + [st("Sbf", [128, 256], BF16) for _ in range(3)]
            Sb_ds = [Dep() for _ in range(4)]

            def stage_a(c):
                ch = slice(c * 128, (c + 1) * 128)
                b = c % 2
                b3 = c % 3
                tq = c // 4
                if c < 15:
                    kb.op("pe", lambda: nc.tensor.transpose(out=psb[:, b * 128:(b + 1) * 128], in_=kendT[:, ch], identity=identb),
                          reads=[ke_d[tq], const_d], writes=[psb_d[b]])
                    kb.op("act", lambda: nc.scalar.copy(out=ketm[b][:], in_=psb[:, b * 128:(b + 1) * 128]),
                          reads=[psb_d[b]], writes=[ketm_d[b]])
                pa = b
                for g in range(4):
                    kb.op("pe", lambda: nc.tensor.matmul(ps[pa][:, g * 128:(g + 1) * 128], lhsT=Kmask[:, g, ch], rhs=QdT[:, ch],
                                                         start=True, stop=True),
                          reads=[Km_d[tq], Qd_d[tq]], writes=[ps_d[pa]], inc=(g == 3))
                kb.op("dve", lambda: nc.vector.tensor_tensor(out=attm[b3][:], in0=ps[pa][:], in1=cmc("causal", 512), op=ALU.mult),
                      reads=[ps_d[pa], const_d], writes=[attm_d[b3]])
                if c < 15:
                    kb.op("pe", lambda: nc.tensor.matmul(ps[4][:, 0:256], lhsT=ketm[b][:], rhs=vt[:, c, :], start=True, stop=True),
                          reads=[ketm_d[b], v_d[c]], writes=[ps_d[4]])
                    if c == 0:
                        kb.op("dve", lambda: nc.vector.tensor_tensor(out=S_run[:], in0=ps[4][:, 0:256], in1=cmc("bdm4", 256), op=ALU.mult),
                              reads=[ps_d[4], const_d], writes=[S_d])
                    else:
                        kb.op("dve", lambda: nc.vector.tensor_tensor(out=S_tmp[:], in0=ps[4][:, 0:256], in1=cmc("bdm4", 256), op=ALU.mult),
                              reads=[ps_d[4], const_d], writes=[St_d])
                        kb.op("dve", lambda: nc.vector.scalar_tensor_tensor(out=S_run[:], in0=S_run[:], scalar=cdec[:, c:c + 1], in1=S_tmp[:],
                                                                            op0=ALU.mult, op1=ALU.add),
                              reads=[S_d, St_d, cdec_d], writes=[S_d])
                    kb.op("act", lambda: nc.scalar.copy(out=Sbfs[c % 4][:], in_=S_run[:]), reads=[S_d], writes=[Sb_ds[c % 4]])

            def stage_b(c):
                ch = slice(c * 128, (c + 1) * 128)
                b = c % 2
                b3 = c % 3
                tq = c // 4
                po = 2 + b
                if c > 0:
                    kb.op("pe", lambda: nc.tensor.matmul(ps[po][:, 0:256], lhsT=QdT[:, ch], rhs=Sbfs[(c - 1) % 4][:], start=True, stop=False),
                          reads=[Qd_d[tq], Sb_ds[(c - 1) % 4]], writes=[ps_d[po]], inc=False)
                for h in range(4):
                    kb.op("pe", lambda: nc.tensor.matmul(ps[po][:, h * 64:(h + 1) * 64], lhsT=attm[b3][:, h * 128:(h + 1) * 128],
                                                         rhs=vt[:, c, h * 64:(h + 1) * 64], start=(c == 0 and h == 0), stop=(h == 3)),
                          reads=[attm_d[b3], v_d[c]], writes=[ps_d[po]], inc=(h == 3))

            def stage_c(c):
                po = 2 + c % 2
                post_norm(c, ps[po][:, 0:256], ps_d[po], 4, gname_idx, sg, sg_d, mixT, mix_d, tl)

            stage_a(0)
            stage_a(1)
            for c in range(16):
                stage_b(c)
                if c + 2 < 16:
                    stage_a(c + 2)
                if c >= 1:
                    stage_c(c - 1)
            stage_c(15)

        def kq_finish(st, qsrc, q_d, ksrc, k_d, dq, dki, dke, dec_d, QdT, Qd_d, Kmask, Km_d, kendT, ke_d, tt, kinv, kinv_d):
            sl = slice(tt * 512, (tt + 1) * 512)
            kb.op("dve", lambda: nc.vector.tensor_tensor(out=QdT[:, sl], in0=qsrc, in1=dq, op=ALU.mult),
                  reads=[q_d, dec_d], writes=[Qd_d[tt]])
            kb.op("dve", lambda: nc.vector.tensor_tensor(out=kinv[:], in0=ksrc, in1=dki, op=ALU.mult),
                  reads=[k_d, dec_d], writes=[kinv_d])
            kb.op("dve", lambda: nc.vector.tensor_tensor(out=kendT[:, sl], in0=ksrc, in1=dke, op=ALU.mult),
                  reads=[k_d, dec_d], writes=[ke_d[tt]])
            for h in range(4):
                if h < 2:
                    kb.op("act", lambda: nc.scalar.activation(out=Kmask[:, h, sl], in_=kinv[:], func=AF.Copy, scale=cmc("hm4", 4)[:, h:h + 1]),
                          reads=[kinv_d, const_d], writes=[Km_d[tt]])
                else:
                    kb.op("dve", lambda: nc.vector.tensor_scalar(out=Kmask[:, h, sl], in0=kinv[:], scalar1=cmc("hm4", 4)[:, h:h + 1], scalar2=None,
                                                                 op0=ALU.mult), reads=[kinv_d, const_d], writes=[Km_d[tt]])

        def la_tiles(st):
            return dict(QdT=st("QdT", [128, S], BF16), Qd_d=[Dep() for _ in range(4)],
                        Kmask=st("Kmask", [128, 4, S], BF16), Km_d=[Dep() for _ in range(4)],
                        kendT=st("kendT", [128, S], BF16), ke_d=[Dep() for _ in range(4)],
                        vt=st("vt", [128, 16, 256], BF16), v_d=[Dep() for _ in range(16)],
                        sg=st("sg", [128, 2, S], BF16), sg_d=[[Dep() for _ in range(4)] for _ in range(2)],
                        kinv=st("kinv", [128, 512], F32), kinv_d=Dep())

        def mix_gla(l, wg, wg_d, mixT, mix_d, st):
            T = la_tiles(st)
            w2f = st("w2f", [16, 128], F32)
            w2b = st("w2b", [16, 128], BF16)
            w2_d = Dep()
            c1 = kb.chan()
            kb.dma(c1, w2f[:], gw2_d[l * 16:(l + 1) * 16, :], writes=[w2_d])
            kb.op("dve", lambda: nc.vector.tensor_copy(out=w2b[:], in_=w2f[:]), reads=[w2_d], writes=[w2_d])
            nb2 = st("nb2", [128, 1], F32)
            kb.op("pool", lambda: nc.gpsimd.tensor_scalar(out=nb2[:], in0=ppc("gla_b2n", l), scalar1=-1.0, scalar2=None, op0=ALU.mult),
                  reads=[const_d], writes=[w2_d])
            ggT = st("ggT", [16, S], BF16)
            gg_d = [Dep() for _ in range(4)]
            bcs = st("bcs", [128, S], F32)
            bcs_d = [Dep() for _ in range(4)]
            spt = [st("spt", [128, 512], F32) for _ in range(2)]
            spt_d = [Dep(), Dep()]
            for tt in range(4):
                sl = slice(tt * 512, (tt + 1) * 512)
                pi = prr.next()
                proj_fm(pi, wg, wg_d, 768, 16, tt)
                kb.op("act", lambda: nc.scalar.copy(out=ggT[:, sl], in_=ps[pi][0:16, :]), reads=[ps_d[pi]], writes=[gg_d[tt]])
                pj = prr.next()
                kb.op("pe", lambda: nc.tensor.matmul(ps[pj][:], lhsT=w2b[:], rhs=ggT[:, sl], start=True, stop=True),
                      reads=[w2_d, gg_d[tt]], writes=[ps_d[pj]])
                b = tt % 2
                kb.op("act", lambda: nc.scalar.activation(out=spt[b][:], in_=ps[pj][:], func=AF.Exp, bias=nb2[:], scale=-1.0),
                      reads=[ps_d[pj], w2_d], writes=[spt_d[b]])
                kb.op("act", lambda: nc.scalar.activation(out=spt[b][:], in_=spt[b][:], func=AF.Ln, bias=cv[:, 1:2], scale=1.0),
                      reads=[spt_d[b], const_d], writes=[spt_d[b]])
                for ci in range(4):
                    cs_ = slice(ci * 128, (ci + 1) * 128)
                    gs_ = slice(tt * 512 + ci * 128, tt * 512 + (ci + 1) * 128)
                    kb.op("dve", lambda: nc.vector.tensor_tensor_scan(out=bcs[:, gs_], data0=cmc("ones", 128), data1=spt[b][:, cs_],
                                                                      initial=0.0, op0=ALU.mult, op1=ALU.add),
                          reads=[spt_d[b], const_d], writes=[bcs_d[tt]])
            nbl = st("nbl", [128, 16], F32)
            cdec = st("cdec", [128, 16], F32)
            nbl_d, cdec_d = Dep(), Dep()
            blast = bcs[:].rearrange("p (c i) -> p c i", i=128)[:, :, 127]
            kb.op("dve", lambda: nc.vector.tensor_scalar(out=nbl[:], in0=blast, scalar1=-1.0 / 16.0, scalar2=None, op0=ALU.mult),
                  reads=bcs_d, writes=[nbl_d])
            kb.op("act", lambda: nc.scalar.activation(out=cdec[:], in_=nbl[:], func=AF.Exp), reads=[nbl_d], writes=[cdec_d])
            dq = [st("dq", [128, 512], F32)] * 2
            dki = [st("dki", [128, 512], F32)] * 2
            dke = [st("dke", [128, 512], F32)] * 2
            dec_d = [Dep()] * 2
            for tt in range(4):
                sl = slice(tt * 512, (tt + 1) * 512)
                b = tt % 2
                kb.op("act", lambda: nc.scalar.activation(out=dq[b][:], in_=bcs[:, sl], func=AF.Exp, bias=cv[:, 2:3], scale=-1.0 / 16.0),
                      reads=[bcs_d[tt], const_d], writes=[dec_d[b]])
                kb.op("act", lambda: nc.scalar.activation(out=dki[b][:], in_=bcs[:, sl], func=AF.Exp, scale=1.0 / 16.0),
                      reads=[bcs_d[tt]], writes=[dec_d[b]])
                for ci in range(4):
                    c = tt * 4 + ci
                    kb.op("act", lambda: nc.scalar.activation(out=dke[b][:, ci * 128:(ci + 1) * 128], in_=bcs[:, c * 128:(c + 1) * 128],
                                                              func=AF.Exp, bias=nbl[:, c:c + 1], scale=1.0 / 16.0),
                          reads=[bcs_d[tt], nbl_d], writes=[dec_d[b]])
                pq = prr.next()
                proj_fm(pq, wg, wg_d, 0, 128, tt)
                pk = prr.next()
                proj_fm(pk, wg, wg_d, 128, 128, tt)
                kq_finish(st, ps[pq][:], ps_d[pq], ps[pk][:], ps_d[pk], dq[b][:], dki[b][:], dke[b][:], dec_d[b],
                          T["QdT"], T["Qd_d"], T["Kmask"], T["Km_d"], T["kendT"], T["ke_d"], tt, T["kinv"], T["kinv_d"])
            v_tokmajor(T["vt"], T["v_d"], wg, wg_d, 256)
            gate_fm(T["sg"], T["sg_d"], wg, wg_d, 512)
            linattn(st, T["Kmask"], T["Km_d"], T["QdT"], T["Qd_d"], T["kendT"], T["ke_d"], T["vt"], T["v_d"], cdec, cdec_d,
                    lambda cc: ("gla_ng", l), T["sg"], T["sg_d"], mixT, mix_d)

        def linattn_v1(st, Kmask, Km_d, QdT, Qd_d, kendT, ke_d, vt, v_d, cdec, cdec_d, gname_idx, sg, sg_d, mixT, mix_d):
            tl = post_tiles(st)
            attm = [st("attm", [128, 512], BF16) for _ in range(2)]
            attm_d = [Dep(), Dep()]
            ketm = [st("ketm", [128, 128], BF16) for _ in range(2)]
            ketm_d = [Dep(), Dep()]
            S_run = st("S_run", [128, 256], F32)
            S_tmp = st("S_tmp", [128, 256], F32)
            Sbf = st("Sbf", [128, 256], BF16)
            S_d, St_d, Sb_d = Dep(), Dep(), Dep()
            for c in range(16):
                ch = slice(c * 128, (c + 1) * 128)
                b = c % 2
                tq = c // 4
                if c < 15:
                    kb.op("pe", lambda: nc.tensor.transpose(out=psb[:, b * 128:(b + 1) * 128], in_=kendT[:, ch], identity=identb),
                          reads=[ke_d[tq], const_d], writes=[psb_d[b]])
                    kb.op("act", lambda: nc.scalar.copy(out=ketm[b][:], in_=psb[:, b * 128:(b + 1) * 128]),
                          reads=[psb_d[b]], writes=[ketm_d[b]])
                pa = b
                for g in range(4):
                    kb.op("pe", lambda: nc.tensor.matmul(ps[pa][:, g * 128:(g + 1) * 128], lhsT=Kmask[:, g, ch], rhs=QdT[:, ch],
                                                         start=True, stop=True),
                          reads=[Km_d[tq], Qd_d[tq]], writes=[ps_d[pa]], inc=(g == 3))
                kb.op("dve", lambda: nc.vector.tensor_tensor(out=attm[b][:], in0=ps[pa][:], in1=cmc("causal", 512), op=ALU.mult),
                      reads=[ps_d[pa], const_d], writes=[attm_d[b]])
                po = 2 + b
                if c > 0:
                    kb.op("pe", lambda: nc.tensor.matmul(ps[po][:, 0:256], lhsT=QdT[:, ch], rhs=Sbf[:], start=True, stop=False),
                          reads=[Qd_d[tq], Sb_d], writes=[ps_d[po]], inc=False)
                for h in range(4):
                    kb.op("pe", lambda: nc.tensor.matmul(ps[po][:, h * 64:(h + 1) * 64], lhsT=attm[b][:, h * 128:(h + 1) * 128],
                                                         rhs=vt[:, c, h * 64:(h + 1) * 64], start=(c == 0 and h == 0), stop=(h == 3)),
                          reads=[attm_d[b], v_d[c]], writes=[ps_d[po]], inc=(h == 3))
                if c < 15:
                    kb.op("pe", lambda: nc.tensor.matmul(ps[4][:, 0:256], lhsT=ketm[b][:], rhs=vt[:, c, :], start=True, stop=True),
                          reads=[ketm_d[b], v_d[c]], writes=[ps_d[4]])
                    if c == 0:
                        kb.op("dve", lambda: nc.vector.tensor_tensor(out=S_run[:], in0=ps[4][:, 0:256], in1=cmc("bdm4", 256), op=ALU.mult),
                              reads=[ps_d[4], const_d, Sb_d], writes=[S_d])
                    else:
                        kb.op("dve", lambda: nc.vector.tensor_tensor(out=S_tmp[:], in0=ps[4][:, 0:256], in1=cmc("bdm4", 256), op=ALU.mult),
                              reads=[ps_d[4], const_d], writes=[St_d])
                        kb.op("dve", lambda: nc.vector.scalar_tensor_tensor(out=S_run[:], in0=S_run[:], scalar=cdec[:, c:c + 1], in1=S_tmp[:],
                                                                            op0=ALU.mult, op1=ALU.add),
                              reads=[S_d, St_d, cdec_d], writes=[S_d])
                    kb.op("act", lambda: nc.scalar.copy(out=Sbf[:], in_=S_run[:]), reads=[S_d], writes=[Sb_d])
                if STAGE == 2:
                    if c == 15:
                        mix_zero(0, None, None, mixT, mix_d, st)
                    continue
                post_norm(c, ps[po][:, 0:256], ps_d[po], 4, gname_idx, sg, sg_d, mixT, mix_d, tl)

        def kq_finish_v1(st, qsrc, q_d, ksrc, k_d, dq, dki, dke, dec_d, QdT, Qd_d, Kmask, Km_d, kendT, ke_d, tt, kinv, kinv_d):
            sl = slice(tt * 512, (tt + 1) * 512)
            kb.op("dve", lambda: nc.vector.tensor_tensor(out=QdT[:, sl], in0=qsrc, in1=dq, op=ALU.mult),
                  reads=[q_d, dec_d], writes=[Qd_d[tt]])
            kb.op("dve", lambda: nc.vector.tensor_tensor(out=kinv[:], in0=ksrc, in1=dki, op=ALU.mult),
                  reads=[k_d, dec_d], writes=[kinv_d])
            kb.op("dve", lambda: nc.vector.tensor_tensor(out=kendT[:, sl], in0=ksrc, in1=dke, op=ALU.mult),
                  reads=[k_d, dec_d], writes=[ke_d[tt]])
            for h in range(4):
                eng = "pool" if h % 2 == 0 else "dve"
                e = nc.gpsimd if eng == "pool" else nc.vector
                kb.op(eng, lambda: e.tensor_scalar(out=Kmask[:, h, sl], in0=kinv[:], scalar1=cmc("hm4", 4)[:, h:h + 1], scalar2=None,
                                                   op0=ALU.mult), reads=[kinv_d, const_d], writes=[Km_d[tt]])

        def mix_ret(l, wg, wg_d, mixT, mix_d, st):
            T = la_tiles(st)
            rcs2 = [st("rcs", [128, 1024], F32) for _ in range(2)]
            rcs_d = [Dep(), Dep()]
            rcs_c = [kb.chan(), kb.chan()]
            rt = st("rt", [128, 1552], F32)
            rt_d = Dep()
            c1 = kb.chan()
            kb.dma(c1, rt[:], rett_d[:, :], writes=[rt_d])
            qb = [st("qb", [128, 512], BF16) for _ in range(2)]
            t1 = [st("t1", [128, 512], F32)] * 2
            t2 = [st("t2", [128, 512], F32)] * 2
            qr = [st("qr", [128, 512], F32) for _ in range(2)]
            qb_d, t1_d, t2_d, qr_d = [Dep(), Dep()], [Dep()] * 2, [Dep()] * 2, [Dep(), Dep()]
            for tt in range(4):
                sl = slice(tt * 512, (tt + 1) * 512)
                rb = tt % 2
                rcs = rcs2[rb]
                kb.dma(rcs_c[rb], rcs[:, 0:512], retcs_d[:, sl], writes=[rcs_d[rb]])
                kb.dma(rcs_c[rb], rcs[:, 512:1024], retcs_d[:, S + tt * 512: S + (tt + 1) * 512], writes=[rcs_d[rb]])
                for w in range(2):
                    pi = prr.next()
                    proj_fm(pi, wg, wg_d, w * 128, 128, tt)
                    kb.op("act", lambda: nc.scalar.copy(out=qb[w][:], in_=ps[pi][:]), reads=[ps_d[pi]], writes=[qb_d[w]])
                    pj = prr.next()
                    kb.op("pe", lambda: nc.tensor.matmul(ps[pj][:], lhsT=cmb[:, 2, :], rhs=qb[w][:], start=True, stop=True),
                          reads=[qb_d[w], const_d], writes=[ps_d[pj]])
                    kb.op("dve", lambda: nc.vector.tensor_tensor(out=t1[w][:], in0=ps[pi][:], in1=rcs[:, 0:512], op=ALU.mult),
                          reads=[ps_d[pi], rcs_d[rb]], writes=[t1_d[w]])
                    kb.op("dve", lambda: nc.vector.tensor_tensor(out=t2[w][:], in0=ps[pj][:], in1=rcs[:, 512:1024],
                                                                 op=ALU.mult), reads=[ps_d[pj], rcs_d[rb]], writes=[t2_d[w]])
                    kb.op("pool", lambda: nc.gpsimd.tensor_tensor(out=qr[w][:], in0=t1[w][:], in1=t2[w][:], op=ALU.add),
                          reads=[t1_d[w], t2_d[w]], writes=[qr_d[w]])
                kq_finish_v1(st, qr[0][:], qr_d[0], qr[1][:], qr_d[1], rt[:, 0:512], rt[:, 512:1024], rt[:, 1024:1536], rt_d,
                          T["QdT"], T["Qd_d"], T["Kmask"], T["Km_d"], T["kendT"], T["ke_d"], tt, T["kinv"], T["kinv_d"])
            v_tokmajor(T["vt"], T["v_d"], wg, wg_d, 256)
            gate_fm(T["sg"], T["sg_d"], wg, wg_d, 512)
            if STAGE == 1:
                return mix_zero(l, wg, wg_d, mixT, mix_d, st)
            linattn_v1(st, T["Kmask"], T["Km_d"], T["QdT"], T["Qd_d"], T["kendT"], T["ke_d"], T["vt"], T["v_d"], rt[:, 1536:1552], rt_d,
                    lambda cc: ("ret_ng", l), T["sg"], T["sg_d"], mixT, mix_d)

        def mix_ssm(l, wg, wg_d, mixT, mix_d, st):
            rw = st("rw", [128, 384], F32)
            rw_d = Dep()
            c1 = kb.chan()
            kb.dma(c1, rw[:], rows_d[:, l * 384:(l + 1) * 384], writes=[rw_d])
            kb.op("act", lambda: nc.scalar.activation(out=rw[:, 64:128], in_=rw[:, 64:128], func=AF.Exp), reads=[rw_d], writes=[rw_d])
            kb.op("dve", lambda: nc.vector.tensor_scalar(out=rw[:, 64:128], in0=rw[:, 64:128], scalar1=-1.0, scalar2=None, op0=ALU.mult),
                  reads=[rw_d], writes=[rw_d])
            dtt = st("dtt", [128, 64], F32)
            at = st("at", [128, 64], F32)
            acs = st("acs", [128, 64], F32)
            alast = st("alast", [128, 64], F32)
            eacs = st("eacs", [128, 64], F32)
            cdec = st("cdec", [128, 64], F32)
            dte = st("dte", [128, 64], F32)
            sm_d = Dep()
            for i in range(16):
                pi = prr.next()
                proj_tm(pi, 0, wg, wg_d, 768, 4, i)
                kb.op("dve", lambda: nc.vector.tensor_tensor(out=dtt[:, i * 4:(i + 1) * 4], in0=ps[pi][:, 0:4], in1=rw[:, i * 4:(i + 1) * 4], op=ALU.add),
                      reads=[ps_d[pi], rw_d], writes=[sm_d])
            kb.op("act", lambda: nc.scalar.activation(out=dtt[:], in_=dtt[:], func=AF.Exp), reads=[sm_d], writes=[sm_d])
            kb.op("act", lambda: nc.scalar.activation(out=dtt[:], in_=dtt[:], func=AF.Ln, bias=cv[:, 1:2], scale=1.0), reads=[sm_d, const_d], writes=[sm_d])
            kb.op("dve", lambda: nc.vector.tensor_tensor(out=at[:], in0=dtt[:], in1=rw[:, 64:128], op=ALU.mult), reads=[sm_d, rw_d], writes=[sm_d])
            pa = prr.next()
            kb.op("pe", lambda: nc.tensor.matmul(ps[pa][:, 0:64], lhsT=cmc("causal", 128), rhs=at[:], start=True, stop=True),
                  reads=[sm_d, const_d], writes=[ps_d[pa]])
            kb.op("act", lambda: nc.scalar.copy(out=acs[:], in_=ps[pa][:, 0:64]), reads=[ps_d[pa]], writes=[sm_d])
            pb = prr.next()
            kb.op("pe", lambda: nc.tensor.matmul(ps[pb][:, 0:64], lhsT=cmc("ones", 128), rhs=at[:], start=True, stop=True),
                  reads=[sm_d, const_d], writes=[ps_d[pb]])
            kb.op("act", lambda: nc.scalar.copy(out=alast[:], in_=ps[pb][:, 0:64]), reads=[ps_d[pb]], writes=[sm_d])
            kb.op("act", lambda: nc.scalar.activation(out=eacs[:], in_=acs[:], func=AF.Exp), reads=[sm_d], writes=[sm_d])
            kb.op("act", lambda: nc.scalar.activation(out=cdec[:], in_=alast[:], func=AF.Exp), reads=[sm_d], writes=[sm_d])
            kb.op("dve", lambda: nc.vector.tensor_tensor(out=dte[:], in0=alast[:], in1=acs[:], op=ALU.subtract), reads=[sm_d], writes=[sm_d])
            kb.op("act", lambda: nc.scalar.activation(out=dte[:], in_=dte[:], func=AF.Exp), reads=[sm_d], writes=[sm_d])
            if STAGE == 11:
                return mix_zero(l, wg, wg_d, mixT, mix_d, st)
            upre = st("upre", [128, S + 3], BF16)
            up_d = [Dep() for _ in range(5)]
            kb.op("pool", lambda: nc.gpsimd.memset(upre[:, 0:3], 0.0), writes=[up_d[0]])
            acc = st("acc", [128, 512], F32)
            acc_d = Dep()
            xsT = st("xsT", [128, 2, S], BF16)
            xs_d = [[Dep() for _ in range(4)] for _ in range(2)]
            Bm = st("Bm", [128, 2, S], BF16)
            Bm_d = [Dep() for _ in range(4)]
            CT = st("CT", [128, S], BF16)
            CT_d = [Dep() for _ in range(4)]
            for cc in range(4):
                for tt in range(4):
                    sl = slice(tt * 512, (tt + 1) * 512)
                    pi = prr.next()
                    proj_fm(pi, wg, wg_d, 256 + cc * 128, 128, tt)
                    kb.op("act", lambda: nc.scalar.copy(out=upre[:, 3 + tt * 512: 3 + (tt + 1) * 512], in_=ps[pi][:]),
                          reads=[ps_d[pi]], writes=[up_d[tt + 1]])
                    kb.op("act", lambda: nc.scalar.activation(out=acc[:], in_=ps[pi][:], func=AF.Identity,
                                                              bias=ppc("ssm_cb", l * 4 + cc), scale=ppc("ssm_cw", (l * 4 + 3) * 4 + cc)),
                          reads=[ps_d[pi], const_d], writes=[acc_d])
                    for k in range(1, 4):
                        kb.op("dve", lambda: nc.vector.scalar_tensor_tensor(
                            out=acc[:], in0=upre[:, 3 - k + tt * 512: 3 - k + (tt + 1) * 512], scalar=ppc("ssm_cw", (l * 4 + 3 - k) * 4 + cc),
                            in1=acc[:], op0=ALU.mult, op1=ALU.add),
                            reads=[up_d[tt + 1], up_d[tt], acc_d, const_d], writes=[acc_d])
                    if cc < 2:
                        kb.op("act", lambda: nc.scalar.activation(out=xsT[:, cc, sl], in_=acc[:], func=AF.Silu), reads=[acc_d], writes=[xs_d[cc][tt]])
                    elif cc == 3:
                        kb.op("act", lambda: nc.scalar.activation(out=CT[:, sl], in_=acc[:], func=AF.Silu), reads=[acc_d], writes=[CT_d[tt]])
                    else:
                        kb.op("act", lambda: nc.scalar.activation(out=acc[:], in_=acc[:], func=AF.Silu), reads=[acc_d], writes=[acc_d])
                        kb.op("act", lambda: nc.scalar.activation(out=Bm[:, 0, sl], in_=acc[:], func=AF.Copy, scale=cmc("hm2", 2)[:, 0:1]),
                              reads=[acc_d, const_d], writes=[Bm_d[tt]])
                        kb.op("dve", lambda: nc.vector.tensor_scalar(out=Bm[:, 1, sl], in0=acc[:], scalar1=cmc("hm2", 2)[:, 1:2],
                                                                     scalar2=None, op0=ALU.mult), reads=[acc_d, const_d], writes=[Bm_d[tt]])
            if STAGE == 12:
                return mix_zero(l, wg, wg_d, mixT, mix_d, st)
            vt = st("vt", [128, 16, 256], BF16)
            xsD = st("xsD", [128, 16, 256], BF16)
            Btm = st("Btm", [128, 16, 128], BF16)
            szt = st("szt", [128, 16, 256], BF16)
            xs_tm = [st("xs_tm", [128, 256], BF16) for _ in range(2)]
            xtm_d = [Dep(), Dep()]
            v_d = [Dep() for _ in range(16)]
            xd_d = [Dep() for _ in range(16)]
            bt_d = [Dep() for _ in range(16)]
            sz_d = [Dep() for _ in range(16)]
            for c in range(16):
                ch = slice(c * 128, (c + 1) * 128)
                s0 = 0
                srcs = [(xsT[:, 0, ch], xs_d[0][c // 4]), (xsT[:, 1, ch], xs_d[1][c // 4]), (Bm[:, 0, ch], Bm_d[c // 4]), (Bm[:, 1, ch], Bm_d[c // 4])]
                for k, (ap_, d_) in enumerate(srcs):
                    kb.op("pe", lambda: nc.tensor.transpose(out=psb[:, (s0 + k) * 128:(s0 + k + 1) * 128], in_=ap_, identity=identb),
                          reads=[d_, const_d], writes=[psb_d[s0 + k]], inc=(k == 3))
                xb = xs_tm[c % 2]
                kb.op("act", lambda: nc.scalar.copy(out=xb[:], in_=psb[:, 0:256]), reads=[psb_d[0]], writes=[xtm_d[c % 2]])
                kb.op("pool", lambda: nc.gpsimd.tensor_tensor(out=xsD[:, c, :], in0=xb[:], in1=rw[:, 128:384], op=ALU.mult),
                      reads=[xtm_d[c % 2], rw_d], writes=[xd_d[c]])
                for cc in range(2):
                    pt_ = psb[:, (s0 + cc) * 128:(s0 + cc + 1) * 128]
                    for hh in range(2):
                        h = cc * 2 + hh
                        kb.op("act", lambda: nc.scalar.activation(out=vt[:, c, h * 64:(h + 1) * 64], in_=pt_[:, hh * 64:(hh + 1) * 64], func=AF.Copy,
                                                                  scale=dtt[:, c * 4 + h: c * 4 + h + 1]),
                              reads=[psb_d[s0 + cc], sm_d], writes=[v_d[c]])
                for g in range(2):
                    kb.op("act", lambda: nc.scalar.copy(out=Btm[:, c, g * 64:(g + 1) * 64],
                                                        in_=psb[:, (s0 + 2 + g) * 128 + g * 64:(s0 + 2 + g) * 128 + (g + 1) * 64]),
                          reads=[psb_d[s0 + 2 + g]], writes=[bt_d[c]])
                pi = prr.next()
                proj_tm(pi, 0, wg, wg_d, 0, 256, c)
                kb.op("act", lambda: nc.scalar.activation(out=szt[:, c, :], in_=ps[pi][:, 0:256], func=AF.Silu), reads=[ps_d[pi]], writes=[sz_d[c]])
            if STAGE == 13:
                return mix_zero(l, wg, wg_d, mixT, mix_d, st)
            tl = post_tiles(st)
            scm = st("scm", [128, 256], F32)
            Mh = [st("Mh", [128, 128], F32) for _ in range(4)]
            dec = st("dec", [128, 512], F32)
            attm = [st("attm", [128, 512], BF16) for _ in range(2)]
            yt = [st("yt", [128, 256], F32) for _ in range(2)]
            vend = st("vend", [128, 256], BF16)
            S_run = st("S_run", [128, 256], F32)
            S_tmp = st("S_tmp", [128, 256], F32)
            Sbf = st("Sbf", [128, 256], BF16)
            scm_d, Mh_d, dec_d, attm_d, yt_d, vend_d = Dep(), [Dep() for _ in range(4)], Dep(), [Dep(), Dep()], [Dep(), Dep()], Dep()
            S_d, St_d, Sb_d = Dep(), Dep(), Dep()
            for c in range(16):
                ch = slice(c * 128, (c + 1) * 128)
                b = c % 2
                tq = c // 4
                pa = b
                for g in range(2):
                    kb.op("pe", lambda: nc.tensor.matmul(ps[pa][:, g * 128:(g + 1) * 128], lhsT=Bm[:, g, ch], rhs=CT[:, ch], start=True, stop=True),
                          reads=[Bm_d[tq], CT_d[tq]], writes=[ps_d[pa]], inc=(g == 1))
                kb.op("dve", lambda: nc.vector.tensor_tensor(out=scm[:], in0=ps[pa][:, 0:256], in1=cmc("causal", 256), op=ALU.mult),
                      reads=[ps_d[pa], const_d], writes=[scm_d])
                pg = 5 + b
                for h in range(4):
                    mb = h
                    kb.op("act", lambda: nc.scalar.activation(out=Mh[mb][:], in_=cmc("sgt", 128), func=AF.Copy, scale=at[:, c * 4 + h: c * 4 + h + 1]),
                          reads=[const_d, sm_d], writes=[Mh_d[mb]])
                    kb.op("pe", lambda: nc.tensor.matmul(ps[pg][:, h * 128:(h + 1) * 128], lhsT=Mh[mb][:], rhs=cmc("causal", 128), start=True, stop=True),
                          reads=[Mh_d[mb], const_d], writes=[ps_d[pg]], inc=(h == 3))
                kb.op("act", lambda: nc.scalar.activation(out=dec[:], in_=ps[pg][:], func=AF.Exp), reads=[ps_d[pg]], writes=[dec_d])
                for h in range(4):
                    g = h // 2
                    kb.op("dve", lambda: nc.vector.tensor_tensor(out=attm[b][:, h * 128:(h + 1) * 128], in0=scm[:, g * 128:(g + 1) * 128],
                                                                 in1=dec[:, h * 128:(h + 1) * 128], op=ALU.mult),
                          reads=[scm_d, dec_d], writes=[attm_d[b]])
                po = 2 + b
                if c > 0:
                    kb.op("pe", lambda: nc.tensor.matmul(ps[po][:, 256:512], lhsT=CT[:, ch], rhs=Sbf[:], start=True, stop=True),
                          reads=[CT_d[tq], Sb_d], writes=[ps_d[po]], inc=False)
                for h in range(4):
                    kb.op("pe", lambda: nc.tensor.matmul(ps[po][:, h * 64:(h + 1) * 64], lhsT=attm[b][:, h * 128:(h + 1) * 128],
                                                         rhs=vt[:, c, h * 64:(h + 1) * 64], start=(h == 0), stop=(h == 3)),
                          reads=[attm_d[b], v_d[c]], writes=[ps_d[po]], inc=(h == 3))
                kb.op("dve", lambda: nc.vector.tensor_tensor(out=yt[b][:], in0=ps[po][:, 0:256], in1=xsD[:, c, :], op=ALU.add),
                      reads=[ps_d[po], xd_d[c]], writes=[yt_d[b]])
                if c > 0:
                    for h in range(4):
                        kb.op("dve", lambda: nc.vector.scalar_tensor_tensor(out=yt[b][:, h * 64:(h + 1) * 64], in0=ps[po][:, 256 + h * 64: 256 + (h + 1) * 64],
                                                                            scalar=eacs[:, c * 4 + h: c * 4 + h + 1], in1=yt[b][:, h * 64:(h + 1) * 64],
                                                                            op0=ALU.mult, op1=ALU.add),
                              reads=[ps_d[po], sm_d, yt_d[b]], writes=[yt_d[b]])
                kb.op("pool", lambda: nc.gpsimd.tensor_tensor(out=yt[b][:], in0=yt[b][:], in1=szt[:, c, :], op=ALU.mult),
                      reads=[yt_d[b], sz_d[c]], writes=[yt_d[b]])
                if c < 15:
                    for h in range(4):
                        if h % 2 == 0:
                            kb.op("act", lambda: nc.scalar.activation(out=vend[:, h * 64:(h + 1) * 64], in_=vt[:, c, h * 64:(h + 1) * 64], func=AF.Copy,
                                                                      scale=dte[:, c * 4 + h: c * 4 + h + 1]), reads=[v_d[c], sm_d], writes=[vend_d])
                        else:
                            kb.op("dve", lambda: nc.vector.tensor_scalar(out=vend[:, h * 64:(h + 1) * 64], in0=vt[:, c, h * 64:(h + 1) * 64],
                                                                         scalar1=dte[:, c * 4 + h: c * 4 + h + 1], scalar2=None, op0=ALU.mult),
                                  reads=[v_d[c], sm_d], writes=[vend_d])
                    kb.op("pe", lambda: nc.tensor.matmul(ps[4][:, 0:256], lhsT=Btm[:, c, :], rhs=vend[:], start=True, stop=True),
                          reads=[bt_d[c], vend_d], writes=[ps_d[4]])
                    if c == 0:
                        kb.op("dve", lambda: nc.vector.tensor_tensor(out=S_run[:], in0=ps[4][:, 0:256], in1=cmc("bdm2", 256), op=ALU.mult),
                              reads=[ps_d[4], const_d, Sb_d], writes=[S_d])
                    else:
                        kb.op("dve", lambda: nc.vector.tensor_tensor(out=S_tmp[:], in0=ps[4][:, 0:256], in1=cmc("bdm2", 256), op=ALU.mult),
                              reads=[ps_d[4], const_d], writes=[St_d])
                        for h in range(4):
                            kb.op("dve", lambda: nc.vector.scalar_tensor_tensor(out=S_run[:, h * 64:(h + 1) * 64], in0=S_run[:, h * 64:(h + 1) * 64],
                                                                                scalar=cdec[:, c * 4 + h: c * 4 + h + 1], in1=S_tmp[:, h * 64:(h + 1) * 64],
                                                                                op0=ALU.mult, op1=ALU.add),
                                  reads=[S_d, St_d, sm_d], writes=[S_d])
                    kb.op("act", lambda: nc.scalar.copy(out=Sbf[:], in_=S_run[:]), reads=[S_d], writes=[Sb_d])
                post_norm(c, yt[b][:], yt_d[b], 2, lambda cc: ("ssm_ng", 2 * l + cc), None, None, mixT, mix_d, tl)

        def mix_moba(l, wg, wg_d, mixT, mix_d, st):
            qa = [st("qa", [72, S], BF16) for _ in range(4)]
            ka = [st("ka", [72, S], BF16) for _ in range(4)]
            qa_d = [[Dep() for _ in range(4)] for _ in range(4)]
            ka_d = [[Dep() for _ in range(4)] for _ in range(4)]
            qb_d = [Dep() for _ in range(4)]
            kb_d = [Dep()] * 4
            va = st("va", [128, 16, 4, 128], BF16)
            va_d = [Dep() for _ in range(16)]
            c1 = kb.chan()
            for h in range(4):
                kb.dma(c1, ka[h][64:72, :], blk_d[:, :], writes=[kb_d[h]], q="pool")
                kb.op("pool", lambda: nc.gpsimd.memset(qa[h][64:72, :], 0.0), writes=[qb_d[h]])
            for i in range(16):
                kb.op("pool", lambda: nc.gpsimd.memset(va[:, i, :, 64:128], 1.0), writes=[va_d[i]])
            mcs = [st("mcs", [128, 1024], F32) for _ in range(2)]
            mcs_d = [Dep(), Dep()]
            mcs_c = [kb.chan(), kb.chan()]
            sqb = st("sqb", [128, 512], BF16)
            rs = st("rs", [128, 512], F32)
            qn = st("qn", [128, 512], F32)
            qnb = st("qnb", [128, 512], BF16)
            t1 = st("t1", [128, 512], F32)
            t2 = st("t2", [128, 512], F32)
            qrot = st("qrot", [128, 512], F32)
            sqb_d, rs_d, qn_d, qnb_d, t1_d, t2_d, qrot_d = Dep(), Dep(), Dep(), Dep(), Dep(), Dep(), Dep()
            kms = st("kms", [128, 2, 8], F32)
            kms_d = Dep()
            km = [st("km", [64, 8], BF16) for _ in range(4)]
            km_d = [Dep() for _ in range(4)]
            for tt in range(4):
                sl = slice(tt * 512, (tt + 1) * 512)
                rb = tt % 2
                kb.dma(mcs_c[rb], mcs[rb][:, 0:512], mobacs_d[:, sl], writes=[mcs_d[rb]])
                kb.dma(mcs_c[rb], mcs[rb][:, 512:1024], mobacs_d[:, S + tt * 512: S + (tt + 1) * 512], writes=[mcs_d[rb]])
                for w in range(2):
                    for pr in range(2):
                        pi = prr.next()
                        proj_fm(pi, wg, wg_d, w * 256 + pr * 128, 128, tt)
                        kb.op("act", lambda: nc.scalar.activation(out=sqb[:], in_=ps[pi][:], func=AF.Square), reads=[ps_d[pi]], writes=[sqb_d])
                        pj = prr.next()
                        kb.op("pe", lambda: nc.tensor.matmul(ps[pj][:], lhsT=cmb[:, 4, :], rhs=sqb[:], start=True, stop=True),
                              reads=[sqb_d, const_d], writes=[ps_d[pj]])
                        kb.op("act", lambda: nc.scalar.activation(out=rs[:], in_=ps[pj][:], func=AF.Ln, bias=cv[:, 0:1], scale=1.0 / 64.0),
                              reads=[ps_d[pj], const_d], writes=[rs_d])
                        kb.op("act", lambda: nc.scalar.activation(out=rs[:], in_=rs[:], func=AF.Exp, scale=-0.5), reads=[rs_d], writes=[rs_d])
                        gn = "moba_qg" if w == 0 else "moba_kg"
                        kb.op("dve", lambda: nc.vector.scalar_tensor_tensor(out=qn[:], in0=ps[pi][:], scalar=ppc(gn, l), in1=rs[:],
                                                                            op0=ALU.mult, op1=ALU.mult),
                              reads=[ps_d[pi], rs_d, const_d], writes=[qn_d])
                        kb.op("act", lambda: nc.scalar.copy(out=qnb[:], in_=qn[:]), reads=[qn_d], writes=[qnb_d])
                        pk = prr.next()
                        kb.op("pe", lambda: nc.tensor.matmul(ps[pk][:], lhsT=cmb[:, 3, :], rhs=qnb[:], start=True, stop=True),
                              reads=[qnb_d, const_d], writes=[ps_d[pk]])
                        kb.op("dve", lambda: nc.vector.tensor_tensor(out=t1[:], in0=qn[:], in1=mcs[rb][:, 0:512], op=ALU.mult),
                              reads=[qn_d, mcs_d[rb]], writes=[t1_d])
                        kb.op("dve", lambda: nc.vector.tensor_tensor(out=t2[:], in0=ps[pk][:], in1=mcs[rb][:, 512:1024], op=ALU.mult),
                              reads=[ps_d[pk], mcs_d[rb]], writes=[t2_d])
                        kb.op("pool", lambda: nc.gpsimd.tensor_tensor(out=qrot[:], in0=t1[:], in1=t2[:], op=ALU.add),
                              reads=[t1_d, t2_d], writes=[qrot_d])
                        for hh in range(2):
                            h = pr * 2 + hh
                            dst = (qa if w == 0 else ka)[h]
                            dd = (qa_d if w == 0 else ka_d)[h][tt]
                            kb.op("act", lambda: nc.scalar.copy(out=dst[0:64, sl], in_=qrot[hh * 64:(hh + 1) * 64, :]), reads=[qrot_d], writes=[dd])
                        if w == 1:
                            kb.op("dve", lambda: nc.vector.tensor_reduce(out=kms[:, pr, 2 * tt:2 * tt + 2], in_=qrot[:].rearrange("p (n j) -> p n j", j=256),
                                                                         axis=AX.X, op=ALU.add), reads=[qrot_d], writes=[kms_d])
            for h in range(4):
                pr, hh = h // 2, h % 2
                kb.op("act", lambda: nc.scalar.activation(out=km[h][:], in_=kms[hh * 64:(hh + 1) * 64, pr, :], func=AF.Copy, scale=1.0 / 256.0),
                      reads=[kms_d], writes=[km_d[h]])
            for i in range(16):
                pi = prr.next()
                proj_tm(pi, 0, wg, wg_d, 512, 256, i)
                evac(i, va[:, i, :, 0:64], ps[pi][:, 0:256].rearrange("p (h e) -> p h e", h=4), [ps_d[pi]], [va_d[i]])
            gm = st("gm", [128, 32], F32)
            mx = st("mx", [128, 32], F32)
            selp = [st("selp", [128, 72], BF16) for _ in range(4)]
            gm_d, mx_d = Dep(), Dep()
            selp_d = [Dep() for _ in range(4)]
            for h in range(4):
                kb.op("pool", lambda: nc.gpsimd.memset(selp[h][:], 0.0), writes=[selp_d[h]])
            for i in range(8, 16):
                b = i // 2
                tq = i // 4
                pg = 6
                for h in range(4):
                    kb.op("pe", lambda: nc.tensor.matmul(ps[pg][:, h * 8:(h + 1) * 8], lhsT=qa[h][0:64, i * 128:(i + 1) * 128], rhs=km[h][:],
                                                         start=True, stop=True), reads=[qa_d[h][tq], km_d[h]], writes=[ps_d[pg]], inc=(h == 3))
                kb.op("dve", lambda: nc.vector.tensor_tensor(out=gm[:], in0=ps[pg][:, 0:32], in1=cmc("gmask", 256)[:, b * 32:(b + 1) * 32], op=ALU.add),
                      reads=[ps_d[pg], const_d], writes=[gm_d])
                for h in range(4):
                    kb.op("dve", lambda: nc.vector.max(out=mx[:, h * 8:(h + 1) * 8], in_=gm[:, h * 8:(h + 1) * 8]), reads=[gm_d], writes=[mx_d])
                for h in range(4):
                    kb.op("dve", lambda: nc.vector.tensor_scalar(out=selp[h][:, 64:72], in0=gm[:, h * 8:(h + 1) * 8], scalar1=mx[:, h * 8 + 2:h * 8 + 3],
                                                                 scalar2=-30000.0, op0=ALU.is_lt, op1=ALU.mult),
                          reads=[gm_d, mx_d], writes=[selp_d[h]])
                for h in range(4):
                    kb.op("pe", lambda: nc.tensor.transpose(out=psb[0:72, h * 128:(h + 1) * 128], in_=selp[h][:], identity=identb),
                          reads=[selp_d[h], const_d], writes=[psb_d[h]], inc=(h == 3))
                for h in range(4):
                    kb.op("act", lambda: nc.scalar.copy(out=qa[h][64:72, i * 128:(i + 1) * 128], in_=psb[64:72, h * 128:(h + 1) * 128]),
                          reads=[psb_d[h]], writes=[qb_d[h]])
            pt = [st("pt", [128, 256], BF16) for _ in range(3)]
            pt_d = [Dep() for _ in range(3)]
            rec = [st("rec", [64, 256], F32) for _ in range(2)]
            rec_d = [Dep(), Dep()]
            items = []
            for h in range(4):
                for b in range(8):
                    for jt in range(2 * b + 2):
                        items.append((h, b, jt))
            LA = 2
            pt = pt + [st("pt", [128, 256], BF16)]
            pt_d = pt_d + [Dep()]

            def emit_score(k):
                h, b, jt = items[k]
                own = jt >= 2 * b
                qlo = jt - 2 * b
                c0 = 128 if (own and qlo == 1) else 0
                n = 256 - c0
                qcols = slice(b * 256 + c0, (b + 1) * 256)
                tq = b // 2
                pi = k % 4
                pb_ = k % 4
                kk = 64 if own else 72
                rds = [ka_d[h][jt // 4], qa_d[h][tq]] + ([] if own else [kb_d[h], qb_d[h]])
                kb.op("pe", lambda: nc.tensor.matmul(ps[pi][:, 0:n], lhsT=ka[h][0:kk, jt * 128:(jt + 1) * 128], rhs=qa[h][0:kk, qcols],
                                                     start=True, stop=True), reads=rds, writes=[ps_d[pi]])
                kb.op("act", lambda: nc.scalar.activation(out=pt[pb_][:, 0:n], in_=ps[pi][:, 0:n], func=AF.Exp, scale=0.125),
                      reads=[ps_d[pi]], writes=[pt_d[pb_]])
                if own:
                    kb.op("dve", lambda: nc.vector.tensor_tensor(out=pt[pb_][:, 0:128], in0=pt[pb_][:, 0:128], in1=cmc("causal", 128), op=ALU.mult),
                          reads=[pt_d[pb_], const_d], writes=[pt_d[pb_]])

            def emit_pv(k):
                h, b, jt = items[k]
                own = jt >= 2 * b
                qlo = jt - 2 * b
                c0 = 128 if (own and qlo == 1) else 0
                n = 256 - c0
                njt = 2 * b + 2
                nacc = h * 8 + b
                po = 4 + nacc % 2
                rbi = nacc % 2
                pb_ = k % 4
                tq = b // 2
                qs = slice(b * 256, (b + 1) * 256)
                kb.op("pe", lambda: nc.tensor.matmul(ps[po][:, c0:256], lhsT=va[:, jt, h, :], rhs=pt[pb_][:, 0:n],
                                                     start=(jt == 0), stop=(jt == njt - 1)),
                      reads=[va_d[jt], pt_d[pb_]], writes=[ps_d[po]])
                if jt == njt - 1:
                    kb.op("dve", lambda: nc.vector.reciprocal(out=rec[rbi][:], in_=ps[po][64:128, 0:256]), reads=[ps_d[po]], writes=[rec_d[rbi]])
                    hh = h % 2
                    kb.op("dve", lambda: nc.vector.tensor_tensor(out=mixT[hh * 64:(hh + 1) * 64, h // 2, qs], in0=ps[po][0:64, 0:256], in1=rec[rbi][:],
                                                                 op=ALU.mult), reads=[ps_d[po], rec_d[rbi]], writes=[mix_d[h // 2][tq]])

            for k in range(len(items) + LA):
                if k < len(items):
                    emit_score(k)
                if k >= LA:
                    emit_pv(k - LA)

        def mix_zero(l, wg, wg_d, mixT, mix_d, st):
            for cc in range(2):
                kb.op("pool", lambda: nc.gpsimd.memset(mixT[:, cc, :], 0.0), writes=mix_d[cc])

        mix_fns = [mix_moba, mix_ssm, mix_gla, mix_ret]

        def mixer_phase(l, s):
            mk_ = kb.mark()
            with contextlib.ExitStack() as sc:
                st = lambda name, shape, dt: sc.enter_context(nc.sbuf_tensor(kb.un(name), shape, dt))
                wg = st("wg", [128, KC, GW], BF16)
                wo = st("wo", [128, 2, D], BF16)
                wg_d, wo_d = Dep(), Dep()
                wg_c, wo_c = kb.chan(), kb.chan()
                mixT = st("mixT", [128, 2, S], BF16)
                mix_d = [[Dep() for _ in range(4)] for _ in range(2)]
                for m in mixers:
                    r0 = (l * 4 + m) * 128
                    kb.dma(wg_c, wg[:].rearrange("p k c -> p (k c)"), win_b[r0:r0 + 128, :], writes=[wg_d])
                    kb.dma(wo_c, wo[:].rearrange("p k c -> p (k c)"), wout_b[r0:r0 + 128, :], writes=[wo_d])
                    with contextlib.ExitStack() as sc2:
                        st2 = lambda name, shape, dt: sc2.enter_context(nc.sbuf_tensor(kb.un(name), shape, dt))
                        mix_fns[m](l, wg, wg_d, mixT, mix_d, st2)
                        kb.barrier()
                    if debug and s == 0 and l == 0:
                        dc_ = kb.chan()
                        kb.dma(dc_, dbg_d[m], mixT[:].rearrange("p k c -> p (k c)"), reads=[d for dd in mix_d for d in dd])
                    for dc in range(KC):
                        for tt in range(4):
                            sl = slice(tt * 512, (tt + 1) * 512)
                            pi = (dc * 4 + tt) % 4
                            for k2 in range(2):
                                kb.op("pe", lambda: nc.tensor.matmul(ps[pi][:], lhsT=wo[:, k2, dc * 128:(dc + 1) * 128], rhs=mixT[:, k2, sl],
                                                                     start=(k2 == 0), stop=(k2 == 1)),
                                      reads=[wo_d, mix_d[k2][tt]], writes=[ps_d[pi]], inc=(k2 == 1))
                            kb.op("dve", lambda: nc.vector.tensor_tensor(out=xT[:, dc, sl], in0=ps[pi][:], in1=xT[:, dc, sl], op=ALU.add),
                                  reads=[ps_d[pi], xT_d[dc][tt]], writes=[xT_d[dc][tt]])
                    kb.barrier()
            kb.release(mk_)

        for s in range(n_seq):
            load_x(s)
            for l in range(L):
                norm("attn_g", l)
                if mixers:
                    mixer_phase(l, s)
                if not SKIP_FFN:
                    norm("ffn_g", l)
                    ffn(l)
            store_x(s)
        kb.barrier()
    return nc


def kernel(**inputs):
    L = 2
    n_seq = 4
    x = np.asarray(inputs["x"], np.float32)
    hp = host_prep(inputs, L)
    nc = build(n_seq, L)
    in_maps = []
    for c in range(NCORES):
        m = dict(hp)
        m["x"] = np.ascontiguousarray(x[c * n_seq:(c + 1) * n_seq].reshape(n_seq * S, D))
        in_maps.append(m)
    res = run_bass_kernel_spmd(nc, in_maps, core_ids=list(range(NCORES)))
    out = np.stack([r["y"].reshape(n_seq, S, D) for r in res.results], axis=0)
    return out.reshape(NCORES * n_seq, S, D).astype(np.float32)
```

```python
import contextlib
import numpy as np
import concourse.bass as bass
import concourse.mybir as mybir
from concourse.bass_utils import run_bass_kernel_spmd
from concourse.alu_op_type import AluOpType as ALU

F32 = mybir.dt.float32
BF16 = mybir.dt.bfloat16
AF = mybir.ActivationFunctionType
AX = mybir.AxisListType

D = 1024
S = 2048
KC = 8
NCORES = 8
FF = 2816
NJ = 22
EPS = 1e-6
IN_W = 3092
GOFF = (0, 768, 1540, 2324)
GW = 784


class Dep:
    __slots__ = ("w", "r")

    def __init__(self):
        self.w = None
        self.r = {}


class Chan:
    def __init__(self, sem):
        self.sem = sem
        self.n = 0


class KB:
    def __init__(self, nc, es):
        self.nc = nc
        self.es = es
        self.eng = {"pe": nc.tensor, "act": nc.scalar, "dve": nc.vector, "pool": nc.gpsimd, "sp": nc.sync}
        self.sem = {e: es.enter_context(nc.semaphore("s_" + e)) for e in self.eng}
        self.cnt = {e: 0 for e in self.eng}
        self.seen = {e: {} for e in self.eng}
        self.chans = []
        self.nchan = 0

    def un(self, name):
        self.nuniq = getattr(self, "nuniq", 0) + 1
        return "%s_u%d" % (name, self.nuniq)

    def chan(self):
        if getattr(self, "free", None):
            c = self.free.pop()
        else:
            self.nchan += 1
            c = Chan(self.es.enter_context(self.nc.semaphore("c%d" % self.nchan)))
            self.chans.append(c)
        if not hasattr(self, "live"):
            self.live = []
        self.live.append(c)
        return c

    def mark(self):
        if not hasattr(self, "live"):
            self.live = []
        return len(self.live)

    def release(self, mark):
        if not hasattr(self, "free"):
            self.free = []
        self.free.extend(self.live[mark:])
        del self.live[mark:]

    @staticmethod
    def _need(reads, writes):
        need = {}

        def add(k, v):
            if need.get(k, 0) < v:
                need[k] = v

        for d in reads:
            if d.w is not None:
                add(*d.w)
        for d in writes:
            if d.w is not None:
                add(*d.w)
            for k, v in d.r.items():
                add(k, v)
        return need

    def _waits(self, eng, need):
        e = self.eng[eng]
        seen = self.seen[eng]
        for k, v in need.items():
            if k == "pe" and eng == "pe":
                continue
            if seen.get(k, 0) >= v:
                continue
            seen[k] = v
            if isinstance(k, Chan):
                e.wait_ge(k.sem, v)
            else:
                e.wait_ge(self.sem[k], v)

    def op(self, eng, fn, reads=(), writes=(), inc=True):
        self._waits(eng, self._need(reads, writes))
        ins = fn()
        if inc:
            self.cnt[eng] += 1
            ins.then_inc(self.sem[eng], 1)
            c = self.cnt[eng]
        else:
            c = self.cnt[eng] + 1
        for d in reads:
            if d.r.get(eng, 0) < c:
                d.r[eng] = c
        for d in writes:
            d.w = (eng, c)
            d.r = {}
        return ins

    def dma(self, chan, out, in_, reads=(), writes=(), q="sp"):
        self._waits(q, self._need(reads, writes))
        ins = self.eng[q].dma_start(out=out, in_=in_)
        chan.n += 16
        ins.then_inc(chan.sem, 16)
        for d in reads:
            d.r[chan] = chan.n
        for d in writes:
            d.w = (chan, chan.n)
            d.r = {}
        return ins

    def barrier(self):
        for e in self.eng:
            need = {k: v for k, v in self.cnt.items() if v > 0 and k != e}
            for c in self.chans:
                if c.n > 0:
                    need[c] = c.n
            self._waits(e, need)
        for e in ("act", "dve", "pool"):
            if self.cnt[e] > 0 and self.seen[e].get(e, 0) < self.cnt[e]:
                self.seen[e][e] = self.cnt[e]
                self.eng[e].wait_ge(self.sem[e], self.cnt[e])


class Ring:
    def __init__(self, items):
        self.items = items
        self.i = 0

    def next(self):
        it = self.items[self.i % len(self.items)]
        self.i += 1
        return it


def pp_layout(L):
    off = {}
    n = 0

    def add(name, cnt):
        nonlocal n
        off[name] = n
        n += cnt

    add("attn_g", L * KC)
    add("ffn_g", L * KC)
    add("fcw", L * 3 * 2 * NJ)
    add("fcb", L * 2 * NJ)
    add("gla_b2n", L)
    add("gla_ng", L)
    add("ret_ng", L)
    add("ssm_ng", L * 2)
    add("ssm_cw", L * 4 * 4)
    add("ssm_cb", L * 4)
    add("moba_qg", L)
    add("moba_kg", L)
    return off, n


def host_prep(inputs, L):
    f = np.float32
    w_in = np.asarray(inputs["w_in"], f)[:L]
    win = np.zeros((L, 4, 128, KC, GW), f)
    for g in range(4):
        w = (GOFF + (IN_W,))[g + 1] - GOFF[g]
        blk = w_in[:, :, GOFF[g]:GOFF[g] + w].reshape(L, KC, 128, w)
        win[:, g, :, :, :w] = blk.transpose(0, 2, 1, 3)
    win = win.reshape(L * 4 * 128, KC * GW)
    w_out = np.asarray(inputs["w_out"], f)[:L]
    wout = w_out.reshape(L, 4, 2, 128, D).transpose(0, 1, 3, 2, 4).reshape(L * 4 * 128, 2 * D)
    w_up = np.asarray(inputs["ffn_w_up"], f)[:L]
    wu = w_up.reshape(L, KC, 128, 2, NJ, 128)
    wup = wu.transpose(0, 4, 2, 1, 3, 5).reshape(L * NJ * 128, KC * 256)
    wdn = np.asarray(inputs["ffn_w_down"], f)[:L].reshape(L * FF, D)
    off, npp = pp_layout(L)
    pp = np.zeros((128, npp), f)

    def fm(v):
        return np.asarray(v, f).reshape(-1, 128).T

    for l in range(L):
        pp[:, off["attn_g"] + l * KC: off["attn_g"] + (l + 1) * KC] = fm(inputs["attn_norm_g"][l])
        pp[:, off["ffn_g"] + l * KC: off["ffn_g"] + (l + 1) * KC] = fm(inputs["ffn_norm_g"][l])
        for i in range(3):
            o = off["fcw"] + (l * 3 + i) * 2 * NJ
            pp[:, o:o + 2 * NJ] = fm(inputs["ffn_conv_w"][l][i])
        o = off["fcb"] + l * 2 * NJ
        pp[:, o:o + 2 * NJ] = fm(inputs["ffn_conv_b"][l])
    for l in range(L):
        pp[:, off["gla_b2n"] + l] = np.asarray(inputs["gla_gate_b"][l], f)
        pp[:, off["gla_ng"] + l] = np.tile(np.asarray(inputs["gla_norm_g"][l], f), 2)
        pp[:, off["ret_ng"] + l] = np.tile(np.asarray(inputs["ret_norm_g"][l], f), 2)
        pp[:, off["ssm_ng"] + 2 * l: off["ssm_ng"] + 2 * l + 2] = fm(inputs["ssm_norm_g"][l])
        for i in range(4):
            o = off["ssm_cw"] + (l * 4 + i) * 4
            pp[:, o:o + 4] = fm(inputs["ssm_conv_w"][l][i])
        pp[:, off["ssm_cb"] + 4 * l: off["ssm_cb"] + 4 * l + 4] = fm(inputs["ssm_conv_b"][l])
        pp[:, off["moba_qg"] + l] = np.tile(np.asarray(inputs["moba_q_norm_g"][l], f), 2)
        pp[:, off["moba_kg"] + l] = np.tile(np.asarray(inputs["moba_k_norm_g"][l], f), 2)
    consts = host_consts()
    w2 = np.asarray(inputs["gla_gate_w2"], f)[:L].reshape(L * 16, 128)
    consts["gw2"] = np.ascontiguousarray(w2)
    rows = np.zeros((128, L * 384), f)
    for l in range(L):
        rows[:, l * 384: l * 384 + 64] = np.tile(np.asarray(inputs["ssm_dt_bias"][l], f), 16)[None, :]
        rows[:, l * 384 + 64: l * 384 + 128] = np.tile(np.asarray(inputs["ssm_a_log"][l], f), 16)[None, :]
        rows[:, l * 384 + 128: l * 384 + 384] = np.repeat(np.asarray(inputs["ssm_d"][l], f), 64)[None, :]
    consts["rows"] = rows
    return dict(win=np.ascontiguousarray(win), wout=np.ascontiguousarray(wout),
                wup=np.ascontiguousarray(wup), wdn=np.ascontiguousarray(wdn), pp=pp, **consts)


CM_OFF = {}
STAGE = 0
SKIP_FFN = False


def host_consts():
    f = np.float32
    p = np.arange(128)
    cols = []

    def add(name, a):
        CM_OFF[name] = sum(c.shape[1] for c in cols)
        cols.append(np.asarray(a, f))

    add("ident", np.eye(128))
    add("ones", np.ones((128, 128)))
    caus = (p[:, None] <= p[None, :]).astype(f)
    add("causal", np.tile(caus, (1, 4)))
    add("bdm4", (p[:, None] // 32 == np.arange(256)[None, :] // 64))
    add("bdm2", (p[:, None] // 64 == np.arange(256)[None, :] // 128))
    add("hm4", (p[:, None] // 32 == np.arange(4)[None, :]))
    add("hm2", (p[:, None] // 64 == np.arange(2)[None, :]))
    def perm(hd):
        half = hd // 2
        m = np.arange(128)
        partner = (m // hd) * hd + ((m % hd) + half) % hd
        P = np.zeros((128, 128), f)
        P[partner, m] = 1.0
        return P
    add("perm32", perm(32))
    add("perm64", perm(64))
    add("bo64", (p[:, None] // 64 == p[None, :] // 64))
    add("sgt", (p[:, None] > p[None, :]))
    gmk = np.zeros((128, 8, 4, 8))
    for b_ in range(8):
        gmk[:, b_, :, b_:] = -1e30
    add("gmask", gmk.reshape(128, 256))
    cm = np.concatenate(cols, axis=1)
    lg = np.log(1.0 - 2.0 ** (-5.0 - np.arange(4, dtype=np.float64)))
    h = p // 32
    d = p % 32
    inv = 1.0 / (10000.0 ** np.linspace(0.0, 1.0, 16))
    t = np.arange(S, dtype=np.float64)
    ang = t[None, :] * inv[d % 16][:, None]
    rcos = np.cos(ang)
    rsin = np.sin(ang) * np.where(d < 16, -1.0, 1.0)[:, None]
    idx = np.arange(512) % 128
    sc = 32.0 ** -0.5
    rdq = np.exp((idx[None, :] + 1.0) * lg[h][:, None])
    rdki = np.exp(-(idx[None, :] + 1.0) * lg[h][:, None]) * sc
    rdke = np.exp((127.0 - idx[None, :]) * lg[h][:, None]) * sc
    rcd = np.repeat(np.exp(128.0 * lg[h])[:, None], 16, axis=1)
    rett = np.concatenate([rdq, rdki, rdke, rcd], axis=1).astype(f)
    dm = p % 64
    invm = 10000.0 ** (-np.arange(0, 64, 2, dtype=np.float64) / 64)
    angm = t[None, :] * invm[dm % 32][:, None]
    mcos = np.cos(angm)
    msin = np.sin(angm) * np.where(dm < 32, -1.0, 1.0)[:, None]
    blk = (np.arange(S)[None, :] // 256 == np.arange(8)[:, None]).astype(f)
    return {"blk1h": blk, "cm": cm, "ret_cs": np.concatenate([rcos, rsin], axis=1).astype(f), "ret_t": rett,
            "moba_cs": np.concatenate([mcos, msin], axis=1).astype(f)}


def build(n_seq, L, mixers=(0, 1, 2, 3), debug=False):
    nc = bass.Bass("TRN2", target_bir_lowering=False)
    off, npp = pp_layout(L)
    x_d = nc.dram_tensor("x", [n_seq * S, D], F32, kind="ExternalInput").ap()
    y_d = nc.dram_tensor("y", [n_seq * S, D], F32, kind="ExternalOutput").ap()
    win_d = nc.dram_tensor("win", [L * 4 * 128, KC * GW], F32, kind="ExternalInput").ap()
    wout_d = nc.dram_tensor("wout", [L * 4 * 128, 2 * D], F32, kind="ExternalInput").ap()
    wup_d = nc.dram_tensor("wup", [L * NJ * 128, KC * 256], F32, kind="ExternalInput").ap()
    wdn_d = nc.dram_tensor("wdn", [L * FF, D], F32, kind="ExternalInput").ap()
    pp_d = nc.dram_tensor("pp", [128, npp], F32, kind="ExternalInput").ap()
    if not CM_OFF:
        host_consts()
    NCM = CM_OFF["gmask"] + 256
    cm_d = nc.dram_tensor("cm", [128, NCM], F32, kind="ExternalInput").ap()
    retcs_d = nc.dram_tensor("ret_cs", [128, 2 * S], F32, kind="ExternalInput").ap()
    rett_d = nc.dram_tensor("ret_t", [128, 1552], F32, kind="ExternalInput").ap()
    mobacs_d = nc.dram_tensor("moba_cs", [128, 2 * S], F32, kind="ExternalInput").ap()
    gw2_d = nc.dram_tensor("gw2", [L * 16, 128], F32, kind="ExternalInput").ap()
    rows_d = nc.dram_tensor("rows", [128, L * 384], F32, kind="ExternalInput").ap()
    blk_d = nc.dram_tensor("blk1h", [8, S], F32, kind="ExternalInput").ap()
    dbg_d = nc.dram_tensor("dbg", [4, 128, 2 * S], BF16, kind="ExternalOutput").ap() if debug else None
    win_b = nc.dram_tensor("win_b", [L * 4 * 128, KC * GW], BF16, kind="Internal").ap()
    wout_b = nc.dram_tensor("wout_b", [L * 4 * 128, 2 * D], BF16, kind="Internal").ap()
    wup_b = nc.dram_tensor("wup_b", [L * NJ * 128, KC * 256], BF16, kind="Internal").ap()
    wdn_b = nc.dram_tensor("wdn_b", [L * FF, D], BF16, kind="Internal").ap()

    with contextlib.ExitStack() as es:
        kb = KB(nc, es)
        sb = lambda name, shape, dt: es.enter_context(nc.sbuf_tensor(name, shape, dt))

        cc = kb.chan()
        for src, dst in ((win_d, win_b), (wout_d, wout_b), (wup_d, wup_b), (wdn_d, wdn_b)):
            rows = src.shape[0]
            for r in range(0, rows, 128):
                kb.dma(cc, dst[r:r + 128, :], src[r:r + 128, :], q="pool")

        xT = sb("xT", [128, KC, S], F32)
        hT = sb("hT", [128, KC, S], BF16)
        xT_d = [[Dep() for _ in range(4)] for _ in range(KC)]
        hT_d = [[Dep() for _ in range(4)] for _ in range(KC)]
        ppt = sb("ppt", [128, npp], F32)
        cmt = sb("cmt", [128, NCM], F32)
        cmb = sb("cmb", [128, 5, 128], BF16)
        identf = cmt[:, 0:128]
        onesb = cmb[:, 1, :]
        identb = cmb[:, 0, :]

        def cmc(name, n):
            return cmt[:, CM_OFF[name]:CM_OFF[name] + n]
        const_d = Dep()
        c0 = kb.chan()
        kb.dma(c0, ppt[:], pp_d[:, :], writes=[const_d])
        kb.dma(c0, cmt[:], cm_d[:, :], writes=[const_d])
        for ii, nm in enumerate(("ident", "ones", "perm32", "perm64", "bo64")):
            kb.op("dve", lambda: nc.vector.tensor_copy(out=cmb[:, ii, :], in_=cmc(nm, 128)), reads=[const_d], writes=[const_d])
        cv = sb("cv", [128, 8], F32)
        kb.op("pool", lambda: nc.gpsimd.memset(cv[:, 0:1], EPS), writes=[const_d])
        ps = [es.enter_context(nc.psum_tensor("ps%d" % i, [128, 512], F32)) for i in range(7)]
        ps_d = [Dep() for _ in range(7)]
        psb = es.enter_context(nc.psum_tensor("psb", [128, 1024], BF16))
        psb_d = [Dep()] * 8
        kb.barrier()

        def ppc(name, idx):
            o = off[name] + idx
            return ppt[:, o:o + 1]

        def load_x(s):
            mk_ = kb.mark()
            with contextlib.ExitStack() as sc:
                xin = [sc.enter_context(nc.sbuf_tensor(kb.un("xin"), [128, D], F32)) for i in range(2)]
                xin_d = [Dep(), Dep()]
                xc = [kb.chan(), kb.chan()]
                for i in range(16):
                    b = i % 2
                    kb.dma(xc[b], xin[b][:], x_d[s * S + i * 128: s * S + (i + 1) * 128, :], writes=[xin_d[b]])
                    for hh in range(2):
                        pi = (i * 2 + hh) % 4
                        for k in range(4):
                            kc = hh * 4 + k
                            kb.op("pe", lambda: nc.tensor.transpose(out=ps[pi][:, k * 128:(k + 1) * 128],
                                                                    in_=xin[b][:, kc * 128:(kc + 1) * 128],
                                                                    identity=identf),
                                  reads=[xin_d[b], const_d], writes=[ps_d[pi]], inc=(k == 3))
                        eng = "act" if hh == 0 else "dve"
                        dst = xT[:, hh * 4:(hh + 1) * 4, i * 128:(i + 1) * 128]
                        src = ps[pi][:].rearrange("p (k c) -> p k c", k=4)
                        wr = [xT_d[hh * 4 + k][i // 4] for k in range(4)]
                        if eng == "act":
                            kb.op("act", lambda: nc.scalar.copy(out=dst, in_=src), reads=[ps_d[pi]], writes=wr)
                        else:
                            kb.op("dve", lambda: nc.vector.tensor_copy(out=dst, in_=src), reads=[ps_d[pi]], writes=wr)
                kb.barrier()
            kb.release(mk_)

        def store_x(s):
            mk_ = kb.mark()
            with contextlib.ExitStack() as sc:
                xo = [sc.enter_context(nc.sbuf_tensor(kb.un("xo"), [128, D], F32)) for i in range(2)]
                xo_d = [Dep(), Dep()]
                xc = [kb.chan(), kb.chan()]
                for i in range(16):
                    b = i % 2
                    for hh in range(2):
                        pi = (i * 2 + hh) % 4
                        for k in range(4):
                            kc = hh * 4 + k
                            kb.op("pe", lambda: nc.tensor.transpose(out=ps[pi][:, k * 128:(k + 1) * 128],
                                                                    in_=xT[:, kc, i * 128:(i + 1) * 128],
                                                                    identity=identf),
                                  reads=[xT_d[kc][i // 4], const_d], writes=[ps_d[pi]], inc=(k == 3))
                        dst = xo[b][:, hh * 512:(hh + 1) * 512]
                        if hh == 0:
                            kb.op("act", lambda: nc.scalar.copy(out=dst, in_=ps[pi][:]), reads=[ps_d[pi]], writes=[xo_d[b]])
                        else:
                            kb.op("dve", lambda: nc.vector.tensor_copy(out=dst, in_=ps[pi][:]), reads=[ps_d[pi]], writes=[xo_d[b]])
                    kb.dma(xc[b], y_d[s * S + i * 128: s * S + (i + 1) * 128, :], xo[b][:], reads=[xo_d[b]])
                kb.barrier()

            kb.release(mk_)

        def norm(gname, l):
            with contextlib.ExitStack() as sc:
                sq = [sc.enter_context(nc.sbuf_tensor(kb.un("sq"), [128, KC, 512], BF16)) for i in range(2)]
                rs = [sc.enter_context(nc.sbuf_tensor(kb.un("rs"), [128, 512], F32)) for i in range(2)]
                sq_d = [Dep(), Dep()]
                rs_d = [Dep(), Dep()]
                for tt in range(4):
                    b = tt % 2
                    sl = slice(tt * 512, (tt + 1) * 512)
                    kb.op("act", lambda: nc.scalar.activation(out=sq[b][:], in_=xT[:, :, sl], func=AF.Square),
                          reads=[xT_d[k][tt] for k in range(KC)], writes=[sq_d[b]])
                    pi = 4 + b
                    for kc in range(KC):
                        kb.op("pe", lambda: nc.tensor.matmul(ps[pi][:], lhsT=onesb, rhs=sq[b][:, kc, :],
                                                             start=(kc == 0), stop=(kc == KC - 1)),
                              reads=[sq_d[b], const_d], writes=[ps_d[pi]], inc=(kc == KC - 1))
                    kb.op("act", lambda: nc.scalar.activation(out=rs[b][:], in_=ps[pi][:], func=AF.Ln, bias=cv[:, 0:1], scale=1.0 / D),
                          reads=[ps_d[pi], const_d], writes=[rs_d[b]])
                    kb.op("act", lambda: nc.scalar.activation(out=rs[b][:], in_=rs[b][:], func=AF.Exp, scale=-0.5),
                          reads=[rs_d[b]], writes=[rs_d[b]])
                    for kc in range(KC):
                        kb.op("dve", lambda: nc.vector.scalar_tensor_tensor(out=hT[:, kc, sl], in0=xT[:, kc, sl],
                                                                            scalar=ppc(gname, l * KC + kc), in1=rs[b][:],
                                                                            op0=ALU.mult, op1=ALU.mult),
                              reads=[xT_d[kc][tt], rs_d[b], const_d], writes=[hT_d[kc][tt]])
                kb.barrier()

        def ffn(l):
            mk_ = kb.mark()
            G = 8
            groups = [list(range(a, min(a + G, NJ))) for a in range(0, NJ, G)]
            with contextlib.ExitStack() as sc:
                st = lambda name, shape, dt: sc.enter_context(nc.sbuf_tensor(kb.un(name), shape, dt))
                upre = [[st("upre%d_%d" % (b, h), [128, S + 2], BF16) for h in range(2)] for b in range(2)]
                upre_d = [[[Dep() for _ in range(5)] for h in range(2)] for b in range(2)]
                acc = [[st("acc%d_%d" % (r, h), [128, 512], F32) for h in range(2)] for r in range(3)]
                acc_d = [[Dep() for h in range(2)] for r in range(3)]
                actT = st("actT", [128, G, S], BF16)
                act_d = [[Dep() for _ in range(4)] for _ in range(G)]
                wup = [st("wup%d" % i, [128, KC, 256], BF16) for i in range(3)]
                wup_dd = [Dep() for _ in range(3)]
                wup_c = [kb.chan() for _ in range(3)]
                wdn = [st("wdn%d" % i, [128, D], BF16) for i in range(G)]
                wdn_dd = [Dep() for _ in range(G)]
                wdn_c = [kb.chan() for _ in range(G)]
                for b in range(2):
                    for h in range(2):
                        kb.op("pool", lambda: nc.gpsimd.memset(upre[b][h][:, 0:2], 0.0), writes=[upre_d[b][h][0]])
                accr = 0
                jcount = 0
                for grp in groups:
                    for jj, j in enumerate(grp):
                        kb.dma(wdn_c[jj], wdn[jj][:], wdn_b[l * FF + j * 128: l * FF + (j + 1) * 128, :], writes=[wdn_dd[jj]])
                    for jj, j in enumerate(grp):
                        ws = jcount % 3
                        ub = jcount % 2
                        jcount += 1
                        r0 = (l * NJ + j) * 128
                        kb.dma(wup_c[ws], wup[ws][:].rearrange("p k c -> p (k c)"), wup_b[r0:r0 + 128, :], writes=[wup_dd[ws]])
                        for tt in range(4):
                            sl = slice(tt * 512, (tt + 1) * 512)
                            ar = accr % 3
                            accr += 1
                            for h in range(2):
                                pi = (tt % 2) * 2 + h
                                for kc in range(KC):
                                    kb.op("pe", lambda: nc.tensor.matmul(ps[pi][:], lhsT=wup[ws][:, kc, h * 128:(h + 1) * 128],
                                                                         rhs=hT[:, kc, sl], start=(kc == 0), stop=(kc == KC - 1)),
                                          reads=[wup_dd[ws], hT_d[kc][tt]], writes=[ps_d[pi]], inc=(kc == KC - 1))
                                kb.op("act", lambda: nc.scalar.copy(out=upre[ub][h][:, 2 + tt * 512: 2 + (tt + 1) * 512], in_=ps[pi][:]),
                                      reads=[ps_d[pi]], writes=[upre_d[ub][h][tt + 1]])
                                w2 = ppc("fcw", (l * 3 + 2) * 2 * NJ + h * NJ + j)
                                w1 = ppc("fcw", (l * 3 + 1) * 2 * NJ + h * NJ + j)
                                w0 = ppc("fcw", (l * 3 + 0) * 2 * NJ + h * NJ + j)
                                bb = ppc("fcb", l * 2 * NJ + h * NJ + j)
                                kb.op("act", lambda: nc.scalar.activation(out=acc[ar][h][:], in_=ps[pi][:], func=AF.Identity,
                                                                          bias=bb, scale=w2),
                                      reads=[ps_d[pi], const_d], writes=[acc_d[ar][h]])
                                kb.op("dve", lambda: nc.vector.scalar_tensor_tensor(
                                    out=acc[ar][h][:], in0=upre[ub][h][:, 1 + tt * 512: 1 + (tt + 1) * 512], scalar=w1,
                                    in1=acc[ar][h][:], op0=ALU.mult, op1=ALU.add),
                                    reads=[upre_d[ub][h][tt + 1], upre_d[ub][h][tt], acc_d[ar][h], const_d], writes=[acc_d[ar][h]])
                                kb.op("dve", lambda: nc.vector.scalar_tensor_tensor(
                                    out=acc[ar][h][:], in0=upre[ub][h][:, tt * 512: (tt + 1) * 512], scalar=w0,
                                    in1=acc[ar][h][:], op0=ALU.mult, op1=ALU.add),
                                    reads=[upre_d[ub][h][tt + 1], upre_d[ub][h][tt], acc_d[ar][h], const_d], writes=[acc_d[ar][h]])
                            kb.op("act", lambda: nc.scalar.activation(out=acc[ar][0][:], in_=acc[ar][0][:], func=AF.Silu),
                                  reads=[acc_d[ar][0]], writes=[acc_d[ar][0]])
                            kb.op("pool", lambda: nc.gpsimd.tensor_tensor(out=actT[:, jj, sl], in0=acc[ar][0][:], in1=acc[ar][1][:],
                                                                          op=ALU.mult),
                                  reads=[acc_d[ar][0], acc_d[ar][1]], writes=[act_d[jj][tt]])
                    for dc in range(KC):
                        for tt in range(4):
                            sl = slice(tt * 512, (tt + 1) * 512)
                            pi = 4 + (dc * 4 + tt) % 3
                            for jj, j in enumerate(grp):
                                kb.op("pe", lambda: nc.tensor.matmul(ps[pi][:], lhsT=wdn[jj][:, dc * 128:(dc + 1) * 128],
                                                                     rhs=actT[:, jj, sl], start=(jj == 0), stop=(jj == len(grp) - 1)),
                                      reads=[wdn_dd[jj], act_d[jj][tt]], writes=[ps_d[pi]], inc=(jj == len(grp) - 1))
                            kb.op("dve", lambda: nc.vector.tensor_tensor(out=xT[:, dc, sl], in0=ps[pi][:], in1=xT[:, dc, sl], op=ALU.add),
                                  reads=[ps_d[pi], xT_d[dc][tt]], writes=[xT_d[dc][tt]])
                kb.barrier()
            kb.release(mk_)

        kb.op("pool", lambda: nc.gpsimd.memset(cv[:, 1:2], 1.0), writes=[const_d])
        kb.op("pool", lambda: nc.gpsimd.memset(cv[:, 2:3], float(np.log(32.0 ** -0.5))), writes=[const_d])
        kb.barrier()
        prr = Ring(list(range(7)))

        def proj_fm(pi, wg, wg_d, c0, M, tt):
            sl = slice(tt * 512, (tt + 1) * 512)
            for kc in range(KC):
                kb.op("pe", lambda: nc.tensor.matmul(ps[pi][0:M, :], lhsT=wg[:, kc, c0:c0 + M], rhs=hT[:, kc, sl],
                                                     start=(kc == 0), stop=(kc == KC - 1)),
                      reads=[wg_d, hT_d[kc][tt]], writes=[ps_d[pi]], inc=(kc == KC - 1))

        def proj_tm(pi, col0, wg, wg_d, c0, N, i):
            for kc in range(KC):
                kb.op("pe", lambda: nc.tensor.matmul(ps[pi][:, col0:col0 + N], lhsT=hT[:, kc, i * 128:(i + 1) * 128],
                                                     rhs=wg[:, kc, c0:c0 + N], start=(kc == 0), stop=(kc == KC - 1)),
                      reads=[wg_d, hT_d[kc][i // 4]], writes=[ps_d[pi]], inc=(kc == KC - 1))

        def evac(i, out, in_, reads, writes):
            if i % 2 == 0:
                kb.op("act", lambda: nc.scalar.copy(out=out, in_=in_), reads=reads, writes=writes)
            else:
                kb.op("dve", lambda: nc.vector.tensor_copy(out=out, in_=in_), reads=reads, writes=writes)

        def v_tokmajor(vt, v_d, wg, wg_d, c0):
            for i in range(16):
                pi = prr.next()
                proj_tm(pi, 0, wg, wg_d, c0, 256, i)
                evac(i, vt[:, i, :], ps[pi][:, 0:256], [ps_d[pi]], [v_d[i]])

        def gate_fm(sg, sg_d, wg, wg_d, c0):
            for cc in range(2):
                for tt in range(4):
                    pi = prr.next()
                    proj_fm(pi, wg, wg_d, c0 + cc * 128, 128, tt)
                    kb.op("act", lambda: nc.scalar.activation(out=sg[:, cc, tt * 512:(tt + 1) * 512], in_=ps[pi][:], func=AF.Silu),
                          reads=[ps_d[pi]], writes=[sg_d[cc][tt]])

        def post_norm(c, src, src_d, ng, gname_idx, sg, sg_d, mixT, mix_d, tl):
            w = 256 // ng
            b = c % 2
            sq, sq_d, ss, ss_d, on, on_d = tl["sq"][b], tl["sq_d"][b], tl["ss"][b], tl["ss_d"][b], tl["on"][b], tl["on_d"][b]
            kb.op("act", lambda: nc.scalar.activation(out=sq[:], in_=src, func=AF.Square), reads=[src_d], writes=[sq_d])
            kb.op("dve", lambda: nc.vector.tensor_reduce(out=ss[:, 0:ng], in_=sq[:].rearrange("p (g e) -> p g e", g=ng),
                                                         axis=AX.X, op=ALU.add), reads=[sq_d], writes=[ss_d])
            kb.op("act", lambda: nc.scalar.activation(out=ss[:, 0:ng], in_=ss[:, 0:ng], func=AF.Ln, bias=cv[:, 0:1], scale=1.0 / w),
                  reads=[ss_d, const_d], writes=[ss_d])
            kb.op("act", lambda: nc.scalar.activation(out=ss[:, 0:ng], in_=ss[:, 0:ng], func=AF.Exp, scale=-0.5),
                  reads=[ss_d], writes=[ss_d])
            for g in range(ng):
                kb.op("act", lambda: nc.scalar.activation(out=on[:, g * w:(g + 1) * w], in_=src[:, g * w:(g + 1) * w], func=AF.Copy,
                                                          scale=ss[:, g:g + 1]), reads=[src_d, ss_d], writes=[on_d])
            if STAGE == 3:
                if c == 15:
                    mix_zero(0, None, None, mixT, mix_d, None)
                return
            pt = 5 + b
            for cc in range(2):
                tin = sq if STAGE == 5 else on
                tin_d = sq_d if STAGE == 5 else on_d
                if STAGE == 7:
                    continue
                kb.op("pe", lambda: nc.tensor.transpose(out=ps[pt][:, cc * 128:(cc + 1) * 128], in_=tin[:, cc * 128:(cc + 1) * 128],
                                                        identity=identf), reads=[tin_d, const_d], writes=[ps_d[pt]], inc=(cc == 1))
            if STAGE == 6:
                if c == 15:
                    mix_zero(0, None, None, mixT, mix_d, None)
                return
            for cc in range(2):
                dst = mixT[:, cc, c * 128:(c + 1) * 128]
                srcT = ps[pt][:, cc * 128:(cc + 1) * 128]
                if sg is not None:
                    kb.op("dve", lambda: nc.vector.scalar_tensor_tensor(out=dst, in0=srcT,
                                                                        scalar=ppc(*gname_idx(cc)), in1=sg[:, cc, c * 128:(c + 1) * 128],
                                                                        op0=ALU.mult, op1=ALU.mult),
                          reads=[ps_d[pt], sg_d[cc][c // 4], const_d], writes=[mix_d[cc][c // 4]])
                else:
                    kb.op("dve", lambda: nc.vector.tensor_scalar(out=dst, in0=srcT,
                                                                 scalar1=ppc(*gname_idx(cc)), scalar2=None, op0=ALU.mult),
                          reads=[ps_d[pt], const_d], writes=[mix_d[cc][c // 4]])

        def post_tiles(st):
            return dict(sq=[st("sq", [128, 256], F32) for _ in range(2)], sq_d=[Dep(), Dep()],
                        ss=[st("ss", [128, 4], F32) for _ in range(2)], ss_d=[Dep(), Dep()],
                        on=[st("on", [128, 256], F32) for _ in range(2)], on_d=[Dep(), Dep()])

        def linattn(st, Kmask, Km_d, QdT, Qd_d, kendT, ke_d, vt, v_d, cdec, cdec_d, gname_idx, sg, sg_d, mixT, mix_d):
            tl = post_tiles(st)
            attm = [st("attm", [128, 512], BF16) for _ in range(2)]
            attm_d = [Dep(), Dep()]
            ketm = [st("ketm", [128, 128], BF16) for _ in range(2)]
            ketm_d = [Dep(), Dep()]
            S_run = st("S_run", [128, 256], F32)
            S_tmp = st("S_tmp", [128, 256], F32)
            Sbf = st("Sbf", [128, 256], BF16)
            S_d, St_d, Sb_d = Dep(), Dep(), Dep()
            attm.append(st("attm", [128, 512], BF16))
            attm_d.append(Dep())
            Sbfs = [Sbf] + [st("Sbf", [128, 256], BF16) for _ in range(3)]
            Sb_ds = [Dep() for _ in range(4)]

            def stage_a(c):
                ch = slice(c * 128, (c + 1) * 128)
                b = c % 2
                b3 = c % 3
                tq = c // 4
                if c < 15:
                    kb.op("pe", lambda: nc.tensor.transpose(out=psb[:, b * 128:(b + 1) * 128], in_=kendT[:, ch], identity=identb),
                          reads=[ke_d[tq], const_d], writes=[psb_d[b]])
                    kb.op("act", lambda: nc.scalar.copy(out=ketm[b][:], in_=psb[:, b * 128:(b + 1) * 128]),
                          reads=[psb_d[b]], writes=[ketm_d[b]])
                pa = b
                for g in range(4):
                    kb.op("pe", lambda: nc.tensor.matmul(ps[pa][:, g * 128:(g + 1) * 128], lhsT=Kmask[:, g, ch], rhs=QdT[:, ch],
                                                         start=True, stop=True),
                          reads=[Km_d[tq], Qd_d[tq]], writes=[ps_d[pa]], inc=(g == 3))
                kb.op("dve", lambda: nc.vector.tensor_tensor(out=attm[b3][:], in0=ps[pa][:], in1=cmc("causal", 512), op=ALU.mult),
                      reads=[ps_d[pa], const_d], writes=[attm_d[b3]])
                if c < 15:
                    kb.op("pe", lambda: nc.tensor.matmul(ps[4][:, 0:256], lhsT=ketm[b][:], rhs=vt[:, c, :], start=True, stop=True),
                          reads=[ketm_d[b], v_d[c]], writes=[ps_d[4]])
                    if c == 0:
                        kb.op("dve", lambda: nc.vector.tensor_tensor(out=S_run[:], in0=ps[4][:, 0:256], in1=cmc("bdm4", 256), op=ALU.mult),
                              reads=[ps_d[4], const_d], writes=[S_d])
                    else:
                        kb.op("dve", lambda: nc.vector.tensor_tensor(out=S_tmp[:], in0=ps[4][:, 0:256], in1=cmc("bdm4", 256), op=ALU.mult),
                              reads=[ps_d[4], const_d], writes=[St_d])
                        kb.op("dve", lambda: nc.vector.scalar_tensor_tensor(out=S_run[:], in0=S_run[:], scalar=cdec[:, c:c + 1], in1=S_tmp[:],
                                                                            op0=ALU.mult, op1=ALU.add),
                              reads=[S_d, St_d, cdec_d], writes=[S_d])
                    kb.op("act", lambda: nc.scalar.copy(out=Sbfs[c % 4][:], in_=S_run[:]), reads=[S_d], writes=[Sb_ds[c % 4]])

            def stage_b(c):
                ch = slice(c * 128, (c + 1) * 128)
                b = c % 2
                b3 = c % 3
                tq = c // 4
                po = 2 + b
                if c > 0:
                    kb.op("pe", lambda: nc.tensor.matmul(ps[po][:, 0:256], lhsT=QdT[:, ch], rhs=Sbfs[(c - 1) % 4][:], start=True, stop=False),
                          reads=[Qd_d[tq], Sb_ds[(c - 1) % 4]], writes=[ps_d[po]], inc=False)
                for h in range(4):
                    kb.op("pe", lambda: nc.tensor.matmul(ps[po][:, h * 64:(h + 1) * 64], lhsT=attm[b3][:, h * 128:(h + 1) * 128],
                                                         rhs=vt[:, c, h * 64:(h + 1) * 64], start=(c == 0 and h == 0), stop=(h == 3)),
                          reads=[attm_d[b3], v_d[c]], writes=[ps_d[po]], inc=(h == 3))

            def stage_c(c):
                po = 2 + c % 2
                post_norm(c, ps[po][:, 0:256], ps_d[po], 4, gname_idx, sg, sg_d, mixT, mix_d, tl)

            stage_a(0)
            stage_a(1)
            for c in range(16):
                stage_b(c)
                if c + 2 < 16:
                    stage_a(c + 2)
                if c >= 1:
                    stage_c(c - 1)
            stage_c(15)

        def kq_finish(st, qsrc, q_d, ksrc, k_d, dq, dki, dke, dec_d, QdT, Qd_d, Kmask, Km_d, kendT, ke_d, tt, kinv, kinv_d):
            sl = slice(tt * 512, (tt + 1) * 512)
            kb.op("dve", lambda: nc.vector.tensor_tensor(out=QdT[:, sl], in0=qsrc, in1=dq, op=ALU.mult),
                  reads=[q_d, dec_d], writes=[Qd_d[tt]])
            kb.op("dve", lambda: nc.vector.tensor_tensor(out=kinv[:], in0=ksrc, in1=dki, op=ALU.mult),
                  reads=[k_d, dec_d], writes=[kinv_d])
            kb.op("dve", lambda: nc.vector.tensor_tensor(out=kendT[:, sl], in0=ksrc, in1=dke, op=ALU.mult),
                  reads=[k_d, dec_d], writes=[ke_d[tt]])
            for h in range(4):
                if h < 2:
                    kb.op("act", lambda: nc.scalar.activation(out=Kmask[:, h, sl], in_=kinv[:], func=AF.Copy, scale=cmc("hm4", 4)[:, h:h + 1]),
                          reads=[kinv_d, const_d], writes=[Km_d[tt]])
                else:
                    kb.op("dve", lambda: nc.vector.tensor_scalar(out=Kmask[:, h, sl], in0=kinv[:], scalar1=cmc("hm4", 4)[:, h:h + 1], scalar2=None,
                                                                 op0=ALU.mult), reads=[kinv_d, const_d], writes=[Km_d[tt]])

        def la_tiles(st):
            return dict(QdT=st("QdT", [128, S], BF16), Qd_d=[Dep() for _ in range(4)],
                        Kmask=st("Kmask", [128, 4, S], BF16), Km_d=[Dep() for _ in range(4)],
                        kendT=st("kendT", [128, S], BF16), ke_d=[Dep() for _ in range(4)],
                        vt=st("vt", [128, 16, 256], BF16), v_d=[Dep() for _ in range(16)],
                        sg=st("sg", [128, 2, S], BF16), sg_d=[[Dep() for _ in range(4)] for _ in range(2)],
                        kinv=st("kinv", [128, 512], F32), kinv_d=Dep())

        def mix_gla(l, wg, wg_d, mixT, mix_d, st):
            T = la_tiles(st)
            w2f = st("w2f", [16, 128], F32)
            w2b = st("w2b", [16, 128], BF16)
            w2_d = Dep()
            c1 = kb.chan()
            kb.dma(c1, w2f[:], gw2_d[l * 16:(l + 1) * 16, :], writes=[w2_d])
            kb.op("dve", lambda: nc.vector.tensor_copy(out=w2b[:], in_=w2f[:]), reads=[w2_d], writes=[w2_d])
            nb2 = st("nb2", [128, 1], F32)
            kb.op("pool", lambda: nc.gpsimd.tensor_scalar(out=nb2[:], in0=ppc("gla_b2n", l), scalar1=-1.0, scalar2=None, op0=ALU.mult),
                  reads=[const_d], writes=[w2_d])
            ggT = st("ggT", [16, S], BF16)
            gg_d = [Dep() for _ in range(4)]
            bcs = st("bcs", [128, S], F32)
            bcs_d = [Dep() for _ in range(4)]
            spt = [st("spt", [128, 512], F32) for _ in range(2)]
            spt_d = [Dep(), Dep()]
            for tt in range(4):
                sl = slice(tt * 512, (tt + 1) * 512)
                pi = prr.next()
                proj_fm(pi, wg, wg_d, 768, 16, tt)
                kb.op("act", lambda: nc.scalar.copy(out=ggT[:, sl], in_=ps[pi][0:16, :]), reads=[ps_d[pi]], writes=[gg_d[tt]])
                pj = prr.next()
                kb.op("pe", lambda: nc.tensor.matmul(ps[pj][:], lhsT=w2b[:], rhs=ggT[:, sl], start=True, stop=True),
                      reads=[w2_d, gg_d[tt]], writes=[ps_d[pj]])
                b = tt % 2
                kb.op("act", lambda: nc.scalar.activation(out=spt[b][:], in_=ps[pj][:], func=AF.Exp, bias=nb2[:], scale=-1.0),
                      reads=[ps_d[pj], w2_d], writes=[spt_d[b]])
                kb.op("act", lambda: nc.scalar.activation(out=spt[b][:], in_=spt[b][:], func=AF.Ln, bias=cv[:, 1:2], scale=1.0),
                      reads=[spt_d[b], const_d], writes=[spt_d[b]])
                for ci in range(4):
                    cs_ = slice(ci * 128, (ci + 1) * 128)
                    gs_ = slice(tt * 512 + ci * 128, tt * 512 + (ci + 1) * 128)
                    kb.op("dve", lambda: nc.vector.tensor_tensor_scan(out=bcs[:, gs_], data0=cmc("ones", 128), data1=spt[b][:, cs_],
                                                                      initial=0.0, op0=ALU.mult, op1=ALU.add),
                          reads=[spt_d[b], const_d], writes=[bcs_d[tt]])
            nbl = st("nbl", [128, 16], F32)
            cdec = st("cdec", [128, 16], F32)
            nbl_d, cdec_d = Dep(), Dep()
            blast = bcs[:].rearrange("p (c i) -> p c i", i=128)[:, :, 127]
            kb.op("dve", lambda: nc.vector.tensor_scalar(out=nbl[:], in0=blast, scalar1=-1.0 / 16.0, scalar2=None, op0=ALU.mult),
                  reads=bcs_d, writes=[nbl_d])
            kb.op("act", lambda: nc.scalar.activation(out=cdec[:], in_=nbl[:], func=AF.Exp), reads=[nbl_d], writes=[cdec_d])
            dq = [st("dq", [128, 512], F32)] * 2
            dki = [st("dki", [128, 512], F32)] * 2
            dke = [st("dke", [128, 512], F32)] * 2
            dec_d = [Dep()] * 2
            for tt in range(4):
                sl = slice(tt * 512, (tt + 1) * 512)
                b = tt % 2
                kb.op("act", lambda: nc.scalar.activation(out=dq[b][:], in_=bcs[:, sl], func=AF.Exp, bias=cv[:, 2:3], scale=-1.0 / 16.0),
                      reads=[bcs_d[tt], const_d], writes=[dec_d[b]])
                kb.op("act", lambda: nc.scalar.activation(out=dki[b][:], in_=bcs[:, sl], func=AF.Exp, scale=1.0 / 16.0),
                      reads=[bcs_d[tt]], writes=[dec_d[b]])
                for ci in range(4):
                    c = tt * 4 + ci
                    kb.op("act", lambda: nc.scalar.activation(out=dke[b][:, ci * 128:(ci + 1) * 128], in_=bcs[:, c * 128:(c + 1) * 128],
                                                              func=AF.Exp, bias=nbl[:, c:c + 1], scale=1.0 / 16.0),
                          reads=[bcs_d[tt], nbl_d], writes=[dec_d[b]])
                pq = prr.next()
                proj_fm(pq, wg, wg_d, 0, 128, tt)
                pk = prr.next()
                proj_fm(pk, wg, wg_d, 128, 128, tt)
                kq_finish(st, ps[pq][:], ps_d[pq], ps[pk][:], ps_d[pk], dq[b][:], dki[b][:], dke[b][:], dec_d[b],
                          T["QdT"], T["Qd_d"], T["Kmask"], T["Km_d"], T["kendT"], T["ke_d"], tt, T["kinv"], T["kinv_d"])
            v_tokmajor(T["vt"], T["v_d"], wg, wg_d, 256)
            gate_fm(T["sg"], T["sg_d"], wg, wg_d, 512)
            linattn(st, T["Kmask"], T["Km_d"], T["QdT"], T["Qd_d"], T["kendT"], T["ke_d"], T["vt"], T["v_d"], cdec, cdec_d,
                    lambda cc: ("gla_ng", l), T["sg"], T["sg_d"], mixT, mix_d)

        def linattn_v1(st, Kmask, Km_d, QdT, Qd_d, kendT, ke_d, vt, v_d, cdec, cdec_d, gname_idx, sg, sg_d, mixT, mix_d):
            tl = post_tiles(st)
            attm = [st("attm", [128, 512], BF16) for _ in range(2)]
            attm_d = [Dep(), Dep()]
            ketm = [st("ketm", [128, 128], BF16) for _ in range(2)]
            ketm_d = [Dep(), Dep()]
            S_run = st("S_run", [128, 256], F32)
            S_tmp = st("S_tmp", [128, 256], F32)
            Sbf = st("Sbf", [128, 256], BF16)
            S_d, St_d, Sb_d = Dep(), Dep(), Dep()
            for c in range(16):
                ch = slice(c * 128, (c + 1) * 128)
                b = c % 2
                tq = c // 4
                if c < 15:
                    kb.op("pe", lambda: nc.tensor.transpose(out=psb[:, b * 128:(b + 1) * 128], in_=kendT[:, ch], identity=identb),
                          reads=[ke_d[tq], const_d], writes=[psb_d[b]])
                    kb.op("act", lambda: nc.scalar.copy(out=ketm[b][:], in_=psb[:, b * 128:(b + 1) * 128]),
                          reads=[psb_d[b]], writes=[ketm_d[b]])
                pa = b
                for g in range(4):
                    kb.op("pe", lambda: nc.tensor.matmul(ps[pa][:, g * 128:(g + 1) * 128], lhsT=Kmask[:, g, ch], rhs=QdT[:, ch],
                                                         start=True, stop=True),
                          reads=[Km_d[tq], Qd_d[tq]], writes=[ps_d[pa]], inc=(g == 3))
                kb.op("dve", lambda: nc.vector.tensor_tensor(out=attm[b][:], in0=ps[pa][:], in1=cmc("causal", 512), op=ALU.mult),
                      reads=[ps_d[pa], const_d], writes=[attm_d[b]])
                po = 2 + b
                if c > 0:
                    kb.op("pe", lambda: nc.tensor.matmul(ps[po][:, 0:256], lhsT=QdT[:, ch], rhs=Sbf[:], start=True, stop=False),
                          reads=[Qd_d[tq], Sb_d], writes=[ps_d[po]], inc=False)
                for h in range(4):
                    kb.op("pe", lambda: nc.tensor.matmul(ps[po][:, h * 64:(h + 1) * 64], lhsT=attm[b][:, h * 128:(h + 1) * 128],
                                                         rhs=vt[:, c, h * 64:(h + 1) * 64], start=(c == 0 and h == 0), stop=(h == 3)),
                          reads=[attm_d[b], v_d[c]], writes=[ps_d[po]], inc=(h == 3))
                if c < 15:
                    kb.op("pe", lambda: nc.tensor.matmul(ps[4][:, 0:256], lhsT=ketm[b][:], rhs=vt[:, c, :], start=True, stop=True),
                          reads=[ketm_d[b], v_d[c]], writes=[ps_d[4]])
                    if c == 0:
                        kb.op("dve", lambda: nc.vector.tensor_tensor(out=S_run[:], in0=ps[4][:, 0:256], in1=cmc("bdm4", 256), op=ALU.mult),
                              reads=[ps_d[4], const_d, Sb_d], writes=[S_d])
                    else:
                        kb.op("dve", lambda: nc.vector.tensor_tensor(out=S_tmp[:], in0=ps[4][:, 0:256], in1=cmc("bdm4", 256), op=ALU.mult),
                              reads=[ps_d[4], const_d], writes=[St_d])
                        kb.op("dve", lambda: nc.vector.scalar_tensor_tensor(out=S_run[:], in0=S_run[:], scalar=cdec[:, c:c + 1], in1=S_tmp[:],
                                                                            op0=ALU.mult, op1=ALU.add),
                              reads=[S_d, St_d, cdec_d], writes=[S_d])
                    kb.op("act", lambda: nc.scalar.copy(out=Sbf[:], in_=S_run[:]), reads=[S_d], writes=[Sb_d])
                if STAGE == 2:
                    if c == 15:
                        mix_zero(0, None, None, mixT, mix_d, st)
                    continue
                post_norm(c, ps[po][:, 0:256], ps_d[po], 4, gname_idx, sg, sg_d, mixT, mix_d, tl)

        def kq_finish_v1(st, qsrc, q_d, ksrc, k_d, dq, dki, dke, dec_d, QdT, Qd_d, Kmask, Km_d, kendT, ke_d, tt, kinv, kinv_d):
            sl = slice(tt * 512, (tt + 1) * 512)
            kb.op("dve", lambda: nc.vector.tensor_tensor(out=QdT[:, sl], in0=qsrc, in1=dq, op=ALU.mult),
                  reads=[q_d, dec_d], writes=[Qd_d[tt]])
            kb.op("dve", lambda: nc.vector.tensor_tensor(out=kinv[:], in0=ksrc, in1=dki, op=ALU.mult),
                  reads=[k_d, dec_d], writes=[kinv_d])
            kb.op("dve", lambda: nc.vector.tensor_tensor(out=kendT[:, sl], in0=ksrc, in1=dke, op=ALU.mult),
                  reads=[k_d, dec_d], writes=[ke_d[tt]])
            for h in range(4):
                eng = "pool" if h % 2 == 0 else "dve"
                e = nc.gpsimd if eng == "pool" else nc.vector
                kb.op(eng, lambda: e.tensor_scalar(out=Kmask[:, h, sl], in0=kinv[:], scalar1=cmc("hm4", 4)[:, h:h + 1], scalar2=None,
                                                   op0=ALU.mult), reads=[kinv_d, const_d], writes=[Km_d[tt]])

        def mix_ret(l, wg, wg_d, mixT, mix_d, st):
            T = la_tiles(st)
            rcs2 = [st("rcs", [128, 1024], F32)] * 2
            rcs_d = [Dep()] * 2
            rcs_c = [kb.chan()] * 2
            rt = st("rt", [128, 1552], F32)
            rt_d = Dep()
            c1 = kb.chan()
            kb.dma(c1, rt[:], rett_d[:, :], writes=[rt_d])
            qb = [st("qb", [128, 512], BF16)] * 2
            t1 = [st("t1", [128, 512], F32)] * 2
            t2 = [st("t2", [128, 512], F32)] * 2
            qr = [st("qr", [128, 512], F32) for _ in range(2)]
            qb_d, t1_d, t2_d, qr_d = [Dep()] * 2, [Dep()] * 2, [Dep()] * 2, [Dep(), Dep()]
            for tt in range(4):
                sl = slice(tt * 512, (tt + 1) * 512)
                rb = tt % 2
                rcs = rcs2[rb]
                kb.dma(rcs_c[rb], rcs[:, 0:512], retcs_d[:, sl], writes=[rcs_d[rb]])
                kb.dma(rcs_c[rb], rcs[:, 512:1024], retcs_d[:, S + tt * 512: S + (tt + 1) * 512], writes=[rcs_d[rb]])
                for w in range(2):
                    pi = prr.next()
                    proj_fm(pi, wg, wg_d, w * 128, 128, tt)
                    kb.op("act", lambda: nc.scalar.copy(out=qb[w][:], in_=ps[pi][:]), reads=[ps_d[pi]], writes=[qb_d[w]])
                    pj = prr.next()
                    kb.op("pe", lambda: nc.tensor.matmul(ps[pj][:], lhsT=cmb[:, 2, :], rhs=qb[w][:], start=True, stop=True),
                          reads=[qb_d[w], const_d], writes=[ps_d[pj]])
                    kb.op("dve", lambda: nc.vector.tensor_tensor(out=t1[w][:], in0=ps[pi][:], in1=rcs[:, 0:512], op=ALU.mult),
                          reads=[ps_d[pi], rcs_d[rb]], writes=[t1_d[w]])
                    kb.op("dve", lambda: nc.vector.tensor_tensor(out=t2[w][:], in0=ps[pj][:], in1=rcs[:, 512:1024],
                                                                 op=ALU.mult), reads=[ps_d[pj], rcs_d[rb]], writes=[t2_d[w]])
                    kb.op("pool", lambda: nc.gpsimd.tensor_tensor(out=qr[w][:], in0=t1[w][:], in1=t2[w][:], op=ALU.add),
                          reads=[t1_d[w], t2_d[w]], writes=[qr_d[w]])
                kq_finish_v1(st, qr[0][:], qr_d[0], qr[1][:], qr_d[1], rt[:, 0:512], rt[:, 512:1024], rt[:, 1024:1536], rt_d,
                          T["QdT"], T["Qd_d"], T["Kmask"], T["Km_d"], T["kendT"], T["ke_d"], tt, T["kinv"], T["kinv_d"])
            v_tokmajor(T["vt"], T["v_d"], wg, wg_d, 256)
            gate_fm(T["sg"], T["sg_d"], wg, wg_d, 512)
            if STAGE == 1:
                return mix_zero(l, wg, wg_d, mixT, mix_d, st)
            cdec_r = st("cdec_r", [128, 16], F32)
            cdec_rd = Dep()
            kb.op("dve", lambda: nc.vector.tensor_copy(out=cdec_r[:], in_=rt[:, 1536:1552]), reads=[rt_d], writes=[cdec_rd])
            linattn(st, T["Kmask"], T["Km_d"], T["QdT"], T["Qd_d"], T["kendT"], T["ke_d"], T["vt"], T["v_d"], cdec_r, cdec_rd,
                    lambda cc: ("ret_ng", l), T["sg"], T["sg_d"], mixT, mix_d)

        def mix_ssm(l, wg, wg_d, mixT, mix_d, st):
            rw = st("rw", [128, 384], F32)
            rw_d = Dep()
            c1 = kb.chan()
            kb.dma(c1, rw[:], rows_d[:, l * 384:(l + 1) * 384], writes=[rw_d])
            kb.op("act", lambda: nc.scalar.activation(out=rw[:, 64:128], in_=rw[:, 64:128], func=AF.Exp), reads=[rw_d], writes=[rw_d])
            kb.op("dve", lambda: nc.vector.tensor_scalar(out=rw[:, 64:128], in0=rw[:, 64:128], scalar1=-1.0, scalar2=None, op0=ALU.mult),
                  reads=[rw_d], writes=[rw_d])
            dtt = st("dtt", [128, 64], F32)
            at = st("at", [128, 64], F32)
            acs = st("acs", [128, 64], F32)
            alast = st("alast", [128, 64], F32)
            eacs = st("eacs", [128, 64], F32)
            cdec = st("cdec", [128, 64], F32)
            dte = st("dte", [128, 64], F32)
            sm_d = Dep()
            for i in range(16):
                pi = prr.next()
                proj_tm(pi, 0, wg, wg_d, 768, 4, i)
                kb.op("dve", lambda: nc.vector.tensor_tensor(out=dtt[:, i * 4:(i + 1) * 4], in0=ps[pi][:, 0:4], in1=rw[:, i * 4:(i + 1) * 4], op=ALU.add),
                      reads=[ps_d[pi], rw_d], writes=[sm_d])
            kb.op("act", lambda: nc.scalar.activation(out=dtt[:], in_=dtt[:], func=AF.Exp), reads=[sm_d], writes=[sm_d])
            kb.op("act", lambda: nc.scalar.activation(out=dtt[:], in_=dtt[:], func=AF.Ln, bias=cv[:, 1:2], scale=1.0), reads=[sm_d, const_d], writes=[sm_d])
            kb.op("dve", lambda: nc.vector.tensor_tensor(out=at[:], in0=dtt[:], in1=rw[:, 64:128], op=ALU.mult), reads=[sm_d, rw_d], writes=[sm_d])
            pa = prr.next()
            kb.op("pe", lambda: nc.tensor.matmul(ps[pa][:, 0:64], lhsT=cmc("causal", 128), rhs=at[:], start=True, stop=True),
                  reads=[sm_d, const_d], writes=[ps_d[pa]])
            kb.op("act", lambda: nc.scalar.copy(out=acs[:], in_=ps[pa][:, 0:64]), reads=[ps_d[pa]], writes=[sm_d])
            pb = prr.next()
            kb.op("pe", lambda: nc.tensor.matmul(ps[pb][:, 0:64], lhsT=cmc("ones", 128), rhs=at[:], start=True, stop=True),
                  reads=[sm_d, const_d], writes=[ps_d[pb]])
            kb.op("act", lambda: nc.scalar.copy(out=alast[:], in_=ps[pb][:, 0:64]), reads=[ps_d[pb]], writes=[sm_d])
            kb.op("act", lambda: nc.scalar.activation(out=eacs[:], in_=acs[:], func=AF.Exp), reads=[sm_d], writes=[sm_d])
            kb.op("act", lambda: nc.scalar.activation(out=cdec[:], in_=alast[:], func=AF.Exp), reads=[sm_d], writes=[sm_d])
            kb.op("dve", lambda: nc.vector.tensor_tensor(out=dte[:], in0=alast[:], in1=acs[:], op=ALU.subtract), reads=[sm_d], writes=[sm_d])
            kb.op("act", lambda: nc.scalar.activation(out=dte[:], in_=dte[:], func=AF.Exp), reads=[sm_d], writes=[sm_d])
            if STAGE == 11:
                return mix_zero(l, wg, wg_d, mixT, mix_d, st)
            upre = st("upre", [128, S + 3], BF16)
            up_d = [Dep() for _ in range(5)]
            kb.op("pool", lambda: nc.gpsimd.memset(upre[:, 0:3], 0.0), writes=[up_d[0]])
            accs = [st("acc", [128, 512], F32) for _ in range(2)]
            accs_d = [Dep(), Dep()]
            xsT = st("xsT", [128, 2, S], BF16)
            xs_d = [[Dep() for _ in range(4)] for _ in range(2)]
            Bm = st("Bm", [128, 2, S], BF16)
            Bm_d = [Dep() for _ in range(4)]
            CT = st("CT", [128, S], BF16)
            CT_d = [Dep() for _ in range(4)]
            cits = [(cc, tt) for cc in range(4) for tt in range(4)]

            def conv1(i):
                cc, tt = cits[i]
                acc, acc_d = accs[i % 2], accs_d[i % 2]
                pi = prr.next()
                proj_fm(pi, wg, wg_d, 256 + cc * 128, 128, tt)
                kb.op("act", lambda: nc.scalar.copy(out=upre[:, 3 + tt * 512: 3 + (tt + 1) * 512], in_=ps[pi][:]),
                      reads=[ps_d[pi]], writes=[up_d[tt + 1]])
                kb.op("act", lambda: nc.scalar.activation(out=acc[:], in_=ps[pi][:], func=AF.Identity,
                                                          bias=ppc("ssm_cb", l * 4 + cc), scale=ppc("ssm_cw", (l * 4 + 3) * 4 + cc)),
                      reads=[ps_d[pi], const_d], writes=[acc_d])
                for k in range(1, 4):
                    kb.op("dve", lambda: nc.vector.scalar_tensor_tensor(
                        out=acc[:], in0=upre[:, 3 - k + tt * 512: 3 - k + (tt + 1) * 512], scalar=ppc("ssm_cw", (l * 4 + 3 - k) * 4 + cc),
                        in1=acc[:], op0=ALU.mult, op1=ALU.add),
                        reads=[up_d[tt + 1], up_d[tt], acc_d, const_d], writes=[acc_d])

            def conv2(i):
                cc, tt = cits[i]
                acc, acc_d = accs[i % 2], accs_d[i % 2]
                sl = slice(tt * 512, (tt + 1) * 512)
                if cc < 2:
                    kb.op("act", lambda: nc.scalar.activation(out=xsT[:, cc, sl], in_=acc[:], func=AF.Silu), reads=[acc_d], writes=[xs_d[cc][tt]])
                elif cc == 3:
                    kb.op("act", lambda: nc.scalar.activation(out=CT[:, sl], in_=acc[:], func=AF.Silu), reads=[acc_d], writes=[CT_d[tt]])
                else:
                    kb.op("act", lambda: nc.scalar.activation(out=acc[:], in_=acc[:], func=AF.Silu), reads=[acc_d], writes=[acc_d])
                    kb.op("act", lambda: nc.scalar.activation(out=Bm[:, 0, sl], in_=acc[:], func=AF.Copy, scale=cmc("hm2", 2)[:, 0:1]),
                          reads=[acc_d, const_d], writes=[Bm_d[tt]])
                    kb.op("dve", lambda: nc.vector.tensor_scalar(out=Bm[:, 1, sl], in0=acc[:], scalar1=cmc("hm2", 2)[:, 1:2],
                                                                 scalar2=None, op0=ALU.mult), reads=[acc_d, const_d], writes=[Bm_d[tt]])

            conv1(0)
            for i in range(16):
                if i + 1 < 16:
                    conv1(i + 1)
                conv2(i)
            if STAGE == 12:
                return mix_zero(l, wg, wg_d, mixT, mix_d, st)
            vt = st("vt", [128, 16, 256], BF16)
            xsD = st("xsD", [128, 16, 256], BF16)
            Btm = st("Btm", [128, 16, 128], BF16)
            szt = st("szt", [128, 16, 256], BF16)
            xs_tm = [st("xs_tm", [128, 256], BF16) for _ in range(2)]
            xtm_d = [Dep(), Dep()]
            v_d = [Dep() for _ in range(16)]
            xd_d = [Dep() for _ in range(16)]
            bt_d = [Dep() for _ in range(16)]
            sz_d = [Dep() for _ in range(16)]
            for c in range(16):
                ch = slice(c * 128, (c + 1) * 128)
                s0 = 0
                srcs = [(xsT[:, 0, ch], xs_d[0][c // 4]), (xsT[:, 1, ch], xs_d[1][c // 4]), (Bm[:, 0, ch], Bm_d[c // 4]), (Bm[:, 1, ch], Bm_d[c // 4])]
                for k, (ap_, d_) in enumerate(srcs):
                    kb.op("pe", lambda: nc.tensor.transpose(out=psb[:, (s0 + k) * 128:(s0 + k + 1) * 128], in_=ap_, identity=identb),
                          reads=[d_, const_d], writes=[psb_d[s0 + k]], inc=(k == 3))
                xb = xs_tm[c % 2]
                kb.op("act", lambda: nc.scalar.copy(out=xb[:], in_=psb[:, 0:256]), reads=[psb_d[0]], writes=[xtm_d[c % 2]])
                kb.op("pool", lambda: nc.gpsimd.tensor_tensor(out=xsD[:, c, :], in0=xb[:], in1=rw[:, 128:384], op=ALU.mult),
                      reads=[xtm_d[c % 2], rw_d], writes=[xd_d[c]])
                for cc in range(2):
                    pt_ = psb[:, (s0 + cc) * 128:(s0 + cc + 1) * 128]
                    for hh in range(2):
                        h = cc * 2 + hh
                        kb.op("act", lambda: nc.scalar.activation(out=vt[:, c, h * 64:(h + 1) * 64], in_=pt_[:, hh * 64:(hh + 1) * 64], func=AF.Copy,
                                                                  scale=dtt[:, c * 4 + h: c * 4 + h + 1]),
                              reads=[psb_d[s0 + cc], sm_d], writes=[v_d[c]])
                for g in range(2):
                    kb.op("act", lambda: nc.scalar.copy(out=Btm[:, c, g * 64:(g + 1) * 64],
                                                        in_=psb[:, (s0 + 2 + g) * 128 + g * 64:(s0 + 2 + g) * 128 + (g + 1) * 64]),
                          reads=[psb_d[s0 + 2 + g]], writes=[bt_d[c]])
                pi = prr.next()
                proj_tm(pi, 0, wg, wg_d, 0, 256, c)
                kb.op("act", lambda: nc.scalar.activation(out=szt[:, c, :], in_=ps[pi][:, 0:256], func=AF.Silu), reads=[ps_d[pi]], writes=[sz_d[c]])
            if STAGE == 13:
                return mix_zero(l, wg, wg_d, mixT, mix_d, st)
            tl = post_tiles(st)
            scm = st("scm", [128, 256], F32)
            Mh = [st("Mh", [128, 128], F32) for _ in range(4)]
            dec = st("dec", [128, 512], F32)
            attm = [st("attm", [128, 512], BF16) for _ in range(2)]
            yt = [st("yt", [128, 256], F32) for _ in range(2)]
            vend = st("vend", [128, 256], BF16)
            S_run = st("S_run", [128, 256], F32)
            S_tmp = st("S_tmp", [128, 256], F32)
            Sbf = st("Sbf", [128, 256], BF16)
            scm_d, Mh_d, dec_d, attm_d, yt_d, vend_d = Dep(), [Dep() for _ in range(4)], Dep(), [Dep(), Dep()], [Dep(), Dep()], Dep()
            S_d, St_d, Sb_d = Dep(), Dep(), Dep()
            for c in range(16):
                ch = slice(c * 128, (c + 1) * 128)
                b = c % 2
                tq = c // 4
                pa = b
                for g in range(2):
                    kb.op("pe", lambda: nc.tensor.matmul(ps[pa][:, g * 128:(g + 1) * 128], lhsT=Bm[:, g, ch], rhs=CT[:, ch], start=True, stop=True),
                          reads=[Bm_d[tq], CT_d[tq]], writes=[ps_d[pa]], inc=(g == 1))
                kb.op("dve", lambda: nc.vector.tensor_tensor(out=scm[:], in0=ps[pa][:, 0:256], in1=cmc("causal", 256), op=ALU.mult),
                      reads=[ps_d[pa], const_d], writes=[scm_d])
                pg = 5 + b
                for h in range(4):
                    mb = h
                    kb.op("act", lambda: nc.scalar.activation(out=Mh[mb][:], in_=cmc("sgt", 128), func=AF.Copy, scale=at[:, c * 4 + h: c * 4 + h + 1]),
                          reads=[const_d, sm_d], writes=[Mh_d[mb]])
                    kb.op("pe", lambda: nc.tensor.matmul(ps[pg][:, h * 128:(h + 1) * 128], lhsT=Mh[mb][:], rhs=cmc("causal", 128), start=True, stop=True),
                          reads=[Mh_d[mb], const_d], writes=[ps_d[pg]], inc=(h == 3))
                kb.op("act", lambda: nc.scalar.activation(out=dec[:], in_=ps[pg][:], func=AF.Exp), reads=[ps_d[pg]], writes=[dec_d])
                for h in range(4):
                    g = h // 2
                    kb.op("dve", lambda: nc.vector.tensor_tensor(out=attm[b][:, h * 128:(h + 1) * 128], in0=scm[:, g * 128:(g + 1) * 128],
                                                                 in1=dec[:, h * 128:(h + 1) * 128], op=ALU.mult),
                          reads=[scm_d, dec_d], writes=[attm_d[b]])
                po = 2 + b
                if c > 0:
                    kb.op("pe", lambda: nc.tensor.matmul(ps[po][:, 256:512], lhsT=CT[:, ch], rhs=Sbf[:], start=True, stop=True),
                          reads=[CT_d[tq], Sb_d], writes=[ps_d[po]], inc=False)
                for h in range(4):
                    kb.op("pe", lambda: nc.tensor.matmul(ps[po][:, h * 64:(h + 1) * 64], lhsT=attm[b][:, h * 128:(h + 1) * 128],
                                                         rhs=vt[:, c, h * 64:(h + 1) * 64], start=(h == 0), stop=(h == 3)),
                          reads=[attm_d[b], v_d[c]], writes=[ps_d[po]], inc=(h == 3))
                kb.op("dve", lambda: nc.vector.tensor_tensor(out=yt[b][:], in0=ps[po][:, 0:256], in1=xsD[:, c, :], op=ALU.add),
                      reads=[ps_d[po], xd_d[c]], writes=[yt_d[b]])
                if c > 0:
                    for h in range(4):
                        kb.op("dve", lambda: nc.vector.scalar_tensor_tensor(out=yt[b][:, h * 64:(h + 1) * 64], in0=ps[po][:, 256 + h * 64: 256 + (h + 1) * 64],
                                                                            scalar=eacs[:, c * 4 + h: c * 4 + h + 1], in1=yt[b][:, h * 64:(h + 1) * 64],
                                                                            op0=ALU.mult, op1=ALU.add),
                              reads=[ps_d[po], sm_d, yt_d[b]], writes=[yt_d[b]])
                kb.op("pool", lambda: nc.gpsimd.tensor_tensor(out=yt[b][:], in0=yt[b][:], in1=szt[:, c, :], op=ALU.mult),
                      reads=[yt_d[b], sz_d[c]], writes=[yt_d[b]])
                if c < 15:
                    for h in range(4):
                        if h % 2 == 0:
                            kb.op("act", lambda: nc.scalar.activation(out=vend[:, h * 64:(h + 1) * 64], in_=vt[:, c, h * 64:(h + 1) * 64], func=AF.Copy,
                                                                      scale=dte[:, c * 4 + h: c * 4 + h + 1]), reads=[v_d[c], sm_d], writes=[vend_d])
                        else:
                            kb.op("dve", lambda: nc.vector.tensor_scalar(out=vend[:, h * 64:(h + 1) * 64], in0=vt[:, c, h * 64:(h + 1) * 64],
                                                                         scalar1=dte[:, c * 4 + h: c * 4 + h + 1], scalar2=None, op0=ALU.mult),
                                  reads=[v_d[c], sm_d], writes=[vend_d])
                    kb.op("pe", lambda: nc.tensor.matmul(ps[4][:, 0:256], lhsT=Btm[:, c, :], rhs=vend[:], start=True, stop=True),
                          reads=[bt_d[c], vend_d], writes=[ps_d[4]])
                    if c == 0:
                        kb.op("dve", lambda: nc.vector.tensor_tensor(out=S_run[:], in0=ps[4][:, 0:256], in1=cmc("bdm2", 256), op=ALU.mult),
                              reads=[ps_d[4], const_d, Sb_d], writes=[S_d])
                    else:
                        kb.op("dve", lambda: nc.vector.tensor_tensor(out=S_tmp[:], in0=ps[4][:, 0:256], in1=cmc("bdm2", 256), op=ALU.mult),
                              reads=[ps_d[4], const_d], writes=[St_d])
                        for h in range(4):
                            kb.op("dve", lambda: nc.vector.scalar_tensor_tensor(out=S_run[:, h * 64:(h + 1) * 64], in0=S_run[:, h * 64:(h + 1) * 64],
                                                                                scalar=cdec[:, c * 4 + h: c * 4 + h + 1], in1=S_tmp[:, h * 64:(h + 1) * 64],
                                                                                op0=ALU.mult, op1=ALU.add),
                                  reads=[S_d, St_d, sm_d], writes=[S_d])
                    kb.op("act", lambda: nc.scalar.copy(out=Sbf[:], in_=S_run[:]), reads=[S_d], writes=[Sb_d])
                post_norm(c, yt[b][:], yt_d[b], 2, lambda cc: ("ssm_ng", 2 * l + cc), None, None, mixT, mix_d, tl)

        def mix_moba(l, wg, wg_d, mixT, mix_d, st):
            qa = [st("qa", [72, S], BF16) for _ in range(4)]
            ka = [st("ka", [72, S], BF16) for _ in range(4)]
            qa_d = [[Dep() for _ in range(4)] for _ in range(4)]
            ka_d = [[Dep() for _ in range(4)] for _ in range(4)]
            qb_d = [Dep() for _ in range(4)]
            kb_d = [Dep()] * 4
            va = st("va", [128, 16, 4, 128], BF16)
            va_d = [Dep() for _ in range(16)]
            c1 = kb.chan()
            for h in range(4):
                kb.dma(c1, ka[h][64:72, :], blk_d[:, :], writes=[kb_d[h]], q="pool")
                kb.op("pool", lambda: nc.gpsimd.memset(qa[h][64:72, :], 0.0), writes=[qb_d[h]])
            for i in range(16):
                kb.op("pool", lambda: nc.gpsimd.memset(va[:, i, :, 64:128], 1.0), writes=[va_d[i]])
            mcs = [st("mcs", [128, 1024], F32) for _ in range(2)]
            mcs_d = [Dep(), Dep()]
            mcs_c = [kb.chan(), kb.chan()]
            sqb = [st("sqb", [128, 512], BF16) for _ in range(2)]
            rs = [st("rs", [128, 512], F32) for _ in range(2)]
            qn = [st("qn", [128, 512], F32) for _ in range(2)]
            qnb = [st("qnb", [128, 512], BF16) for _ in range(2)]
            sqb_d, rs_d, qn_d, qnb_d = [Dep(), Dep()], [Dep(), Dep()], [Dep(), Dep()], [Dep(), Dep()]
            kms = st("kms", [128, 2, 8], F32)
            kms_d = Dep()
            km = [st("km", [64, 8], BF16) for _ in range(4)]
            km_d = [Dep() for _ in range(4)]
            its = [(tt, w, pr) for tt in range(4) for w in range(2) for pr in range(2)]

            def prep1(i):
                tt, w, pr = its[i]
                s_ = i % 2
                sl = slice(tt * 512, (tt + 1) * 512)
                rb = tt % 2
                if w == 0 and pr == 0:
                    kb.dma(mcs_c[rb], mcs[rb][:, 0:512], mobacs_d[:, sl], writes=[mcs_d[rb]])
                    kb.dma(mcs_c[rb], mcs[rb][:, 512:1024], mobacs_d[:, S + tt * 512: S + (tt + 1) * 512], writes=[mcs_d[rb]])
                pi = prr.next()
                proj_fm(pi, wg, wg_d, w * 256 + pr * 128, 128, tt)
                kb.op("act", lambda: nc.scalar.activation(out=sqb[s_][:], in_=ps[pi][:], func=AF.Square), reads=[ps_d[pi]], writes=[sqb_d[s_]])
                pj = prr.next()
                kb.op("pe", lambda: nc.tensor.matmul(ps[pj][:], lhsT=cmb[:, 4, :], rhs=sqb[s_][:], start=True, stop=True),
                      reads=[sqb_d[s_], const_d], writes=[ps_d[pj]])
                kb.op("act", lambda: nc.scalar.activation(out=rs[s_][:], in_=ps[pj][:], func=AF.Ln, bias=cv[:, 0:1], scale=1.0 / 64.0),
                      reads=[ps_d[pj], const_d], writes=[rs_d[s_]])
                kb.op("act", lambda: nc.scalar.activation(out=rs[s_][:], in_=rs[s_][:], func=AF.Exp, scale=-0.5), reads=[rs_d[s_]], writes=[rs_d[s_]])
                gn = "moba_qg" if w == 0 else "moba_kg"
                kb.op("dve", lambda: nc.vector.scalar_tensor_tensor(out=qn[s_][:], in0=ps[pi][:], scalar=ppc(gn, l), in1=rs[s_][:],
                                                                    op0=ALU.mult, op1=ALU.mult),
                      reads=[ps_d[pi], rs_d[s_], const_d], writes=[qn_d[s_]])
                kb.op("act", lambda: nc.scalar.copy(out=qnb[s_][:], in_=qn[s_][:]), reads=[qn_d[s_]], writes=[qnb_d[s_]])

            def prep2(i):
                tt, w, pr = its[i]
                s_ = i % 2
                sl = slice(tt * 512, (tt + 1) * 512)
                rb = tt % 2
                pk = prr.next()
                kb.op("pe", lambda: nc.tensor.matmul(ps[pk][:], lhsT=cmb[:, 3, :], rhs=qnb[s_][:], start=True, stop=True),
                      reads=[qnb_d[s_], const_d], writes=[ps_d[pk]])
                kb.op("dve", lambda: nc.vector.tensor_tensor(out=qn[s_][:], in0=qn[s_][:], in1=mcs[rb][:, 0:512], op=ALU.mult),
                      reads=[qn_d[s_], mcs_d[rb]], writes=[qn_d[s_]])
                kb.op("dve", lambda: nc.vector.tensor_tensor(out=rs[s_][:], in0=ps[pk][:], in1=mcs[rb][:, 512:1024], op=ALU.mult),
                      reads=[ps_d[pk], mcs_d[rb]], writes=[rs_d[s_]])
                kb.op("pool", lambda: nc.gpsimd.tensor_tensor(out=qn[s_][:], in0=qn[s_][:], in1=rs[s_][:], op=ALU.add),
                      reads=[qn_d[s_], rs_d[s_]], writes=[qn_d[s_]])
                for hh in range(2):
                    h = pr * 2 + hh
                    dst = (qa if w == 0 else ka)[h]
                    dd = (qa_d if w == 0 else ka_d)[h][tt]
                    kb.op("act", lambda: nc.scalar.copy(out=dst[0:64, sl], in_=qn[s_][hh * 64:(hh + 1) * 64, :]), reads=[qn_d[s_]], writes=[dd])
                if w == 1:
                    kb.op("dve", lambda: nc.vector.tensor_reduce(out=kms[:, pr, 2 * tt:2 * tt + 2], in_=qn[s_][:].rearrange("p (n j) -> p n j", j=256),
                                                                 axis=AX.X, op=ALU.add), reads=[qn_d[s_]], writes=[kms_d])

            prep1(0)
            for i in range(16):
                if i + 1 < 16:
                    prep1(i + 1)
                prep2(i)
            for h in range(4):
                pr, hh = h // 2, h % 2
                kb.op("act", lambda: nc.scalar.activation(out=km[h][:], in_=kms[hh * 64:(hh + 1) * 64, pr, :], func=AF.Copy, scale=1.0 / 256.0),
                      reads=[kms_d], writes=[km_d[h]])
            for i in range(16):
                pi = prr.next()
                proj_tm(pi, 0, wg, wg_d, 512, 256, i)
                evac(i, va[:, i, :, 0:64], ps[pi][:, 0:256].rearrange("p (h e) -> p h e", h=4), [ps_d[pi]], [va_d[i]])
            gm = st("gm", [128, 32], F32)
            mx = st("mx", [128, 32], F32)
            selp = [st("selp", [128, 72], BF16) for _ in range(4)]
            gm_d, mx_d = Dep(), Dep()
            selp_d = [Dep() for _ in range(4)]
            for h in range(4):
                kb.op("pool", lambda: nc.gpsimd.memset(selp[h][:], 0.0), writes=[selp_d[h]])
            for i in range(8, 16):
                b = i // 2
                tq = i // 4
                pg = 6
                for h in range(4):
                    kb.op("pe", lambda: nc.tensor.matmul(ps[pg][:, h * 8:(h + 1) * 8], lhsT=qa[h][0:64, i * 128:(i + 1) * 128], rhs=km[h][:],
                                                         start=True, stop=True), reads=[qa_d[h][tq], km_d[h]], writes=[ps_d[pg]], inc=(h == 3))
                kb.op("dve", lambda: nc.vector.tensor_tensor(out=gm[:], in0=ps[pg][:, 0:32], in1=cmc("gmask", 256)[:, b * 32:(b + 1) * 32], op=ALU.add),
                      reads=[ps_d[pg], const_d], writes=[gm_d])
                for h in range(4):
                    kb.op("dve", lambda: nc.vector.max(out=mx[:, h * 8:(h + 1) * 8], in_=gm[:, h * 8:(h + 1) * 8]), reads=[gm_d], writes=[mx_d])
                for h in range(4):
                    kb.op("dve", lambda: nc.vector.tensor_scalar(out=selp[h][:, 64:72], in0=gm[:, h * 8:(h + 1) * 8], scalar1=mx[:, h * 8 + 2:h * 8 + 3],
                                                                 scalar2=-30000.0, op0=ALU.is_lt, op1=ALU.mult),
                          reads=[gm_d, mx_d], writes=[selp_d[h]])
                for h in range(4):
                    kb.op("pe", lambda: nc.tensor.transpose(out=psb[0:72, h * 128:(h + 1) * 128], in_=selp[h][:], identity=identb),
                          reads=[selp_d[h], const_d], writes=[psb_d[h]], inc=(h == 3))
                for h in range(4):
                    kb.op("act", lambda: nc.scalar.copy(out=qa[h][64:72, i * 128:(i + 1) * 128], in_=psb[64:72, h * 128:(h + 1) * 128]),
                          reads=[psb_d[h]], writes=[qb_d[h]])
            pt = [st("pt", [128, 256], BF16) for _ in range(3)]
            pt_d = [Dep() for _ in range(3)]
            rec = [st("rec", [64, 256], F32) for _ in range(2)]
            rec_d = [Dep(), Dep()]
            items = []
            for h in range(4):
                for b in range(8):
                    for jt in range(2 * b + 2):
                        items.append((h, b, jt))
            LA = 2
            pt = pt + [st("pt", [128, 256], BF16)]
            pt_d = pt_d + [Dep()]

            def emit_score(k):
                h, b, jt = items[k]
                own = jt >= 2 * b
                qlo = jt - 2 * b
                c0 = 128 if (own and qlo == 1) else 0
                n = 256 - c0
                qcols = slice(b * 256 + c0, (b + 1) * 256)
                tq = b // 2
                pi = k % 4
                pb_ = k % 4
                kk = 64 if own else 72
                rds = [ka_d[h][jt // 4], qa_d[h][tq]] + ([] if own else [kb_d[h], qb_d[h]])
                kb.op("pe", lambda: nc.tensor.matmul(ps[pi][:, 0:n], lhsT=ka[h][0:kk, jt * 128:(jt + 1) * 128], rhs=qa[h][0:kk, qcols],
                                                     start=True, stop=True), reads=rds, writes=[ps_d[pi]])
                kb.op("act", lambda: nc.scalar.activation(out=pt[pb_][:, 0:n], in_=ps[pi][:, 0:n], func=AF.Exp, scale=0.125),
                      reads=[ps_d[pi]], writes=[pt_d[pb_]])
                if own:
                    kb.op("dve", lambda: nc.vector.tensor_tensor(out=pt[pb_][:, 0:128], in0=pt[pb_][:, 0:128], in1=cmc("causal", 128), op=ALU.mult),
                          reads=[pt_d[pb_], const_d], writes=[pt_d[pb_]])

            def emit_pv(k):
                h, b, jt = items[k]
                own = jt >= 2 * b
                qlo = jt - 2 * b
                c0 = 128 if (own and qlo == 1) else 0
                n = 256 - c0
                njt = 2 * b + 2
                nacc = h * 8 + b
                po = 4 + nacc % 2
                rbi = nacc % 2
                pb_ = k % 4
                tq = b // 2
                qs = slice(b * 256, (b + 1) * 256)
                kb.op("pe", lambda: nc.tensor.matmul(ps[po][:, c0:256], lhsT=va[:, jt, h, :], rhs=pt[pb_][:, 0:n],
                                                     start=(jt == 0), stop=(jt == njt - 1)),
                      reads=[va_d[jt], pt_d[pb_]], writes=[ps_d[po]])
                if jt == njt - 1:
                    kb.op("dve", lambda: nc.vector.reciprocal(out=rec[rbi][:], in_=ps[po][64:128, 0:256]), reads=[ps_d[po]], writes=[rec_d[rbi]])
                    hh = h % 2
                    kb.op("dve", lambda: nc.vector.tensor_tensor(out=mixT[hh * 64:(hh + 1) * 64, h // 2, qs], in0=ps[po][0:64, 0:256], in1=rec[rbi][:],
                                                                 op=ALU.mult), reads=[ps_d[po], rec_d[rbi]], writes=[mix_d[h // 2][tq]])

            for k in range(len(items) + LA):
                if k < len(items):
                    emit_score(k)
                if k >= LA:
                    emit_pv(k - LA)

        def mix_zero(l, wg, wg_d, mixT, mix_d, st):
            for cc in range(2):
                kb.op("pool", lambda: nc.gpsimd.memset(mixT[:, cc, :], 0.0), writes=mix_d[cc])

        mix_fns = [mix_moba, mix_ssm, mix_gla, mix_ret]

        def mixer_phase(l, s):
            mk_ = kb.mark()
            with contextlib.ExitStack() as sc:
                st = lambda name, shape, dt: sc.enter_context(nc.sbuf_tensor(kb.un(name), shape, dt))
                wg = st("wg", [128, KC, GW], BF16)
                wo = st("wo", [128, 2, D], BF16)
                wg_d, wo_d = Dep(), Dep()
                wg_c, wo_c = kb.chan(), kb.chan()
                mixT = st("mixT", [128, 2, S], BF16)
                mix_d = [[Dep() for _ in range(4)] for _ in range(2)]
                for m in mixers:
                    r0 = (l * 4 + m) * 128
                    kb.dma(wg_c, wg[:].rearrange("p k c -> p (k c)"), win_b[r0:r0 + 128, :], writes=[wg_d])
                    kb.dma(wo_c, wo[:].rearrange("p k c -> p (k c)"), wout_b[r0:r0 + 128, :], writes=[wo_d])
                    with contextlib.ExitStack() as sc2:
                        st2 = lambda name, shape, dt: sc2.enter_context(nc.sbuf_tensor(kb.un(name), shape, dt))
                        mix_fns[m](l, wg, wg_d, mixT, mix_d, st2)
                        kb.barrier()
                    if debug and s == 0 and l == 0:
                        dc_ = kb.chan()
                        kb.dma(dc_, dbg_d[m], mixT[:].rearrange("p k c -> p (k c)"), reads=[d for dd in mix_d for d in dd])
                    for dc in range(KC):
                        for tt in range(4):
                            sl = slice(tt * 512, (tt + 1) * 512)
                            pi = (dc * 4 + tt) % 4
                            for k2 in range(2):
                                kb.op("pe", lambda: nc.tensor.matmul(ps[pi][:], lhsT=wo[:, k2, dc * 128:(dc + 1) * 128], rhs=mixT[:, k2, sl],
                                                                     start=(k2 == 0), stop=(k2 == 1)),
                                      reads=[wo_d, mix_d[k2][tt]], writes=[ps_d[pi]], inc=(k2 == 1))
                            kb.op("dve", lambda: nc.vector.tensor_tensor(out=xT[:, dc, sl], in0=ps[pi][:], in1=xT[:, dc, sl], op=ALU.add),
                                  reads=[ps_d[pi], xT_d[dc][tt]], writes=[xT_d[dc][tt]])
                    kb.barrier()
            kb.release(mk_)

        for s in range(n_seq):
            load_x(s)
            for l in range(L):
                norm("attn_g", l)
                if mixers:
                    mixer_phase(l, s)
                if not SKIP_FFN:
                    norm("ffn_g", l)
                    ffn(l)
            store_x(s)
        kb.barrier()
    return nc


def kernel(**inputs):
    L = 2
    n_seq = 4
    x = np.asarray(inputs["x"], np.float32)
    hp = host_prep(inputs, L)
    nc = build(n_seq, L)
    in_maps = []
    for c in range(NCORES):
        m = dict(hp)
        m["x"] = np.ascontiguousarray(x[c * n_seq:(c + 1) * n_seq].reshape(n_seq * S, D))
        in_maps.append(m)
    res = run_bass_kernel_spmd(nc, in_maps, core_ids=list(range(NCORES)))
    out = np.stack([r["y"].reshape(n_seq, S, D) for r in res.results], axis=0)
    return out.reshape(NCORES * n_seq, S, D).astype(np.float32)
```

```python
import contextlib
import numpy as np
import concourse.bass as bass
import concourse.mybir as mybir
from concourse.bass_utils import run_bass_kernel_spmd
from concourse.alu_op_type import AluOpType as ALU

F32 = mybir.dt.float32
BF16 = mybir.dt.bfloat16
AF = mybir.ActivationFunctionType
AX = mybir.AxisListType

D = 1024
S = 2048
KC = 8
NCORES = 8
FF = 2816
NJ = 22
EPS = 1e-6
IN_W = 3092
GOFF = (0, 768, 1540, 2324)
GW = 784


class Dep:
    __slots__ = ("w", "r")

    def __init__(self):
        self.w = None
        self.r = {}


class Chan:
    def __init__(self, sem):
        self.sem = sem
        self.n = 0


class KB:
    def __init__(self, nc, es):
        self.nc = nc
        self.es = es
        self.eng = {"pe": nc.tensor, "act": nc.scalar, "dve": nc.vector, "pool": nc.gpsimd, "sp": nc.sync}
        self.sem = {e: es.enter_context(nc.semaphore("s_" + e)) for e in self.eng}
        self.cnt = {e: 0 for e in self.eng}
        self.seen = {e: {} for e in self.eng}
        self.chans = []
        self.nchan = 0

    def un(self, name):
        self.nuniq = getattr(self, "nuniq", 0) + 1
        return "%s_u%d" % (name, self.nuniq)

    def chan(self):
        if getattr(self, "free", None):
            c = self.free.pop()
        else:
            self.nchan += 1
            c = Chan(self.es.enter_context(self.nc.semaphore("c%d" % self.nchan)))
            self.chans.append(c)
        if not hasattr(self, "live"):
            self.live = []
        self.live.append(c)
        return c

    def mark(self):
        if not hasattr(self, "live"):
            self.live = []
        return len(self.live)

    def release(self, mark):
        if not hasattr(self, "free"):
            self.free = []
        self.free.extend(self.live[mark:])
        del self.live[mark:]

    @staticmethod
    def _need(reads, writes):
        need = {}

        def add(k, v):
            if need.get(k, 0) < v:
                need[k] = v

        for d in reads:
            if d.w is not None:
                add(*d.w)
        for d in writes:
            if d.w is not None:
                add(*d.w)
            for k, v in d.r.items():
                add(k, v)
        return need

    def _waits(self, eng, need):
        e = self.eng[eng]
        seen = self.seen[eng]
        for k, v in need.items():
            if k == "pe" and eng == "pe":
                continue
            if seen.get(k, 0) >= v:
                continue
            seen[k] = v
            if isinstance(k, Chan):
                e.wait_ge(k.sem, v)
            else:
                e.wait_ge(self.sem[k], v)

    def op(self, eng, fn, reads=(), writes=(), inc=True):
        self._waits(eng, self._need(reads, writes))
        ins = fn()
        if inc:
            self.cnt[eng] += 1
            ins.then_inc(self.sem[eng], 1)
            c = self.cnt[eng]
        else:
            c = self.cnt[eng] + 1
        for d in reads:
            if d.r.get(eng, 0) < c:
                d.r[eng] = c
        for d in writes:
            d.w = (eng, c)
            d.r = {}
        return ins

    def dma(self, chan, out, in_, reads=(), writes=(), q="sp"):
        self._waits(q, self._need(reads, writes))
        ins = self.eng[q].dma_start(out=out, in_=in_)
        chan.n += 16
        ins.then_inc(chan.sem, 16)
        for d in reads:
            d.r[chan] = chan.n
        for d in writes:
            d.w = (chan, chan.n)
            d.r = {}
        return ins

    def barrier(self):
        for e in self.eng:
            need = {k: v for k, v in self.cnt.items() if v > 0 and k != e}
            for c in self.chans:
                if c.n > 0:
                    need[c] = c.n
            self._waits(e, need)
        for e in ("act", "dve", "pool"):
            if self.cnt[e] > 0 and self.seen[e].get(e, 0) < self.cnt[e]:
                self.seen[e][e] = self.cnt[e]
                self.eng[e].wait_ge(self.sem[e], self.cnt[e])


class Ring:
    def __init__(self, items):
        self.items = items
        self.i = 0

    def next(self):
        it = self.items[self.i % len(self.items)]
        self.i += 1
        return it


def pp_layout(L):
    off = {}
    n = 0

    def add(name, cnt):
        nonlocal n
        off[name] = n
        n += cnt

    add("attn_g", L * KC)
    add("ffn_g", L * KC)
    add("fcw", L * 3 * 2 * NJ)
    add("fcb", L * 2 * NJ)
    add("gla_b2n", L)
    add("gla_ng", L)
    add("ret_ng", L)
    add("ssm_ng", L * 2)
    add("ssm_cw", L * 4 * 4)
    add("ssm_cb", L * 4)
    add("moba_qg", L)
    add("moba_kg", L)
    return off, n


def host_prep(inputs, L):
    f = np.float32
    w_in = np.asarray(inputs["w_in"], f)[:L]
    win = np.zeros((L, 4, 128, KC, GW), f)
    for g in range(4):
        w = (GOFF + (IN_W,))[g + 1] - GOFF[g]
        blk = w_in[:, :, GOFF[g]:GOFF[g] + w].reshape(L, KC, 128, w)
        win[:, g, :, :, :w] = blk.transpose(0, 2, 1, 3)
    win = win.reshape(L * 4 * 128, KC * GW)
    w_out = np.asarray(inputs["w_out"], f)[:L]
    wout = w_out.reshape(L, 4, 2, 128, D).transpose(0, 1, 3, 2, 4).reshape(L * 4 * 128, 2 * D)
    w_up = np.asarray(inputs["ffn_w_up"], f)[:L]
    wu = w_up.reshape(L, KC, 128, 2, NJ, 128)
    wup = wu.transpose(0, 4, 2, 1, 3, 5).reshape(L * NJ * 128, KC * 256)
    wdn = np.asarray(inputs["ffn_w_down"], f)[:L].reshape(L * FF, D)
    off, npp = pp_layout(L)
    pp = np.zeros((128, npp), f)

    def fm(v):
        return np.asarray(v, f).reshape(-1, 128).T

    for l in range(L):
        pp[:, off["attn_g"] + l * KC: off["attn_g"] + (l + 1) * KC] = fm(inputs["attn_norm_g"][l])
        pp[:, off["ffn_g"] + l * KC: off["ffn_g"] + (l + 1) * KC] = fm(inputs["ffn_norm_g"][l])
        for i in range(3):
            o = off["fcw"] + (l * 3 + i) * 2 * NJ
            pp[:, o:o + 2 * NJ] = fm(inputs["ffn_conv_w"][l][i])
        o = off["fcb"] + l * 2 * NJ
        pp[:, o:o + 2 * NJ] = fm(inputs["ffn_conv_b"][l])
    for l in range(L):
        pp[:, off["gla_b2n"] + l] = np.asarray(inputs["gla_gate_b"][l], f)
        pp[:, off["gla_ng"] + l] = np.tile(np.asarray(inputs["gla_norm_g"][l], f), 2)
        pp[:, off["ret_ng"] + l] = np.tile(np.asarray(inputs["ret_norm_g"][l], f), 2)
        pp[:, off["ssm_ng"] + 2 * l: off["ssm_ng"] + 2 * l + 2] = fm(inputs["ssm_norm_g"][l])
        for i in range(4):
            o = off["ssm_cw"] + (l * 4 + i) * 4
            pp[:, o:o + 4] = fm(inputs["ssm_conv_w"][l][i])
        pp[:, off["ssm_cb"] + 4 * l: off["ssm_cb"] + 4 * l + 4] = fm(inputs["ssm_conv_b"][l])
        pp[:, off["moba_qg"] + l] = np.tile(np.asarray(inputs["moba_q_norm_g"][l], f), 2)
        pp[:, off["moba_kg"] + l] = np.tile(np.asarray(inputs["moba_k_norm_g"][l], f), 2)
    consts = host_consts()
    w2 = np.asarray(inputs["gla_gate_w2"], f)[:L].reshape(L * 16, 128)
    consts["gw2"] = np.ascontiguousarray(w2)
    rows = np.zeros((128, L * 384), f)
    for l in range(L):
        rows[:, l * 384: l * 384 + 64] = np.tile(np.asarray(inputs["ssm_dt_bias"][l], f), 16)[None, :]
        rows[:, l * 384 + 64: l * 384 + 128] = np.tile(np.asarray(inputs["ssm_a_log"][l], f), 16)[None, :]
        rows[:, l * 384 + 128: l * 384 + 384] = np.repeat(np.asarray(inputs["ssm_d"][l], f), 64)[None, :]
    consts["rows"] = rows
    return dict(win=np.ascontiguousarray(win), wout=np.ascontiguousarray(wout),
                wup=np.ascontiguousarray(wup), wdn=np.ascontiguousarray(wdn), pp=pp, **consts)


CM_OFF = {}
STAGE = 0
SKIP_FFN = False


def host_consts():
    f = np.float32
    p = np.arange(128)
    cols = []

    def add(name, a):
        CM_OFF[name] = sum(c.shape[1] for c in cols)
        cols.append(np.asarray(a, f))

    add("ident", np.eye(128))
    add("ones", np.ones((128, 128)))
    caus = (p[:, None] <= p[None, :]).astype(f)
    add("causal", np.tile(caus, (1, 4)))
    add("bdm4", (p[:, None] // 32 == np.arange(256)[None, :] // 64))
    add("bdm2", (p[:, None] // 64 == np.arange(256)[None, :] // 128))
    add("hm4", (p[:, None] // 32 == np.arange(4)[None, :]))
    add("hm2", (p[:, None] // 64 == np.arange(2)[None, :]))
    def perm(hd):
        half = hd // 2
        m = np.arange(128)
        partner = (m // hd) * hd + ((m % hd) + half) % hd
        P = np.zeros((128, 128), f)
        P[partner, m] = 1.0
        return P
    add("perm32", perm(32))
    add("perm64", perm(64))
    add("bo64", (p[:, None] // 64 == p[None, :] // 64))
    add("sgt", (p[:, None] > p[None, :]))
    gmk = np.zeros((128, 8, 4, 8))
    for b_ in range(8):
        gmk[:, b_, :, b_:] = -1e30
    add("gmask", gmk.reshape(128, 256))
    cm = np.concatenate(cols, axis=1)
    lg = np.log(1.0 - 2.0 ** (-5.0 - np.arange(4, dtype=np.float64)))
    h = p // 32
    d = p % 32
    inv = 1.0 / (10000.0 ** np.linspace(0.0, 1.0, 16))
    t = np.arange(S, dtype=np.float64)
    ang = t[None, :] * inv[d % 16][:, None]
    rcos = np.cos(ang)
    rsin = np.sin(ang) * np.where(d < 16, -1.0, 1.0)[:, None]
    idx = np.arange(512) % 128
    sc = 32.0 ** -0.5
    rdq = np.exp((idx[None, :] + 1.0) * lg[h][:, None])
    rdki = np.exp(-(idx[None, :] + 1.0) * lg[h][:, None]) * sc
    rdke = np.exp((127.0 - idx[None, :]) * lg[h][:, None]) * sc
    rcd = np.repeat(np.exp(128.0 * lg[h])[:, None], 16, axis=1)
    rett = np.concatenate([rdq, rdki, rdke, rcd], axis=1).astype(f)
    dm = p % 64
    invm = 10000.0 ** (-np.arange(0, 64, 2, dtype=np.float64) / 64)
    angm = t[None, :] * invm[dm % 32][:, None]
    mcos = np.cos(angm)
    msin = np.sin(angm) * np.where(dm < 32, -1.0, 1.0)[:, None]
    blk = (np.arange(S)[None, :] // 256 == np.arange(8)[:, None]).astype(f)
    return {"blk1h": blk, "cm": cm, "ret_cs": np.concatenate([rcos, rsin], axis=1).astype(f), "ret_t": rett,
            "moba_cs": np.concatenate([mcos, msin], axis=1).astype(f)}


def build(n_seq, L, mixers=(0, 1, 2, 3), debug=False):
    nc = bass.Bass("TRN2", target_bir_lowering=False)
    off, npp = pp_layout(L)
    x_d = nc.dram_tensor("x", [n_seq * S, D], F32, kind="ExternalInput").ap()
    y_d = nc.dram_tensor("y", [n_seq * S, D], F32, kind="ExternalOutput").ap()
    win_d = nc.dram_tensor("win", [L * 4 * 128, KC * GW], F32, kind="ExternalInput").ap()
    wout_d = nc.dram_tensor("wout", [L * 4 * 128, 2 * D], F32, kind="ExternalInput").ap()
    wup_d = nc.dram_tensor("wup", [L * NJ * 128, KC * 256], F32, kind="ExternalInput").ap()
    wdn_d = nc.dram_tensor("wdn", [L * FF, D], F32, kind="ExternalInput").ap()
    pp_d = nc.dram_tensor("pp", [128, npp], F32, kind="ExternalInput").ap()
    if not CM_OFF:
        host_consts()
    NCM = CM_OFF["gmask"] + 256
    cm_d = nc.dram_tensor("cm", [128, NCM], F32, kind="ExternalInput").ap()
    retcs_d = nc.dram_tensor("ret_cs", [128, 2 * S], F32, kind="ExternalInput").ap()
    rett_d = nc.dram_tensor("ret_t", [128, 1552], F32, kind="ExternalInput").ap()
    mobacs_d = nc.dram_tensor("moba_cs", [128, 2 * S], F32, kind="ExternalInput").ap()
    gw2_d = nc.dram_tensor("gw2", [L * 16, 128], F32, kind="ExternalInput").ap()
    rows_d = nc.dram_tensor("rows", [128, L * 384], F32, kind="ExternalInput").ap()
    blk_d = nc.dram_tensor("blk1h", [8, S], F32, kind="ExternalInput").ap()
    dbg_d = nc.dram_tensor("dbg", [4, 128, 2 * S], BF16, kind="ExternalOutput").ap() if debug else None
    win_b = nc.dram_tensor("win_b", [L * 4 * 128, KC * GW], BF16, kind="Internal").ap()
    wout_b = nc.dram_tensor("wout_b", [L * 4 * 128, 2 * D], BF16, kind="Internal").ap()
    wup_b = nc.dram_tensor("wup_b", [L * NJ * 128, KC * 256], BF16, kind="Internal").ap()
    wdn_b = nc.dram_tensor("wdn_b", [L * FF, D], BF16, kind="Internal").ap()

    with contextlib.ExitStack() as es:
        kb = KB(nc, es)
        sb = lambda name, shape, dt: es.enter_context(nc.sbuf_tensor(name, shape, dt))

        cc = kb.chan()
        for src, dst in ((win_d, win_b), (wout_d, wout_b), (wup_d, wup_b), (wdn_d, wdn_b)):
            rows = src.shape[0]
            for r in range(0, rows, 128):
                kb.dma(cc, dst[r:r + 128, :], src[r:r + 128, :], q="pool")

        xT = sb("xT", [128, KC, S], F32)
        hT = sb("hT", [128, KC, S], BF16)
        xT_d = [[Dep() for _ in range(4)] for _ in range(KC)]
        hT_d = [[Dep() for _ in range(4)] for _ in range(KC)]
        ppt = sb("ppt", [128, npp], F32)
        cmt = sb("cmt", [128, NCM], F32)
        cmb = sb("cmb", [128, 5, 128], BF16)
        identf = cmt[:, 0:128]
        onesb = cmb[:, 1, :]
        identb = cmb[:, 0, :]

        def cmc(name, n):
            return cmt[:, CM_OFF[name]:CM_OFF[name] + n]
        const_d = Dep()
        c0 = kb.chan()
        kb.dma(c0, ppt[:], pp_d[:, :], writes=[const_d])
        kb.dma(c0, cmt[:], cm_d[:, :], writes=[const_d])
        for ii, nm in enumerate(("ident", "ones", "perm32", "perm64", "bo64")):
            kb.op("dve", lambda: nc.vector.tensor_copy(out=cmb[:, ii, :], in_=cmc(nm, 128)), reads=[const_d], writes=[const_d])
        cv = sb("cv", [128, 8], F32)
        kb.op("pool", lambda: nc.gpsimd.memset(cv[:, 0:1], EPS), writes=[const_d])
        ps = [es.enter_context(nc.psum_tensor("ps%d" % i, [128, 512], F32)) for i in range(7)]
        ps_d = [Dep() for _ in range(7)]
        psb = es.enter_context(nc.psum_tensor("psb", [128, 1024], BF16))
        psb_d = [Dep()] * 8
        kb.barrier()

        def ppc(name, idx):
            o = off[name] + idx
            return ppt[:, o:o + 1]

        def load_x(s):
            mk_ = kb.mark()
            with contextlib.ExitStack() as sc:
                xin = [sc.enter_context(nc.sbuf_tensor(kb.un("xin"), [128, D], F32)) for i in range(2)]
                xin_d = [Dep(), Dep()]
                xc = [kb.chan(), kb.chan()]
                for i in range(16):
                    b = i % 2
                    kb.dma(xc[b], xin[b][:], x_d[s * S + i * 128: s * S + (i + 1) * 128, :], writes=[xin_d[b]])
                    for hh in range(2):
                        pi = (i * 2 + hh) % 4
                        for k in range(4):
                            kc = hh * 4 + k
                            kb.op("pe", lambda: nc.tensor.transpose(out=ps[pi][:, k * 128:(k + 1) * 128],
                                                                    in_=xin[b][:, kc * 128:(kc + 1) * 128],
                                                                    identity=identf),
                                  reads=[xin_d[b], const_d], writes=[ps_d[pi]], inc=(k == 3))
                        eng = "act" if hh == 0 else "dve"
                        dst = xT[:, hh * 4:(hh + 1) * 4, i * 128:(i + 1) * 128]
                        src = ps[pi][:].rearrange("p (k c) -> p k c", k=4)
                        wr = [xT_d[hh * 4 + k][i // 4] for k in range(4)]
                        if eng == "act":
                            kb.op("act", lambda: nc.scalar.copy(out=dst, in_=src), reads=[ps_d[pi]], writes=wr)
                        else:
                            kb.op("dve", lambda: nc.vector.tensor_copy(out=dst, in_=src), reads=[ps_d[pi]], writes=wr)
                kb.barrier()
            kb.release(mk_)

        def store_x(s):
            mk_ = kb.mark()
            with contextlib.ExitStack() as sc:
                xo = [sc.enter_context(nc.sbuf_tensor(kb.un("xo"), [128, D], F32)) for i in range(2)]
                xo_d = [Dep(), Dep()]
                xc = [kb.chan(), kb.chan()]
                for i in range(16):
                    b = i % 2
                    for hh in range(2):
                        pi = (i * 2 + hh) % 4
                        for k in range(4):
                            kc = hh * 4 + k
                            kb.op("pe", lambda: nc.tensor.transpose(out=ps[pi][:, k * 128:(k + 1) * 128],
                                                                    in_=xT[:, kc, i * 128:(i + 1) * 128],
                                                                    identity=identf),
                                  reads=[xT_d[kc][i // 4], const_d], writes=[ps_d[pi]], inc=(k == 3))
                        dst = xo[b][:, hh * 512:(hh + 1) * 512]
                        if hh == 0:
                            kb.op("act", lambda: nc.scalar.copy(out=dst, in_=ps[pi][:]), reads=[ps_d[pi]], writes=[xo_d[b]])
                        else:
                            kb.op("dve", lambda: nc.vector.tensor_copy(out=dst, in_=ps[pi][:]), reads=[ps_d[pi]], writes=[xo_d[b]])
                    kb.dma(xc[b], y_d[s * S + i * 128: s * S + (i + 1) * 128, :], xo[b][:], reads=[xo_d[b]])
                kb.barrier()

            kb.release(mk_)

        def norm(gname, l):
            with contextlib.ExitStack() as sc:
                sq = [sc.enter_context(nc.sbuf_tensor(kb.un("sq"), [128, KC, 512], BF16)) for i in range(2)]
                rs = [sc.enter_context(nc.sbuf_tensor(kb.un("rs"), [128, 512], F32)) for i in range(2)]
                sq_d = [Dep(), Dep()]
                rs_d = [Dep(), Dep()]
                for tt in range(4):
                    b = tt % 2
                    sl = slice(tt * 512, (tt + 1) * 512)
                    kb.op("act", lambda: nc.scalar.activation(out=sq[b][:], in_=xT[:, :, sl], func=AF.Square),
                          reads=[xT_d[k][tt] for k in range(KC)], writes=[sq_d[b]])
                    pi = 4 + b
                    for kc in range(KC):
                        kb.op("pe", lambda: nc.tensor.matmul(ps[pi][:], lhsT=onesb, rhs=sq[b][:, kc, :],
                                                             start=(kc == 0), stop=(kc == KC - 1)),
                              reads=[sq_d[b], const_d], writes=[ps_d[pi]], inc=(kc == KC - 1))
                    kb.op("act", lambda: nc.scalar.activation(out=rs[b][:], in_=ps[pi][:], func=AF.Ln, bias=cv[:, 0:1], scale=1.0 / D),
                          reads=[ps_d[pi], const_d], writes=[rs_d[b]])
                    kb.op("act", lambda: nc.scalar.activation(out=rs[b][:], in_=rs[b][:], func=AF.Exp, scale=-0.5),
                          reads=[rs_d[b]], writes=[rs_d[b]])
                    for kc in range(KC):
                        kb.op("dve", lambda: nc.vector.scalar_tensor_tensor(out=hT[:, kc, sl], in0=xT[:, kc, sl],
                                                                            scalar=ppc(gname, l * KC + kc), in1=rs[b][:],
                                                                            op0=ALU.mult, op1=ALU.mult),
                              reads=[xT_d[kc][tt], rs_d[b], const_d], writes=[hT_d[kc][tt]])
                kb.barrier()

        def ffn(l):
            mk_ = kb.mark()
            G = 8
            groups = [list(range(a, min(a + G, NJ))) for a in range(0, NJ, G)]
            with contextlib.ExitStack() as sc:
                st = lambda name, shape, dt: sc.enter_context(nc.sbuf_tensor(kb.un(name), shape, dt))
                upre = [[st("upre%d_%d" % (b, h), [128, S + 2], BF16) for h in range(2)] for b in range(2)]
                upre_d = [[[Dep() for _ in range(5)] for h in range(2)] for b in range(2)]
                acc = [[st("acc%d_%d" % (r, h), [128, 512], F32) for h in range(2)] for r in range(3)]
                acc_d = [[Dep() for h in range(2)] for r in range(3)]
                actT = st("actT", [128, G, S], BF16)
                act_d = [[Dep() for _ in range(4)] for _ in range(G)]
                wup = [st("wup%d" % i, [128, KC, 256], BF16) for i in range(3)]
                wup_dd = [Dep() for _ in range(3)]
                wup_c = [kb.chan() for _ in range(3)]
                wdn = [st("wdn%d" % i, [128, D], BF16) for i in range(G)]
                wdn_dd = [Dep() for _ in range(G)]
                wdn_c = [kb.chan() for _ in range(G)]
                for b in range(2):
                    for h in range(2):
                        kb.op("pool", lambda: nc.gpsimd.memset(upre[b][h][:, 0:2], 0.0), writes=[upre_d[b][h][0]])
                accr = 0
                jcount = 0
                for grp in groups:
                    for jj, j in enumerate(grp):
                        kb.dma(wdn_c[jj], wdn[jj][:], wdn_b[l * FF + j * 128: l * FF + (j + 1) * 128, :], writes=[wdn_dd[jj]])
                    for jj, j in enumerate(grp):
                        ws = jcount % 3
                        ub = jcount % 2
                        jcount += 1
                        r0 = (l * NJ + j) * 128
                        kb.dma(wup_c[ws], wup[ws][:].rearrange("p k c -> p (k c)"), wup_b[r0:r0 + 128, :], writes=[wup_dd[ws]])
                        for tt in range(4):
                            sl = slice(tt * 512, (tt + 1) * 512)
                            ar = accr % 3
                            accr += 1
                            for h in range(2):
                                pi = (tt % 2) * 2 + h
                                for kc in range(KC):
                                    kb.op("pe", lambda: nc.tensor.matmul(ps[pi][:], lhsT=wup[ws][:, kc, h * 128:(h + 1) * 128],
                                                                         rhs=hT[:, kc, sl], start=(kc == 0), stop=(kc == KC - 1)),
                                          reads=[wup_dd[ws], hT_d[kc][tt]], writes=[ps_d[pi]], inc=(kc == KC - 1))
                                kb.op("act", lambda: nc.scalar.copy(out=upre[ub][h][:, 2 + tt * 512: 2 + (tt + 1) * 512], in_=ps[pi][:]),
                                      reads=[ps_d[pi]], writes=[upre_d[ub][h][tt + 1]])
                                w2 = ppc("fcw", (l * 3 + 2) * 2 * NJ + h * NJ + j)
                                w1 = ppc("fcw", (l * 3 + 1) * 2 * NJ + h * NJ + j)
                                w0 = ppc("fcw", (l * 3 + 0) * 2 * NJ + h * NJ + j)
                                bb = ppc("fcb", l * 2 * NJ + h * NJ + j)
                                kb.op("act", lambda: nc.scalar.activation(out=acc[ar][h][:], in_=ps[pi][:], func=AF.Identity,
                                                                          bias=bb, scale=w2),
                                      reads=[ps_d[pi], const_d], writes=[acc_d[ar][h]])
                                kb.op("dve", lambda: nc.vector.scalar_tensor_tensor(
                                    out=acc[ar][h][:], in0=upre[ub][h][:, 1 + tt * 512: 1 + (tt + 1) * 512], scalar=w1,
                                    in1=acc[ar][h][:], op0=ALU.mult, op1=ALU.add),
                                    reads=[upre_d[ub][h][tt + 1], upre_d[ub][h][tt], acc_d[ar][h], const_d], writes=[acc_d[ar][h]])
                                kb.op("dve", lambda: nc.vector.scalar_tensor_tensor(
                                    out=acc[ar][h][:], in0=upre[ub][h][:, tt * 512: (tt + 1) * 512], scalar=w0,
                                    in1=acc[ar][h][:], op0=ALU.mult, op1=ALU.add),
                                    reads=[upre_d[ub][h][tt + 1], upre_d[ub][h][tt], acc_d[ar][h], const_d], writes=[acc_d[ar][h]])
                            kb.op("act", lambda: nc.scalar.activation(out=acc[ar][0][:], in_=acc[ar][0][:], func=AF.Silu),
                                  reads=[acc_d[ar][0]], writes=[acc_d[ar][0]])
                            kb.op("pool", lambda: nc.gpsimd.tensor_tensor(out=actT[:, jj, sl], in0=acc[ar][0][:], in1=acc[ar][1][:],
                                                                          op=ALU.mult),
                                  reads=[acc_d[ar][0], acc_d[ar][1]], writes=[act_d[jj][tt]])
                    for dc in range(KC):
                        for tt in range(4):
                            sl = slice(tt * 512, (tt + 1) * 512)
                            pi = 4 + (dc * 4 + tt) % 3
                            for jj, j in enumerate(grp):
                                kb.op("pe", lambda: nc.tensor.matmul(ps[pi][:], lhsT=wdn[jj][:, dc * 128:(dc + 1) * 128],
                                                                     rhs=actT[:, jj, sl], start=(jj == 0), stop=(jj == len(grp) - 1)),
                                      reads=[wdn_dd[jj], act_d[jj][tt]], writes=[ps_d[pi]], inc=(jj == len(grp) - 1))
                            kb.op("dve", lambda: nc.vector.tensor_tensor(out=xT[:, dc, sl], in0=ps[pi][:], in1=xT[:, dc, sl], op=ALU.add),
                                  reads=[ps_d[pi], xT_d[dc][tt]], writes=[xT_d[dc][tt]])
                kb.barrier()
            kb.release(mk_)

        kb.op("pool", lambda: nc.gpsimd.memset(cv[:, 1:2], 1.0), writes=[const_d])
        kb.op("pool", lambda: nc.gpsimd.memset(cv[:, 2:3], float(np.log(32.0 ** -0.5))), writes=[const_d])
        kb.barrier()
        prr = Ring(list(range(7)))

        def proj_fm(pi, wg, wg_d, c0, M, tt):
            sl = slice(tt * 512, (tt + 1) * 512)
            for kc in range(KC):
                kb.op("pe", lambda: nc.tensor.matmul(ps[pi][0:M, :], lhsT=wg[:, kc, c0:c0 + M], rhs=hT[:, kc, sl],
                                                     start=(kc == 0), stop=(kc == KC - 1)),
                      reads=[wg_d, hT_d[kc][tt]], writes=[ps_d[pi]], inc=(kc == KC - 1))

        def proj_tm(pi, col0, wg, wg_d, c0, N, i):
            for kc in range(KC):
                kb.op("pe", lambda: nc.tensor.matmul(ps[pi][:, col0:col0 + N], lhsT=hT[:, kc, i * 128:(i + 1) * 128],
                                                     rhs=wg[:, kc, c0:c0 + N], start=(kc == 0), stop=(kc == KC - 1)),
                      reads=[wg_d, hT_d[kc][i // 4]], writes=[ps_d[pi]], inc=(kc == KC - 1))

        def evac(i, out, in_, reads, writes):
            if i % 2 == 0:
                kb.op("act", lambda: nc.scalar.copy(out=out, in_=in_), reads=reads, writes=writes)
            else:
                kb.op("dve", lambda: nc.vector.tensor_copy(out=out, in_=in_), reads=reads, writes=writes)

        def v_tokmajor(vt, v_d, wg, wg_d, c0):
            for i in range(16):
                pi = prr.next()
                proj_tm(pi, 0, wg, wg_d, c0, 256, i)
                evac(i, vt[:, i, :], ps[pi][:, 0:256], [ps_d[pi]], [v_d[i]])

        def gate_fm(sg, sg_d, wg, wg_d, c0):
            for cc in range(2):
                for tt in range(4):
                    pi = prr.next()
                    proj_fm(pi, wg, wg_d, c0 + cc * 128, 128, tt)
                    kb.op("act", lambda: nc.scalar.activation(out=sg[:, cc, tt * 512:(tt + 1) * 512], in_=ps[pi][:], func=AF.Silu),
                          reads=[ps_d[pi]], writes=[sg_d[cc][tt]])

        def post_norm(c, src, src_d, ng, gname_idx, sg, sg_d, mixT, mix_d, tl):
            w = 256 // ng
            b = c % 2
            sq, sq_d, ss, ss_d, on, on_d = tl["sq"][b], tl["sq_d"][b], tl["ss"][b], tl["ss_d"][b], tl["on"][b], tl["on_d"][b]
            kb.op("act", lambda: nc.scalar.activation(out=sq[:], in_=src, func=AF.Square), reads=[src_d], writes=[sq_d])
            kb.op("dve", lambda: nc.vector.tensor_reduce(out=ss[:, 0:ng], in_=sq[:].rearrange("p (g e) -> p g e", g=ng),
                                                         axis=AX.X, op=ALU.add), reads=[sq_d], writes=[ss_d])
            kb.op("act", lambda: nc.scalar.activation(out=ss[:, 0:ng], in_=ss[:, 0:ng], func=AF.Ln, bias=cv[:, 0:1], scale=1.0 / w),
                  reads=[ss_d, const_d], writes=[ss_d])
            kb.op("act", lambda: nc.scalar.activation(out=ss[:, 0:ng], in_=ss[:, 0:ng], func=AF.Exp, scale=-0.5),
                  reads=[ss_d], writes=[ss_d])
            for g in range(ng):
                kb.op("act", lambda: nc.scalar.activation(out=on[:, g * w:(g + 1) * w], in_=src[:, g * w:(g + 1) * w], func=AF.Copy,
                                                          scale=ss[:, g:g + 1]), reads=[src_d, ss_d], writes=[on_d])
            if STAGE == 3:
                if c == 15:
                    mix_zero(0, None, None, mixT, mix_d, None)
                return
            pt = 5 + b
            for cc in range(2):
                tin = sq if STAGE == 5 else on
                tin_d = sq_d if STAGE == 5 else on_d
                if STAGE == 7:
                    continue
                kb.op("pe", lambda: nc.tensor.transpose(out=ps[pt][:, cc * 128:(cc + 1) * 128], in_=tin[:, cc * 128:(cc + 1) * 128],
                                                        identity=identf), reads=[tin_d, const_d], writes=[ps_d[pt]], inc=(cc == 1))
            if STAGE == 6:
                if c == 15:
                    mix_zero(0, None, None, mixT, mix_d, None)
                return
            for cc in range(2):
                dst = mixT[:, cc, c * 128:(c + 1) * 128]
                srcT = ps[pt][:, cc * 128:(cc + 1) * 128]
                if sg is not None:
                    kb.op("dve", lambda: nc.vector.scalar_tensor_tensor(out=dst, in0=srcT,
                                                                        scalar=ppc(*gname_idx(cc)), in1=sg[:, cc, c * 128:(c + 1) * 128],
                                                                        op0=ALU.mult, op1=ALU.mult),
                          reads=[ps_d[pt], sg_d[cc][c // 4], const_d], writes=[mix_d[cc][c // 4]])
                else:
                    kb.op("dve", lambda: nc.vector.tensor_scalar(out=dst, in0=srcT,
                                                                 scalar1=ppc(*gname_idx(cc)), scalar2=None, op0=ALU.mult),
                          reads=[ps_d[pt], const_d], writes=[mix_d[cc][c // 4]])

        def post_tiles(st):
            return dict(sq=[st("sq", [128, 256], F32) for _ in range(2)], sq_d=[Dep(), Dep()],
                        ss=[st("ss", [128, 4], F32) for _ in range(2)], ss_d=[Dep(), Dep()],
                        on=[st("on", [128, 256], F32) for _ in range(2)], on_d=[Dep(), Dep()])

        def linattn(st, Kmask, Km_d, QdT, Qd_d, kendT, ke_d, vt, v_d, cdec, cdec_d, gname_idx, sg, sg_d, mixT, mix_d):
            tl = post_tiles(st)
            attm = [st("attm", [128, 512], BF16) for _ in range(2)]
            attm_d = [Dep(), Dep()]
            ketm = [st("ketm", [128, 128], BF16) for _ in range(2)]
            ketm_d = [Dep(), Dep()]
            S_run = st("S_run", [128, 256], F32)
            S_tmp = st("S_tmp", [128, 256], F32)
            Sbf = st("Sbf", [128, 256], BF16)
            S_d, St_d, Sb_d = Dep(), Dep(), Dep()
            attm.append(st("attm", [128, 512], BF16))
            attm_d.append(Dep())
            Sbfs = [Sbf] + [st("Sbf", [128, 256], BF16) for _ in range(3)]
            Sb_ds = [Dep() for _ in range(4)]

            def stage_a(c):
                ch = slice(c * 128, (c + 1) * 128)
                b = c % 2
                b3 = c % 3
                tq = c // 4
                if c < 15:
                    kb.op("pe", lambda: nc.tensor.transpose(out=psb[:, b * 128:(b + 1) * 128], in_=kendT[:, ch], identity=identb),
                          reads=[ke_d[tq], const_d], writes=[psb_d[b]])
                    kb.op("act", lambda: nc.scalar.copy(out=ketm[b][:], in_=psb[:, b * 128:(b + 1) * 128]),
                          reads=[psb_d[b]], writes=[ketm_d[b]])
                pa = b
                for g in range(4):
                    kb.op("pe", lambda: nc.tensor.matmul(ps[pa][:, g * 128:(g + 1) * 128], lhsT=Kmask[:, g, ch], rhs=QdT[:, ch],
                                                         start=True, stop=True),
                          reads=[Km_d[tq], Qd_d[tq]], writes=[ps_d[pa]], inc=(g == 3))
                kb.op("dve", lambda: nc.vector.tensor_tensor(out=attm[b3][:], in0=ps[pa][:], in1=cmc("causal", 512), op=ALU.mult),
                      reads=[ps_d[pa], const_d], writes=[attm_d[b3]])
                if c < 15:
                    kb.op("pe", lambda: nc.tensor.matmul(ps[4][:, 0:256], lhsT=ketm[b][:], rhs=vt[:, c, :], start=True, stop=True),
                          reads=[ketm_d[b], v_d[c]], writes=[ps_d[4]])
                    if c == 0:
                        kb.op("dve", lambda: nc.vector.tensor_tensor(out=S_run[:], in0=ps[4][:, 0:256], in1=cmc("bdm4", 256), op=ALU.mult),
                              reads=[ps_d[4], const_d], writes=[S_d])
                    else:
                        kb.op("dve", lambda: nc.vector.tensor_tensor(out=S_tmp[:], in0=ps[4][:, 0:256], in1=cmc("bdm4", 256), op=ALU.mult),
                              reads=[ps_d[4], const_d], writes=[St_d])
                        kb.op("dve", lambda: nc.vector.scalar_tensor_tensor(out=S_run[:], in0=S_run[:], scalar=cdec[:, c:c + 1], in1=S_tmp[:],
                                                                            op0=ALU.mult, op1=ALU.add),
                              reads=[S_d, St_d, cdec_d], writes=[S_d])
                    kb.op("act", lambda: nc.scalar.copy(out=Sbfs[c % 4][:], in_=S_run[:]), reads=[S_d], writes=[Sb_ds[c % 4]])

            def stage_b(c):
                ch = slice(c * 128, (c + 1) * 128)
                b = c % 2
                b3 = c % 3
                tq = c // 4
                po = 2 + b
                if c > 0:
                    kb.op("pe", lambda: nc.tensor.matmul(ps[po][:, 0:256], lhsT=QdT[:, ch], rhs=Sbfs[(c - 1) % 4][:], start=True, stop=False),
                          reads=[Qd_d[tq], Sb_ds[(c - 1) % 4]], writes=[ps_d[po]], inc=False)
                for h in range(4):
                    kb.op("pe", lambda: nc.tensor.matmul(ps[po][:, h * 64:(h + 1) * 64], lhsT=attm[b3][:, h * 128:(h + 1) * 128],
                                                         rhs=vt[:, c, h * 64:(h + 1) * 64], start=(c == 0 and h == 0), stop=(h == 3)),
                          reads=[attm_d[b3], v_d[c]], writes=[ps_d[po]], inc=(h == 3))

            def stage_c(c):
                po = 2 + c % 2
                post_norm(c, ps[po][:, 0:256], ps_d[po], 4, gname_idx, sg, sg_d, mixT, mix_d, tl)

            stage_a(0)
            stage_a(1)
            for c in range(16):
                stage_b(c)
                if c + 2 < 16:
                    stage_a(c + 2)
                if c >= 1:
                    stage_c(c - 1)
            stage_c(15)

        def kq_finish(st, qsrc, q_d, ksrc, k_d, dq, dki, dke, dec_d, QdT, Qd_d, Kmask, Km_d, kendT, ke_d, tt, kinv, kinv_d):
            sl = slice(tt * 512, (tt + 1) * 512)
            kb.op("dve", lambda: nc.vector.tensor_tensor(out=QdT[:, sl], in0=qsrc, in1=dq, op=ALU.mult),
                  reads=[q_d, dec_d], writes=[Qd_d[tt]])
            kb.op("dve", lambda: nc.vector.tensor_tensor(out=kinv[:], in0=ksrc, in1=dki, op=ALU.mult),
                  reads=[k_d, dec_d], writes=[kinv_d])
            kb.op("dve", lambda: nc.vector.tensor_tensor(out=kendT[:, sl], in0=ksrc, in1=dke, op=ALU.mult),
                  reads=[k_d, dec_d], writes=[ke_d[tt]])
            for h in range(4):
                if h < 2:
                    kb.op("act", lambda: nc.scalar.activation(out=Kmask[:, h, sl], in_=kinv[:], func=AF.Copy, scale=cmc("hm4", 4)[:, h:h + 1]),
                          reads=[kinv_d, const_d], writes=[Km_d[tt]])
                else:
                    kb.op("dve", lambda: nc.vector.tensor_scalar(out=Kmask[:, h, sl], in0=kinv[:], scalar1=cmc("hm4", 4)[:, h:h + 1], scalar2=None,
                                                                 op0=ALU.mult), reads=[kinv_d, const_d], writes=[Km_d[tt]])

        def la_tiles(st):
            return dict(QdT=st("QdT", [128, S], BF16), Qd_d=[Dep() for _ in range(4)],
                        Kmask=st("Kmask", [128, 4, S], BF16), Km_d=[Dep() for _ in range(4)],
                        kendT=st("kendT", [128, S], BF16), ke_d=[Dep() for _ in range(4)],
                        vt=st("vt", [128, 16, 256], BF16), v_d=[Dep() for _ in range(16)],
                        sg=st("sg", [128, 2, S], BF16), sg_d=[[Dep() for _ in range(4)] for _ in range(2)],
                        kinv=st("kinv", [128, 512], F32), kinv_d=Dep())

        def mix_gla(l, wg, wg_d, mixT, mix_d, st):
            T = la_tiles(st)
            w2f = st("w2f", [16, 128], F32)
            w2b = st("w2b", [16, 128], BF16)
            w2_d = Dep()
            c1 = kb.chan()
            kb.dma(c1, w2f[:], gw2_d[l * 16:(l + 1) * 16, :], writes=[w2_d])
            kb.op("dve", lambda: nc.vector.tensor_copy(out=w2b[:], in_=w2f[:]), reads=[w2_d], writes=[w2_d])
            nb2 = st("nb2", [128, 1], F32)
            kb.op("pool", lambda: nc.gpsimd.tensor_scalar(out=nb2[:], in0=ppc("gla_b2n", l), scalar1=-1.0, scalar2=None, op0=ALU.mult),
                  reads=[const_d], writes=[w2_d])
            ggT = st("ggT", [16, S], BF16)
            gg_d = [Dep() for _ in range(4)]
            bcs = st("bcs", [128, S], F32)
            bcs_d = [Dep() for _ in range(4)]
            spt = [st("spt", [128, 512], F32) for _ in range(2)]
            spt_d = [Dep(), Dep()]
            for tt in range(4):
                sl = slice(tt * 512, (tt + 1) * 512)
                pi = prr.next()
                proj_fm(pi, wg, wg_d, 768, 16, tt)
                kb.op("act", lambda: nc.scalar.copy(out=ggT[:, sl], in_=ps[pi][0:16, :]), reads=[ps_d[pi]], writes=[gg_d[tt]])
                pj = prr.next()
                kb.op("pe", lambda: nc.tensor.matmul(ps[pj][:], lhsT=w2b[:], rhs=ggT[:, sl], start=True, stop=True),
                      reads=[w2_d, gg_d[tt]], writes=[ps_d[pj]])
                b = tt % 2
                kb.op("act", lambda: nc.scalar.activation(out=spt[b][:], in_=ps[pj][:], func=AF.Exp, bias=nb2[:], scale=-1.0),
                      reads=[ps_d[pj], w2_d], writes=[spt_d[b]])
                kb.op("act", lambda: nc.scalar.activation(out=spt[b][:], in_=spt[b][:], func=AF.Ln, bias=cv[:, 1:2], scale=1.0),
                      reads=[spt_d[b], const_d], writes=[spt_d[b]])
                for ci in range(4):
                    cs_ = slice(ci * 128, (ci + 1) * 128)
                    gs_ = slice(tt * 512 + ci * 128, tt * 512 + (ci + 1) * 128)
                    kb.op("dve", lambda: nc.vector.tensor_tensor_scan(out=bcs[:, gs_], data0=cmc("ones", 128), data1=spt[b][:, cs_],
                                                                      initial=0.0, op0=ALU.mult, op1=ALU.add),
                          reads=[spt_d[b], const_d], writes=[bcs_d[tt]])
            nbl = st("nbl", [128, 16], F32)
            cdec = st("cdec", [128, 16], F32)
            nbl_d, cdec_d = Dep(), Dep()
            blast = bcs[:].rearrange("p (c i) -> p c i", i=128)[:, :, 127]
            kb.op("dve", lambda: nc.vector.tensor_scalar(out=nbl[:], in0=blast, scalar1=-1.0 / 16.0, scalar2=None, op0=ALU.mult),
                  reads=bcs_d, writes=[nbl_d])
            kb.op("act", lambda: nc.scalar.activation(out=cdec[:], in_=nbl[:], func=AF.Exp), reads=[nbl_d], writes=[cdec_d])
            dq = [st("dq", [128, 512], F32)] * 2
            dki = [st("dki", [128, 512], F32)] * 2
            dke = [st("dke", [128, 512], F32)] * 2
            dec_d = [Dep()] * 2
            for tt in range(4):
                sl = slice(tt * 512, (tt + 1) * 512)
                b = tt % 2
                kb.op("act", lambda: nc.scalar.activation(out=dq[b][:], in_=bcs[:, sl], func=AF.Exp, bias=cv[:, 2:3], scale=-1.0 / 16.0),
                      reads=[bcs_d[tt], const_d], writes=[dec_d[b]])
                kb.op("act", lambda: nc.scalar.activation(out=dki[b][:], in_=bcs[:, sl], func=AF.Exp, scale=1.0 / 16.0),
                      reads=[bcs_d[tt]], writes=[dec_d[b]])
                for ci in range(4):
                    c = tt * 4 + ci
                    kb.op("act", lambda: nc.scalar.activation(out=dke[b][:, ci * 128:(ci + 1) * 128], in_=bcs[:, c * 128:(c + 1) * 128],
                                                              func=AF.Exp, bias=nbl[:, c:c + 1], scale=1.0 / 16.0),
                          reads=[bcs_d[tt], nbl_d], writes=[dec_d[b]])
                pq = prr.next()
                proj_fm(pq, wg, wg_d, 0, 128, tt)
                pk = prr.next()
                proj_fm(pk, wg, wg_d, 128, 128, tt)
                kq_finish(st, ps[pq][:], ps_d[pq], ps[pk][:], ps_d[pk], dq[b][:], dki[b][:], dke[b][:], dec_d[b],
                          T["QdT"], T["Qd_d"], T["Kmask"], T["Km_d"], T["kendT"], T["ke_d"], tt, T["kinv"], T["kinv_d"])
            v_tokmajor(T["vt"], T["v_d"], wg, wg_d, 256)
            gate_fm(T["sg"], T["sg_d"], wg, wg_d, 512)
            linattn(st, T["Kmask"], T["Km_d"], T["QdT"], T["Qd_d"], T["kendT"], T["ke_d"], T["vt"], T["v_d"], cdec, cdec_d,
                    lambda cc: ("gla_ng", l), T["sg"], T["sg_d"], mixT, mix_d)

        def linattn_v1(st, Kmask, Km_d, QdT, Qd_d, kendT, ke_d, vt, v_d, cdec, cdec_d, gname_idx, sg, sg_d, mixT, mix_d):
            tl = post_tiles(st)
            attm = [st("attm", [128, 512], BF16) for _ in range(2)]
            attm_d = [Dep(), Dep()]
            ketm = [st("ketm", [128, 128], BF16) for _ in range(2)]
            ketm_d = [Dep(), Dep()]
            S_run = st("S_run", [128, 256], F32)
            S_tmp = st("S_tmp", [128, 256], F32)
            Sbf = st("Sbf", [128, 256], BF16)
            S_d, St_d, Sb_d = Dep(), Dep(), Dep()
            for c in range(16):
                ch = slice(c * 128, (c + 1) * 128)
                b = c % 2
                tq = c // 4
                if c < 15:
                    kb.op("pe", lambda: nc.tensor.transpose(out=psb[:, b * 128:(b + 1) * 128], in_=kendT[:, ch], identity=identb),
                          reads=[ke_d[tq], const_d], writes=[psb_d[b]])
                    kb.op("act", lambda: nc.scalar.copy(out=ketm[b][:], in_=psb[:, b * 128:(b + 1) * 128]),
                          reads=[psb_d[b]], writes=[ketm_d[b]])
                pa = b
                for g in range(4):
                    kb.op("pe", lambda: nc.tensor.matmul(ps[pa][:, g * 128:(g + 1) * 128], lhsT=Kmask[:, g, ch], rhs=QdT[:, ch],
                                                         start=True, stop=True),
                          reads=[Km_d[tq], Qd_d[tq]], writes=[ps_d[pa]], inc=(g == 3))
                kb.op("dve", lambda: nc.vector.tensor_tensor(out=attm[b][:], in0=ps[pa][:], in1=cmc("causal", 512), op=ALU.mult),
                      reads=[ps_d[pa], const_d], writes=[attm_d[b]])
                po = 2 + b
                if c > 0:
                    kb.op("pe", lambda: nc.tensor.matmul(ps[po][:, 0:256], lhsT=QdT[:, ch], rhs=Sbf[:], start=True, stop=False),
                          reads=[Qd_d[tq], Sb_d], writes=[ps_d[po]], inc=False)
                for h in range(4):
                    kb.op("pe", lambda: nc.tensor.matmul(ps[po][:, h * 64:(h + 1) * 64], lhsT=attm[b][:, h * 128:(h + 1) * 128],
                                                         rhs=vt[:, c, h * 64:(h + 1) * 64], start=(c == 0 and h == 0), stop=(h == 3)),
                          reads=[attm_d[b], v_d[c]], writes=[ps_d[po]], inc=(h == 3))
                if c < 15:
                    kb.op("pe", lambda: nc.tensor.matmul(ps[4][:, 0:256], lhsT=ketm[b][:], rhs=vt[:, c, :], start=True, stop=True),
                          reads=[ketm_d[b], v_d[c]], writes=[ps_d[4]])
                    if c == 0:
                        kb.op("dve", lambda: nc.vector.tensor_tensor(out=S_run[:], in0=ps[4][:, 0:256], in1=cmc("bdm4", 256), op=ALU.mult),
                              reads=[ps_d[4], const_d, Sb_d], writes=[S_d])
                    else:
                        kb.op("dve", lambda: nc.vector.tensor_tensor(out=S_tmp[:], in0=ps[4][:, 0:256], in1=cmc("bdm4", 256), op=ALU.mult),
                              reads=[ps_d[4], const_d], writes=[St_d])
                        kb.op("dve", lambda: nc.vector.scalar_tensor_tensor(out=S_run[:], in0=S_run[:], scalar=cdec[:, c:c + 1], in1=S_tmp[:],
                                                                            op0=ALU.mult, op1=ALU.add),
                              reads=[S_d, St_d, cdec_d], writes=[S_d])
                    kb.op("act", lambda: nc.scalar.copy(out=Sbf[:], in_=S_run[:]), reads=[S_d], writes=[Sb_d])
                if STAGE == 2:
                    if c == 15:
                        mix_zero(0, None, None, mixT, mix_d, st)
                    continue
                post_norm(c, ps[po][:, 0:256], ps_d[po], 4, gname_idx, sg, sg_d, mixT, mix_d, tl)

        def kq_finish_v1(st, qsrc, q_d, ksrc, k_d, dq, dki, dke, dec_d, QdT, Qd_d, Kmask, Km_d, kendT, ke_d, tt, kinv, kinv_d):
            sl = slice(tt * 512, (tt + 1) * 512)
            kb.op("dve", lambda: nc.vector.tensor_tensor(out=QdT[:, sl], in0=qsrc, in1=dq, op=ALU.mult),
                  reads=[q_d, dec_d], writes=[Qd_d[tt]])
            kb.op("dve", lambda: nc.vector.tensor_tensor(out=kinv[:], in0=ksrc, in1=dki, op=ALU.mult),
                  reads=[k_d, dec_d], writes=[kinv_d])
            kb.op("dve", lambda: nc.vector.tensor_tensor(out=kendT[:, sl], in0=ksrc, in1=dke, op=ALU.mult),
                  reads=[k_d, dec_d], writes=[ke_d[tt]])
            for h in range(4):
                eng = "pool" if h % 2 == 0 else "dve"
                e = nc.gpsimd if eng == "pool" else nc.vector
                kb.op(eng, lambda: e.tensor_scalar(out=Kmask[:, h, sl], in0=kinv[:], scalar1=cmc("hm4", 4)[:, h:h + 1], scalar2=None,
                                                   op0=ALU.mult), reads=[kinv_d, const_d], writes=[Km_d[tt]])

        def mix_ret(l, wg, wg_d, mixT, mix_d, st):
            T = la_tiles(st)
            rcs2 = [st("rcs", [128, 1024], F32)] * 2
            rcs_d = [Dep()] * 2
            rcs_c = [kb.chan()] * 2
            rt = st("rt", [128, 1552], F32)
            rt_d = Dep()
            c1 = kb.chan()
            kb.dma(c1, rt[:], rett_d[:, :], writes=[rt_d])
            qb = [st("qb", [128, 512], BF16)] * 2
            t1 = [st("t1", [128, 512], F32)] * 2
            t2 = [st("t2", [128, 512], F32)] * 2
            qr = [st("qr", [128, 512], F32) for _ in range(2)]
            qb_d, t1_d, t2_d, qr_d = [Dep()] * 2, [Dep()] * 2, [Dep()] * 2, [Dep(), Dep()]
            for tt in range(4):
                sl = slice(tt * 512, (tt + 1) * 512)
                rb = tt % 2
                rcs = rcs2[rb]
                kb.dma(rcs_c[rb], rcs[:, 0:512], retcs_d[:, sl], writes=[rcs_d[rb]])
                kb.dma(rcs_c[rb], rcs[:, 512:1024], retcs_d[:, S + tt * 512: S + (tt + 1) * 512], writes=[rcs_d[rb]])
                for w in range(2):
                    pi = prr.next()
                    proj_fm(pi, wg, wg_d, w * 128, 128, tt)
                    kb.op("act", lambda: nc.scalar.copy(out=qb[w][:], in_=ps[pi][:]), reads=[ps_d[pi]], writes=[qb_d[w]])
                    pj = prr.next()
                    kb.op("pe", lambda: nc.tensor.matmul(ps[pj][:], lhsT=cmb[:, 2, :], rhs=qb[w][:], start=True, stop=True),
                          reads=[qb_d[w], const_d], writes=[ps_d[pj]])
                    kb.op("dve", lambda: nc.vector.tensor_tensor(out=t1[w][:], in0=ps[pi][:], in1=rcs[:, 0:512], op=ALU.mult),
                          reads=[ps_d[pi], rcs_d[rb]], writes=[t1_d[w]])
                    kb.op("dve", lambda: nc.vector.tensor_tensor(out=t2[w][:], in0=ps[pj][:], in1=rcs[:, 512:1024],
                                                                 op=ALU.mult), reads=[ps_d[pj], rcs_d[rb]], writes=[t2_d[w]])
                    kb.op("pool", lambda: nc.gpsimd.tensor_tensor(out=qr[w][:], in0=t1[w][:], in1=t2[w][:], op=ALU.add),
                          reads=[t1_d[w], t2_d[w]], writes=[qr_d[w]])
                kq_finish_v1(st, qr[0][:], qr_d[0], qr[1][:], qr_d[1], rt[:, 0:512], rt[:, 512:1024], rt[:, 1024:1536], rt_d,
                          T["QdT"], T["Qd_d"], T["Kmask"], T["Km_d"], T["kendT"], T["ke_d"], tt, T["kinv"], T["kinv_d"])
            v_tokmajor(T["vt"], T["v_d"], wg, wg_d, 256)
            gate_fm(T["sg"], T["sg_d"], wg, wg_d, 512)
            if STAGE == 1:
                return mix_zero(l, wg, wg_d, mixT, mix_d, st)
            cdec_r = st("cdec_r", [128, 16], F32)
            cdec_rd = Dep()
            kb.op("dve", lambda: nc.vector.tensor_copy(out=cdec_r[:], in_=rt[:, 1536:1552]), reads=[rt_d], writes=[cdec_rd])
            linattn(st, T["Kmask"], T["Km_d"], T["QdT"], T["Qd_d"], T["kendT"], T["ke_d"], T["vt"], T["v_d"], cdec_r, cdec_rd,
                    lambda cc: ("ret_ng", l), T["sg"], T["sg_d"], mixT, mix_d)

        def mix_ssm(l, wg, wg_d, mixT, mix_d, st):
            rw = st("rw", [128, 384], F32)
            rw_d = Dep()
            c1 = kb.chan()
            kb.dma(c1, rw[:], rows_d[:, l * 384:(l + 1) * 384], writes=[rw_d])
            kb.op("act", lambda: nc.scalar.activation(out=rw[:, 64:128], in_=rw[:, 64:128], func=AF.Exp), reads=[rw_d], writes=[rw_d])
            kb.op("dve", lambda: nc.vector.tensor_scalar(out=rw[:, 64:128], in0=rw[:, 64:128], scalar1=-1.0, scalar2=None, op0=ALU.mult),
                  reads=[rw_d], writes=[rw_d])
            dtt = st("dtt", [128, 64], F32)
            at = st("at", [128, 64], F32)
            acs = st("acs", [128, 64], F32)
            alast = st("alast", [128, 64], F32)
            eacs = st("eacs", [128, 64], F32)
            cdec = st("cdec", [128, 64], F32)
            dte = st("dte", [128, 64], F32)
            sm_d = Dep()
            for i in range(16):
                pi = prr.next()
                proj_tm(pi, 0, wg, wg_d, 768, 4, i)
                kb.op("dve", lambda: nc.vector.tensor_tensor(out=dtt[:, i * 4:(i + 1) * 4], in0=ps[pi][:, 0:4], in1=rw[:, i * 4:(i + 1) * 4], op=ALU.add),
                      reads=[ps_d[pi], rw_d], writes=[sm_d])
            kb.op("act", lambda: nc.scalar.activation(out=dtt[:], in_=dtt[:], func=AF.Exp), reads=[sm_d], writes=[sm_d])
            kb.op("act", lambda: nc.scalar.activation(out=dtt[:], in_=dtt[:], func=AF.Ln, bias=cv[:, 1:2], scale=1.0), reads=[sm_d, const_d], writes=[sm_d])
            kb.op("dve", lambda: nc.vector.tensor_tensor(out=at[:], in0=dtt[:], in1=rw[:, 64:128], op=ALU.mult), reads=[sm_d, rw_d], writes=[sm_d])
            pa = prr.next()
            kb.op("pe", lambda: nc.tensor.matmul(ps[pa][:, 0:64], lhsT=cmc("causal", 128), rhs=at[:], start=True, stop=True),
                  reads=[sm_d, const_d], writes=[ps_d[pa]])
            kb.op("act", lambda: nc.scalar.copy(out=acs[:], in_=ps[pa][:, 0:64]), reads=[ps_d[pa]], writes=[sm_d])
            pb = prr.next()
            kb.op("pe", lambda: nc.tensor.matmul(ps[pb][:, 0:64], lhsT=cmc("ones", 128), rhs=at[:], start=True, stop=True),
                  reads=[sm_d, const_d], writes=[ps_d[pb]])
            kb.op("act", lambda: nc.scalar.copy(out=alast[:], in_=ps[pb][:, 0:64]), reads=[ps_d[pb]], writes=[sm_d])
            kb.op("act", lambda: nc.scalar.activation(out=eacs[:], in_=acs[:], func=AF.Exp), reads=[sm_d], writes=[sm_d])
            kb.op("act", lambda: nc.scalar.activation(out=cdec[:], in_=alast[:], func=AF.Exp), reads=[sm_d], writes=[sm_d])
            kb.op("dve", lambda: nc.vector.tensor_tensor(out=dte[:], in0=alast[:], in1=acs[:], op=ALU.subtract), reads=[sm_d], writes=[sm_d])
            kb.op("act", lambda: nc.scalar.activation(out=dte[:], in_=dte[:], func=AF.Exp), reads=[sm_d], writes=[sm_d])
            if STAGE == 11:
                return mix_zero(l, wg, wg_d, mixT, mix_d, st)
            xsT = st("xsT", [128, 2, S], BF16)
            xs_d = [[Dep() for _ in range(4)] for _ in range(2)]
            Bm = st("Bm", [128, 2, S], BF16)
            Bm_d = [Dep() for _ in range(4)]
            CT = st("CT", [128, S], BF16)
            CT_d = [Dep() for _ in range(4)]
            cs_ = contextlib.ExitStack()
            upre = cs_.enter_context(nc.sbuf_tensor(kb.un("upre"), [128, S + 3], BF16))
            up_d = [Dep() for _ in range(5)]
            kb.op("pool", lambda: nc.gpsimd.memset(upre[:, 0:3], 0.0), writes=[up_d[0]])
            accs = [cs_.enter_context(nc.sbuf_tensor(kb.un("acc"), [128, 512], F32)) for _ in range(2)]
            accs_d = [Dep(), Dep()]
            cits = [(cc, tt) for cc in range(4) for tt in range(4)]

            def conv1(i):
                cc, tt = cits[i]
                acc, acc_d = accs[i % 2], accs_d[i % 2]
                pi = prr.next()
                proj_fm(pi, wg, wg_d, 256 + cc * 128, 128, tt)
                kb.op("act", lambda: nc.scalar.copy(out=upre[:, 3 + tt * 512: 3 + (tt + 1) * 512], in_=ps[pi][:]),
                      reads=[ps_d[pi]], writes=[up_d[tt + 1]])
                kb.op("act", lambda: nc.scalar.activation(out=acc[:], in_=ps[pi][:], func=AF.Identity,
                                                          bias=ppc("ssm_cb", l * 4 + cc), scale=ppc("ssm_cw", (l * 4 + 3) * 4 + cc)),
                      reads=[ps_d[pi], const_d], writes=[acc_d])
                for k in range(1, 4):
                    kb.op("dve", lambda: nc.vector.scalar_tensor_tensor(
                        out=acc[:], in0=upre[:, 3 - k + tt * 512: 3 - k + (tt + 1) * 512], scalar=ppc("ssm_cw", (l * 4 + 3 - k) * 4 + cc),
                        in1=acc[:], op0=ALU.mult, op1=ALU.add),
                        reads=[up_d[tt + 1], up_d[tt], acc_d, const_d], writes=[acc_d])

            def conv2(i):
                cc, tt = cits[i]
                acc, acc_d = accs[i % 2], accs_d[i % 2]
                sl = slice(tt * 512, (tt + 1) * 512)
                if cc < 2:
                    kb.op("act", lambda: nc.scalar.activation(out=xsT[:, cc, sl], in_=acc[:], func=AF.Silu), reads=[acc_d], writes=[xs_d[cc][tt]])
                elif cc == 3:
                    kb.op("act", lambda: nc.scalar.activation(out=CT[:, sl], in_=acc[:], func=AF.Silu), reads=[acc_d], writes=[CT_d[tt]])
                else:
                    kb.op("act", lambda: nc.scalar.activation(out=acc[:], in_=acc[:], func=AF.Silu), reads=[acc_d], writes=[acc_d])
                    kb.op("act", lambda: nc.scalar.activation(out=Bm[:, 0, sl], in_=acc[:], func=AF.Copy, scale=cmc("hm2", 2)[:, 0:1]),
                          reads=[acc_d, const_d], writes=[Bm_d[tt]])
                    kb.op("dve", lambda: nc.vector.tensor_scalar(out=Bm[:, 1, sl], in0=acc[:], scalar1=cmc("hm2", 2)[:, 1:2],
                                                                 scalar2=None, op0=ALU.mult), reads=[acc_d, const_d], writes=[Bm_d[tt]])

            conv1(0)
            for i in range(16):
                if i + 1 < 16:
                    conv1(i + 1)
                conv2(i)
            kb.barrier()
            cs_.close()
            if STAGE == 12:
                return mix_zero(l, wg, wg_d, mixT, mix_d, st)
            vt = st("vt", [128, 16, 256], BF16)
            xsD = st("xsD", [128, 16, 256], BF16)
            Btm = st("Btm", [128, 16, 128], BF16)
            szt = st("szt", [128, 16, 256], BF16)
            xs_tm = [st("xs_tm", [128, 256], BF16) for _ in range(2)]
            xtm_d = [Dep(), Dep()]
            v_d = [Dep() for _ in range(16)]
            xd_d = [Dep() for _ in range(16)]
            bt_d = [Dep() for _ in range(16)]
            sz_d = [Dep() for _ in range(16)]
            for c in range(16):
                ch = slice(c * 128, (c + 1) * 128)
                s0 = 0
                srcs = [(xsT[:, 0, ch], xs_d[0][c // 4]), (xsT[:, 1, ch], xs_d[1][c // 4]), (Bm[:, 0, ch], Bm_d[c // 4]), (Bm[:, 1, ch], Bm_d[c // 4])]
                for k, (ap_, d_) in enumerate(srcs):
                    kb.op("pe", lambda: nc.tensor.transpose(out=psb[:, (s0 + k) * 128:(s0 + k + 1) * 128], in_=ap_, identity=identb),
                          reads=[d_, const_d], writes=[psb_d[s0 + k]], inc=(k == 3))
                xb = xs_tm[c % 2]
                kb.op("act", lambda: nc.scalar.copy(out=xb[:], in_=psb[:, 0:256]), reads=[psb_d[0]], writes=[xtm_d[c % 2]])
                kb.op("pool", lambda: nc.gpsimd.tensor_tensor(out=xsD[:, c, :], in0=xb[:], in1=rw[:, 128:384], op=ALU.mult),
                      reads=[xtm_d[c % 2], rw_d], writes=[xd_d[c]])
                for cc in range(2):
                    pt_ = psb[:, (s0 + cc) * 128:(s0 + cc + 1) * 128]
                    for hh in range(2):
                        h = cc * 2 + hh
                        kb.op("act", lambda: nc.scalar.activation(out=vt[:, c, h * 64:(h + 1) * 64], in_=pt_[:, hh * 64:(hh + 1) * 64], func=AF.Copy,
                                                                  scale=dtt[:, c * 4 + h: c * 4 + h + 1]),
                              reads=[psb_d[s0 + cc], sm_d], writes=[v_d[c]])
                for g in range(2):
                    kb.op("act", lambda: nc.scalar.copy(out=Btm[:, c, g * 64:(g + 1) * 64],
                                                        in_=psb[:, (s0 + 2 + g) * 128 + g * 64:(s0 + 2 + g) * 128 + (g + 1) * 64]),
                          reads=[psb_d[s0 + 2 + g]], writes=[bt_d[c]])
                pi = prr.next()
                proj_tm(pi, 0, wg, wg_d, 0, 256, c)
                kb.op("act", lambda: nc.scalar.activation(out=szt[:, c, :], in_=ps[pi][:, 0:256], func=AF.Silu), reads=[ps_d[pi]], writes=[sz_d[c]])
            if STAGE == 13:
                return mix_zero(l, wg, wg_d, mixT, mix_d, st)
            tl = post_tiles(st)
            scm = st("scm", [128, 256], F32)
            Mh = [st("Mh", [128, 128], F32) for _ in range(4)]
            dec = st("dec", [128, 512], F32)
            attm = [st("attm", [128, 512], BF16) for _ in range(3)]
            yt = [st("yt", [128, 256], F32) for _ in range(2)]
            vend = st("vend", [128, 256], BF16)
            S_run = st("S_run", [128, 256], F32)
            S_tmp = st("S_tmp", [128, 256], F32)
            Sbfs = [st("Sbf", [128, 256], BF16) for _ in range(4)]
            scm_d, Mh_d, dec_d, attm_d, yt_d, vend_d = Dep(), [Dep() for _ in range(4)], Dep(), [Dep() for _ in range(3)], [Dep(), Dep()], Dep()
            S_d, St_d = Dep(), Dep()
            Sb_ds = [Dep() for _ in range(4)]

            def sa(c):
                ch = slice(c * 128, (c + 1) * 128)
                b = c % 2
                b3 = c % 3
                tq = c // 4
                pa = b
                for g in range(2):
                    kb.op("pe", lambda: nc.tensor.matmul(ps[pa][:, g * 128:(g + 1) * 128], lhsT=Bm[:, g, ch], rhs=CT[:, ch], start=True, stop=True),
                          reads=[Bm_d[tq], CT_d[tq]], writes=[ps_d[pa]], inc=(g == 1))
                kb.op("dve", lambda: nc.vector.tensor_tensor(out=scm[:], in0=ps[pa][:, 0:256], in1=cmc("causal", 256), op=ALU.mult),
                      reads=[ps_d[pa], const_d], writes=[scm_d])
                pg = 5 + b
                for h in range(4):
                    kb.op("act", lambda: nc.scalar.activation(out=Mh[h][:], in_=cmc("sgt", 128), func=AF.Copy, scale=at[:, c * 4 + h: c * 4 + h + 1]),
                          reads=[const_d, sm_d], writes=[Mh_d[h]])
                    kb.op("pe", lambda: nc.tensor.matmul(ps[pg][:, h * 128:(h + 1) * 128], lhsT=Mh[h][:], rhs=cmc("causal", 128), start=True, stop=True),
                          reads=[Mh_d[h], const_d], writes=[ps_d[pg]], inc=(h == 3))
                kb.op("act", lambda: nc.scalar.activation(out=dec[:], in_=ps[pg][:], func=AF.Exp), reads=[ps_d[pg]], writes=[dec_d])
                for h in range(4):
                    g = h // 2
                    kb.op("dve", lambda: nc.vector.tensor_tensor(out=attm[b3][:, h * 128:(h + 1) * 128], in0=scm[:, g * 128:(g + 1) * 128],
                                                                 in1=dec[:, h * 128:(h + 1) * 128], op=ALU.mult),
                          reads=[scm_d, dec_d], writes=[attm_d[b3]])
                if c < 15:
                    for h in range(4):
                        if h % 2 == 0:
                            kb.op("act", lambda: nc.scalar.activation(out=vend[:, h * 64:(h + 1) * 64], in_=vt[:, c, h * 64:(h + 1) * 64], func=AF.Copy,
                                                                      scale=dte[:, c * 4 + h: c * 4 + h + 1]), reads=[v_d[c], sm_d], writes=[vend_d])
                        else:
                            kb.op("dve", lambda: nc.vector.tensor_scalar(out=vend[:, h * 64:(h + 1) * 64], in0=vt[:, c, h * 64:(h + 1) * 64],
                                                                         scalar1=dte[:, c * 4 + h: c * 4 + h + 1], scalar2=None, op0=ALU.mult),
                                  reads=[v_d[c], sm_d], writes=[vend_d])
                    kb.op("pe", lambda: nc.tensor.matmul(ps[4][:, 0:256], lhsT=Btm[:, c, :], rhs=vend[:], start=True, stop=True),
                          reads=[bt_d[c], vend_d], writes=[ps_d[4]])
                    if c == 0:
                        kb.op("dve", lambda: nc.vector.tensor_tensor(out=S_run[:], in0=ps[4][:, 0:256], in1=cmc("bdm2", 256), op=ALU.mult),
                              reads=[ps_d[4], const_d], writes=[S_d])
                    else:
                        kb.op("dve", lambda: nc.vector.tensor_tensor(out=S_tmp[:], in0=ps[4][:, 0:256], in1=cmc("bdm2", 256), op=ALU.mult),
                              reads=[ps_d[4], const_d], writes=[St_d])
                        for h in range(4):
                            kb.op("dve", lambda: nc.vector.scalar_tensor_tensor(out=S_run[:, h * 64:(h + 1) * 64], in0=S_run[:, h * 64:(h + 1) * 64],
                                                                                scalar=cdec[:, c * 4 + h: c * 4 + h + 1], in1=S_tmp[:, h * 64:(h + 1) * 64],
                                                                                op0=ALU.mult, op1=ALU.add),
                                  reads=[S_d, St_d, sm_d], writes=[S_d])
                    kb.op("act", lambda: nc.scalar.copy(out=Sbfs[c % 4][:], in_=S_run[:]), reads=[S_d], writes=[Sb_ds[c % 4]])

            def sb_(c):
                ch = slice(c * 128, (c + 1) * 128)
                b = c % 2
                b3 = c % 3
                tq = c // 4
                po = 2 + b
                if c > 0:
                    kb.op("pe", lambda: nc.tensor.matmul(ps[po][:, 256:512], lhsT=CT[:, ch], rhs=Sbfs[(c - 1) % 4][:], start=True, stop=True),
                          reads=[CT_d[tq], Sb_ds[(c - 1) % 4]], writes=[ps_d[po]], inc=False)
                for h in range(4):
                    kb.op("pe", lambda: nc.tensor.matmul(ps[po][:, h * 64:(h + 1) * 64], lhsT=attm[b3][:, h * 128:(h + 1) * 128],
                                                         rhs=vt[:, c, h * 64:(h + 1) * 64], start=(h == 0), stop=(h == 3)),
                          reads=[attm_d[b3], v_d[c]], writes=[ps_d[po]], inc=(h == 3))
                kb.op("dve", lambda: nc.vector.tensor_tensor(out=yt[b][:], in0=ps[po][:, 0:256], in1=xsD[:, c, :], op=ALU.add),
                      reads=[ps_d[po], xd_d[c]], writes=[yt_d[b]])
                if c > 0:
                    for h in range(4):
                        kb.op("dve", lambda: nc.vector.scalar_tensor_tensor(out=yt[b][:, h * 64:(h + 1) * 64], in0=ps[po][:, 256 + h * 64: 256 + (h + 1) * 64],
                                                                            scalar=eacs[:, c * 4 + h: c * 4 + h + 1], in1=yt[b][:, h * 64:(h + 1) * 64],
                                                                            op0=ALU.mult, op1=ALU.add),
                              reads=[ps_d[po], sm_d, yt_d[b]], writes=[yt_d[b]])
                kb.op("pool", lambda: nc.gpsimd.tensor_tensor(out=yt[b][:], in0=yt[b][:], in1=szt[:, c, :], op=ALU.mult),
                      reads=[yt_d[b], sz_d[c]], writes=[yt_d[b]])

            def sc_(c):
                b = c % 2
                post_norm(c, yt[b][:], yt_d[b], 2, lambda cc: ("ssm_ng", 2 * l + cc), None, None, mixT, mix_d, tl)

            sa(0)
            sa(1)
            for c in range(16):
                sb_(c)
                if c + 2 < 16:
                    sa(c + 2)
                if c >= 1:
                    sc_(c - 1)
            sc_(15)

        def mix_moba(l, wg, wg_d, mixT, mix_d, st):
            qa = [st("qa", [72, S], BF16) for _ in range(4)]
            ka = [st("ka", [72, S], BF16) for _ in range(4)]
            qa_d = [[Dep() for _ in range(4)] for _ in range(4)]
            ka_d = [[Dep() for _ in range(4)] for _ in range(4)]
            qb_d = [Dep() for _ in range(4)]
            kb_d = [Dep()] * 4
            va = st("va", [128, 16, 4, 128], BF16)
            va_d = [Dep() for _ in range(16)]
            c1 = kb.chan()
            for h in range(4):
                kb.dma(c1, ka[h][64:72, :], blk_d[:, :], writes=[kb_d[h]], q="pool")
                kb.op("pool", lambda: nc.gpsimd.memset(qa[h][64:72, :], 0.0), writes=[qb_d[h]])
            for i in range(16):
                kb.op("pool", lambda: nc.gpsimd.memset(va[:, i, :, 64:128], 1.0), writes=[va_d[i]])
            mcs = [st("mcs", [128, 1024], F32) for _ in range(2)]
            mcs_d = [Dep(), Dep()]
            mcs_c = [kb.chan(), kb.chan()]
            sqb = [st("sqb", [128, 512], BF16) for _ in range(2)]
            rs = [st("rs", [128, 512], F32) for _ in range(2)]
            qn = [st("qn", [128, 512], F32) for _ in range(2)]
            qnb = [st("qnb", [128, 512], BF16) for _ in range(2)]
            sqb_d, rs_d, qn_d, qnb_d = [Dep(), Dep()], [Dep(), Dep()], [Dep(), Dep()], [Dep(), Dep()]
            kms = st("kms", [128, 2, 8], F32)
            kms_d = Dep()
            km = [st("km", [64, 8], BF16) for _ in range(4)]
            km_d = [Dep() for _ in range(4)]
            its = [(tt, w, pr) for tt in range(4) for w in range(2) for pr in range(2)]

            def prep1(i):
                tt, w, pr = its[i]
                s_ = i % 2
                sl = slice(tt * 512, (tt + 1) * 512)
                rb = tt % 2
                if w == 0 and pr == 0:
                    kb.dma(mcs_c[rb], mcs[rb][:, 0:512], mobacs_d[:, sl], writes=[mcs_d[rb]])
                    kb.dma(mcs_c[rb], mcs[rb][:, 512:1024], mobacs_d[:, S + tt * 512: S + (tt + 1) * 512], writes=[mcs_d[rb]])
                pi = prr.next()
                proj_fm(pi, wg, wg_d, w * 256 + pr * 128, 128, tt)
                kb.op("act", lambda: nc.scalar.activation(out=sqb[s_][:], in_=ps[pi][:], func=AF.Square), reads=[ps_d[pi]], writes=[sqb_d[s_]])
                pj = prr.next()
                kb.op("pe", lambda: nc.tensor.matmul(ps[pj][:], lhsT=cmb[:, 4, :], rhs=sqb[s_][:], start=True, stop=True),
                      reads=[sqb_d[s_], const_d], writes=[ps_d[pj]])
                kb.op("act", lambda: nc.scalar.activation(out=rs[s_][:], in_=ps[pj][:], func=AF.Ln, bias=cv[:, 0:1], scale=1.0 / 64.0),
                      reads=[ps_d[pj], const_d], writes=[rs_d[s_]])
                kb.op("act", lambda: nc.scalar.activation(out=rs[s_][:], in_=rs[s_][:], func=AF.Exp, scale=-0.5), reads=[rs_d[s_]], writes=[rs_d[s_]])
                gn = "moba_qg" if w == 0 else "moba_kg"
                kb.op("dve", lambda: nc.vector.scalar_tensor_tensor(out=qn[s_][:], in0=ps[pi][:], scalar=ppc(gn, l), in1=rs[s_][:],
                                                                    op0=ALU.mult, op1=ALU.mult),
                      reads=[ps_d[pi], rs_d[s_], const_d], writes=[qn_d[s_]])
                kb.op("act", lambda: nc.scalar.copy(out=qnb[s_][:], in_=qn[s_][:]), reads=[qn_d[s_]], writes=[qnb_d[s_]])

            def prep2(i):
                tt, w, pr = its[i]
                s_ = i % 2
                sl = slice(tt * 512, (tt + 1) * 512)
                rb = tt % 2
                pk = prr.next()
                kb.op("pe", lambda: nc.tensor.matmul(ps[pk][:], lhsT=cmb[:, 3, :], rhs=qnb[s_][:], start=True, stop=True),
                      reads=[qnb_d[s_], const_d], writes=[ps_d[pk]])
                kb.op("dve", lambda: nc.vector.tensor_tensor(out=qn[s_][:], in0=qn[s_][:], in1=mcs[rb][:, 0:512], op=ALU.mult),
                      reads=[qn_d[s_], mcs_d[rb]], writes=[qn_d[s_]])
                kb.op("dve", lambda: nc.vector.tensor_tensor(out=rs[s_][:], in0=ps[pk][:], in1=mcs[rb][:, 512:1024], op=ALU.mult),
                      reads=[ps_d[pk], mcs_d[rb]], writes=[rs_d[s_]])
                kb.op("pool", lambda: nc.gpsimd.tensor_tensor(out=qn[s_][:], in0=qn[s_][:], in1=rs[s_][:], op=ALU.add),
                      reads=[qn_d[s_], rs_d[s_]], writes=[qn_d[s_]])
                for hh in range(2):
                    h = pr * 2 + hh
                    dst = (qa if w == 0 else ka)[h]
                    dd = (qa_d if w == 0 else ka_d)[h][tt]
                    kb.op("act", lambda: nc.scalar.copy(out=dst[0:64, sl], in_=qn[s_][hh * 64:(hh + 1) * 64, :]), reads=[qn_d[s_]], writes=[dd])
                if w == 1:
                    kb.op("dve", lambda: nc.vector.tensor_reduce(out=kms[:, pr, 2 * tt:2 * tt + 2], in_=qn[s_][:].rearrange("p (n j) -> p n j", j=256),
                                                                 axis=AX.X, op=ALU.add), reads=[qn_d[s_]], writes=[kms_d])

            prep1(0)
            for i in range(16):
                if i + 1 < 16:
                    prep1(i + 1)
                prep2(i)
            for h in range(4):
                pr, hh = h // 2, h % 2
                kb.op("act", lambda: nc.scalar.activation(out=km[h][:], in_=kms[hh * 64:(hh + 1) * 64, pr, :], func=AF.Copy, scale=1.0 / 256.0),
                      reads=[kms_d], writes=[km_d[h]])
            for i in range(16):
                pi = prr.next()
                proj_tm(pi, 0, wg, wg_d, 512, 256, i)
                evac(i, va[:, i, :, 0:64], ps[pi][:, 0:256].rearrange("p (h e) -> p h e", h=4), [ps_d[pi]], [va_d[i]])
            gm = st("gm", [128, 32], F32)
            mx = st("mx", [128, 32], F32)
            selp = [st("selp", [128, 72], BF16) for _ in range(4)]
            gm_d, mx_d = Dep(), Dep()
            selp_d = [Dep() for _ in range(4)]
            for h in range(4):
                kb.op("pool", lambda: nc.gpsimd.memset(selp[h][:], 0.0), writes=[selp_d[h]])
            for i in range(8, 16):
                b = i // 2
                tq = i // 4
                pg = 6
                for h in range(4):
                    kb.op("pe", lambda: nc.tensor.matmul(ps[pg][:, h * 8:(h + 1) * 8], lhsT=qa[h][0:64, i * 128:(i + 1) * 128], rhs=km[h][:],
                                                         start=True, stop=True), reads=[qa_d[h][tq], km_d[h]], writes=[ps_d[pg]], inc=(h == 3))
                kb.op("dve", lambda: nc.vector.tensor_tensor(out=gm[:], in0=ps[pg][:, 0:32], in1=cmc("gmask", 256)[:, b * 32:(b + 1) * 32], op=ALU.add),
                      reads=[ps_d[pg], const_d], writes=[gm_d])
                for h in range(4):
                    kb.op("dve", lambda: nc.vector.max(out=mx[:, h * 8:(h + 1) * 8], in_=gm[:, h * 8:(h + 1) * 8]), reads=[gm_d], writes=[mx_d])
                for h in range(4):
                    kb.op("dve", lambda: nc.vector.tensor_scalar(out=selp[h][:, 64:72], in0=gm[:, h * 8:(h + 1) * 8], scalar1=mx[:, h * 8 + 2:h * 8 + 3],
                                                                 scalar2=-30000.0, op0=ALU.is_lt, op1=ALU.mult),
                          reads=[gm_d, mx_d], writes=[selp_d[h]])
                for h in range(4):
                    kb.op("pe", lambda: nc.tensor.transpose(out=psb[0:72, h * 128:(h + 1) * 128], in_=selp[h][:], identity=identb),
                          reads=[selp_d[h], const_d], writes=[psb_d[h]], inc=(h == 3))
                for h in range(4):
                    kb.op("act", lambda: nc.scalar.copy(out=qa[h][64:72, i * 128:(i + 1) * 128], in_=psb[64:72, h * 128:(h + 1) * 128]),
                          reads=[psb_d[h]], writes=[qb_d[h]])
            pt = [st("pt", [128, 256], BF16) for _ in range(3)]
            pt_d = [Dep() for _ in range(3)]
            rec = [st("rec", [64, 256], F32) for _ in range(2)]
            rec_d = [Dep(), Dep()]
            items = []
            for h in range(4):
                for b in range(8):
                    for jt in range(2 * b + 2):
                        items.append((h, b, jt))
            LA = 2
            pt = pt + [st("pt", [128, 256], BF16)]
            pt_d = pt_d + [Dep()]

            def emit_score(k):
                h, b, jt = items[k]
                own = jt >= 2 * b
                qlo = jt - 2 * b
                c0 = 128 if (own and qlo == 1) else 0
                n = 256 - c0
                qcols = slice(b * 256 + c0, (b + 1) * 256)
                tq = b // 2
                pi = k % 4
                pb_ = k % 4
                kk = 64 if own else 72
                rds = [ka_d[h][jt // 4], qa_d[h][tq]] + ([] if own else [kb_d[h], qb_d[h]])
                kb.op("pe", lambda: nc.tensor.matmul(ps[pi][:, 0:n], lhsT=ka[h][0:kk, jt * 128:(jt + 1) * 128], rhs=qa[h][0:kk, qcols],
                                                     start=True, stop=True), reads=rds, writes=[ps_d[pi]])
                kb.op("act", lambda: nc.scalar.activation(out=pt[pb_][:, 0:n], in_=ps[pi][:, 0:n], func=AF.Exp, scale=0.125),
                      reads=[ps_d[pi]], writes=[pt_d[pb_]])
                if own:
                    kb.op("dve", lambda: nc.vector.tensor_tensor(out=pt[pb_][:, 0:128], in0=pt[pb_][:, 0:128], in1=cmc("causal", 128), op=ALU.mult),
                          reads=[pt_d[pb_], const_d], writes=[pt_d[pb_]])

            def emit_pv(k):
                h, b, jt = items[k]
                own = jt >= 2 * b
                qlo = jt - 2 * b
                c0 = 128 if (own and qlo == 1) else 0
                n = 256 - c0
                njt = 2 * b + 2
                nacc = h * 8 + b
                po = 4 + nacc % 2
                rbi = nacc % 2
                pb_ = k % 4
                tq = b // 2
                qs = slice(b * 256, (b + 1) * 256)
                kb.op("pe", lambda: nc.tensor.matmul(ps[po][:, c0:256], lhsT=va[:, jt, h, :], rhs=pt[pb_][:, 0:n],
                                                     start=(jt == 0), stop=(jt == njt - 1)),
                      reads=[va_d[jt], pt_d[pb_]], writes=[ps_d[po]])
                if jt == njt - 1:
                    kb.op("dve", lambda: nc.vector.reciprocal(out=rec[rbi][:], in_=ps[po][64:128, 0:256]), reads=[ps_d[po]], writes=[rec_d[rbi]])
                    hh = h % 2
                    kb.op("dve", lambda: nc.vector.tensor_tensor(out=mixT[hh * 64:(hh + 1) * 64, h // 2, qs], in0=ps[po][0:64, 0:256], in1=rec[rbi][:],
                                                                 op=ALU.mult), reads=[ps_d[po], rec_d[rbi]], writes=[mix_d[h // 2][tq]])

            for k in range(len(items) + LA):
                if k < len(items):
                    emit_score(k)
                if k >= LA:
                    emit_pv(k - LA)

        def mix_zero(l, wg, wg_d, mixT, mix_d, st):
            for cc in range(2):
                kb.op("pool", lambda: nc.gpsimd.memset(mixT[:, cc, :], 0.0), writes=mix_d[cc])

        mix_fns = [mix_moba, mix_ssm, mix_gla, mix_ret]

        def mixer_phase(l, s):
            mk_ = kb.mark()
            with contextlib.ExitStack() as sc:
                st = lambda name, shape, dt: sc.enter_context(nc.sbuf_tensor(kb.un(name), shape, dt))
                wg = st("wg", [128, KC, GW], BF16)
                wo = st("wo", [128, 2, D], BF16)
                wg_d, wo_d = Dep(), Dep()
                wg_c, wo_c = kb.chan(), kb.chan()
                mixT = st("mixT", [128, 2, S], BF16)
                mix_d = [[Dep() for _ in range(4)] for _ in range(2)]
                for mi_, m in enumerate(mixers):
                    r0 = (l * 4 + m) * 128
                    if mi_ == 0:
                        kb.dma(wg_c, wg[:].rearrange("p k c -> p (k c)"), win_b[r0:r0 + 128, :], writes=[wg_d])
                    kb.dma(wo_c, wo[:].rearrange("p k c -> p (k c)"), wout_b[r0:r0 + 128, :], writes=[wo_d])
                    with contextlib.ExitStack() as sc2:
                        st2 = lambda name, shape, dt: sc2.enter_context(nc.sbuf_tensor(kb.un(name), shape, dt))
                        mix_fns[m](l, wg, wg_d, mixT, mix_d, st2)
                        kb.barrier()
                    if debug and s == 0 and l == 0:
                        dc_ = kb.chan()
                        kb.dma(dc_, dbg_d[m], mixT[:].rearrange("p k c -> p (k c)"), reads=[d for dd in mix_d for d in dd])
                    if mi_ + 1 < len(mixers):
                        rn = (l * 4 + mixers[mi_ + 1]) * 128
                        kb.dma(wg_c, wg[:].rearrange("p k c -> p (k c)"), win_b[rn:rn + 128, :], writes=[wg_d])
                    for dc in range(KC):
                        for tt in range(4):
                            sl = slice(tt * 512, (tt + 1) * 512)
                            pi = (dc * 4 + tt) % 4
                            for k2 in range(2):
                                kb.op("pe", lambda: nc.tensor.matmul(ps[pi][:], lhsT=wo[:, k2, dc * 128:(dc + 1) * 128], rhs=mixT[:, k2, sl],
                                                                     start=(k2 == 0), stop=(k2 == 1)),
                                      reads=[wo_d, mix_d[k2][tt]], writes=[ps_d[pi]], inc=(k2 == 1))
                            kb.op("dve", lambda: nc.vector.tensor_tensor(out=xT[:, dc, sl], in0=ps[pi][:], in1=xT[:, dc, sl], op=ALU.add),
                                  reads=[ps_d[pi], xT_d[dc][tt]], writes=[xT_d[dc][tt]])
                    kb.barrier()
            kb.release(mk_)

        for s in range(n_seq):
            load_x(s)
            for l in range(L):
                norm("attn_g", l)
                if mixers:
                    mixer_phase(l, s)
                if not SKIP_FFN:
                    norm("ffn_g", l)
                    ffn(l)
            store_x(s)
        kb.barrier()
    return nc


def kernel(**inputs):
    L = 2
    n_seq = 4
    x = np.asarray(inputs["x"], np.float32)
    hp = host_prep(inputs, L)
    nc = build(n_seq, L)
    in_maps = []
    for c in range(NCORES):
        m = dict(hp)
        m["x"] = np.ascontiguousarray(x[c * n_seq:(c + 1) * n_seq].reshape(n_seq * S, D))
        in_maps.append(m)
    res = run_bass_kernel_spmd(nc, in_maps, core_ids=list(range(NCORES)))
    out = np.stack([r["y"].reshape(n_seq, S, D) for r in res.results], axis=0)
    return out.reshape(NCORES * n_seq, S, D).astype(np.float32)
```

```python
import contextlib
import numpy as np
import concourse.bass as bass
import concourse.mybir as mybir
from concourse.bass_utils import run_bass_kernel_spmd
from concourse.alu_op_type import AluOpType as ALU

F32 = mybir.dt.float32
BF16 = mybir.dt.bfloat16
AF = mybir.ActivationFunctionType
AX = mybir.AxisListType

D = 1024
S = 2048
KC = 8
NCORES = 8
FF = 2816
NJ = 22
EPS = 1e-6
IN_W = 3092
GOFF = (0, 768, 1540, 2324)
GW = 784


class Dep:
    __slots__ = ("w", "r")

    def __init__(self):
        self.w = None
        self.r = {}


class Chan:
    def __init__(self, sem):
        self.sem = sem
        self.n = 0


class KB:
    def __init__(self, nc, es):
        self.nc = nc
        self.es = es
        self.eng = {"pe": nc.tensor, "act": nc.scalar, "dve": nc.vector, "pool": nc.gpsimd, "sp": nc.sync}
        self.sem = {e: es.enter_context(nc.semaphore("s_" + e)) for e in self.eng}
        self.cnt = {e: 0 for e in self.eng}
        self.seen = {e: {} for e in self.eng}
        self.chans = []
        self.nchan = 0

    def un(self, name):
        self.nuniq = getattr(self, "nuniq", 0) + 1
        return "%s_u%d" % (name, self.nuniq)

    def chan(self):
        if getattr(self, "free", None):
            c = self.free.pop()
        else:
            self.nchan += 1
            c = Chan(self.es.enter_context(self.nc.semaphore("c%d" % self.nchan)))
            self.chans.append(c)
        if not hasattr(self, "live"):
            self.live = []
        self.live.append(c)
        return c

    def mark(self):
        if not hasattr(self, "live"):
            self.live = []
        return len(self.live)

    def release(self, mark):
        if not hasattr(self, "free"):
            self.free = []
        self.free.extend(self.live[mark:])
        del self.live[mark:]

    @staticmethod
    def _need(reads, writes):
        need = {}

        def add(k, v):
            if need.get(k, 0) < v:
                need[k] = v

        for d in reads:
            if d.w is not None:
                add(*d.w)
        for d in writes:
            if d.w is not None:
                add(*d.w)
            for k, v in d.r.items():
                add(k, v)
        return need

    def _waits(self, eng, need):
        e = self.eng[eng]
        seen = self.seen[eng]
        for k, v in need.items():
            if k == "pe" and eng == "pe":
                continue
            if seen.get(k, 0) >= v:
                continue
            seen[k] = v
            if isinstance(k, Chan):
                e.wait_ge(k.sem, v)
            else:
                e.wait_ge(self.sem[k], v)

    def op(self, eng, fn, reads=(), writes=(), inc=True):
        self._waits(eng, self._need(reads, writes))
        ins = fn()
        if inc:
            self.cnt[eng] += 1
            ins.then_inc(self.sem[eng], 1)
            c = self.cnt[eng]
        else:
            c = self.cnt[eng] + 1
        for d in reads:
            if d.r.get(eng, 0) < c:
                d.r[eng] = c
        for d in writes:
            d.w = (eng, c)
            d.r = {}
        return ins

    def dma(self, chan, out, in_, reads=(), writes=(), q="sp"):
        self._waits(q, self._need(reads, writes))
        ins = self.eng[q].dma_start(out=out, in_=in_)
        chan.n += 16
        ins.then_inc(chan.sem, 16)
        for d in reads:
            d.r[chan] = chan.n
        for d in writes:
            d.w = (chan, chan.n)
            d.r = {}
        return ins

    def barrier(self):
        for e in self.eng:
            need = {k: v for k, v in self.cnt.items() if v > 0 and k != e}
            for c in self.chans:
                if c.n > 0:
                    need[c] = c.n
            self._waits(e, need)
        for e in ("act", "dve", "pool"):
            if self.cnt[e] > 0 and self.seen[e].get(e, 0) < self.cnt[e]:
                self.seen[e][e] = self.cnt[e]
                self.eng[e].wait_ge(self.sem[e], self.cnt[e])


class Ring:
    def __init__(self, items):
        self.items = items
        self.i = 0

    def next(self):
        it = self.items[self.i % len(self.items)]
        self.i += 1
        return it


def pp_layout(L):
    off = {}
    n = 0

    def add(name, cnt):
        nonlocal n
        off[name] = n
        n += cnt

    add("attn_g", L * KC)
    add("ffn_g", L * KC)
    add("fcw", L * 3 * 2 * NJ)
    add("fcb", L * 2 * NJ)
    add("gla_b2n", L)
    add("gla_ng", L)
    add("ret_ng", L)
    add("ssm_ng", L * 2)
    add("ssm_cw", L * 4 * 4)
    add("ssm_cb", L * 4)
    add("moba_qg", L)
    add("moba_kg", L)
    return off, n


def host_prep(inputs, L):
    f = np.float32
    w_in = np.asarray(inputs["w_in"], f)[:L]
    win = np.zeros((L, 4, 128, KC, GW), f)
    for g in range(4):
        w = (GOFF + (IN_W,))[g + 1] - GOFF[g]
        blk = w_in[:, :, GOFF[g]:GOFF[g] + w].reshape(L, KC, 128, w)
        win[:, g, :, :, :w] = blk.transpose(0, 2, 1, 3)
    win = win.reshape(L * 4 * 128, KC * GW)
    w_out = np.asarray(inputs["w_out"], f)[:L]
    wout = w_out.reshape(L, 4, 2, 128, D).transpose(0, 1, 3, 2, 4).reshape(L * 4 * 128, 2 * D)
    w_up = np.asarray(inputs["ffn_w_up"], f)[:L]
    wu = w_up.reshape(L, KC, 128, 2, NJ, 128)
    wup = wu.transpose(0, 4, 2, 1, 3, 5).reshape(L * NJ * 128, KC * 256)
    wdn = np.asarray(inputs["ffn_w_down"], f)[:L].reshape(L * FF, D)
    off, npp = pp_layout(L)
    pp = np.zeros((128, npp), f)

    def fm(v):
        return np.asarray(v, f).reshape(-1, 128).T

    for l in range(L):
        pp[:, off["attn_g"] + l * KC: off["attn_g"] + (l + 1) * KC] = fm(inputs["attn_norm_g"][l])
        pp[:, off["ffn_g"] + l * KC: off["ffn_g"] + (l + 1) * KC] = fm(inputs["ffn_norm_g"][l])
        for i in range(3):
            o = off["fcw"] + (l * 3 + i) * 2 * NJ
            pp[:, o:o + 2 * NJ] = fm(inputs["ffn_conv_w"][l][i])
        o = off["fcb"] + l * 2 * NJ
        pp[:, o:o + 2 * NJ] = fm(inputs["ffn_conv_b"][l])
    for l in range(L):
        pp[:, off["gla_b2n"] + l] = np.asarray(inputs["gla_gate_b"][l], f)
        pp[:, off["gla_ng"] + l] = np.tile(np.asarray(inputs["gla_norm_g"][l], f), 2)
        pp[:, off["ret_ng"] + l] = np.tile(np.asarray(inputs["ret_norm_g"][l], f), 2)
        pp[:, off["ssm_ng"] + 2 * l: off["ssm_ng"] + 2 * l + 2] = fm(inputs["ssm_norm_g"][l])
        for i in range(4):
            o = off["ssm_cw"] + (l * 4 + i) * 4
            pp[:, o:o + 4] = fm(inputs["ssm_conv_w"][l][i])
        pp[:, off["ssm_cb"] + 4 * l: off["ssm_cb"] + 4 * l + 4] = fm(inputs["ssm_conv_b"][l])
        pp[:, off["moba_qg"] + l] = np.tile(np.asarray(inputs["moba_q_norm_g"][l], f), 2)
        pp[:, off["moba_kg"] + l] = np.tile(np.asarray(inputs["moba_k_norm_g"][l], f), 2)
    consts = host_consts()
    w2 = np.asarray(inputs["gla_gate_w2"], f)[:L].reshape(L * 16, 128)
    consts["gw2"] = np.ascontiguousarray(w2)
    rows = np.zeros((128, L * 384), f)
    for l in range(L):
        rows[:, l * 384: l * 384 + 64] = np.tile(np.asarray(inputs["ssm_dt_bias"][l], f), 16)[None, :]
        rows[:, l * 384 + 64: l * 384 + 128] = np.tile(np.asarray(inputs["ssm_a_log"][l], f), 16)[None, :]
        rows[:, l * 384 + 128: l * 384 + 384] = np.repeat(np.asarray(inputs["ssm_d"][l], f), 64)[None, :]
    consts["rows"] = rows
    return dict(win=np.ascontiguousarray(win), wout=np.ascontiguousarray(wout),
                wup=np.ascontiguousarray(wup), wdn=np.ascontiguousarray(wdn), pp=pp, **consts)


CM_OFF = {}
STAGE = 0
SKIP_FFN = False


def host_consts():
    f = np.float32
    p = np.arange(128)
    cols = []

    def add(name, a):
        CM_OFF[name] = sum(c.shape[1] for c in cols)
        cols.append(np.asarray(a, f))

    add("ident", np.eye(128))
    add("ones", np.ones((128, 128)))
    caus = (p[:, None] <= p[None, :]).astype(f)
    add("causal", np.tile(caus, (1, 4)))
    add("bdm4", (p[:, None] // 32 == np.arange(256)[None, :] // 64))
    add("bdm2", (p[:, None] // 64 == np.arange(256)[None, :] // 128))
    add("hm4", (p[:, None] // 32 == np.arange(4)[None, :]))
    add("hm2", (p[:, None] // 64 == np.arange(2)[None, :]))
    def perm(hd):
        half = hd // 2
        m = np.arange(128)
        partner = (m // hd) * hd + ((m % hd) + half) % hd
        P = np.zeros((128, 128), f)
        P[partner, m] = 1.0
        return P
    add("perm32", perm(32))
    add("perm64", perm(64))
    add("bo64", (p[:, None] // 64 == p[None, :] // 64))
    add("sgt", (p[:, None] > p[None, :]))
    gmk = np.zeros((128, 8, 4, 8))
    for b_ in range(8):
        gmk[:, b_, :, b_:] = -1e30
    add("gmask", gmk.reshape(128, 256))
    cm = np.concatenate(cols, axis=1)
    lg = np.log(1.0 - 2.0 ** (-5.0 - np.arange(4, dtype=np.float64)))
    h = p // 32
    d = p % 32
    inv = 1.0 / (10000.0 ** np.linspace(0.0, 1.0, 16))
    t = np.arange(S, dtype=np.float64)
    ang = t[None, :] * inv[d % 16][:, None]
    rcos = np.cos(ang)
    rsin = np.sin(ang) * np.where(d < 16, -1.0, 1.0)[:, None]
    idx = np.arange(512) % 128
    sc = 32.0 ** -0.5
    rdq = np.exp((idx[None, :] + 1.0) * lg[h][:, None])
    rdki = np.exp(-(idx[None, :] + 1.0) * lg[h][:, None]) * sc
    rdke = np.exp((127.0 - idx[None, :]) * lg[h][:, None]) * sc
    rcd = np.repeat(np.exp(128.0 * lg[h])[:, None], 16, axis=1)
    rett = np.concatenate([rdq, rdki, rdke, rcd], axis=1).astype(f)
    dm = p % 64
    invm = 10000.0 ** (-np.arange(0, 64, 2, dtype=np.float64) / 64)
    angm = t[None, :] * invm[dm % 32][:, None]
    mcos = np.cos(angm)
    msin = np.sin(angm) * np.where(dm < 32, -1.0, 1.0)[:, None]
    blk = (np.arange(S)[None, :] // 256 == np.arange(8)[:, None]).astype(f)
    return {"blk1h": blk, "cm": cm, "ret_cs": np.concatenate([rcos, rsin], axis=1).astype(f), "ret_t": rett,
            "moba_cs": np.concatenate([mcos, msin], axis=1).astype(f)}


def build(n_seq, L, mixers=(0, 1, 2, 3), debug=False):
    nc = bass.Bass("TRN2", target_bir_lowering=False)
    off, npp = pp_layout(L)
    x_d = nc.dram_tensor("x", [n_seq * S, D], F32, kind="ExternalInput").ap()
    y_d = nc.dram_tensor("y", [n_seq * S, D], F32, kind="ExternalOutput").ap()
    win_d = nc.dram_tensor("win", [L * 4 * 128, KC * GW], F32, kind="ExternalInput").ap()
    wout_d = nc.dram_tensor("wout", [L * 4 * 128, 2 * D], F32, kind="ExternalInput").ap()
    wup_d = nc.dram_tensor("wup", [L * NJ * 128, KC * 256], F32, kind="ExternalInput").ap()
    wdn_d = nc.dram_tensor("wdn", [L * FF, D], F32, kind="ExternalInput").ap()
    pp_d = nc.dram_tensor("pp", [128, npp], F32, kind="ExternalInput").ap()
    if not CM_OFF:
        host_consts()
    NCM = CM_OFF["gmask"] + 256
    cm_d = nc.dram_tensor("cm", [128, NCM], F32, kind="ExternalInput").ap()
    retcs_d = nc.dram_tensor("ret_cs", [128, 2 * S], F32, kind="ExternalInput").ap()
    rett_d = nc.dram_tensor("ret_t", [128, 1552], F32, kind="ExternalInput").ap()
    mobacs_d = nc.dram_tensor("moba_cs", [128, 2 * S], F32, kind="ExternalInput").ap()
    gw2_d = nc.dram_tensor("gw2", [L * 16, 128], F32, kind="ExternalInput").ap()
    rows_d = nc.dram_tensor("rows", [128, L * 384], F32, kind="ExternalInput").ap()
    blk_d = nc.dram_tensor("blk1h", [8, S], F32, kind="ExternalInput").ap()
    dbg_d = nc.dram_tensor("dbg", [4, 128, 2 * S], BF16, kind="ExternalOutput").ap() if debug else None
    win_b = nc.dram_tensor("win_b", [L * 4 * 128, KC * GW], BF16, kind="Internal").ap()
    wout_b = nc.dram_tensor("wout_b", [L * 4 * 128, 2 * D], BF16, kind="Internal").ap()
    wup_b = nc.dram_tensor("wup_b", [L * NJ * 128, KC * 256], BF16, kind="Internal").ap()
    wdn_b = nc.dram_tensor("wdn_b", [L * FF, D], BF16, kind="Internal").ap()

    with contextlib.ExitStack() as es:
        kb = KB(nc, es)
        sb = lambda name, shape, dt: es.enter_context(nc.sbuf_tensor(name, shape, dt))

        cc = kb.chan()
        for src, dst in ((win_d, win_b), (wout_d, wout_b), (wup_d, wup_b), (wdn_d, wdn_b)):
            rows = src.shape[0]
            for r in range(0, rows, 128):
                kb.dma(cc, dst[r:r + 128, :], src[r:r + 128, :], q="pool")

        xT = sb("xT", [128, KC, S], F32)
        hT = sb("hT", [128, KC, S], BF16)
        xT_d = [[Dep() for _ in range(4)] for _ in range(KC)]
        hT_d = [[Dep() for _ in range(4)] for _ in range(KC)]
        ppt = sb("ppt", [128, npp], F32)
        cmt = sb("cmt", [128, NCM], F32)
        cmb = sb("cmb", [128, 5, 128], BF16)
        identf = cmt[:, 0:128]
        onesb = cmb[:, 1, :]
        identb = cmb[:, 0, :]

        def cmc(name, n):
            return cmt[:, CM_OFF[name]:CM_OFF[name] + n]
        const_d = Dep()
        c0 = kb.chan()
        kb.dma(c0, ppt[:], pp_d[:, :], writes=[const_d])
        kb.dma(c0, cmt[:], cm_d[:, :], writes=[const_d])
        for ii, nm in enumerate(("ident", "ones", "perm32", "perm64", "bo64")):
            kb.op("dve", lambda: nc.vector.tensor_copy(out=cmb[:, ii, :], in_=cmc(nm, 128)), reads=[const_d], writes=[const_d])
        cv = sb("cv", [128, 8], F32)
        kb.op("pool", lambda: nc.gpsimd.memset(cv[:, 0:1], EPS), writes=[const_d])
        ps = [es.enter_context(nc.psum_tensor("ps%d" % i, [128, 512], F32)) for i in range(7)]
        ps_d = [Dep() for _ in range(7)]
        psb = es.enter_context(nc.psum_tensor("psb", [128, 1024], BF16))
        psb_d = [Dep()] * 8
        kb.barrier()

        def ppc(name, idx):
            o = off[name] + idx
            return ppt[:, o:o + 1]

        def load_x(s):
            mk_ = kb.mark()
            with contextlib.ExitStack() as sc:
                xin = [sc.enter_context(nc.sbuf_tensor(kb.un("xin"), [128, D], F32)) for i in range(2)]
                xin_d = [Dep(), Dep()]
                xc = [kb.chan(), kb.chan()]
                for i in range(16):
                    b = i % 2
                    kb.dma(xc[b], xin[b][:], x_d[s * S + i * 128: s * S + (i + 1) * 128, :], writes=[xin_d[b]])
                    for hh in range(2):
                        pi = (i * 2 + hh) % 4
                        for k in range(4):
                            kc = hh * 4 + k
                            kb.op("pe", lambda: nc.tensor.transpose(out=ps[pi][:, k * 128:(k + 1) * 128],
                                                                    in_=xin[b][:, kc * 128:(kc + 1) * 128],
                                                                    identity=identf),
                                  reads=[xin_d[b], const_d], writes=[ps_d[pi]], inc=(k == 3))
                        eng = "act" if hh == 0 else "dve"
                        dst = xT[:, hh * 4:(hh + 1) * 4, i * 128:(i + 1) * 128]
                        src = ps[pi][:].rearrange("p (k c) -> p k c", k=4)
                        wr = [xT_d[hh * 4 + k][i // 4] for k in range(4)]
                        if eng == "act":
                            kb.op("act", lambda: nc.scalar.copy(out=dst, in_=src), reads=[ps_d[pi]], writes=wr)
                        else:
                            kb.op("dve", lambda: nc.vector.tensor_copy(out=dst, in_=src), reads=[ps_d[pi]], writes=wr)
                kb.barrier()
            kb.release(mk_)

        def store_x(s):
            mk_ = kb.mark()
            with contextlib.ExitStack() as sc:
                xo = [sc.enter_context(nc.sbuf_tensor(kb.un("xo"), [128, D], F32)) for i in range(2)]
                xo_d = [Dep(), Dep()]
                xc = [kb.chan(), kb.chan()]
                for i in range(16):
                    b = i % 2
                    for hh in range(2):
                        pi = (i * 2 + hh) % 4
                        for k in range(4):
                            kc = hh * 4 + k
                            kb.op("pe", lambda: nc.tensor.transpose(out=ps[pi][:, k * 128:(k + 1) * 128],
                                                                    in_=xT[:, kc, i * 128:(i + 1) * 128],
                                                                    identity=identf),
                                  reads=[xT_d[kc][i // 4], const_d], writes=[ps_d[pi]], inc=(k == 3))
                        dst = xo[b][:, hh * 512:(hh + 1) * 512]
                        if hh == 0:
                            kb.op("act", lambda: nc.scalar.copy(out=dst, in_=ps[pi][:]), reads=[ps_d[pi]], writes=[xo_d[b]])
                        else:
                            kb.op("dve", lambda: nc.vector.tensor_copy(out=dst, in_=ps[pi][:]), reads=[ps_d[pi]], writes=[xo_d[b]])
                    kb.dma(xc[b], y_d[s * S + i * 128: s * S + (i + 1) * 128, :], xo[b][:], reads=[xo_d[b]])
                kb.barrier()

            kb.release(mk_)

        def norm(gname, l):
            with contextlib.ExitStack() as sc:
                sq = [sc.enter_context(nc.sbuf_tensor(kb.un("sq"), [128, KC, 512], BF16)) for i in range(2)]
                rs = [sc.enter_context(nc.sbuf_tensor(kb.un("rs"), [128, 512], F32)) for i in range(2)]
                sq_d = [Dep(), Dep()]
                rs_d = [Dep(), Dep()]
                for tt in range(4):
                    b = tt % 2
                    sl = slice(tt * 512, (tt + 1) * 512)
                    kb.op("act", lambda: nc.scalar.activation(out=sq[b][:], in_=xT[:, :, sl], func=AF.Square),
                          reads=[xT_d[k][tt] for k in range(KC)], writes=[sq_d[b]])
                    pi = 4 + b
                    for kc in range(KC):
                        kb.op("pe", lambda: nc.tensor.matmul(ps[pi][:], lhsT=onesb, rhs=sq[b][:, kc, :],
                                                             start=(kc == 0), stop=(kc == KC - 1)),
                              reads=[sq_d[b], const_d], writes=[ps_d[pi]], inc=(kc == KC - 1))
                    kb.op("act", lambda: nc.scalar.activation(out=rs[b][:], in_=ps[pi][:], func=AF.Ln, bias=cv[:, 0:1], scale=1.0 / D),
                          reads=[ps_d[pi], const_d], writes=[rs_d[b]])
                    kb.op("act", lambda: nc.scalar.activation(out=rs[b][:], in_=rs[b][:], func=AF.Exp, scale=-0.5),
                          reads=[rs_d[b]], writes=[rs_d[b]])
                    for kc in range(KC):
                        kb.op("dve", lambda: nc.vector.scalar_tensor_tensor(out=hT[:, kc, sl], in0=xT[:, kc, sl],
                                                                            scalar=ppc(gname, l * KC + kc), in1=rs[b][:],
                                                                            op0=ALU.mult, op1=ALU.mult),
                              reads=[xT_d[kc][tt], rs_d[b], const_d], writes=[hT_d[kc][tt]])
                kb.barrier()

        def ffn(l):
            mk_ = kb.mark()
            G = 8
            groups = [list(range(a, min(a + G, NJ))) for a in range(0, NJ, G)]
            with contextlib.ExitStack() as sc:
                st = lambda name, shape, dt: sc.enter_context(nc.sbuf_tensor(kb.un(name), shape, dt))
                upre = [[st("upre%d_%d" % (b, h), [128, S + 2], BF16) for h in range(2)] for b in range(2)]
                upre_d = [[[Dep() for _ in range(5)] for h in range(2)] for b in range(2)]
                acc = [[st("acc%d_%d" % (r, h), [128, 512], F32) for h in range(2)] for r in range(3)]
                acc_d = [[Dep() for h in range(2)] for r in range(3)]
                actT = st("actT", [128, G, S], BF16)
                act_d = [[Dep() for _ in range(4)] for _ in range(G)]
                wup = [st("wup%d" % i, [128, KC, 256], BF16) for i in range(3)]
                wup_dd = [Dep() for _ in range(3)]
                wup_c = [kb.chan() for _ in range(3)]
                wdn = [st("wdn%d" % i, [128, D], BF16) for i in range(G)]
                wdn_dd = [Dep() for _ in range(G)]
                wdn_c = [kb.chan() for _ in range(G)]
                for b in range(2):
                    for h in range(2):
                        kb.op("pool", lambda: nc.gpsimd.memset(upre[b][h][:, 0:2], 0.0), writes=[upre_d[b][h][0]])
                accr = 0
                jcount = 0
                for grp in groups:
                    for jj, j in enumerate(grp):
                        kb.dma(wdn_c[jj], wdn[jj][:], wdn_b[l * FF + j * 128: l * FF + (j + 1) * 128, :], writes=[wdn_dd[jj]])
                    for jj, j in enumerate(grp):
                        ws = jcount % 3
                        ub = jcount % 2
                        jcount += 1
                        r0 = (l * NJ + j) * 128
                        kb.dma(wup_c[ws], wup[ws][:].rearrange("p k c -> p (k c)"), wup_b[r0:r0 + 128, :], writes=[wup_dd[ws]])
                        for tt in range(4):
                            sl = slice(tt * 512, (tt + 1) * 512)
                            ar = accr % 3
                            accr += 1
                            for h in range(2):
                                pi = (tt % 2) * 2 + h
                                for kc in range(KC):
                                    kb.op("pe", lambda: nc.tensor.matmul(ps[pi][:], lhsT=wup[ws][:, kc, h * 128:(h + 1) * 128],
                                                                         rhs=hT[:, kc, sl], start=(kc == 0), stop=(kc == KC - 1)),
                                          reads=[wup_dd[ws], hT_d[kc][tt]], writes=[ps_d[pi]], inc=(kc == KC - 1))
                                kb.op("act", lambda: nc.scalar.copy(out=upre[ub][h][:, 2 + tt * 512: 2 + (tt + 1) * 512], in_=ps[pi][:]),
                                      reads=[ps_d[pi]], writes=[upre_d[ub][h][tt + 1]])
                                w2 = ppc("fcw", (l * 3 + 2) * 2 * NJ + h * NJ + j)
                                w1 = ppc("fcw", (l * 3 + 1) * 2 * NJ + h * NJ + j)
                                w0 = ppc("fcw", (l * 3 + 0) * 2 * NJ + h * NJ + j)
                                bb = ppc("fcb", l * 2 * NJ + h * NJ + j)
                                kb.op("act", lambda: nc.scalar.activation(out=acc[ar][h][:], in_=ps[pi][:], func=AF.Identity,
                                                                          bias=bb, scale=w2),
                                      reads=[ps_d[pi], const_d], writes=[acc_d[ar][h]])
                                kb.op("dve", lambda: nc.vector.scalar_tensor_tensor(
                                    out=acc[ar][h][:], in0=upre[ub][h][:, 1 + tt * 512: 1 + (tt + 1) * 512], scalar=w1,
                                    in1=acc[ar][h][:], op0=ALU.mult, op1=ALU.add),
                                    reads=[upre_d[ub][h][tt + 1], upre_d[ub][h][tt], acc_d[ar][h], const_d], writes=[acc_d[ar][h]])
                                kb.op("dve", lambda: nc.vector.scalar_tensor_tensor(
                                    out=acc[ar][h][:], in0=upre[ub][h][:, tt * 512: (tt + 1) * 512], scalar=w0,
                                    in1=acc[ar][h][:], op0=ALU.mult, op1=ALU.add),
                                    reads=[upre_d[ub][h][tt + 1], upre_d[ub][h][tt], acc_d[ar][h], const_d], writes=[acc_d[ar][h]])
                            kb.op("act", lambda: nc.scalar.activation(out=acc[ar][0][:], in_=acc[ar][0][:], func=AF.Silu),
                                  reads=[acc_d[ar][0]], writes=[acc_d[ar][0]])
                            kb.op("pool", lambda: nc.gpsimd.tensor_tensor(out=actT[:, jj, sl], in0=acc[ar][0][:], in1=acc[ar][1][:],
                                                                          op=ALU.mult),
                                  reads=[acc_d[ar][0], acc_d[ar][1]], writes=[act_d[jj][tt]])
                    for dc in range(KC):
                        for tt in range(4):
                            sl = slice(tt * 512, (tt + 1) * 512)
                            pi = 4 + (dc * 4 + tt) % 3
                            for jj, j in enumerate(grp):
                                kb.op("pe", lambda: nc.tensor.matmul(ps[pi][:], lhsT=wdn[jj][:, dc * 128:(dc + 1) * 128],
                                                                     rhs=actT[:, jj, sl], start=(jj == 0), stop=(jj == len(grp) - 1)),
                                      reads=[wdn_dd[jj], act_d[jj][tt]], writes=[ps_d[pi]], inc=(jj == len(grp) - 1))
                            kb.op("dve", lambda: nc.vector.tensor_tensor(out=xT[:, dc, sl], in0=ps[pi][:], in1=xT[:, dc, sl], op=ALU.add),
                                  reads=[ps_d[pi], xT_d[dc][tt]], writes=[xT_d[dc][tt]])
                kb.barrier()
            kb.release(mk_)

        kb.op("pool", lambda: nc.gpsimd.memset(cv[:, 1:2], 1.0), writes=[const_d])
        kb.op("pool", lambda: nc.gpsimd.memset(cv[:, 2:3], float(np.log(32.0 ** -0.5))), writes=[const_d])
        kb.barrier()
        prr = Ring(list(range(7)))

        def proj_fm(pi, wg, wg_d, c0, M, tt):
            sl = slice(tt * 512, (tt + 1) * 512)
            for kc in range(KC):
                kb.op("pe", lambda: nc.tensor.matmul(ps[pi][0:M, :], lhsT=wg[:, kc, c0:c0 + M], rhs=hT[:, kc, sl],
                                                     start=(kc == 0), stop=(kc == KC - 1)),
                      reads=[wg_d, hT_d[kc][tt]], writes=[ps_d[pi]], inc=(kc == KC - 1))

        def proj_tm(pi, col0, wg, wg_d, c0, N, i):
            for kc in range(KC):
                kb.op("pe", lambda: nc.tensor.matmul(ps[pi][:, col0:col0 + N], lhsT=hT[:, kc, i * 128:(i + 1) * 128],
                                                     rhs=wg[:, kc, c0:c0 + N], start=(kc == 0), stop=(kc == KC - 1)),
                      reads=[wg_d, hT_d[kc][i // 4]], writes=[ps_d[pi]], inc=(kc == KC - 1))

        def evac(i, out, in_, reads, writes):
            if i % 2 == 0:
                kb.op("act", lambda: nc.scalar.copy(out=out, in_=in_), reads=reads, writes=writes)
            else:
                kb.op("dve", lambda: nc.vector.tensor_copy(out=out, in_=in_), reads=reads, writes=writes)

        def v_tokmajor(vt, v_d, wg, wg_d, c0):
            for i in range(16):
                pi = prr.next()
                proj_tm(pi, 0, wg, wg_d, c0, 256, i)
                evac(i, vt[:, i, :], ps[pi][:, 0:256], [ps_d[pi]], [v_d[i]])

        def gate_fm(sg, sg_d, wg, wg_d, c0):
            for cc in range(2):
                for tt in range(4):
                    pi = prr.next()
                    proj_fm(pi, wg, wg_d, c0 + cc * 128, 128, tt)
                    kb.op("act", lambda: nc.scalar.activation(out=sg[:, cc, tt * 512:(tt + 1) * 512], in_=ps[pi][:], func=AF.Silu),
                          reads=[ps_d[pi]], writes=[sg_d[cc][tt]])

        def post_norm(c, src, src_d, ng, gname_idx, sg, sg_d, mixT, mix_d, tl):
            w = 256 // ng
            b = c % 2
            sq, sq_d, ss, ss_d, on, on_d = tl["sq"][b], tl["sq_d"][b], tl["ss"][b], tl["ss_d"][b], tl["on"][b], tl["on_d"][b]
            kb.op("act", lambda: nc.scalar.activation(out=sq[:], in_=src, func=AF.Square), reads=[src_d], writes=[sq_d])
            kb.op("dve", lambda: nc.vector.tensor_reduce(out=ss[:, 0:ng], in_=sq[:].rearrange("p (g e) -> p g e", g=ng),
                                                         axis=AX.X, op=ALU.add), reads=[sq_d], writes=[ss_d])
            kb.op("act", lambda: nc.scalar.activation(out=ss[:, 0:ng], in_=ss[:, 0:ng], func=AF.Ln, bias=cv[:, 0:1], scale=1.0 / w),
                  reads=[ss_d, const_d], writes=[ss_d])
            kb.op("act", lambda: nc.scalar.activation(out=ss[:, 0:ng], in_=ss[:, 0:ng], func=AF.Exp, scale=-0.5),
                  reads=[ss_d], writes=[ss_d])
            for g in range(ng):
                kb.op("act", lambda: nc.scalar.activation(out=on[:, g * w:(g + 1) * w], in_=src[:, g * w:(g + 1) * w], func=AF.Copy,
                                                          scale=ss[:, g:g + 1]), reads=[src_d, ss_d], writes=[on_d])
            if STAGE == 3:
                if c == 15:
                    mix_zero(0, None, None, mixT, mix_d, None)
                return
            pt = 5 + b
            for cc in range(2):
                tin = sq if STAGE == 5 else on
                tin_d = sq_d if STAGE == 5 else on_d
                if STAGE == 7:
                    continue
                kb.op("pe", lambda: nc.tensor.transpose(out=ps[pt][:, cc * 128:(cc + 1) * 128], in_=tin[:, cc * 128:(cc + 1) * 128],
                                                        identity=identf), reads=[tin_d, const_d], writes=[ps_d[pt]], inc=(cc == 1))
            if STAGE == 6:
                if c == 15:
                    mix_zero(0, None, None, mixT, mix_d, None)
                return
            for cc in range(2):
                dst = mixT[:, cc, c * 128:(c + 1) * 128]
                srcT = ps[pt][:, cc * 128:(cc + 1) * 128]
                if sg is not None:
                    kb.op("dve", lambda: nc.vector.scalar_tensor_tensor(out=dst, in0=srcT,
                                                                        scalar=ppc(*gname_idx(cc)), in1=sg[:, cc, c * 128:(c + 1) * 128],
                                                                        op0=ALU.mult, op1=ALU.mult),
                          reads=[ps_d[pt], sg_d[cc][c // 4], const_d], writes=[mix_d[cc][c // 4]])
                else:
                    kb.op("dve", lambda: nc.vector.tensor_scalar(out=dst, in0=srcT,
                                                                 scalar1=ppc(*gname_idx(cc)), scalar2=None, op0=ALU.mult),
                          reads=[ps_d[pt], const_d], writes=[mix_d[cc][c // 4]])

        def post_tiles(st):
            return dict(sq=[st("sq", [128, 256], F32) for _ in range(2)], sq_d=[Dep(), Dep()],
                        ss=[st("ss", [128, 4], F32) for _ in range(2)], ss_d=[Dep(), Dep()],
                        on=[st("on", [128, 256], F32) for _ in range(2)], on_d=[Dep(), Dep()])

        def linattn(st, Kmask, Km_d, QdT, Qd_d, kendT, ke_d, vt, v_d, cdec, cdec_d, gname_idx, sg, sg_d, mixT, mix_d):
            tl = post_tiles(st)
            attm = [st("attm", [128, 512], BF16) for _ in range(2)]
            attm_d = [Dep(), Dep()]
            ketm = [st("ketm", [128, 128], BF16) for _ in range(2)]
            ketm_d = [Dep(), Dep()]
            S_run = st("S_run", [128, 256], F32)
            S_tmp = st("S_tmp", [128, 256], F32)
            Sbf = st("Sbf", [128, 256], BF16)
            S_d, St_d, Sb_d = Dep(), Dep(), Dep()
            attm.append(st("attm", [128, 512], BF16))
            attm_d.append(Dep())
            Sbfs = [Sbf] + [st("Sbf", [128, 256], BF16) for _ in range(3)]
            Sb_ds = [Dep() for _ in range(4)]

            def stage_a(c):
                ch = slice(c * 128, (c + 1) * 128)
                b = c % 2
                b3 = c % 3
                tq = c // 4
                if c < 15:
                    kb.op("pe", lambda: nc.tensor.transpose(out=psb[:, b * 128:(b + 1) * 128], in_=kendT[:, ch], identity=identb),
                          reads=[ke_d[tq], const_d], writes=[psb_d[b]])
                    kb.op("act", lambda: nc.scalar.copy(out=ketm[b][:], in_=psb[:, b * 128:(b + 1) * 128]),
                          reads=[psb_d[b]], writes=[ketm_d[b]])
                pa = b
                for g in range(4):
                    kb.op("pe", lambda: nc.tensor.matmul(ps[pa][:, g * 128:(g + 1) * 128], lhsT=Kmask[:, g, ch], rhs=QdT[:, ch],
                                                         start=True, stop=True),
                          reads=[Km_d[tq], Qd_d[tq]], writes=[ps_d[pa]], inc=(g == 3))
                kb.op("dve", lambda: nc.vector.tensor_tensor(out=attm[b3][:], in0=ps[pa][:], in1=cmc("causal", 512), op=ALU.mult),
                      reads=[ps_d[pa], const_d], writes=[attm_d[b3]])
                if c < 15:
                    kb.op("pe", lambda: nc.tensor.matmul(ps[4][:, 0:256], lhsT=ketm[b][:], rhs=vt[:, c, :], start=True, stop=True),
                          reads=[ketm_d[b], v_d[c]], writes=[ps_d[4]])
                    if c == 0:
                        kb.op("dve", lambda: nc.vector.tensor_tensor(out=S_run[:], in0=ps[4][:, 0:256], in1=cmc("bdm4", 256), op=ALU.mult),
                              reads=[ps_d[4], const_d], writes=[S_d])
                    else:
                        kb.op("dve", lambda: nc.vector.tensor_tensor(out=S_tmp[:], in0=ps[4][:, 0:256], in1=cmc("bdm4", 256), op=ALU.mult),
                              reads=[ps_d[4], const_d], writes=[St_d])
                        kb.op("dve", lambda: nc.vector.scalar_tensor_tensor(out=S_run[:], in0=S_run[:], scalar=cdec[:, c:c + 1], in1=S_tmp[:],
                                                                            op0=ALU.mult, op1=ALU.add),
                              reads=[S_d, St_d, cdec_d], writes=[S_d])
                    kb.op("act", lambda: nc.scalar.copy(out=Sbfs[c % 4][:], in_=S_run[:]), reads=[S_d], writes=[Sb_ds[c % 4]])

            def stage_b(c):
                ch = slice(c * 128, (c + 1) * 128)
                b = c % 2
                b3 = c % 3
                tq = c // 4
                po = 2 + b
                if c > 0:
                    kb.op("pe", lambda: nc.tensor.matmul(ps[po][:, 0:256], lhsT=QdT[:, ch], rhs=Sbfs[(c - 1) % 4][:], start=True, stop=False),
                          reads=[Qd_d[tq], Sb_ds[(c - 1) % 4]], writes=[ps_d[po]], inc=False)
                for h in range(4):
                    kb.op("pe", lambda: nc.tensor.matmul(ps[po][:, h * 64:(h + 1) * 64], lhsT=attm[b3][:, h * 128:(h + 1) * 128],
                                                         rhs=vt[:, c, h * 64:(h + 1) * 64], start=(c == 0 and h == 0), stop=(h == 3)),
                          reads=[attm_d[b3], v_d[c]], writes=[ps_d[po]], inc=(h == 3))

            def stage_c(c):
                po = 2 + c % 2
                post_norm(c, ps[po][:, 0:256], ps_d[po], 4, gname_idx, sg, sg_d, mixT, mix_d, tl)

            stage_a(0)
            stage_a(1)
            for c in range(16):
                stage_b(c)
                if c + 2 < 16:
                    stage_a(c + 2)
                if c >= 1:
                    stage_c(c - 1)
            stage_c(15)

        def kq_finish(st, qsrc, q_d, ksrc, k_d, dq, dki, dke, dec_d, QdT, Qd_d, Kmask, Km_d, kendT, ke_d, tt, kinv, kinv_d):
            sl = slice(tt * 512, (tt + 1) * 512)
            kb.op("dve", lambda: nc.vector.tensor_tensor(out=QdT[:, sl], in0=qsrc, in1=dq, op=ALU.mult),
                  reads=[q_d, dec_d], writes=[Qd_d[tt]])
            kb.op("dve", lambda: nc.vector.tensor_tensor(out=kinv[:], in0=ksrc, in1=dki, op=ALU.mult),
                  reads=[k_d, dec_d], writes=[kinv_d])
            kb.op("dve", lambda: nc.vector.tensor_tensor(out=kendT[:, sl], in0=ksrc, in1=dke, op=ALU.mult),
                  reads=[k_d, dec_d], writes=[ke_d[tt]])
            for h in range(4):
                if h < 2:
                    kb.op("act", lambda: nc.scalar.activation(out=Kmask[:, h, sl], in_=kinv[:], func=AF.Copy, scale=cmc("hm4", 4)[:, h:h + 1]),
                          reads=[kinv_d, const_d], writes=[Km_d[tt]])
                else:
                    kb.op("dve", lambda: nc.vector.tensor_scalar(out=Kmask[:, h, sl], in0=kinv[:], scalar1=cmc("hm4", 4)[:, h:h + 1], scalar2=None,
                                                                 op0=ALU.mult), reads=[kinv_d, const_d], writes=[Km_d[tt]])

        def la_tiles(st):
            return dict(QdT=st("QdT", [128, S], BF16), Qd_d=[Dep() for _ in range(4)],
                        Kmask=st("Kmask", [128, 4, S], BF16), Km_d=[Dep() for _ in range(4)],
                        kendT=st("kendT", [128, S], BF16), ke_d=[Dep() for _ in range(4)],
                        vt=st("vt", [128, 16, 256], BF16), v_d=[Dep() for _ in range(16)],
                        sg=st("sg", [128, 2, S], BF16), sg_d=[[Dep() for _ in range(4)] for _ in range(2)],
                        kinv=st("kinv", [128, 512], F32), kinv_d=Dep())

        def mix_gla(l, wg, wg_d, mixT, mix_d, st):
            T = la_tiles(st)
            w2f = st("w2f", [16, 128], F32)
            w2b = st("w2b", [16, 128], BF16)
            w2_d = Dep()
            c1 = kb.chan()
            kb.dma(c1, w2f[:], gw2_d[l * 16:(l + 1) * 16, :], writes=[w2_d])
            kb.op("dve", lambda: nc.vector.tensor_copy(out=w2b[:], in_=w2f[:]), reads=[w2_d], writes=[w2_d])
            nb2 = st("nb2", [128, 1], F32)
            kb.op("pool", lambda: nc.gpsimd.tensor_scalar(out=nb2[:], in0=ppc("gla_b2n", l), scalar1=-1.0, scalar2=None, op0=ALU.mult),
                  reads=[const_d], writes=[w2_d])
            ggT = st("ggT", [16, S], BF16)
            gg_d = [Dep() for _ in range(4)]
            bcs = st("bcs", [128, S], F32)
            bcs_d = [Dep() for _ in range(4)]
            spt = [st("spt", [128, 512], F32) for _ in range(2)]
            spt_d = [Dep(), Dep()]
            for tt in range(4):
                sl = slice(tt * 512, (tt + 1) * 512)
                pi = prr.next()
                proj_fm(pi, wg, wg_d, 768, 16, tt)
                kb.op("act", lambda: nc.scalar.copy(out=ggT[:, sl], in_=ps[pi][0:16, :]), reads=[ps_d[pi]], writes=[gg_d[tt]])
                pj = prr.next()
                kb.op("pe", lambda: nc.tensor.matmul(ps[pj][:], lhsT=w2b[:], rhs=ggT[:, sl], start=True, stop=True),
                      reads=[w2_d, gg_d[tt]], writes=[ps_d[pj]])
                b = tt % 2
                kb.op("act", lambda: nc.scalar.activation(out=spt[b][:], in_=ps[pj][:], func=AF.Exp, bias=nb2[:], scale=-1.0),
                      reads=[ps_d[pj], w2_d], writes=[spt_d[b]])
                kb.op("act", lambda: nc.scalar.activation(out=spt[b][:], in_=spt[b][:], func=AF.Ln, bias=cv[:, 1:2], scale=1.0),
                      reads=[spt_d[b], const_d], writes=[spt_d[b]])
                for ci in range(4):
                    cs_ = slice(ci * 128, (ci + 1) * 128)
                    gs_ = slice(tt * 512 + ci * 128, tt * 512 + (ci + 1) * 128)
                    kb.op("dve", lambda: nc.vector.tensor_tensor_scan(out=bcs[:, gs_], data0=cmc("ones", 128), data1=spt[b][:, cs_],
                                                                      initial=0.0, op0=ALU.mult, op1=ALU.add),
                          reads=[spt_d[b], const_d], writes=[bcs_d[tt]])
            nbl = st("nbl", [128, 16], F32)
            cdec = st("cdec", [128, 16], F32)
            nbl_d, cdec_d = Dep(), Dep()
            blast = bcs[:].rearrange("p (c i) -> p c i", i=128)[:, :, 127]
            kb.op("dve", lambda: nc.vector.tensor_scalar(out=nbl[:], in0=blast, scalar1=-1.0 / 16.0, scalar2=None, op0=ALU.mult),
                  reads=bcs_d, writes=[nbl_d])
            kb.op("act", lambda: nc.scalar.activation(out=cdec[:], in_=nbl[:], func=AF.Exp), reads=[nbl_d], writes=[cdec_d])
            dq = [st("dq", [128, 512], F32)] * 2
            dki = [st("dki", [128, 512], F32)] * 2
            dke = [st("dke", [128, 512], F32)] * 2
            dec_d = [Dep()] * 2
            for tt in range(4):
                sl = slice(tt * 512, (tt + 1) * 512)
                b = tt % 2
                kb.op("act", lambda: nc.scalar.activation(out=dq[b][:], in_=bcs[:, sl], func=AF.Exp, bias=cv[:, 2:3], scale=-1.0 / 16.0),
                      reads=[bcs_d[tt], const_d], writes=[dec_d[b]])
                kb.op("act", lambda: nc.scalar.activation(out=dki[b][:], in_=bcs[:, sl], func=AF.Exp, scale=1.0 / 16.0),
                      reads=[bcs_d[tt]], writes=[dec_d[b]])
                for ci in range(4):
                    c = tt * 4 + ci
                    kb.op("act", lambda: nc.scalar.activation(out=dke[b][:, ci * 128:(ci + 1) * 128], in_=bcs[:, c * 128:(c + 1) * 128],
                                                              func=AF.Exp, bias=nbl[:, c:c + 1], scale=1.0 / 16.0),
                          reads=[bcs_d[tt], nbl_d], writes=[dec_d[b]])
                pq = prr.next()
                proj_fm(pq, wg, wg_d, 0, 128, tt)
                pk = prr.next()
                proj_fm(pk, wg, wg_d, 128, 128, tt)
                kq_finish(st, ps[pq][:], ps_d[pq], ps[pk][:], ps_d[pk], dq[b][:], dki[b][:], dke[b][:], dec_d[b],
                          T["QdT"], T["Qd_d"], T["Kmask"], T["Km_d"], T["kendT"], T["ke_d"], tt, T["kinv"], T["kinv_d"])
            v_tokmajor(T["vt"], T["v_d"], wg, wg_d, 256)
            gate_fm(T["sg"], T["sg_d"], wg, wg_d, 512)
            linattn(st, T["Kmask"], T["Km_d"], T["QdT"], T["Qd_d"], T["kendT"], T["ke_d"], T["vt"], T["v_d"], cdec, cdec_d,
                    lambda cc: ("gla_ng", l), T["sg"], T["sg_d"], mixT, mix_d)

        def linattn_v1(st, Kmask, Km_d, QdT, Qd_d, kendT, ke_d, vt, v_d, cdec, cdec_d, gname_idx, sg, sg_d, mixT, mix_d):
            tl = post_tiles(st)
            attm = [st("attm", [128, 512], BF16) for _ in range(2)]
            attm_d = [Dep(), Dep()]
            ketm = [st("ketm", [128, 128], BF16) for _ in range(2)]
            ketm_d = [Dep(), Dep()]
            S_run = st("S_run", [128, 256], F32)
            S_tmp = st("S_tmp", [128, 256], F32)
            Sbf = st("Sbf", [128, 256], BF16)
            S_d, St_d, Sb_d = Dep(), Dep(), Dep()
            for c in range(16):
                ch = slice(c * 128, (c + 1) * 128)
                b = c % 2
                tq = c // 4
                if c < 15:
                    kb.op("pe", lambda: nc.tensor.transpose(out=psb[:, b * 128:(b + 1) * 128], in_=kendT[:, ch], identity=identb),
                          reads=[ke_d[tq], const_d], writes=[psb_d[b]])
                    kb.op("act", lambda: nc.scalar.copy(out=ketm[b][:], in_=psb[:, b * 128:(b + 1) * 128]),
                          reads=[psb_d[b]], writes=[ketm_d[b]])
                pa = b
                for g in range(4):
                    kb.op("pe", lambda: nc.tensor.matmul(ps[pa][:, g * 128:(g + 1) * 128], lhsT=Kmask[:, g, ch], rhs=QdT[:, ch],
                                                         start=True, stop=True),
                          reads=[Km_d[tq], Qd_d[tq]], writes=[ps_d[pa]], inc=(g == 3))
                kb.op("dve", lambda: nc.vector.tensor_tensor(out=attm[b][:], in0=ps[pa][:], in1=cmc("causal", 512), op=ALU.mult),
                      reads=[ps_d[pa], const_d], writes=[attm_d[b]])
                po = 2 + b
                if c > 0:
                    kb.op("pe", lambda: nc.tensor.matmul(ps[po][:, 0:256], lhsT=QdT[:, ch], rhs=Sbf[:], start=True, stop=False),
                          reads=[Qd_d[tq], Sb_d], writes=[ps_d[po]], inc=False)
                for h in range(4):
                    kb.op("pe", lambda: nc.tensor.matmul(ps[po][:, h * 64:(h + 1) * 64], lhsT=attm[b][:, h * 128:(h + 1) * 128],
                                                         rhs=vt[:, c, h * 64:(h + 1) * 64], start=(c == 0 and h == 0), stop=(h == 3)),
                          reads=[attm_d[b], v_d[c]], writes=[ps_d[po]], inc=(h == 3))
                if c < 15:
                    kb.op("pe", lambda: nc.tensor.matmul(ps[4][:, 0:256], lhsT=ketm[b][:], rhs=vt[:, c, :], start=True, stop=True),
                          reads=[ketm_d[b], v_d[c]], writes=[ps_d[4]])
                    if c == 0:
                        kb.op("dve", lambda: nc.vector.tensor_tensor(out=S_run[:], in0=ps[4][:, 0:256], in1=cmc("bdm4", 256), op=ALU.mult),
                              reads=[ps_d[4], const_d, Sb_d], writes=[S_d])
                    else:
                        kb.op("dve", lambda: nc.vector.tensor_tensor(out=S_tmp[:], in0=ps[4][:, 0:256], in1=cmc("bdm4", 256), op=ALU.mult),
                              reads=[ps_d[4], const_d], writes=[St_d])
                        kb.op("dve", lambda: nc.vector.scalar_tensor_tensor(out=S_run[:], in0=S_run[:], scalar=cdec[:, c:c + 1], in1=S_tmp[:],
                                                                            op0=ALU.mult, op1=ALU.add),
                              reads=[S_d, St_d, cdec_d], writes=[S_d])
                    kb.op("act", lambda: nc.scalar.copy(out=Sbf[:], in_=S_run[:]), reads=[S_d], writes=[Sb_d])
                if STAGE == 2:
                    if c == 15:
                        mix_zero(0, None, None, mixT, mix_d, st)
                    continue
                post_norm(c, ps[po][:, 0:256], ps_d[po], 4, gname_idx, sg, sg_d, mixT, mix_d, tl)

        def kq_finish_v1(st, qsrc, q_d, ksrc, k_d, dq, dki, dke, dec_d, QdT, Qd_d, Kmask, Km_d, kendT, ke_d, tt, kinv, kinv_d):
            sl = slice(tt * 512, (tt + 1) * 512)
            kb.op("dve", lambda: nc.vector.tensor_tensor(out=QdT[:, sl], in0=qsrc, in1=dq, op=ALU.mult),
                  reads=[q_d, dec_d], writes=[Qd_d[tt]])
            kb.op("dve", lambda: nc.vector.tensor_tensor(out=kinv[:], in0=ksrc, in1=dki, op=ALU.mult),
                  reads=[k_d, dec_d], writes=[kinv_d])
            kb.op("dve", lambda: nc.vector.tensor_tensor(out=kendT[:, sl], in0=ksrc, in1=dke, op=ALU.mult),
                  reads=[k_d, dec_d], writes=[ke_d[tt]])
            for h in range(4):
                eng = "pool" if h % 2 == 0 else "dve"
                e = nc.gpsimd if eng == "pool" else nc.vector
                kb.op(eng, lambda: e.tensor_scalar(out=Kmask[:, h, sl], in0=kinv[:], scalar1=cmc("hm4", 4)[:, h:h + 1], scalar2=None,
                                                   op0=ALU.mult), reads=[kinv_d, const_d], writes=[Km_d[tt]])

        def mix_ret(l, wg, wg_d, mixT, mix_d, st):
            T = la_tiles(st)
            rcs2 = [st("rcs", [128, 1024], F32)] * 2
            rcs_d = [Dep()] * 2
            rcs_c = [kb.chan()] * 2
            rt = st("rt", [128, 1552], F32)
            rt_d = Dep()
            c1 = kb.chan()
            kb.dma(c1, rt[:], rett_d[:, :], writes=[rt_d])
            qb = [st("qb", [128, 512], BF16)] * 2
            t1 = [st("t1", [128, 512], F32)] * 2
            t2 = [st("t2", [128, 512], F32)] * 2
            qr = [st("qr", [128, 512], F32) for _ in range(2)]
            qb_d, t1_d, t2_d, qr_d = [Dep()] * 2, [Dep()] * 2, [Dep()] * 2, [Dep(), Dep()]
            for tt in range(4):
                sl = slice(tt * 512, (tt + 1) * 512)
                rb = tt % 2
                rcs = rcs2[rb]
                kb.dma(rcs_c[rb], rcs[:, 0:512], retcs_d[:, sl], writes=[rcs_d[rb]])
                kb.dma(rcs_c[rb], rcs[:, 512:1024], retcs_d[:, S + tt * 512: S + (tt + 1) * 512], writes=[rcs_d[rb]])
                for w in range(2):
                    pi = prr.next()
                    proj_fm(pi, wg, wg_d, w * 128, 128, tt)
                    kb.op("act", lambda: nc.scalar.copy(out=qb[w][:], in_=ps[pi][:]), reads=[ps_d[pi]], writes=[qb_d[w]])
                    pj = prr.next()
                    kb.op("pe", lambda: nc.tensor.matmul(ps[pj][:], lhsT=cmb[:, 2, :], rhs=qb[w][:], start=True, stop=True),
                          reads=[qb_d[w], const_d], writes=[ps_d[pj]])
                    kb.op("dve", lambda: nc.vector.tensor_tensor(out=t1[w][:], in0=ps[pi][:], in1=rcs[:, 0:512], op=ALU.mult),
                          reads=[ps_d[pi], rcs_d[rb]], writes=[t1_d[w]])
                    kb.op("dve", lambda: nc.vector.tensor_tensor(out=t2[w][:], in0=ps[pj][:], in1=rcs[:, 512:1024],
                                                                 op=ALU.mult), reads=[ps_d[pj], rcs_d[rb]], writes=[t2_d[w]])
                    kb.op("pool", lambda: nc.gpsimd.tensor_tensor(out=qr[w][:], in0=t1[w][:], in1=t2[w][:], op=ALU.add),
                          reads=[t1_d[w], t2_d[w]], writes=[qr_d[w]])
                kq_finish_v1(st, qr[0][:], qr_d[0], qr[1][:], qr_d[1], rt[:, 0:512], rt[:, 512:1024], rt[:, 1024:1536], rt_d,
                          T["QdT"], T["Qd_d"], T["Kmask"], T["Km_d"], T["kendT"], T["ke_d"], tt, T["kinv"], T["kinv_d"])
            v_tokmajor(T["vt"], T["v_d"], wg, wg_d, 256)
            gate_fm(T["sg"], T["sg_d"], wg, wg_d, 512)
            if STAGE == 1:
                return mix_zero(l, wg, wg_d, mixT, mix_d, st)
            cdec_r = st("cdec_r", [128, 16], F32)
            cdec_rd = Dep()
            kb.op("dve", lambda: nc.vector.tensor_copy(out=cdec_r[:], in_=rt[:, 1536:1552]), reads=[rt_d], writes=[cdec_rd])
            linattn(st, T["Kmask"], T["Km_d"], T["QdT"], T["Qd_d"], T["kendT"], T["ke_d"], T["vt"], T["v_d"], cdec_r, cdec_rd,
                    lambda cc: ("ret_ng", l), T["sg"], T["sg_d"], mixT, mix_d)

        def mix_ssm(l, wg, wg_d, mixT, mix_d, st):
            rw = st("rw", [128, 384], F32)
            rw_d = Dep()
            c1 = kb.chan()
            kb.dma(c1, rw[:], rows_d[:, l * 384:(l + 1) * 384], writes=[rw_d])
            kb.op("act", lambda: nc.scalar.activation(out=rw[:, 64:128], in_=rw[:, 64:128], func=AF.Exp), reads=[rw_d], writes=[rw_d])
            kb.op("dve", lambda: nc.vector.tensor_scalar(out=rw[:, 64:128], in0=rw[:, 64:128], scalar1=-1.0, scalar2=None, op0=ALU.mult),
                  reads=[rw_d], writes=[rw_d])
            dtt = st("dtt", [128, 64], F32)
            at = st("at", [128, 64], F32)
            acs = st("acs", [128, 64], F32)
            alast = st("alast", [128, 64], F32)
            eacs = st("eacs", [128, 64], F32)
            cdec = st("cdec", [128, 64], F32)
            dte = st("dte", [128, 64], F32)
            sm_d = Dep()
            for i in range(16):
                pi = prr.next()
                proj_tm(pi, 0, wg, wg_d, 768, 4, i)
                kb.op("dve", lambda: nc.vector.tensor_tensor(out=dtt[:, i * 4:(i + 1) * 4], in0=ps[pi][:, 0:4], in1=rw[:, i * 4:(i + 1) * 4], op=ALU.add),
                      reads=[ps_d[pi], rw_d], writes=[sm_d])
            kb.op("act", lambda: nc.scalar.activation(out=dtt[:], in_=dtt[:], func=AF.Exp), reads=[sm_d], writes=[sm_d])
            kb.op("act", lambda: nc.scalar.activation(out=dtt[:], in_=dtt[:], func=AF.Ln, bias=cv[:, 1:2], scale=1.0), reads=[sm_d, const_d], writes=[sm_d])
            kb.op("dve", lambda: nc.vector.tensor_tensor(out=at[:], in0=dtt[:], in1=rw[:, 64:128], op=ALU.mult), reads=[sm_d, rw_d], writes=[sm_d])
            pa = prr.next()
            kb.op("pe", lambda: nc.tensor.matmul(ps[pa][:, 0:64], lhsT=cmc("causal", 128), rhs=at[:], start=True, stop=True),
                  reads=[sm_d, const_d], writes=[ps_d[pa]])
            kb.op("act", lambda: nc.scalar.copy(out=acs[:], in_=ps[pa][:, 0:64]), reads=[ps_d[pa]], writes=[sm_d])
            pb = prr.next()
            kb.op("pe", lambda: nc.tensor.matmul(ps[pb][:, 0:64], lhsT=cmc("ones", 128), rhs=at[:], start=True, stop=True),
                  reads=[sm_d, const_d], writes=[ps_d[pb]])
            kb.op("act", lambda: nc.scalar.copy(out=alast[:], in_=ps[pb][:, 0:64]), reads=[ps_d[pb]], writes=[sm_d])
            kb.op("act", lambda: nc.scalar.activation(out=eacs[:], in_=acs[:], func=AF.Exp), reads=[sm_d], writes=[sm_d])
            kb.op("act", lambda: nc.scalar.activation(out=cdec[:], in_=alast[:], func=AF.Exp), reads=[sm_d], writes=[sm_d])
            kb.op("dve", lambda: nc.vector.tensor_tensor(out=dte[:], in0=alast[:], in1=acs[:], op=ALU.subtract), reads=[sm_d], writes=[sm_d])
            kb.op("act", lambda: nc.scalar.activation(out=dte[:], in_=dte[:], func=AF.Exp), reads=[sm_d], writes=[sm_d])
            if STAGE == 11:
                return mix_zero(l, wg, wg_d, mixT, mix_d, st)
            xsT = st("xsT", [128, 2, S], BF16)
            xs_d = [[Dep() for _ in range(4)] for _ in range(2)]
            Bm = st("Bm", [128, 2, S], BF16)
            Bm_d = [Dep() for _ in range(4)]
            CT = st("CT", [128, S], BF16)
            CT_d = [Dep() for _ in range(4)]
            cs_ = contextlib.ExitStack()
            upre = cs_.enter_context(nc.sbuf_tensor(kb.un("upre"), [128, S + 3], BF16))
            up_d = [Dep() for _ in range(5)]
            kb.op("pool", lambda: nc.gpsimd.memset(upre[:, 0:3], 0.0), writes=[up_d[0]])
            accs = [cs_.enter_context(nc.sbuf_tensor(kb.un("acc"), [128, 512], F32)) for _ in range(2)]
            accs_d = [Dep(), Dep()]
            cits = [(cc, tt) for cc in range(4) for tt in range(4)]

            def conv1(i):
                cc, tt = cits[i]
                acc, acc_d = accs[i % 2], accs_d[i % 2]
                pi = prr.next()
                proj_fm(pi, wg, wg_d, 256 + cc * 128, 128, tt)
                kb.op("act", lambda: nc.scalar.copy(out=upre[:, 3 + tt * 512: 3 + (tt + 1) * 512], in_=ps[pi][:]),
                      reads=[ps_d[pi]], writes=[up_d[tt + 1]])
                kb.op("act", lambda: nc.scalar.activation(out=acc[:], in_=ps[pi][:], func=AF.Identity,
                                                          bias=ppc("ssm_cb", l * 4 + cc), scale=ppc("ssm_cw", (l * 4 + 3) * 4 + cc)),
                      reads=[ps_d[pi], const_d], writes=[acc_d])
                for k in range(1, 4):
                    kb.op("dve", lambda: nc.vector.scalar_tensor_tensor(
                        out=acc[:], in0=upre[:, 3 - k + tt * 512: 3 - k + (tt + 1) * 512], scalar=ppc("ssm_cw", (l * 4 + 3 - k) * 4 + cc),
                        in1=acc[:], op0=ALU.mult, op1=ALU.add),
                        reads=[up_d[tt + 1], up_d[tt], acc_d, const_d], writes=[acc_d])

            def conv2(i):
                cc, tt = cits[i]
                acc, acc_d = accs[i % 2], accs_d[i % 2]
                sl = slice(tt * 512, (tt + 1) * 512)
                if cc < 2:
                    kb.op("act", lambda: nc.scalar.activation(out=xsT[:, cc, sl], in_=acc[:], func=AF.Silu), reads=[acc_d], writes=[xs_d[cc][tt]])
                elif cc == 3:
                    kb.op("act", lambda: nc.scalar.activation(out=CT[:, sl], in_=acc[:], func=AF.Silu), reads=[acc_d], writes=[CT_d[tt]])
                else:
                    kb.op("act", lambda: nc.scalar.activation(out=acc[:], in_=acc[:], func=AF.Silu), reads=[acc_d], writes=[acc_d])
                    kb.op("act", lambda: nc.scalar.activation(out=Bm[:, 0, sl], in_=acc[:], func=AF.Copy, scale=cmc("hm2", 2)[:, 0:1]),
                          reads=[acc_d, const_d], writes=[Bm_d[tt]])
                    kb.op("dve", lambda: nc.vector.tensor_scalar(out=Bm[:, 1, sl], in0=acc[:], scalar1=cmc("hm2", 2)[:, 1:2],
                                                                 scalar2=None, op0=ALU.mult), reads=[acc_d, const_d], writes=[Bm_d[tt]])

            conv1(0)
            for i in range(16):
                if i + 1 < 16:
                    conv1(i + 1)
                conv2(i)
            kb.barrier()
            cs_.close()
            if STAGE == 12:
                return mix_zero(l, wg, wg_d, mixT, mix_d, st)
            vt = st("vt", [128, 16, 256], BF16)
            xsD = st("xsD", [128, 16, 256], BF16)
            Btm = st("Btm", [128, 16, 128], BF16)
            szt = st("szt", [128, 16, 256], BF16)
            xs_tm = [st("xs_tm", [128, 256], BF16) for _ in range(2)]
            xtm_d = [Dep(), Dep()]
            v_d = [Dep() for _ in range(16)]
            xd_d = [Dep() for _ in range(16)]
            bt_d = [Dep() for _ in range(16)]
            sz_d = [Dep() for _ in range(16)]
            for c in range(16):
                ch = slice(c * 128, (c + 1) * 128)
                s0 = 0
                srcs = [(xsT[:, 0, ch], xs_d[0][c // 4]), (xsT[:, 1, ch], xs_d[1][c // 4]), (Bm[:, 0, ch], Bm_d[c // 4]), (Bm[:, 1, ch], Bm_d[c // 4])]
                for k, (ap_, d_) in enumerate(srcs):
                    kb.op("pe", lambda: nc.tensor.transpose(out=psb[:, (s0 + k) * 128:(s0 + k + 1) * 128], in_=ap_, identity=identb),
                          reads=[d_, const_d], writes=[psb_d[s0 + k]], inc=(k == 3))
                xb = xs_tm[c % 2]
                kb.op("act", lambda: nc.scalar.copy(out=xb[:], in_=psb[:, 0:256]), reads=[psb_d[0]], writes=[xtm_d[c % 2]])
                kb.op("pool", lambda: nc.gpsimd.tensor_tensor(out=xsD[:, c, :], in0=xb[:], in1=rw[:, 128:384], op=ALU.mult),
                      reads=[xtm_d[c % 2], rw_d], writes=[xd_d[c]])
                for cc in range(2):
                    pt_ = psb[:, (s0 + cc) * 128:(s0 + cc + 1) * 128]
                    for hh in range(2):
                        h = cc * 2 + hh
                        kb.op("act", lambda: nc.scalar.activation(out=vt[:, c, h * 64:(h + 1) * 64], in_=pt_[:, hh * 64:(hh + 1) * 64], func=AF.Copy,
                                                                  scale=dtt[:, c * 4 + h: c * 4 + h + 1]),
                              reads=[psb_d[s0 + cc], sm_d], writes=[v_d[c]])
                for g in range(2):
                    kb.op("act", lambda: nc.scalar.copy(out=Btm[:, c, g * 64:(g + 1) * 64],
                                                        in_=psb[:, (s0 + 2 + g) * 128 + g * 64:(s0 + 2 + g) * 128 + (g + 1) * 64]),
                          reads=[psb_d[s0 + 2 + g]], writes=[bt_d[c]])
                pi = prr.next()
                proj_tm(pi, 0, wg, wg_d, 0, 256, c)
                kb.op("act", lambda: nc.scalar.activation(out=szt[:, c, :], in_=ps[pi][:, 0:256], func=AF.Silu), reads=[ps_d[pi]], writes=[sz_d[c]])
            if STAGE == 13:
                return mix_zero(l, wg, wg_d, mixT, mix_d, st)
            tl = post_tiles(st)
            scm = st("scm", [128, 256], F32)
            Mh = [st("Mh", [128, 128], F32) for _ in range(4)]
            dec = st("dec", [128, 512], F32)
            attm = [st("attm", [128, 512], BF16) for _ in range(3)]
            yt = [st("yt", [128, 256], F32) for _ in range(2)]
            vend = st("vend", [128, 256], BF16)
            S_run = st("S_run", [128, 256], F32)
            S_tmp = st("S_tmp", [128, 256], F32)
            Sbfs = [st("Sbf", [128, 256], BF16) for _ in range(4)]
            scm_d, Mh_d, dec_d, attm_d, yt_d, vend_d = Dep(), [Dep() for _ in range(4)], Dep(), [Dep() for _ in range(3)], [Dep(), Dep()], Dep()
            S_d, St_d = Dep(), Dep()
            Sb_ds = [Dep() for _ in range(4)]

            def sa(c):
                ch = slice(c * 128, (c + 1) * 128)
                b = c % 2
                b3 = c % 3
                tq = c // 4
                pa = b
                for g in range(2):
                    kb.op("pe", lambda: nc.tensor.matmul(ps[pa][:, g * 128:(g + 1) * 128], lhsT=Bm[:, g, ch], rhs=CT[:, ch], start=True, stop=True),
                          reads=[Bm_d[tq], CT_d[tq]], writes=[ps_d[pa]], inc=(g == 1))
                kb.op("dve", lambda: nc.vector.tensor_tensor(out=scm[:], in0=ps[pa][:, 0:256], in1=cmc("causal", 256), op=ALU.mult),
                      reads=[ps_d[pa], const_d], writes=[scm_d])
                pg = 5 + b
                for h in range(4):
                    kb.op("act", lambda: nc.scalar.activation(out=Mh[h][:], in_=cmc("sgt", 128), func=AF.Copy, scale=at[:, c * 4 + h: c * 4 + h + 1]),
                          reads=[const_d, sm_d], writes=[Mh_d[h]])
                    kb.op("pe", lambda: nc.tensor.matmul(ps[pg][:, h * 128:(h + 1) * 128], lhsT=Mh[h][:], rhs=cmc("causal", 128), start=True, stop=True),
                          reads=[Mh_d[h], const_d], writes=[ps_d[pg]], inc=(h == 3))
                kb.op("act", lambda: nc.scalar.activation(out=dec[:], in_=ps[pg][:], func=AF.Exp), reads=[ps_d[pg]], writes=[dec_d])
                for h in range(4):
                    g = h // 2
                    kb.op("dve", lambda: nc.vector.tensor_tensor(out=attm[b3][:, h * 128:(h + 1) * 128], in0=scm[:, g * 128:(g + 1) * 128],
                                                                 in1=dec[:, h * 128:(h + 1) * 128], op=ALU.mult),
                          reads=[scm_d, dec_d], writes=[attm_d[b3]])
                if c < 15:
                    for h in range(4):
                        if h % 2 == 0:
                            kb.op("act", lambda: nc.scalar.activation(out=vend[:, h * 64:(h + 1) * 64], in_=vt[:, c, h * 64:(h + 1) * 64], func=AF.Copy,
                                                                      scale=dte[:, c * 4 + h: c * 4 + h + 1]), reads=[v_d[c], sm_d], writes=[vend_d])
                        else:
                            kb.op("dve", lambda: nc.vector.tensor_scalar(out=vend[:, h * 64:(h + 1) * 64], in0=vt[:, c, h * 64:(h + 1) * 64],
                                                                         scalar1=dte[:, c * 4 + h: c * 4 + h + 1], scalar2=None, op0=ALU.mult),
                                  reads=[v_d[c], sm_d], writes=[vend_d])
                    kb.op("pe", lambda: nc.tensor.matmul(ps[4][:, 0:256], lhsT=Btm[:, c, :], rhs=vend[:], start=True, stop=True),
                          reads=[bt_d[c], vend_d], writes=[ps_d[4]])
                    if c == 0:
                        kb.op("dve", lambda: nc.vector.tensor_tensor(out=S_run[:], in0=ps[4][:, 0:256], in1=cmc("bdm2", 256), op=ALU.mult),
                              reads=[ps_d[4], const_d], writes=[S_d])
                    else:
                        kb.op("dve", lambda: nc.vector.tensor_tensor(out=S_tmp[:], in0=ps[4][:, 0:256], in1=cmc("bdm2", 256), op=ALU.mult),
                              reads=[ps_d[4], const_d], writes=[St_d])
                        for h in range(4):
                            kb.op("dve", lambda: nc.vector.scalar_tensor_tensor(out=S_run[:, h * 64:(h + 1) * 64], in0=S_run[:, h * 64:(h + 1) * 64],
                                                                                scalar=cdec[:, c * 4 + h: c * 4 + h + 1], in1=S_tmp[:, h * 64:(h + 1) * 64],
                                                                                op0=ALU.mult, op1=ALU.add),
                                  reads=[S_d, St_d, sm_d], writes=[S_d])
                    kb.op("act", lambda: nc.scalar.copy(out=Sbfs[c % 4][:], in_=S_run[:]), reads=[S_d], writes=[Sb_ds[c % 4]])

            def sb_(c):
                ch = slice(c * 128, (c + 1) * 128)
                b = c % 2
                b3 = c % 3
                tq = c // 4
                po = 2 + b
                if c > 0:
                    kb.op("pe", lambda: nc.tensor.matmul(ps[po][:, 256:512], lhsT=CT[:, ch], rhs=Sbfs[(c - 1) % 4][:], start=True, stop=True),
                          reads=[CT_d[tq], Sb_ds[(c - 1) % 4]], writes=[ps_d[po]], inc=False)
                for h in range(4):
                    kb.op("pe", lambda: nc.tensor.matmul(ps[po][:, h * 64:(h + 1) * 64], lhsT=attm[b3][:, h * 128:(h + 1) * 128],
                                                         rhs=vt[:, c, h * 64:(h + 1) * 64], start=(h == 0), stop=(h == 3)),
                          reads=[attm_d[b3], v_d[c]], writes=[ps_d[po]], inc=(h == 3))
                kb.op("dve", lambda: nc.vector.tensor_tensor(out=yt[b][:], in0=ps[po][:, 0:256], in1=xsD[:, c, :], op=ALU.add),
                      reads=[ps_d[po], xd_d[c]], writes=[yt_d[b]])
                if c > 0:
                    for h in range(4):
                        kb.op("dve", lambda: nc.vector.scalar_tensor_tensor(out=yt[b][:, h * 64:(h + 1) * 64], in0=ps[po][:, 256 + h * 64: 256 + (h + 1) * 64],
                                                                            scalar=eacs[:, c * 4 + h: c * 4 + h + 1], in1=yt[b][:, h * 64:(h + 1) * 64],
                                                                            op0=ALU.mult, op1=ALU.add),
                              reads=[ps_d[po], sm_d, yt_d[b]], writes=[yt_d[b]])
                kb.op("pool", lambda: nc.gpsimd.tensor_tensor(out=yt[b][:], in0=yt[b][:], in1=szt[:, c, :], op=ALU.mult),
                      reads=[yt_d[b], sz_d[c]], writes=[yt_d[b]])

            def sc_(c):
                b = c % 2
                post_norm(c, yt[b][:], yt_d[b], 2, lambda cc: ("ssm_ng", 2 * l + cc), None, None, mixT, mix_d, tl)

            sa(0)
            sa(1)
            for c in range(16):
                sb_(c)
                if c + 2 < 16:
                    sa(c + 2)
                if c >= 1:
                    sc_(c - 1)
            sc_(15)

        def mix_moba(l, wg, wg_d, mixT, mix_d, st):
            qa = [st("qa", [72, S], BF16) for _ in range(4)]
            ka = [st("ka", [72, S], BF16) for _ in range(4)]
            qa_d = [[Dep() for _ in range(4)] for _ in range(4)]
            ka_d = [[Dep() for _ in range(4)] for _ in range(4)]
            qb_d = [Dep() for _ in range(4)]
            kb_d = [Dep()] * 4
            va = st("va", [128, 16, 4, 128], BF16)
            va_d = [Dep() for _ in range(16)]
            c1 = kb.chan()
            for h in range(4):
                kb.dma(c1, ka[h][64:72, :], blk_d[:, :], writes=[kb_d[h]], q="pool")
                kb.op("pool", lambda: nc.gpsimd.memset(qa[h][64:72, :], 0.0), writes=[qb_d[h]])
            for i in range(16):
                kb.op("pool", lambda: nc.gpsimd.memset(va[:, i, :, 64:128], 1.0), writes=[va_d[i]])
            mcs = [st("mcs", [128, 1024], F32) for _ in range(2)]
            mcs_d = [Dep(), Dep()]
            mcs_c = [kb.chan(), kb.chan()]
            sqb = [st("sqb", [128, 512], BF16) for _ in range(2)]
            rs = [st("rs", [128, 512], F32) for _ in range(2)]
            qn = [st("qn", [128, 512], F32) for _ in range(2)]
            qnb = [st("qnb", [128, 512], BF16) for _ in range(2)]
            sqb_d, rs_d, qn_d, qnb_d = [Dep(), Dep()], [Dep(), Dep()], [Dep(), Dep()], [Dep(), Dep()]
            kms = st("kms", [128, 2, 8], F32)
            kms_d = Dep()
            km = [st("km", [64, 8], BF16) for _ in range(4)]
            km_d = [Dep() for _ in range(4)]
            its = [(tt, w, pr) for tt in range(4) for w in range(2) for pr in range(2)]

            def prep1(i):
                tt, w, pr = its[i]
                s_ = i % 2
                sl = slice(tt * 512, (tt + 1) * 512)
                rb = tt % 2
                if w == 0 and pr == 0:
                    kb.dma(mcs_c[rb], mcs[rb][:, 0:512], mobacs_d[:, sl], writes=[mcs_d[rb]])
                    kb.dma(mcs_c[rb], mcs[rb][:, 512:1024], mobacs_d[:, S + tt * 512: S + (tt + 1) * 512], writes=[mcs_d[rb]])
                pi = prr.next()
                proj_fm(pi, wg, wg_d, w * 256 + pr * 128, 128, tt)
                kb.op("act", lambda: nc.scalar.activation(out=sqb[s_][:], in_=ps[pi][:], func=AF.Square), reads=[ps_d[pi]], writes=[sqb_d[s_]])
                pj = prr.next()
                kb.op("pe", lambda: nc.tensor.matmul(ps[pj][:], lhsT=cmb[:, 4, :], rhs=sqb[s_][:], start=True, stop=True),
                      reads=[sqb_d[s_], const_d], writes=[ps_d[pj]])
                kb.op("act", lambda: nc.scalar.activation(out=rs[s_][:], in_=ps[pj][:], func=AF.Ln, bias=cv[:, 0:1], scale=1.0 / 64.0),
                      reads=[ps_d[pj], const_d], writes=[rs_d[s_]])
                kb.op("act", lambda: nc.scalar.activation(out=rs[s_][:], in_=rs[s_][:], func=AF.Exp, scale=-0.5), reads=[rs_d[s_]], writes=[rs_d[s_]])
                gn = "moba_qg" if w == 0 else "moba_kg"
                kb.op("dve", lambda: nc.vector.scalar_tensor_tensor(out=qn[s_][:], in0=ps[pi][:], scalar=ppc(gn, l), in1=rs[s_][:],
                                                                    op0=ALU.mult, op1=ALU.mult),
                      reads=[ps_d[pi], rs_d[s_], const_d], writes=[qn_d[s_]])
                kb.op("act", lambda: nc.scalar.copy(out=qnb[s_][:], in_=qn[s_][:]), reads=[qn_d[s_]], writes=[qnb_d[s_]])

            def prep2(i):
                tt, w, pr = its[i]
                s_ = i % 2
                sl = slice(tt * 512, (tt + 1) * 512)
                rb = tt % 2
                pk = prr.next()
                kb.op("pe", lambda: nc.tensor.matmul(ps[pk][:], lhsT=cmb[:, 3, :], rhs=qnb[s_][:], start=True, stop=True),
                      reads=[qnb_d[s_], const_d], writes=[ps_d[pk]])
                kb.op("dve", lambda: nc.vector.tensor_tensor(out=qn[s_][:], in0=qn[s_][:], in1=mcs[rb][:, 0:512], op=ALU.mult),
                      reads=[qn_d[s_], mcs_d[rb]], writes=[qn_d[s_]])
                kb.op("dve", lambda: nc.vector.tensor_tensor(out=rs[s_][:], in0=ps[pk][:], in1=mcs[rb][:, 512:1024], op=ALU.mult),
                      reads=[ps_d[pk], mcs_d[rb]], writes=[rs_d[s_]])
                kb.op("pool", lambda: nc.gpsimd.tensor_tensor(out=qn[s_][:], in0=qn[s_][:], in1=rs[s_][:], op=ALU.add),
                      reads=[qn_d[s_], rs_d[s_]], writes=[qn_d[s_]])
                for hh in range(2):
                    h = pr * 2 + hh
                    dst = (qa if w == 0 else ka)[h]
                    dd = (qa_d if w == 0 else ka_d)[h][tt]
                    kb.op("act", lambda: nc.scalar.copy(out=dst[0:64, sl], in_=qn[s_][hh * 64:(hh + 1) * 64, :]), reads=[qn_d[s_]], writes=[dd])
                if w == 1:
                    kb.op("dve", lambda: nc.vector.tensor_reduce(out=kms[:, pr, 2 * tt:2 * tt + 2], in_=qn[s_][:].rearrange("p (n j) -> p n j", j=256),
                                                                 axis=AX.X, op=ALU.add), reads=[qn_d[s_]], writes=[kms_d])

            prep1(0)
            for i in range(16):
                if i + 1 < 16:
                    prep1(i + 1)
                prep2(i)
            for h in range(4):
                pr, hh = h // 2, h % 2
                kb.op("act", lambda: nc.scalar.activation(out=km[h][:], in_=kms[hh * 64:(hh + 1) * 64, pr, :], func=AF.Copy, scale=1.0 / 256.0),
                      reads=[kms_d], writes=[km_d[h]])
            for i in range(16):
                pi = prr.next()
                proj_tm(pi, 0, wg, wg_d, 512, 256, i)
                evac(i, va[:, i, :, 0:64], ps[pi][:, 0:256].rearrange("p (h e) -> p h e", h=4), [ps_d[pi]], [va_d[i]])
            gm = st("gm", [128, 32], F32)
            mx = st("mx", [128, 32], F32)
            selp = [st("selp", [128, 72], BF16) for _ in range(4)]
            gm_d, mx_d = Dep(), Dep()
            selp_d = [Dep() for _ in range(4)]
            for h in range(4):
                kb.op("pool", lambda: nc.gpsimd.memset(selp[h][:], 0.0), writes=[selp_d[h]])
            for i in range(8, 16):
                b = i // 2
                tq = i // 4
                pg = 6
                for h in range(4):
                    kb.op("pe", lambda: nc.tensor.matmul(ps[pg][:, h * 8:(h + 1) * 8], lhsT=qa[h][0:64, i * 128:(i + 1) * 128], rhs=km[h][:],
                                                         start=True, stop=True), reads=[qa_d[h][tq], km_d[h]], writes=[ps_d[pg]], inc=(h == 3))
                kb.op("dve", lambda: nc.vector.tensor_tensor(out=gm[:], in0=ps[pg][:, 0:32], in1=cmc("gmask", 256)[:, b * 32:(b + 1) * 32], op=ALU.add),
                      reads=[ps_d[pg], const_d], writes=[gm_d])
                for h in range(4):
                    kb.op("dve", lambda: nc.vector.max(out=mx[:, h * 8:(h + 1) * 8], in_=gm[:, h * 8:(h + 1) * 8]), reads=[gm_d], writes=[mx_d])
                for h in range(4):
                    kb.op("dve", lambda: nc.vector.tensor_scalar(out=selp[h][:, 64:72], in0=gm[:, h * 8:(h + 1) * 8], scalar1=mx[:, h * 8 + 2:h * 8 + 3],
                                                                 scalar2=-30000.0, op0=ALU.is_lt, op1=ALU.mult),
                          reads=[gm_d, mx_d], writes=[selp_d[h]])
                for h in range(4):
                    kb.op("pe", lambda: nc.tensor.transpose(out=psb[0:72, h * 128:(h + 1) * 128], in_=selp[h][:], identity=identb),
                          reads=[selp_d[h], const_d], writes=[psb_d[h]], inc=(h == 3))
                for h in range(4):
                    kb.op("act", lambda: nc.scalar.copy(out=qa[h][64:72, i * 128:(i + 1) * 128], in_=psb[64:72, h * 128:(h + 1) * 128]),
                          reads=[psb_d[h]], writes=[qb_d[h]])
            pt = [st("pt", [128, 256], BF16) for _ in range(3)]
            pt_d = [Dep() for _ in range(3)]
            rec = [st("rec", [64, 256], F32) for _ in range(2)]
            rec_d = [Dep(), Dep()]
            items = []
            for h in range(4):
                for b in range(8):
                    for jt in range(2 * b + 2):
                        items.append((h, b, jt))
            LA = 3
            pt = pt + [st("pt", [128, 256], BF16)]
            pt_d = pt_d + [Dep()]

            def emit_score(k):
                h, b, jt = items[k]
                own = jt >= 2 * b
                qlo = jt - 2 * b
                c0 = 128 if (own and qlo == 1) else 0
                n = 256 - c0
                qcols = slice(b * 256 + c0, (b + 1) * 256)
                tq = b // 2
                pi = k % 4
                pb_ = k % 4
                kk = 64 if own else 72
                rds = [ka_d[h][jt // 4], qa_d[h][tq]] + ([] if own else [kb_d[h], qb_d[h]])
                kb.op("pe", lambda: nc.tensor.matmul(ps[pi][:, 0:n], lhsT=ka[h][0:kk, jt * 128:(jt + 1) * 128], rhs=qa[h][0:kk, qcols],
                                                     start=True, stop=True), reads=rds, writes=[ps_d[pi]])
                kb.op("act", lambda: nc.scalar.activation(out=pt[pb_][:, 0:n], in_=ps[pi][:, 0:n], func=AF.Exp, scale=0.125),
                      reads=[ps_d[pi]], writes=[pt_d[pb_]])
                if own:
                    kb.op("dve", lambda: nc.vector.tensor_tensor(out=pt[pb_][:, 0:128], in0=pt[pb_][:, 0:128], in1=cmc("causal", 128), op=ALU.mult),
                          reads=[pt_d[pb_], const_d], writes=[pt_d[pb_]])

            def emit_pv(k):
                h, b, jt = items[k]
                own = jt >= 2 * b
                qlo = jt - 2 * b
                c0 = 128 if (own and qlo == 1) else 0
                n = 256 - c0
                njt = 2 * b + 2
                nacc = h * 8 + b
                po = 4 + nacc % 2
                rbi = nacc % 2
                pb_ = k % 4
                tq = b // 2
                qs = slice(b * 256, (b + 1) * 256)
                kb.op("pe", lambda: nc.tensor.matmul(ps[po][:, c0:256], lhsT=va[:, jt, h, :], rhs=pt[pb_][:, 0:n],
                                                     start=(jt == 0), stop=(jt == njt - 1)),
                      reads=[va_d[jt], pt_d[pb_]], writes=[ps_d[po]])
                if jt == njt - 1:
                    kb.op("dve", lambda: nc.vector.reciprocal(out=rec[rbi][:], in_=ps[po][64:128, 0:256]), reads=[ps_d[po]], writes=[rec_d[rbi]])
                    hh = h % 2
                    kb.op("dve", lambda: nc.vector.tensor_tensor(out=mixT[hh * 64:(hh + 1) * 64, h // 2, qs], in0=ps[po][0:64, 0:256], in1=rec[rbi][:],
                                                                 op=ALU.mult), reads=[ps_d[po], rec_d[rbi]], writes=[mix_d[h // 2][tq]])

            for k in range(len(items) + LA):
                if k < len(items):
                    emit_score(k)
                if k >= LA:
                    emit_pv(k - LA)

        def mix_zero(l, wg, wg_d, mixT, mix_d, st):
            for cc in range(2):
                kb.op("pool", lambda: nc.gpsimd.memset(mixT[:, cc, :], 0.0), writes=mix_d[cc])

        mix_fns = [mix_moba, mix_ssm, mix_gla, mix_ret]

        def mixer_phase(l, s):
            mk_ = kb.mark()
            with contextlib.ExitStack() as sc:
                st = lambda name, shape, dt: sc.enter_context(nc.sbuf_tensor(kb.un(name), shape, dt))
                wg = st("wg", [128, KC, GW], BF16)
                wo = st("wo", [128, 2, D], BF16)
                wg_d, wo_d = Dep(), Dep()
                wg_c, wo_c = kb.chan(), kb.chan()
                mixT = st("mixT", [128, 2, S], BF16)
                mix_d = [[Dep() for _ in range(4)] for _ in range(2)]
                for mi_, m in enumerate(mixers):
                    r0 = (l * 4 + m) * 128
                    if mi_ == 0:
                        kb.dma(wg_c, wg[:].rearrange("p k c -> p (k c)"), win_b[r0:r0 + 128, :], writes=[wg_d])
                    kb.dma(wo_c, wo[:].rearrange("p k c -> p (k c)"), wout_b[r0:r0 + 128, :], writes=[wo_d])
                    with contextlib.ExitStack() as sc2:
                        st2 = lambda name, shape, dt: sc2.enter_context(nc.sbuf_tensor(kb.un(name), shape, dt))
                        mix_fns[m](l, wg, wg_d, mixT, mix_d, st2)
                        kb.barrier()
                    if debug and s == 0 and l == 0:
                        dc_ = kb.chan()
                        kb.dma(dc_, dbg_d[m], mixT[:].rearrange("p k c -> p (k c)"), reads=[d for dd in mix_d for d in dd])
                    if mi_ + 1 < len(mixers):
                        rn = (l * 4 + mixers[mi_ + 1]) * 128
                        kb.dma(wg_c, wg[:].rearrange("p k c -> p (k c)"), win_b[rn:rn + 128, :], writes=[wg_d])
                    for dc in range(KC):
                        for tt in range(4):
                            sl = slice(tt * 512, (tt + 1) * 512)
                            pi = (dc * 4 + tt) % 4
                            for k2 in range(2):
                                kb.op("pe", lambda: nc.tensor.matmul(ps[pi][:], lhsT=wo[:, k2, dc * 128:(dc + 1) * 128], rhs=mixT[:, k2, sl],
                                                                     start=(k2 == 0), stop=(k2 == 1)),
                                      reads=[wo_d, mix_d[k2][tt]], writes=[ps_d[pi]], inc=(k2 == 1))
                            kb.op("dve", lambda: nc.vector.tensor_tensor(out=xT[:, dc, sl], in0=ps[pi][:], in1=xT[:, dc, sl], op=ALU.add),
                                  reads=[ps_d[pi], xT_d[dc][tt]], writes=[xT_d[dc][tt]])
                    kb.barrier()
            kb.release(mk_)

        for s in range(n_seq):
            load_x(s)
            for l in range(L):
                norm("attn_g", l)
                if mixers:
                    mixer_phase(l, s)
                if not SKIP_FFN:
                    norm("ffn_g", l)
                    ffn(l)
            store_x(s)
        kb.barrier()
    return nc


def kernel(**inputs):
    L = 2
    n_seq = 4
    x = np.asarray(inputs["x"], np.float32)
    hp = host_prep(inputs, L)
    nc = build(n_seq, L)
    in_maps = []
    for c in range(NCORES):
        m = dict(hp)
        m["x"] = np.ascontiguousarray(x[c * n_seq:(c + 1) * n_seq].reshape(n_seq * S, D))
        in_maps.append(m)
    res = run_bass_kernel_spmd(nc, in_maps, core_ids=list(range(NCORES)))
    out = np.stack([r["y"].reshape(n_seq, S, D) for r in res.results], axis=0)
    return out.reshape(NCORES * n_seq, S, D).astype(np.float32)
```
